# Optimizing a Trainium2 kernel written in Bass

```python
import math
import jax, jax.numpy as jnp
from jax import lax
import numpy as np

D_MODEL = 1024
BATCH = 8
SEQ = 2048
DEPTH = 4

f32 = jnp.float32
N_A_LAYERS = DEPTH // 2

SSM_EXPAND = 2
D_INNER = SSM_EXPAND * D_MODEL
SSM_HEAD_DIM = 64
SSM_HEADS = D_INNER // SSM_HEAD_DIM
SSM_GROUPS = 4
SSM_STATE = 128
CONV_WIDTH = 4
SSM_CHUNK = 128
CONV_DIM = D_INNER + 2 * SSM_GROUPS * SSM_STATE
IN_PROJ_DIM = D_INNER + CONV_DIM + SSM_HEADS

ATTN_HEADS = 16
ATTN_HEAD_DIM = 64
ATTN_DIM = ATTN_HEADS * ATTN_HEAD_DIM
ROT_DIM = ATTN_HEAD_DIM // 4
ROPE_THETA = 500000.0
MOBA_BLOCK = 256
MOBA_TOPK = 3
MOBA_Q_CHUNK = 64

N_EXPERTS = 32
TOP_K = 4
D_EXPERT = D_MODEL
SWIGLU_LIMIT = 7.0
SWIGLU_ALPHA = 1.702
MOE_ROW_BLOCK = 512

LN_EPS = 1e-5
RMS_EPS = 1e-5
DEEPNORM_ALPHA = (2 * DEPTH) ** 0.25
DEEPNORM_BETA = (8 * DEPTH) ** -0.25

kernel_name = "yoco_mamba2_moba_moe_deepnorm"


def layer_norm(x, g, b):
    xf = x.astype(f32)
    mu = jnp.mean(xf, -1, keepdims=True)
    xc = xf - mu
    var = jnp.mean(xc * xc, -1, keepdims=True)
    return (xc * lax.rsqrt(var + LN_EPS) * g + b).astype(x.dtype)


def causal_depthwise_conv(u, w, b):
    c = u.shape[-1]
    out = lax.conv_general_dilated(u, w[:, None, :], window_strides=(1,),
                                   padding=[(CONV_WIDTH - 1, 0)],
                                   dimension_numbers=('NWC', 'WIO', 'NWC'),
                                   feature_group_count=c)
    return out + b


def gated_rmsnorm(y, z, g):
    h = y.astype(f32) * jax.nn.silu(z.astype(f32))
    hg = h.reshape(*h.shape[:-1], SSM_GROUPS, D_INNER // SSM_GROUPS)
    hg = hg * lax.rsqrt(jnp.mean(hg * hg, -1, keepdims=True) + RMS_EPS)
    return (hg.reshape(h.shape) * g).astype(y.dtype)


def ssd_chunked_scan(xs, dt, a, b_in, c_in):
    bsz, s, _, _ = xs.shape
    nc, L, G, R = s // SSM_CHUNK, SSM_CHUNK, SSM_GROUPS, SSM_HEADS // SSM_GROUPS
    x_dt = (xs.astype(f32) * dt[..., None]).reshape(bsz, nc, L, G, R, SSM_HEAD_DIM)
    a_dt = (dt * a).reshape(bsz, nc, L, G, R).transpose(0, 3, 4, 1, 2)
    bb = b_in.astype(f32).reshape(bsz, nc, L, G, SSM_STATE)
    cc = c_in.astype(f32).reshape(bsz, nc, L, G, SSM_STATE)
    a_cs = jnp.cumsum(a_dt, -1)
    tril = jnp.tril(jnp.ones((L, L), bool))
    decay = jnp.exp(jnp.where(tril, a_cs[..., :, None] - a_cs[..., None, :], -jnp.inf))
    cb = jnp.einsum('bclgn,bcsgn->bgcls', cc, bb)
    y_diag = jnp.einsum('bgcls,bgrcls,bcsgrp->bclgrp', cb, decay, x_dt)
    decay_to_end = jnp.exp(a_cs[..., -1:] - a_cs)
    chunk_states = jnp.einsum('bclgn,bgrcl,bclgrp->bcgrpn', bb, decay_to_end, x_dt)
    chunk_decay = jnp.exp(a_cs[..., -1])

    def step(state, inp):
        st, dec = inp
        return state * dec[..., None, None] + st, state

    init = jnp.zeros((bsz, G, R, SSM_HEAD_DIM, SSM_STATE), f32)
    _, prev_states = lax.scan(step, init, (jnp.moveaxis(chunk_states, 1, 0),
                                           jnp.moveaxis(chunk_decay, -1, 0)))
    y_off = jnp.einsum('bclgn,cbgrpn,bgrcl->bclgrp', cc, prev_states, jnp.exp(a_cs))
    return (y_diag + y_off).reshape(bsz, s, SSM_HEADS, SSM_HEAD_DIM)


def mamba2_mixer(x, w_in, conv_w, conv_b, dt_bias, a_log, d_skip, norm_g, w_out):
    bsz, s, _ = x.shape
    zxbcdt = x @ w_in
    z, xbc, dt = jnp.split(zxbcdt, [D_INNER, D_INNER + CONV_DIM], axis=-1)
    xbc = jax.nn.silu(causal_depthwise_conv(xbc, conv_w, conv_b))
    xs, b_in, c_in = jnp.split(xbc, [D_INNER, D_INNER + SSM_GROUPS * SSM_STATE], axis=-1)
    dt = jax.nn.softplus((dt + dt_bias).astype(f32))
    a = -jnp.exp(a_log.astype(f32))
    xs = xs.reshape(bsz, s, SSM_HEADS, SSM_HEAD_DIM)
    y = ssd_chunked_scan(xs, dt, a, b_in.reshape(bsz, s, SSM_GROUPS, SSM_STATE),
                         c_in.reshape(bsz, s, SSM_GROUPS, SSM_STATE))
    y = y + d_skip.astype(f32)[:, None] * xs.astype(f32)
    y = gated_rmsnorm(y.reshape(bsz, s, D_INNER).astype(x.dtype), z, norm_g)
    return y @ w_out


def rope_tables(s):
    inv_freq = ROPE_THETA ** (-jnp.arange(0, ROT_DIM, 2, dtype=f32) / ROT_DIM)
    ang = jnp.arange(s, dtype=f32)[:, None] * inv_freq[None, :]
    return jnp.cos(ang), jnp.sin(ang)


def apply_partial_rope(t, cos, sin):
    tf = t.astype(f32)
    half = ROT_DIM // 2
    t1, t2 = tf[..., :half], tf[..., half:ROT_DIM]
    c, s = cos[None, :, None, :], sin[None, :, None, :]
    return jnp.concatenate([t1 * c - t2 * s, t2 * c + t1 * s, tf[..., ROT_DIM:]], -1).astype(t.dtype)


def shared_kv(h, w_k, w_v, cos, sin):
    bsz, s, _ = h.shape
    nb = -(-s // MOBA_BLOCK)
    pad = nb * MOBA_BLOCK - s
    k = apply_partial_rope((h @ w_k).reshape(bsz, s, ATTN_HEADS, ATTN_HEAD_DIM), cos, sin)
    v = (h @ w_v).reshape(bsz, s, ATTN_HEADS, ATTN_HEAD_DIM)
    padw = ((0, 0), (0, pad), (0, 0), (0, 0))
    k_blk = jnp.pad(k, padw).reshape(bsz, nb, MOBA_BLOCK, ATTN_HEADS, ATTN_HEAD_DIM).transpose(0, 3, 1, 2, 4)
    v_blk = jnp.pad(v, padw).reshape(bsz, nb, MOBA_BLOCK, ATTN_HEADS, ATTN_HEAD_DIM).transpose(0, 3, 1, 2, 4)
    k_mean = jnp.mean(k_blk.astype(f32), axis=3)
    return k_blk, v_blk, k_mean


def moba_attention(h, w_q, w_o, k_blk, v_blk, k_mean, cos, sin):
    bsz, s, _ = h.shape
    nb = k_blk.shape[2]
    n_sel = min(MOBA_TOPK, nb)
    q = apply_partial_rope((h @ w_q).reshape(bsz, s, ATTN_HEADS, ATTN_HEAD_DIM), cos, sin).transpose(0, 2, 1, 3)
    q_blk = jnp.arange(s) // MOBA_BLOCK
    gate = jnp.einsum('bhtd,bhnd->bhtn', q.astype(f32), k_mean)
    past = jnp.arange(nb)[None, :] < q_blk[:, None]
    gate = jnp.where(past, gate, -jnp.inf)
    _, sel = lax.top_k(gate, n_sel)
    n_qc = s // MOBA_Q_CHUNK
    scale = ATTN_HEAD_DIM ** -0.5
    head_ix = jnp.arange(ATTN_HEADS)[:, None, None]

    def attend_chunk(ix):
        b = ix // n_qc
        t0 = (ix % n_qc) * MOBA_Q_CHUNK
        qc = lax.dynamic_slice_in_dim(q[b], t0, MOBA_Q_CHUNK, axis=1).astype(f32)
        sc = lax.dynamic_slice_in_dim(sel[b], t0, MOBA_Q_CHUNK, axis=1)
        kb, vb = k_blk[b], v_blk[b]
        k_sel = kb[head_ix, sc].astype(f32)
        v_sel = vb[head_ix, sc].astype(f32)
        cblk = t0 // MOBA_BLOCK
        k_own = lax.dynamic_index_in_dim(kb, cblk, axis=1, keepdims=False).astype(f32)
        v_own = lax.dynamic_index_in_dim(vb, cblk, axis=1, keepdims=False).astype(f32)
        s_sel = jnp.einsum('htd,htjsd->htjs', qc, k_sel) * scale
        s_sel = jnp.where((jnp.arange(n_sel) < cblk)[None, None, :, None], s_sel, -jnp.inf)
        s_own = jnp.einsum('htd,hsd->hts', qc, k_own) * scale
        t_abs = t0 + jnp.arange(MOBA_Q_CHUNK)
        s_abs = cblk * MOBA_BLOCK + jnp.arange(MOBA_BLOCK)
        s_own = jnp.where(s_abs[None, :] <= t_abs[:, None], s_own, -jnp.inf)
        scores = jnp.concatenate([s_sel.reshape(ATTN_HEADS, MOBA_Q_CHUNK, n_sel * MOBA_BLOCK), s_own], -1)
        p = jax.nn.softmax(scores, -1)
        p_sel = p[..., :n_sel * MOBA_BLOCK].reshape(ATTN_HEADS, MOBA_Q_CHUNK, n_sel, MOBA_BLOCK)
        p_own = p[..., n_sel * MOBA_BLOCK:]
        o = jnp.einsum('htjs,htjsd->htd', p_sel, v_sel) + jnp.einsum('hts,hsd->htd', p_own, v_own)
        return o.astype(h.dtype)

    out = lax.map(attend_chunk, jnp.arange(bsz * n_qc))
    out = out.reshape(bsz, n_qc, ATTN_HEADS, MOBA_Q_CHUNK, ATTN_HEAD_DIM).transpose(0, 1, 3, 2, 4)
    return out.reshape(bsz, s, ATTN_DIM) @ w_o


def moe_ffn(h, w_router, b_router, w_gu, b_gu, w_down, b_down):
    bsz, s, d = h.shape
    t = h.reshape(-1, d)
    n_tok = t.shape[0]
    n_assign = n_tok * TOP_K
    logits = (t @ w_router + b_router).astype(f32)
    top_val, top_idx = lax.top_k(logits, TOP_K)
    gate = jax.nn.softmax(top_val, axis=-1)
    flat_e = top_idx.reshape(-1)
    flat_tok = jnp.arange(n_assign) // TOP_K
    order = jnp.argsort(flat_e)
    e_sorted, tok_sorted = flat_e[order], flat_tok[order]
    g_sorted = gate.reshape(-1)[order]
    counts = jnp.bincount(flat_e, length=N_EXPERTS)
    starts = jnp.cumsum(counts) - counts
    padded = (counts + MOE_ROW_BLOCK - 1) // MOE_ROW_BLOCK * MOE_ROW_BLOCK
    pad_ends = jnp.cumsum(padded)
    pad_starts = pad_ends - padded
    dest = pad_starts[e_sorted] + (jnp.arange(n_assign) - starts[e_sorted])
    n_blocks = -(-n_assign // MOE_ROW_BLOCK) + N_EXPERTS
    n_rows = n_blocks * MOE_ROW_BLOCK
    buf = jnp.zeros((n_rows, d), t.dtype).at[dest].set(t[tok_sorted])
    blk_e = jnp.minimum(jnp.searchsorted(pad_ends, jnp.arange(n_blocks) * MOE_ROW_BLOCK, side='right'),
                        N_EXPERTS - 1)

    def expert_block(args):
        xb, e = args
        gu = xb @ w_gu[e] + b_gu[e]
        g_lin = jnp.minimum(gu[:, :D_EXPERT], SWIGLU_LIMIT)
        u_lin = jnp.clip(gu[:, D_EXPERT:], -SWIGLU_LIMIT, SWIGLU_LIMIT)
        act = (u_lin + 1.0) * (g_lin * jax.nn.sigmoid(g_lin * SWIGLU_ALPHA))
        return act @ w_down[e] + b_down[e]

    ybuf = lax.map(expert_block, (buf.reshape(n_blocks, MOE_ROW_BLOCK, d), blk_e)).reshape(n_rows, d)
    y = jnp.zeros((n_tok, d), f32).at[tok_sorted].add(g_sorted[:, None] * ybuf[dest].astype(f32))
    return y.astype(h.dtype).reshape(bsz, s, d)


def setup_inputs(seed: int = 0) -> dict:
    key = jax.random.key(seed)
    ks = jax.random.split(key, 24)
    n_a, n_b = N_A_LAYERS, DEPTH - N_A_LAYERS

    def nrm(k, shape, scale):
        return jax.random.normal(k, shape, f32) * scale

    dt0 = jnp.exp(jax.random.uniform(ks[4], (n_a, SSM_HEADS), f32, math.log(1e-3), math.log(1e-1)))
    return {
        "x": nrm(ks[0], (BATCH, SEQ, D_MODEL), 1.0),
        "ssm_w_in": nrm(ks[1], (n_a, D_MODEL, IN_PROJ_DIM), D_MODEL ** -0.5),
        "ssm_conv_w": nrm(ks[2], (n_a, CONV_WIDTH, CONV_DIM), CONV_WIDTH ** -0.5),
        "ssm_conv_b": nrm(ks[3], (n_a, CONV_DIM), 0.02),
        "ssm_dt_bias": dt0 + jnp.log(-jnp.expm1(-dt0)),
        "ssm_a_log": jnp.log(jax.random.uniform(ks[5], (n_a, SSM_HEADS), f32, 1.0, 16.0)),
        "ssm_d": 1.0 + nrm(ks[6], (n_a, SSM_HEADS), 0.01),
        "ssm_norm_g": 1.0 + nrm(ks[7], (n_a, D_INNER), 0.01),
        "ssm_w_out": nrm(ks[8], (n_a, D_INNER, D_MODEL), D_INNER ** -0.5 * DEEPNORM_BETA),
        "kv_w_k": nrm(ks[9], (D_MODEL, ATTN_DIM), D_MODEL ** -0.5),
        "kv_w_v": nrm(ks[10], (D_MODEL, ATTN_DIM), D_MODEL ** -0.5 * DEEPNORM_BETA),
        "attn_w_q": nrm(ks[11], (n_b, D_MODEL, ATTN_DIM), D_MODEL ** -0.5),
        "attn_w_o": nrm(ks[12], (n_b, ATTN_DIM, D_MODEL), ATTN_DIM ** -0.5 * DEEPNORM_BETA),
        "moe_w_router": nrm(ks[13], (DEPTH, D_MODEL, N_EXPERTS), D_MODEL ** -0.5),
        "moe_b_router": nrm(ks[14], (DEPTH, N_EXPERTS), 0.01),
        "moe_w_gate_up": nrm(ks[15], (DEPTH, N_EXPERTS, D_MODEL, 2 * D_EXPERT), D_MODEL ** -0.5),
        "moe_b_gate_up": nrm(ks[16], (DEPTH, N_EXPERTS, 2 * D_EXPERT), 0.01),
        "moe_w_down": nrm(ks[17], (DEPTH, N_EXPERTS, D_EXPERT, D_MODEL), D_EXPERT ** -0.5 * DEEPNORM_BETA),
        "moe_b_down": nrm(ks[18], (DEPTH, N_EXPERTS, D_MODEL), 0.01),
        "ln_mix_g": 1.0 + nrm(ks[19], (DEPTH, D_MODEL), 0.01),
        "ln_mix_b": nrm(ks[20], (DEPTH, D_MODEL), 0.01),
        "ln_ffn_g": 1.0 + nrm(ks[21], (DEPTH, D_MODEL), 0.01),
        "ln_ffn_b": nrm(ks[22], (DEPTH, D_MODEL), 0.01),
    }


def reference(x, ssm_w_in, ssm_conv_w, ssm_conv_b, ssm_dt_bias, ssm_a_log, ssm_d, ssm_norm_g, ssm_w_out,
              kv_w_k, kv_w_v, attn_w_q, attn_w_o, moe_w_router, moe_b_router, moe_w_gate_up, moe_b_gate_up,
              moe_w_down, moe_b_down, ln_mix_g, ln_mix_b, ln_ffn_g, ln_ffn_b):
    s = x.shape[1]
    cos, sin = rope_tables(s)
    h = x
    k_blk = v_blk = k_mean = None
    for l in range(DEPTH):
        if l < N_A_LAYERS:
            mix = mamba2_mixer(h, ssm_w_in[l], ssm_conv_w[l], ssm_conv_b[l], ssm_dt_bias[l], ssm_a_log[l],
                               ssm_d[l], ssm_norm_g[l], ssm_w_out[l])
        else:
            if l == N_A_LAYERS:
                k_blk, v_blk, k_mean = shared_kv(h, kv_w_k, kv_w_v, cos, sin)
            j = l - N_A_LAYERS
            mix = moba_attention(h, attn_w_q[j], attn_w_o[j], k_blk, v_blk, k_mean, cos, sin)
        h = layer_norm(DEEPNORM_ALPHA * h + mix, ln_mix_g[l], ln_mix_b[l])
        ffn = moe_ffn(h, moe_w_router[l], moe_b_router[l], moe_w_gate_up[l], moe_b_gate_up[l],
                      moe_w_down[l], moe_b_down[l])
        h = layer_norm(DEEPNORM_ALPHA * h + ffn, ln_ffn_g[l], ln_ffn_b[l])
    return h
```

```python
import math
from contextlib import ExitStack

import numpy as np
import ml_dtypes

import concourse.bass as bass
import concourse.mybir as mybir
from concourse.bass_utils import run_bass_kernel_spmd

F32 = mybir.dt.float32
BF16 = mybir.dt.bfloat16
AF = mybir.ActivationFunctionType
ALU = mybir.AluOpType
AX = mybir.AxisListType

D = 1024
S = 2048
NT = 16
DEPTH = 4
NE = 32
CAP = 512
NJT = CAP // 128
ALPHA = (2 * DEPTH) ** 0.25
LN_EPS = 1e-5
G = 1024


class Op:
    __slots__ = ("eng", "fn", "deps", "dma", "signal", "sig", "idx")

    def __init__(self, eng, fn, deps, dma, idx):
        self.eng = eng
        self.fn = fn
        self.deps = deps
        self.dma = dma
        self.signal = False
        self.sig = None
        self.idx = idx


class Prog:
    NDMA = 24

    def __init__(self, nc):
        self.nc = nc
        self.ops = []
        self.last_writer = {}
        self.readers = {}

    def add(self, eng, fn, reads=(), writes=(), dma=False):
        idx = len(self.ops)
        ops = self.ops
        deps = set()
        for k in reads:
            w = self.last_writer.get(k)
            if w is not None:
                deps.add(w)
        for k in writes:
            w = self.last_writer.get(k)
            if w is not None:
                o = ops[w]
                if o.dma or dma or o.eng != eng:
                    deps.add(w)
            for r in self.readers.get(k, ()):
                o = ops[r]
                if o.dma or dma or o.eng != eng:
                    deps.add(r)
        if eng == "pe" and not dma:
            deps = {d for d in deps if ops[d].dma or ops[d].eng != "pe"}
        latest = {}
        pruned = set()
        for d in deps:
            o = ops[d]
            if o.dma:
                pruned.add(d)
            elif latest.get(o.eng, -1) < d:
                latest[o.eng] = d
        pruned.update(latest.values())
        deps = pruned
        op = Op(eng, fn, deps, dma, idx)
        ops.append(op)
        for k in reads:
            self.readers.setdefault(k, []).append(idx)
        for k in writes:
            self.last_writer[k] = idx
            self.readers[k] = []
        return op

    def emit(self, stack):
        nc = self.nc
        ops = self.ops
        for op in ops:
            for d in op.deps:
                ops[d].signal = True
        engs = ["pe", "act", "dve", "pool", "sp"]
        esem = {e: stack.enter_context(nc.semaphore("s_" + e)) for e in engs}
        dsem = {e: [stack.enter_context(nc.semaphore("d_%s_%d" % (e, i))) for i in range(self.NDMA)]
                for e in ("sp", "act", "pool")}
        cnt = {e: 0 for e in engs}
        dcnt = {e: 0 for e in dsem}
        prewait = {}
        for op in ops:
            if op.dma:
                op.signal = True
                i = dcnt[op.eng]
                dcnt[op.eng] += 1
                sem = dsem[op.eng][i % self.NDMA]
                op.sig = (sem, 16 * (i // self.NDMA + 1))
                if i >= self.NDMA:
                    prewait[op.idx] = (sem, 16 * (i // self.NDMA))
            elif op.signal:
                cnt[op.eng] += 1
                op.sig = (esem[op.eng], cnt[op.eng])
        per = {e: [op for op in ops if op.eng == e] for e in engs}
        block = stack.enter_context(nc.Block())
        self.nwaits = 0

        def body(e):
            def run(h):
                waited = {}
                for op in per[e]:
                    need = {}
                    if op.idx in prewait:
                        s, v = prewait[op.idx]
                        need[id(s)] = (s, v)
                    for d in op.deps:
                        s, v = ops[d].sig
                        cur = need.get(id(s))
                        if cur is None or cur[1] < v:
                            need[id(s)] = (s, v)
                    for sid, (s, v) in need.items():
                        if waited.get(sid, 0) < v:
                            h.wait_ge(s, v)
                            waited[sid] = v
                            self.nwaits += 1
                    ins = op.fn(h)
                    if op.signal and ins is not None:
                        s, v = op.sig
                        ins.then_inc(s, 16 if op.dma else 1)
            return run

        block.tensor(body("pe"))
        block.scalar(body("act"))
        block.vector(body("dve"))
        block.gpsimd(body("pool"))
        block.sync(body("sp"))
        self.counts = dict(cnt)


class V:
    __slots__ = ("ap", "keys")

    def __init__(self, ap, keys):
        self.ap = ap
        self.keys = keys


def _prod(s):
    n = 1
    for x in s:
        n *= x
    return n


_RE = {2: "p (a b) -> p a b", 3: "p (a b c) -> p a b c"}


class Buf:
    def __init__(self, M, name, nbytes):
        self.name = name
        self.nbytes = nbytes
        self.t = M.st.enter_context(M.nc.sbuf_tensor(name, [128, nbytes // 2], BF16))

    def view(self, off, shape, dt=BF16, p0=0, parts=128):
        esz = 4 if dt == F32 else 2
        n = _prod(shape)
        assert off % 4 == 0 and off + n * esz <= self.nbytes, (self.name, off, shape)
        a = self.t[p0:p0 + parts, off // 2:(off + n * esz) // 2]
        if dt == F32:
            a = a.bitcast(F32)
        if len(shape) == 2:
            a = a.rearrange("p (a b) -> p a b", a=shape[0])
        elif len(shape) == 3:
            a = a.rearrange("p (a b c) -> p a b c", a=shape[0], b=shape[1])
        keys = [(self.name, g) for g in range(off // G, (off + n * esz - 1) // G + 1)]
        return V(a, keys)


def keys_of(vs):
    out = []
    for v in vs:
        if isinstance(v, V):
            out.extend(v.keys)
        elif isinstance(v, (list, tuple)) and v and isinstance(v[0], (V,)):
            out.extend(keys_of(v))
        else:
            out.append(v)
    return out


def make_consts():
    c = {}
    c["ident_f"] = np.eye(128, dtype=np.float32)
    c["iota_row"] = np.broadcast_to(np.arange(CAP, dtype=np.float32), (128, CAP)).copy()
    c["piota"] = (np.arange(128, dtype=np.float32)[:, None] + 128.0 * np.arange(NJT, dtype=np.float32)[None, :]).copy()
    c["triu"] = np.triu(np.ones((128, 128), np.float32), k=1)
    c["ones"] = np.ones((128, 128), np.float32)
    m = np.where(np.arange(128)[None, :] >= np.arange(128)[:, None], 0.0, -30000.0).astype(np.float32)
    c["maskneg"] = np.tile(m, (1, 4))
    inv_freq = (500000.0 ** (-np.arange(0, 16, 2, dtype=np.float32) / 16.0)).astype(np.float32)
    ang = np.arange(S, dtype=np.float32)[:, None] * inv_freq[None, :]
    cos, sin = np.cos(ang).astype(np.float32), np.sin(ang).astype(np.float32)
    cosf = np.ones((128, S), np.float32)
    sinf = np.zeros((128, S), np.float32)
    rotm = np.zeros((128, 128), np.float32)
    for hl in range(2):
        for dd in range(16):
            cosf[64 * hl + dd] = cos[:, dd % 8]
            sinf[64 * hl + dd] = sin[:, dd % 8]
        for dd in range(8):
            rotm[64 * hl + dd + 8, 64 * hl + dd] = -1.0
            rotm[64 * hl + dd, 64 * hl + dd + 8] = 1.0
    c["cosf"], c["sinf"], c["rotm"] = cosf, sinf, rotm
    c["tri"] = np.triu(np.ones((128, 128), np.float32), k=0)
    padm = np.zeros((8, 8), np.float32)
    for cb in range(8):
        padm[cb, cb:] = -1e30
    c["padm"] = np.broadcast_to(padm.reshape(1, 64), (128, 64)).copy()
    return c


CONST_SHAPES = {"ident_f": [128, 128], "iota_row": [128, CAP], "piota": [128, NJT],
                "triu": [128, 128], "ones": [128, 128], "maskneg": [128, 512], "cosf": [128, S], "sinf": [128, S],
                "rotm": [128, 128], "tri": [128, 128], "padm": [128, 64]}


class MK:
    def __init__(self, layers, phases, debug_in=None):
        self.layers = layers
        self.phases = phases
        self.debug = debug_in
        self.nc = bass.Bass("TRN2", target_bir_lowering=False)
        self.st = ExitStack()

    def op(self, eng, fn, r=(), w=(), dma=False):
        return self.P.add(eng, fn, keys_of(r), keys_of(w), dma)

    def dram_in(self, name, shape):
        return self.nc.dram_tensor(name, list(shape), F32, kind="ExternalInput").ap()

    def psb(self, b, cols=512, parts=128, c0=0):
        return V(self.ps[0:parts, b, c0:c0 + cols], [("ps", b)])

    def build(self):
        nc, st = self.nc, self.st
        nl = len(self.layers)
        self.x = self.dram_in("x", [S, D])
        self.y = nc.dram_tensor("y", [S, D], F32, kind="ExternalOutput").ap()
        self.w_router = self.dram_in("moe_w_router", [nl, D, NE])
        self.b_router = self.dram_in("moe_b_router", [nl, NE])
        self.w_gu = self.dram_in("moe_w_gate_up", [nl, NE, D, 2 * D])
        self.b_gu = self.dram_in("moe_b_gate_up", [nl, NE, 2 * D])
        self.w_dn = self.dram_in("moe_w_down", [nl, NE, D, D])
        self.b_dn = self.dram_in("moe_b_down", [nl, NE, D])
        self.ln_g = {"mix": self.dram_in("ln_mix_g", [nl, D]), "moe": self.dram_in("ln_ffn_g", [nl, D])}
        self.ln_b = {"mix": self.dram_in("ln_mix_b", [nl, D]), "moe": self.dram_in("ln_ffn_b", [nl, D])}
        self.cst = {k: self.dram_in("c_" + k, s) for k, s in CONST_SHAPES.items()}
        na = max(1, sum(1 for l in self.layers if l < 2))
        self.ssm_w_in = self.dram_in("ssm_w_in", [na, D, 5152])
        self.ssm_conv_w = self.dram_in("ssm_conv_w", [na, 4, 3072])
        self.ssm_conv_b = self.dram_in("ssm_conv_b", [na, 3072])
        self.ssm_dt_bias = self.dram_in("ssm_dt_bias", [na, 32])
        self.ssm_a_log = self.dram_in("ssm_a_log", [na, 32])
        self.ssm_d = self.dram_in("ssm_d", [na, 32])
        self.ssm_norm_g = self.dram_in("ssm_norm_g", [na, 2048])
        self.ssm_w_out = self.dram_in("ssm_w_out", [na, 2048, D])
        self.attn_layers = [l for l in self.layers if l >= 2]
        nbl = max(1, len(self.attn_layers))
        self.kv_w_k = self.dram_in("kv_w_k", [D, D])
        self.kv_w_v = self.dram_in("kv_w_v", [D, D])
        self.attn_w_q = self.dram_in("attn_w_q", [nbl, D, D])
        self.attn_w_o = self.dram_in("attn_w_o", [nbl, D, D])
        self.kt_s = self.y[0:1024, :].rearrange("(p a) c -> p (a c)", p=128)
        self.va_s = self.y[1024:2048, :].rearrange("(p a) c -> p (a c)", p=128)

        self.dbg = nc.dram_tensor("dbg", [128, 2048], F32, kind="ExternalOutput").ap() if self.debug else None
        self.P = Prog(nc)
        self.ps = st.enter_context(nc.psum_tensor("ps", [128, 8, 512], F32))
        self.Rb = Buf(self, "R", NT * D * 4)
        self.HBb = Buf(self, "HB", NT * D * 2)
        self.HTb = Buf(self, "HT", 8 * S * 2)
        self.Cb = Buf(self, "CST", 6 * 1024)
        self.Ab = Buf(self, "ARENA", 72 * 1024)
        self.R = [self.Rb.view(i * D * 4, [D], F32) for i in range(NT)]
        self.HB = [self.HBb.view(i * D * 2, [D], BF16) for i in range(NT)]
        self.HT = self.HTb.view(0, [8, S], BF16)

        self.load_consts()
        for i in range(NT):
            self.op("sp", lambda h, i=i: h.dma_start(out=self.R[i].ap, in_=self.x[i * 128:(i + 1) * 128, :]),
                    r=["x"], w=[self.R[i]], dma=True)
        first = True
        for kind, l in self.phases:
            li = self.layers.index(l)
            if kind == "prep":
                for i in range(NT):
                    self.make_copies(i)
            elif kind == "nomix":
                for i in range(NT):
                    Ri = self.R[i]
                    self.op("act", lambda h, Ri=Ri: h.activation(out=Ri.ap, in_=Ri.ap, func=AF.Copy, scale=ALPHA), r=[Ri], w=[Ri])
                self.layer_norm("mix", li)
            elif kind == "attn":
                self.attention(l, li)
                self.layer_norm("mix", li)
            elif kind == "mamba":
                self.mamba(li)
                self.layer_norm("mix", li)
            elif kind == "moe":
                self.moe(li)
                self.layer_norm("moe", li)
        for i in range(NT):
            self.op("sp", lambda h, i=i: h.dma_start(out=self.y[i * 128:(i + 1) * 128, :], in_=self.R[i].ap),
                    r=[self.R[i]], w=["y%d" % i, "kt_s", "va_s"], dma=True)
        self.P.add("sp", lambda h: None, reads=["y%d" % i for i in range(NT)])
        self.P.emit(st)
        return nc

    def load_consts(self):
        cb = self.Cb
        off = 0
        self.C = {}

        def alloc(n_bytes):
            nonlocal off
            o = off
            off += (n_bytes + 3) // 4 * 4
            return o
        for k in ("maskneg",):
            v = cb.view(alloc(512 * 4), [512], F32)
            self.C[k] = v
            self.op("sp", lambda h, v=v, k=k: h.dma_start(out=v.ap, in_=self.cst[k]), r=[], w=[v], dma=True)
        v = cb.view(alloc(128 * 4), [128], F32)
        self.C["ones_f"] = v
        self.op("sp", lambda h, v=v: h.dma_start(out=v.ap, in_=self.cst["ones"]), r=[], w=[v], dma=True)
        for k in ("ident_f", "piota"):
            shp = CONST_SHAPES[k]
            v = cb.view(alloc(shp[1] * 4), [shp[1]], F32)
            self.C[k] = v
            self.op("sp", lambda h, v=v, k=k: h.dma_start(out=v.ap, in_=self.cst[k]), r=[], w=[v], dma=True)
        v = cb.view(alloc(CAP * 4), [CAP], F32)
        self.C["iota_row"] = v
        self.op("sp", lambda h, v=v: h.dma_start(out=v.ap, in_=self.cst["iota_row"]), r=[], w=[v], dma=True)
        for k in ("triu", "ones"):
            v = cb.view(alloc(128 * 2), [128], BF16)
            self.C[k] = v
            self.op("pool", lambda h, v=v, k=k: h.dma_start(out=v.ap, in_=self.cst[k]), r=[], w=[v], dma=True)
        v = cb.view(alloc(128 * 2), [128], BF16)
        self.C["ident_b"] = v
        self.op("pool", lambda h, v=v: h.dma_start(out=v.ap, in_=self.cst["ident_f"]), r=[], w=[v], dma=True)
        self.cst_off = off

    def layer_norm(self, which, li, scale_in_place=True):
        A = self.Ab
        gt = A.view(0, [D], F32)
        bt = A.view(4096, [D], F32)
        st6 = A.view(8192, [2, 6], F32)
        mv = A.view(8192 + 64, [2], F32)
        rstd = A.view(8192 + 128, [1], F32)
        nmr = A.view(8192 + 192, [1], F32)
        tmp = A.view(9216, [D], F32)
        self.op("sp", lambda h: h.dma_start(out=gt.ap, in_=self.ln_g[which][li:li + 1, :].partition_broadcast(128)),
                r=[], w=[gt], dma=True)
        self.op("sp", lambda h: h.dma_start(out=bt.ap, in_=self.ln_b[which][li:li + 1, :].partition_broadcast(128)),
                r=[], w=[bt], dma=True)
        for i in range(NT):
            Ri = self.R[i]
            for c in range(2):
                self.op("dve", lambda h, c=c, Ri=Ri: h.bn_stats(out=st6.ap[:, c, :], in_=Ri.ap[:, c * 512:(c + 1) * 512]),
                        r=[Ri], w=[st6])
            self.op("dve", lambda h: h.bn_aggr(out=mv.ap, in_=st6.ap.rearrange("p a b -> p (a b)")), r=[st6], w=[mv])
            self.op("act", lambda h: h.activation(out=rstd.ap, in_=mv.ap[:, 1:2], func=AF.Sqrt, bias=LN_EPS, scale=1.0),
                    r=[mv], w=[rstd])
            self.op("dve", lambda h: h.reciprocal(out=rstd.ap, in_=rstd.ap), r=[rstd], w=[rstd])
            self.op("dve", lambda h: h.scalar_tensor_tensor(out=nmr.ap, in0=mv.ap[:, 0:1], scalar=-1.0, in1=rstd.ap, op0=ALU.mult, op1=ALU.mult),
                    r=[mv, rstd], w=[nmr])
            self.op("act", lambda h, Ri=Ri: h.activation(out=tmp.ap, in_=Ri.ap, func=AF.Identity, bias=nmr.ap, scale=rstd.ap), r=[Ri, nmr, rstd], w=[tmp])
            self.op("dve", lambda h: h.tensor_tensor(out=tmp.ap, in0=tmp.ap, in1=gt.ap, op=ALU.mult), r=[tmp, gt], w=[tmp])
            self.op("dve", lambda h, Ri=Ri: h.tensor_tensor(out=Ri.ap, in0=tmp.ap, in1=bt.ap, op=ALU.add), r=[tmp, bt], w=[Ri])
            self.make_copies(i)

    def make_copies(self, i):
            Ri = self.R[i]
            HBi = self.HB[i]
            self.op("act", lambda h, Ri=Ri, HBi=HBi: h.activation(out=HBi.ap, in_=Ri.ap, func=AF.Copy), r=[Ri], w=[HBi])
            pb = 6 + (i % 2)
            pt = V(self.ps[:, pb, :].bitcast(BF16)[:, 0:1024].rearrange("p (a b) -> p a b", a=8), [("ps", pb)])
            for m in range(8):
                self.op("pe", lambda h, m=m, HBi=HBi, pt=pt: h.transpose(out=pt.ap[:, m, :], in_=HBi.ap[:, m * 128:(m + 1) * 128],
                                                                          identity=self.C["ident_b"].ap),
                        r=[HBi, self.C["ident_b"]], w=[pt])
            htv = V(self.HT.ap[:, :, i * 128:(i + 1) * 128], [("HT", g) for g in range(8 * S * 2 // G)])
            self.op("act", lambda h, pt=pt, htv=htv: h.activation(out=htv.ap, in_=pt.ap, func=AF.Copy), r=[pt], w=[htv])


    def mamba(self, li):
        A, HBb, C = self.Ab, self.HBb, self.C
        identf = C["ident_f"].ap
        op = self.op
        ACSF = HBb.view(0, [S], F32, parts=32)
        NBF = HBb.view(8192, [S], F32, parts=32)
        NBH = [HBb.view(16384 + 2048 * i, [512], F32, parts=32) for i in range(2)]
        ONESF = HBb.view(20480, [128], F32)
        PAR = HBb.view(21504, [8], F32, parts=32)
        ACST = HBb.view(24576, [16, 32], F32)
        W2T = HBb.view(26624, [16, 32], F32)
        EACST = HBb.view(28672, [16, 32], F32)
        DECC = HBb.view(30720, [16, 32], F32)
        o = 0

        def alloc(n):
            nonlocal o
            a = o
            o += (n + G - 1) // G * G
            return a
        CWT = A.view(alloc(6 * 4 * 4), [6, 4], F32)
        CBT = A.view(alloc(6 * 4), [6], F32)
        DBC = A.view(alloc(32 * 4), [32], F32)
        NGg = A.view(alloc(512 * 4), [512], F32)
        xf_off = alloc(4 * S * 2)
        XF = A.view(xf_off, [4, S], BF16)
        T1 = A.view(xf_off, [S], F32, parts=32)
        T2 = A.view(xf_off + S * 4, [S], F32, parts=32)
        bf_off = alloc(S * 2)
        BF = A.view(bf_off, [S], BF16)
        WDT = A.view(bf_off, [8, 32], BF16)
        CF = A.view(alloc(S * 2), [S], BF16)
        STATE = A.view(alloc(512 * 4), [512], F32)
        reg = o
        o = reg
        WX = A.view(alloc(8 * 512 * 2), [8, 512], BF16)
        WB = A.view(alloc(8 * 128 * 2), [8, 128], BF16)
        WC = A.view(alloc(8 * 128 * 2), [8, 128], BF16)
        U = A.view(alloc((S + 4) * 4), [S + 4], F32)
        ACC = [A.view(alloc(512 * 4), [512], F32) for _ in range(2)]
        o_conv = o
        o = reg
        WZ = A.view(alloc(8 * 512 * 2), [8, 512], BF16)
        WO = A.view(alloc(4 * D * 2), [4, D], BF16)
        GT4 = A.view(alloc(512 * 4), [4, 128], F32)
        ARG = [A.view(alloc(512 * 4), [512], F32) for _ in range(2)]
        MT = [A.view(alloc(512 * 2), [4, 128], BF16) for _ in range(8)]
        XTOK = A.view(alloc(512 * 2), [512], BF16)
        BTOK = A.view(alloc(128 * 2), [128], BF16)
        PREV = A.view(alloc(512 * 2), [512], BF16)
        xdt_off = alloc(512 * 2)
        XDT2 = A.view(xdt_off, [512], BF16)
        Y1 = A.view(alloc(512 * 4), [512], F32)
        xd_off = alloc(512 * 4)
        XD = A.view(xd_off, [512], F32)
        ZS = A.view(alloc(512 * 2), [512], BF16)
        JUNK = A.view(xd_off, [512], F32)
        SS = A.view(alloc(16), [4], F32)
        YN = A.view(alloc(512 * 2), [512], BF16)
        YNT = A.view(xdt_off, [4, 128], BF16)
        assert max(o, o_conv) <= A.nbytes, (o, o_conv)
        w_in = self.ssm_w_in[li].rearrange("(kc kp) c -> kp kc c", kp=128)

        for i in range(NT):
            Ri = self.R[i]
            op("act", lambda h, Ri=Ri: h.activation(out=Ri.ap, in_=Ri.ap, func=AF.Copy, scale=ALPHA), r=[Ri], w=[Ri])

        op("pool", lambda h: h.dma_start(out=WDT.ap, in_=w_in[:, :, 5120:5152]), r=[], w=[WDT], dma=True)
        op("sp", lambda h: h.dma_start(out=PAR.ap[:, 0:1], in_=self.ssm_dt_bias[li:li + 1, :].rearrange("o h -> h o")), r=[], w=[PAR], dma=True)
        op("sp", lambda h: h.dma_start(out=PAR.ap[:, 1:2], in_=self.ssm_a_log[li:li + 1, :].rearrange("o h -> h o")), r=[], w=[PAR], dma=True)
        op("sp", lambda h: h.dma_start(out=DBC.ap, in_=self.ssm_d[li:li + 1, :].partition_broadcast(128)), r=[], w=[DBC], dma=True)
        op("dve", lambda h: h.memset(ONESF.ap, 1.0), r=[], w=[ONESF])
        for r in range(4):
            pd = self.psb(r % 2, parts=32)
            for k in range(8):
                op("pe", lambda h, k=k, r=r, pd=pd: h.matmul(pd.ap, lhsT=WDT.ap[:, k, :], rhs=self.HT.ap[:, k, r * 512:(r + 1) * 512],
                                                           start=(k == 0), stop=(k == 7)), r=[WDT, self.HT], w=[pd])
            op("act", lambda h, r=r, pd=pd: h.activation(out=T1.ap[:, r * 512:(r + 1) * 512], in_=pd.ap, func=AF.Exp, bias=PAR.ap[:, 0:1], scale=1.0),
               r=[pd, PAR], w=[T1])
        op("act", lambda h: h.activation(out=T1.ap, in_=T1.ap, func=AF.Ln, bias=1.0, scale=1.0), r=[T1], w=[T1])
        op("act", lambda h: h.activation(out=T2.ap, in_=T1.ap, func=AF.Ln), r=[T1], w=[T2])
        op("act", lambda h: h.activation(out=PAR.ap[:, 2:3], in_=PAR.ap[:, 1:2], func=AF.Exp), r=[PAR], w=[PAR])
        op("dve", lambda h: h.tensor_scalar(out=PAR.ap[:, 2:3], in0=PAR.ap[:, 2:3], scalar1=-1.0, scalar2=None, op0=ALU.mult), r=[PAR], w=[PAR])
        op("dve", lambda h: h.tensor_scalar(out=T1.ap, in0=T1.ap, scalar1=PAR.ap[:, 2:3], scalar2=None, op0=ALU.mult), r=[T1, PAR], w=[T1])
        for c in range(16):
            op("dve", lambda h, c=c: h.tensor_tensor_scan(out=ACSF.ap[:, c * 128:(c + 1) * 128], data0=ONESF.ap[0:32, :], data1=T1.ap[:, c * 128:(c + 1) * 128],
                                                         initial=0.0, op0=ALU.mult, op1=ALU.add), r=[ONESF, T1], w=[ACSF])
        op("dve", lambda h: h.tensor_tensor(out=NBF.ap, in0=T2.ap, in1=ACSF.ap, op=ALU.subtract), r=[T2, ACSF], w=[NBF])
        for c in range(16):
            op("act", lambda h, c=c: h.activation(out=T2.ap[:, c * 128:(c + 1) * 128], in_=NBF.ap[:, c * 128:(c + 1) * 128], func=AF.Exp,
                                                  bias=ACSF.ap[:, c * 128 + 127:c * 128 + 128], scale=1.0), r=[NBF, ACSF], w=[T2])
        pa = self.psb(2)
        pw = self.psb(3)
        for c in range(16):
            op("pe", lambda h, c=c: h.transpose(out=pa.ap[:, c * 32:(c + 1) * 32], in_=ACSF.ap[:, c * 128:(c + 1) * 128], identity=identf[0:32, 0:32]),
               r=[ACSF, C["ident_f"]], w=[pa])
            op("pe", lambda h, c=c: h.transpose(out=pw.ap[:, c * 32:(c + 1) * 32], in_=T2.ap[:, c * 128:(c + 1) * 128], identity=identf[0:32, 0:32]),
               r=[T2, C["ident_f"]], w=[pw])
        op("act", lambda h: h.activation(out=ACST.ap.rearrange("p a b -> p (a b)"), in_=pa.ap, func=AF.Copy), r=[pa], w=[ACST])
        op("act", lambda h: h.activation(out=W2T.ap.rearrange("p a b -> p (a b)"), in_=pw.ap, func=AF.Copy), r=[pw], w=[W2T])
        op("act", lambda h: h.activation(out=EACST.ap, in_=ACST.ap, func=AF.Exp), r=[ACST], w=[EACST])
        op("dve", lambda h: h.tensor_scalar(out=Y1.ap, in0=ACST.ap.rearrange("p a b -> p (a b)"), scalar1=identf[:, 127:128], scalar2=None, op0=ALU.mult),
           r=[ACST, C["ident_f"]], w=[Y1])
        pdc = self.psb(0)
        op("pe", lambda h: h.matmul(pdc.ap, lhsT=C["ones_f"].ap, rhs=Y1.ap, start=True, stop=True), r=[C["ones_f"], Y1], w=[pdc])
        op("act", lambda h: h.activation(out=DECC.ap.rearrange("p a b -> p (a b)"), in_=pdc.ap, func=AF.Exp), r=[pdc], w=[DECC])

        if getattr(self, "dbg", None) is not None:
            for q, tv in enumerate((ACST, W2T, EACST, DECC)[3:], 3):
                op("sp", lambda h, q=q, tv=tv: h.dma_start(out=self.dbg[:, q * 512:(q + 1) * 512], in_=tv.ap.rearrange("p a b -> p (a b)")),
                   r=[tv], w=["dbg%d" % q], dma=True)
        for g in range(4):
            op("pool", lambda h, g=g: h.dma_start(out=WX.ap, in_=w_in[:, :, 2048 + g * 512:2048 + (g + 1) * 512]), r=[], w=[WX], dma=True)
            op("pool", lambda h, g=g: h.dma_start(out=WB.ap, in_=w_in[:, :, 4096 + g * 128:4096 + (g + 1) * 128]), r=[], w=[WB], dma=True)
            op("pool", lambda h, g=g: h.dma_start(out=WC.ap, in_=w_in[:, :, 4608 + g * 128:4608 + (g + 1) * 128]), r=[], w=[WC], dma=True)
            op("dve", lambda h: h.memset(U.ap[:, 0:4], 0.0), r=[], w=[U])
            bases = [g * 512 + j * 128 for j in range(4)] + [2048 + g * 128, 2560 + g * 128]
            for j, b0 in enumerate(bases):
                op("sp", lambda h, j=j, b0=b0: h.dma_start(out=CWT.ap[:, j, :], in_=self.ssm_conv_w[li, :, b0:b0 + 128].rearrange("k p -> p k"),
                                                          allow_slow_non_contiguous=True), r=[], w=[CWT], dma=True)
                op("sp", lambda h, j=j, b0=b0: h.dma_start(out=CBT.ap[:, j:j + 1], in_=self.ssm_conv_b[li:li + 1, b0:b0 + 128].rearrange("o p -> p o")),
                   r=[], w=[CBT], dma=True)
            n = 0
            for j in range(6):
                for r in range(4):
                    pc = self.psb(6 + (n % 2))
                    for k in range(8):
                        if j < 4:
                            lw = WX.ap[:, k, j * 128:(j + 1) * 128]
                            wv = WX
                        else:
                            wv = WB if j == 4 else WC
                            lw = wv.ap[:, k, :]
                        op("pe", lambda h, k=k, r=r, lw=lw, pc=pc: h.matmul(pc.ap, lhsT=lw, rhs=self.HT.ap[:, k, r * 512:(r + 1) * 512],
                                                                       start=(k == 0), stop=(k == 7)), r=[wv, self.HT], w=[pc])
                    op("act", lambda h, r=r, pc=pc: h.activation(out=U.ap[:, 4 + r * 512:4 + (r + 1) * 512], in_=pc.ap, func=AF.Copy), r=[pc], w=[U])
                    acc = ACC[n % 2]
                    eng = "dve"
                    op(eng, lambda h, r=r, j=j, acc=acc: h.tensor_scalar(out=acc.ap, in0=U.ap[:, 1 + r * 512:1 + (r + 1) * 512], scalar1=CWT.ap[:, j, 0:1],
                                                                        scalar2=None, op0=ALU.mult), r=[U, CWT], w=[acc])
                    for t in range(1, 4):
                        op(eng, lambda h, r=r, j=j, t=t, acc=acc: h.scalar_tensor_tensor(out=acc.ap, in0=U.ap[:, 1 + t + r * 512:1 + t + (r + 1) * 512],
                                                                                         scalar=CWT.ap[:, j, t:t + 1], in1=acc.ap, op0=ALU.mult, op1=ALU.add),
                           r=[U, CWT, acc], w=[acc])
                    if j < 4:
                        dst, dv = XF.ap[:, j, r * 512:(r + 1) * 512], XF
                    elif j == 4:
                        dst, dv = BF.ap[:, r * 512:(r + 1) * 512], BF
                    else:
                        dst, dv = CF.ap[:, r * 512:(r + 1) * 512], CF
                    op("act", lambda h, j=j, acc=acc, dst=dst: h.activation(out=dst, in_=acc.ap, func=AF.Silu, bias=CBT.ap[:, j:j + 1], scale=1.0),
                       r=[acc, CBT], w=[dv])
                    n += 1
            op("pool", lambda h, g=g: h.dma_start(out=WZ.ap, in_=w_in[:, :, g * 512:(g + 1) * 512]), r=[], w=[WZ], dma=True)
            op("pool", lambda h, g=g: h.dma_start(out=WO.ap, in_=self.ssm_w_out[li, g * 512:(g + 1) * 512, :].rearrange("(c p) d -> p c d", p=128)),
               r=[], w=[WO], dma=True)
            op("sp", lambda h, g=g: h.dma_start(out=NGg.ap, in_=self.ssm_norm_g[li:li + 1, g * 512:(g + 1) * 512].partition_broadcast(128)),
               r=[], w=[NGg], dma=True)
            op("dve", lambda h: h.memset(STATE.ap, 0.0), r=[], w=[STATE])
            hs = slice(8 * g, 8 * g + 8)
            for cb in range(4):
                pgt = self.psb(5)
                for c in range(4):
                    cc = 4 * cb + c
                    op("pe", lambda h, c=c, cc=cc: h.matmul(pgt.ap[:, c * 128:(c + 1) * 128], lhsT=BF.ap[:, cc * 128:(cc + 1) * 128],
                                                           rhs=CF.ap[:, cc * 128:(cc + 1) * 128], start=True, stop=True), r=[BF, CF], w=[pgt])
                op("act", lambda h: h.activation(out=GT4.ap.rearrange("p a b -> p (a b)"), in_=pgt.ap, func=AF.Copy), r=[pgt], w=[GT4])
                for hh in range(8):
                    hd = 8 * g + hh
                    nbh = NBH[hh % 2]
                    op("dve", lambda h, hd=hd, cb=cb, nbh=nbh: h.tensor_scalar(out=nbh.ap, in0=NBF.ap[:, cb * 512:(cb + 1) * 512], scalar1=identf[0:32, hd:hd + 1],
                                                                               scalar2=None, op0=ALU.mult), r=[NBF, C["ident_f"]], w=[nbh])
                    parg = self.psb(4)
                    op("pe", lambda h, hd=hd, cb=cb: h.matmul(parg.ap, lhsT=identf[0:32, hd:hd + 1].broadcast_to([32, 128]), rhs=ACSF.ap[:, cb * 512:(cb + 1) * 512],
                                                              start=True, stop=False), r=[C["ident_f"], ACSF], w=[parg])
                    for c in range(4):
                        op("pe", lambda h, c=c, nbh=nbh: h.matmul(parg.ap[:, c * 128:(c + 1) * 128], lhsT=nbh.ap[:, c * 128:(c + 1) * 128], rhs=ONESF.ap[0:32, :],
                                                                  start=False, stop=(c == 3)), r=[nbh, ONESF], w=[parg])
                    arg = ARG[hh % 2]
                    op("dve", lambda h, arg=arg: h.tensor_tensor(out=arg.ap, in0=parg.ap, in1=C["maskneg"].ap, op=ALU.add), r=[parg, C["maskneg"]], w=[arg])
                    op("act", lambda h, arg=arg: h.activation(out=arg.ap, in_=arg.ap, func=AF.Exp), r=[arg], w=[arg])
                    op("dve", lambda h, arg=arg, hh=hh: h.tensor_tensor(out=MT[hh].ap.rearrange("p a b -> p (a b)"), in0=arg.ap,
                                                                        in1=GT4.ap.rearrange("p a b -> p (a b)"), op=ALU.mult), r=[arg, GT4], w=[MT[hh]])
                for c in range(4):
                    cc = 4 * cb + c
                    tk = slice(cc * 128, (cc + 1) * 128)
                    ptx = V(self.ps[:, 5, :].bitcast(BF16)[:, 0:512], [("ps", 5)])
                    ptb = V(self.ps[:, 5, :].bitcast(BF16)[:, 512:640], [("ps", 5)])
                    for j in range(4):
                        op("pe", lambda h, j=j, tk=tk: h.transpose(out=ptx.ap[:, j * 128:(j + 1) * 128], in_=XF.ap[:, j, tk], identity=C["ident_b"].ap),
                           r=[XF, C["ident_b"]], w=[ptx])
                    op("pe", lambda h, tk=tk: h.transpose(out=ptb.ap, in_=BF.ap[:, tk], identity=C["ident_b"].ap), r=[BF, C["ident_b"]], w=[ptb])
                    op("act", lambda h: h.activation(out=XTOK.ap, in_=ptx.ap, func=AF.Copy), r=[ptx], w=[XTOK])
                    op("act", lambda h: h.activation(out=BTOK.ap, in_=ptb.ap, func=AF.Copy), r=[ptb], w=[BTOK])
                    op("act", lambda h: h.activation(out=PREV.ap, in_=STATE.ap, func=AF.Copy), r=[STATE], w=[PREV])
                    pyd, pyo, pst, pz = self.psb(0), self.psb(1), self.psb(2), self.psb(3)
                    for hh in range(8):
                        op("pe", lambda h, hh=hh, c=c: h.matmul(pyd.ap[:, hh * 64:(hh + 1) * 64], lhsT=MT[hh].ap[:, c, :], rhs=XTOK.ap[:, hh * 64:(hh + 1) * 64],
                                                               start=True, stop=True), r=[MT[hh], XTOK], w=[pyd])
                    op("pe", lambda h, tk=tk: h.matmul(pyo.ap, lhsT=CF.ap[:, tk], rhs=PREV.ap, start=True, stop=True), r=[CF, PREV], w=[pyo])
                    op("dve", lambda h, cc=cc, hs=hs: h.tensor_tensor(out=XDT2.ap.rearrange("p (a b) -> p a b", a=8), in0=XTOK.ap.rearrange("p (a b) -> p a b", a=8),
                                                               in1=W2T.ap[:, cc, hs].unsqueeze(2).broadcast_to([128, 8, 64]), op=ALU.mult), r=[XTOK, W2T], w=[XDT2])
                    op("pe", lambda h: h.matmul(pst.ap, lhsT=BTOK.ap, rhs=XDT2.ap, start=True, stop=True), r=[BTOK, XDT2], w=[pst])
                    op("dve", lambda h, cc=cc, hs=hs: h.tensor_tensor(out=Y1.ap.rearrange("p (a b) -> p a b", a=8), in0=pyo.ap.rearrange("p (a b) -> p a b", a=8),
                                                              in1=EACST.ap[:, cc, hs].unsqueeze(2).broadcast_to([128, 8, 64]), op=ALU.mult), r=[pyo, EACST], w=[Y1])
                    op("dve", lambda h: h.tensor_tensor(out=Y1.ap, in0=Y1.ap, in1=pyd.ap, op=ALU.add), r=[Y1, pyd], w=[Y1])
                    op("dve", lambda h, hs=hs: h.tensor_tensor(out=XD.ap.rearrange("p (a b) -> p a b", a=8), in0=XTOK.ap.rearrange("p (a b) -> p a b", a=8),
                                                        in1=DBC.ap[:, hs].unsqueeze(2).broadcast_to([128, 8, 64]), op=ALU.mult), r=[XTOK, DBC], w=[XD])
                    op("dve", lambda h: h.tensor_tensor(out=Y1.ap, in0=Y1.ap, in1=XD.ap, op=ALU.add), r=[Y1, XD], w=[Y1])
                    op("dve", lambda h, cc=cc, hs=hs: h.tensor_tensor(out=STATE.ap.rearrange("p (a b) -> p a b", a=8), in0=STATE.ap.rearrange("p (a b) -> p a b", a=8),
                                                              in1=DECC.ap[:, cc, hs].unsqueeze(2).broadcast_to([128, 8, 64]), op=ALU.mult), r=[STATE, DECC], w=[STATE])
                    op("dve", lambda h: h.tensor_tensor(out=STATE.ap, in0=STATE.ap, in1=pst.ap, op=ALU.add), r=[STATE, pst], w=[STATE])
                    if getattr(self, "dbg", None) is not None and g == 0 and cc == 0:
                        op("sp", lambda h: h.dma_start(out=self.dbg[:, 0:512], in_=STATE.ap), r=[STATE], w=["dbg0"], dma=True)
                        op("act", lambda h: h.activation(out=ARG[0].ap, in_=XDT2.ap, func=AF.Copy), r=[XDT2], w=[ARG[0]])
                        op("act", lambda h: h.activation(out=ARG[1].ap[:, 0:128], in_=BTOK.ap, func=AF.Copy), r=[BTOK], w=[ARG[1]])
                        op("sp", lambda h: h.dma_start(out=self.dbg[:, 1024:1536], in_=ARG[0].ap), r=[ARG[0]], w=["dbg2"], dma=True)
                        op("sp", lambda h: h.dma_start(out=self.dbg[:, 512:640], in_=ARG[1].ap[:, 0:128]), r=[ARG[1]], w=["dbg1"], dma=True)
                    for k in range(8):
                        op("pe", lambda h, k=k, tk=tk: h.matmul(pz.ap, lhsT=self.HT.ap[:, k, tk], rhs=WZ.ap[:, k, :], start=(k == 0), stop=(k == 7)),
                           r=[self.HT, WZ], w=[pz])
                    op("act", lambda h: h.activation(out=ZS.ap, in_=pz.ap, func=AF.Silu), r=[pz], w=[ZS])
                    op("dve", lambda h: h.tensor_tensor(out=Y1.ap, in0=Y1.ap, in1=ZS.ap, op=ALU.mult), r=[Y1, ZS], w=[Y1])
                    op("act", lambda h: h.activation(out=JUNK.ap, in_=Y1.ap, func=AF.Square, accum_out=SS.ap[:, 0:1]), r=[Y1], w=[JUNK, SS])
                    op("act", lambda h: h.activation(out=SS.ap[:, 1:2], in_=SS.ap[:, 0:1], func=AF.Sqrt, bias=1e-5, scale=1.0 / 512.0), r=[SS], w=[SS])
                    op("dve", lambda h: h.reciprocal(out=SS.ap[:, 2:3], in_=SS.ap[:, 1:2]), r=[SS], w=[SS])
                    op("dve", lambda h: h.scalar_tensor_tensor(out=YN.ap, in0=Y1.ap, scalar=SS.ap[:, 2:3], in1=NGg.ap, op0=ALU.mult, op1=ALU.mult),
                       r=[Y1, SS, NGg], w=[YN])
                    pty = V(self.ps[:, 5, :].bitcast(BF16)[:, 0:512].rearrange("p (a b) -> p a b", a=4), [("ps", 5)])
                    for j in range(4):
                        op("pe", lambda h, j=j: h.transpose(out=pty.ap[:, j, :], in_=YN.ap[:, j * 128:(j + 1) * 128], identity=C["ident_b"].ap),
                           r=[YN, C["ident_b"]], w=[pty])
                    op("act", lambda h: h.activation(out=YNT.ap, in_=pty.ap, func=AF.Copy), r=[pty], w=[YNT])
                    Rc = self.R[cc]
                    for dr in range(2):
                        pm = self.psb(6 + dr)
                        for j in range(4):
                            op("pe", lambda h, j=j, dr=dr, pm=pm: h.matmul(pm.ap, lhsT=YNT.ap[:, j, :], rhs=WO.ap[:, j, dr * 512:(dr + 1) * 512],
                                                                        start=(j == 0), stop=(j == 3)), r=[YNT, WO], w=[pm])
                        op("dve", lambda h, dr=dr, pm=pm, Rc=Rc: h.tensor_tensor(out=Rc.ap[:, dr * 512:(dr + 1) * 512], in0=Rc.ap[:, dr * 512:(dr + 1) * 512],
                                                                                in1=pm.ap, op=ALU.add), r=[Rc, pm], w=[Rc])


    def attention(self, l, li):
        A, HBb, C = self.Ab, self.HBb, self.C
        op = self.op
        ja = self.attn_layers.index(l)
        build_kv = (ja == 0)
        KT = HBb.view(0, [8, S], BF16)
        o = 0

        def alloc(n):
            nonlocal o
            a = o
            o += (n + G - 1) // G * G
            return a
        VA = A.view(alloc(16 * 16 * 66 * 2), [16, 16, 66], BF16)
        VAF = A.view(0, [16, 16, 33], F32)
        KTF = HBb.view(0, [8 * S // 2], F32)
        KMB = A.view(alloc(8 * 8 * 2), [8, 8], BF16)
        W1R = A.view(alloc(8 * 128 * 2), [8, 128], BF16)
        TRI = A.view(alloc(256), [128], BF16)
        PADM = A.view(alloc(256), [8, 8], F32)
        WSEL = A.view(alloc(16 * 2 * 8 * 4), [16, 2, 8], F32)
        GSC = A.view(alloc(1024), [256], F32)
        W1 = A.view(alloc(8 * 512 * 2), [8, 512], BF16)
        WOp = A.view(alloc(D * 2), [D], BF16)
        QTp = A.view(alloc(S * 2), [S], BF16)
        OTp = A.view(alloc(S * 2), [S], BF16)
        CS = A.view(alloc(512 * 4), [512], F32)
        SN = A.view(alloc(512 * 4), [512], F32)
        T1 = A.view(alloc(512 * 4), [512], F32)
        T2 = A.view(alloc(512 * 4), [512], F32)
        PT = [A.view(alloc(512 * 2), [512], BF16) for _ in range(2)]
        ACC = A.view(alloc(4 * 65 * 4), [4, 65], F32)
        OTOK = A.view(alloc(4 * 128 * 2), [4, 128], BF16)
        assert o <= A.nbytes, o

        for i in range(NT):
            Ri = self.R[i]
            op("act", lambda h, Ri=Ri: h.activation(out=Ri.ap, in_=Ri.ap, func=AF.Copy, scale=ALPHA), r=[Ri], w=[Ri])
        op("pool", lambda h: h.dma_start(out=TRI.ap, in_=self.cst["tri"]), r=[], w=[TRI], dma=True)
        op("sp", lambda h: h.dma_start(out=PADM.ap.rearrange("p a b -> p (a b)"), in_=self.cst["padm"]), r=[], w=[PADM], dma=True)

        def make_rot_weights(pp):
            op("dve", lambda h: h.memset(W1R.ap, 0.0), r=[], w=[W1R])
            for hl in range(2):
                b0 = pp * 128 + 64 * hl
                op("act", lambda h, hl=hl, b0=b0: h.activation(out=W1R.ap[:, :, 64 * hl:64 * hl + 8], in_=W1.ap[:, :, b0 + 8:b0 + 16], func=AF.Copy, scale=-1.0),
                   r=[W1], w=[W1R])
                op("act", lambda h, hl=hl, b0=b0: h.activation(out=W1R.ap[:, :, 64 * hl + 8:64 * hl + 16], in_=W1.ap[:, :, b0:b0 + 8], func=AF.Copy),
                   r=[W1], w=[W1R])

        def rope(pk, dst, dv, r):
            prot = self.psb(6)
            for k in range(8):
                op("pe", lambda h, k=k, r=r, prot=prot: h.matmul(prot.ap, lhsT=W1R.ap[:, k, :], rhs=self.HT.ap[:, k, r * 512:(r + 1) * 512],
                                                              start=(k == 0), stop=(k == 7)), r=[W1R, self.HT], w=[prot])
            op("sp", lambda h, r=r: h.dma_start(out=CS.ap, in_=self.cst["cosf"][:, r * 512:(r + 1) * 512]), r=[], w=[CS], dma=True)
            op("sp", lambda h, r=r: h.dma_start(out=SN.ap, in_=self.cst["sinf"][:, r * 512:(r + 1) * 512]), r=[], w=[SN], dma=True)
            op("dve", lambda h, pk=pk: h.tensor_tensor(out=T1.ap, in0=pk.ap, in1=CS.ap, op=ALU.mult), r=[pk, CS], w=[T1])
            op("dve", lambda h, prot=prot: h.tensor_tensor(out=T2.ap, in0=prot.ap, in1=SN.ap, op=ALU.mult), r=[prot, SN], w=[T2])
            op("dve", lambda h, dst=dst: h.tensor_tensor(out=dst, in0=T1.ap, in1=T2.ap, op=ALU.add), r=[T1, T2], w=[dv])

        def wsrc(w2d, half):
            return w2d.rearrange("(kc kp) c -> kp kc c", kp=128)[:, :, half * 512:(half + 1) * 512]

        def kmeans():
            for p in range(8):
                op("dve", lambda h, p=p: h.tensor_reduce(out=GSC.ap[:, 0:8], in_=KT.ap[:, p, :].rearrange("p (a b) -> p a b", a=8), axis=AX.X, op=ALU.add),
                   r=[KT], w=[GSC])
                op("dve", lambda h, p=p: h.tensor_scalar(out=KMB.ap[:, p, :], in0=GSC.ap[:, 0:8], scalar1=1.0 / 256.0, scalar2=None, op0=ALU.mult),
                   r=[GSC], w=[KMB])

        stop = getattr(self, "attn_stop", 0)
        if stop == -1:
            return
        if build_kv:
            for half in range(2):
                if stop == -2 and half == 1:
                    return
                op("pool", lambda h, half=half: h.dma_start(out=W1.ap, in_=wsrc(self.kv_w_k, half)), r=[], w=[W1], dma=True)
                for pp in range(4):
                    p = half * 4 + pp
                    make_rot_weights(pp)
                    for r in range(4):
                        pk = self.psb(4 + r % 2)
                        for k in range(8):
                            op("pe", lambda h, k=k, r=r, pp=pp, pk=pk: h.matmul(pk.ap, lhsT=W1.ap[:, k, pp * 128:(pp + 1) * 128],
                                                                             rhs=self.HT.ap[:, k, r * 512:(r + 1) * 512], start=(k == 0), stop=(k == 7)),
                               r=[W1, self.HT], w=[pk])
                        rope(pk, KT.ap[:, p, r * 512:(r + 1) * 512], KT, r)
            for half in range(2):
                op("pool", lambda h, half=half: h.dma_start(out=W1.ap, in_=wsrc(self.kv_w_v, half)), r=[], w=[W1], dma=True)
                for i in range(NT):
                    pv = self.psb(4 + i % 2)
                    for k in range(8):
                        op("pe", lambda h, k=k, i=i, pv=pv: h.matmul(pv.ap, lhsT=self.HT.ap[:, k, i * 128:(i + 1) * 128], rhs=W1.ap[:, k, :],
                                                                 start=(k == 0), stop=(k == 7)), r=[self.HT, W1], w=[pv])
                    op("act", lambda h, i=i, half=half, pv=pv: h.activation(out=VA.ap[:, i, half * 8:(half + 1) * 8, 0:64],
                                                                          in_=pv.ap.rearrange("p (a b) -> p a b", a=8), func=AF.Copy), r=[pv], w=[VA])
            if stop == -3:
                return
            op("dve", lambda h: h.memset(VA.ap[:, :, :, 64:65], 1.0), r=[], w=[VA])
            if stop == -4:
                return
            kmeans()
            if not getattr(self, "no_spill", False):
                op("sp", lambda h: h.dma_start(out=self.kt_s, in_=KTF.ap), r=[KT], w=["kt_s"], dma=True)
                for kt in range(16):
                    op("sp", lambda h, kt=kt: h.dma_start(out=self.va_s[:, kt * 512:(kt + 1) * 512].rearrange("p (b c) -> p b c", c=32), in_=VAF.ap[:, kt, :, 0:32]),
                       r=[VA], w=["va_s"], dma=True)
        else:
            op("sp", lambda h: h.dma_start(out=KTF.ap, in_=self.kt_s), r=["kt_s"], w=[KT], dma=True)
            for kt in range(16):
                op("sp", lambda h, kt=kt: h.dma_start(out=VAF.ap[:, kt, :, 0:32], in_=self.va_s[:, kt * 512:(kt + 1) * 512].rearrange("p (b c) -> p b c", c=32)),
                   r=["va_s"], w=[VA], dma=True)
            op("dve", lambda h: h.memset(VA.ap[:, :, :, 64:65], 1.0), r=[], w=[VA])
            kmeans()
        wq = self.attn_w_q[ja]
        stop = getattr(self, "attn_stop", 0)
        if stop == 1:
            return
        for p in range(8 if stop == 0 else 1):
            half, pp = divmod(p, 4)
            if pp == 0:
                op("pool", lambda h, half=half: h.dma_start(out=W1.ap, in_=wsrc(wq, half)), r=[], w=[W1], dma=True)
            op("pool", lambda h, p=p: h.dma_start(out=WOp.ap, in_=self.attn_w_o[ja, p * 128:(p + 1) * 128, :]), r=[], w=[WOp], dma=True)
            make_rot_weights(pp)
            for r in range(4):
                pq = self.psb(4 + r % 2)
                for k in range(8):
                    op("pe", lambda h, k=k, r=r, pp=pp, pq=pq: h.matmul(pq.ap, lhsT=W1.ap[:, k, pp * 128:(pp + 1) * 128],
                                                                     rhs=self.HT.ap[:, k, r * 512:(r + 1) * 512], start=(k == 0), stop=(k == 7)),
                       r=[W1, self.HT], w=[pq])
                rope(pq, QTp.ap[:, r * 512:(r + 1) * 512], QTp, r)
            for hl in range(2):
                rows = slice(64 * hl, 64 * hl + 64)
                for i in range(8, NT):
                    cblk = i // 2
                    pg = self.psb(6, cols=8)
                    op("pe", lambda h, i=i, rows=rows, p=p, pg=pg: h.matmul(pg.ap, lhsT=QTp.ap[rows, i * 128:(i + 1) * 128], rhs=KMB.ap[rows, p, :],
                                                                       start=True, stop=True), r=[QTp, KMB], w=[pg])
                    op("dve", lambda h, cblk=cblk, pg=pg: h.tensor_tensor(out=GSC.ap[:, 0:8], in0=pg.ap, in1=PADM.ap[:, cblk, :], op=ALU.add),
                       r=[pg, PADM], w=[GSC])
                    op("dve", lambda h: h.max(out=GSC.ap[:, 8:16], in_=GSC.ap[:, 0:8]), r=[GSC], w=[GSC])
                    op("dve", lambda h, i=i, hl=hl: h.tensor_scalar(out=WSEL.ap[:, i, hl, :], in0=GSC.ap[:, 0:8], scalar1=GSC.ap[:, 10:11], scalar2=None,
                                                                   op0=ALU.is_ge), r=[GSC], w=[WSEL])
            nst = 0
            if stop == 2:
                return
            for r in range(4):
                for hl in range(2):
                    rows = slice(64 * hl, 64 * hl + 64)
                    hd = 2 * p + hl
                    for n in range(2 * r + 2):
                        jqs = [jq for jq in range(4) if (4 * r + jq) // 2 >= n]
                        c0 = jqs[0] * 128
                        ncol = 512 - c0
                        pob = self.psb(2 + n % 2)
                        pts = {}
                        for kt in (2 * n, 2 * n + 1):
                            use = [jq for jq in jqs if kt <= 4 * r + jq]
                            if not use:
                                continue
                            pss = self.psb(nst % 2)
                            pt = PT[nst % 2]
                            nst += 1
                            pts[kt] = pt
                            op("pe", lambda h, kt=kt, rows=rows, r=r, c0=c0, ncol=ncol, pss=pss, p=p: h.matmul(
                                pss.ap[:, c0:c0 + ncol], lhsT=KT.ap[rows, p, kt * 128:(kt + 1) * 128], rhs=QTp.ap[rows, r * 512 + c0:(r + 1) * 512],
                                start=True, stop=True), r=[KT, QTp], w=[pss])
                            op("act", lambda h, c0=c0, ncol=ncol, pss=pss, pt=pt: h.activation(out=pt.ap[:, c0:c0 + ncol], in_=pss.ap[:, c0:c0 + ncol],
                                                                                           func=AF.Exp, scale=0.125), r=[pss], w=[pt])
                            for jq in use:
                                if kt == 4 * r + jq:
                                    op("dve", lambda h, jq=jq, pt=pt: h.tensor_tensor(out=pt.ap[:, jq * 128:(jq + 1) * 128], in0=pt.ap[:, jq * 128:(jq + 1) * 128],
                                                                                 in1=TRI.ap, op=ALU.mult), r=[pt, TRI], w=[pt])
                        for jq in jqs:
                            kts = [kt for kt in (2 * n, 2 * n + 1) if kt <= 4 * r + jq]
                            for kt in kts:
                                pt = pts[kt]
                                op("pe", lambda h, jq=jq, kt=kt, hd=hd, pt=pt, pob=pob, st=(kt == kts[0]), last=(kt == kts[-1]): h.matmul(
                                    pob.ap[:, jq * 65:(jq + 1) * 65], lhsT=pt.ap[:, jq * 128:(jq + 1) * 128], rhs=VA.ap[:, kt, hd, 0:65], start=st, stop=last),
                                    r=[pt, VA], w=[pob])
                        for jq in jqs:
                            i = 4 * r + jq
                            cblk = i // 2
                            if n == cblk or i < 8:
                                wgt = 1.0
                                rd = [pob]
                            else:
                                wgt = WSEL.ap[:, i, hl, n:n + 1]
                                rd = [pob, WSEL]
                            if n == 0:
                                op("dve", lambda h, jq=jq, wgt=wgt, pob=pob: h.tensor_scalar(out=ACC.ap[:, jq, :], in0=pob.ap[:, jq * 65:(jq + 1) * 65], scalar1=wgt,
                                                                                        scalar2=None, op0=ALU.mult), r=rd, w=[ACC])
                            else:
                                op("dve", lambda h, jq=jq, wgt=wgt, pob=pob: h.scalar_tensor_tensor(out=ACC.ap[:, jq, :], in0=pob.ap[:, jq * 65:(jq + 1) * 65], scalar=wgt,
                                                                                               in1=ACC.ap[:, jq, :], op0=ALU.mult, op1=ALU.add), r=rd + [ACC], w=[ACC])
                    for jq in range(4):
                        op("dve", lambda h, jq=jq: h.reciprocal(out=GSC.ap[:, 32 + jq:33 + jq], in_=ACC.ap[:, jq, 64:65]), r=[ACC], w=[GSC])
                        op("dve", lambda h, jq=jq, hl=hl: h.tensor_scalar(out=OTOK.ap[:, jq, 64 * hl:64 * hl + 64], in0=ACC.ap[:, jq, 0:64],
                                                                         scalar1=GSC.ap[:, 32 + jq:33 + jq], scalar2=None, op0=ALU.mult), r=[ACC, GSC], w=[OTOK])
                ptt = V(self.ps[:, 7, :].bitcast(BF16)[:, 0:512], [("ps", 7)])
                for jq in range(4):
                    op("pe", lambda h, jq=jq: h.transpose(out=ptt.ap[:, jq * 128:(jq + 1) * 128], in_=OTOK.ap[:, jq, :], identity=C["ident_b"].ap),
                       r=[OTOK, C["ident_b"]], w=[ptt])
                op("act", lambda h, r=r: h.activation(out=OTp.ap[:, r * 512:(r + 1) * 512], in_=ptt.ap, func=AF.Copy), r=[ptt], w=[OTp])
            for i in range(NT):
                Ri = self.R[i]
                for dr in range(2):
                    pm = self.psb(4 + dr)
                    op("pe", lambda h, i=i, dr=dr, pm=pm: h.matmul(pm.ap, lhsT=OTp.ap[:, i * 128:(i + 1) * 128], rhs=WOp.ap[:, dr * 512:(dr + 1) * 512],
                                                              start=True, stop=True), r=[OTp, WOp], w=[pm])
                    op("dve", lambda h, dr=dr, pm=pm, Ri=Ri: h.tensor_tensor(out=Ri.ap[:, dr * 512:(dr + 1) * 512], in0=Ri.ap[:, dr * 512:(dr + 1) * 512],
                                                                            in1=pm.ap, op=ALU.add), r=[Ri, pm], w=[Ri])

    def moe(self, li):
        A = self.Ab
        C = self.C
        o = 0

        def alloc(n):
            nonlocal o
            a = o
            o += (n + G - 1) // G * G
            return a
        WR = A.view(alloc(8 * NE * 2), [8, NE], BF16)
        BRT = A.view(alloc(NE * 4), [NE], F32)
        RG = A.view(alloc(NT * 64 * 4), [NT, 64], F32)
        RGT = A.view(alloc(S * 4), [S], F32, parts=64)
        MSK = A.view(alloc(NT * NE * 2), [NT, NE], BF16)
        CM = A.view(alloc(NT * NE * 2), [NT, NE], BF16)
        SM = A.view(alloc(1024), [256], F32)
        BGU = A.view(alloc(NE * 16 * 4), [NE, 16], F32)
        BDN = [A.view(alloc(D * 2), [D], BF16, parts=1) for _ in range(2)]
        NRING = 4
        RING = [A.view(alloc(8 * 256 * 2), [8, 256], BF16) for _ in range(NRING)]
        xt_off = alloc(8 * CAP * 2)
        XT = A.view(xt_off, [8, CAP], BF16)
        ACTT = A.view(alloc(8 * CAP * 2), [8, CAP], BF16)
        YB = A.view(xt_off, [NJT, D], BF16)
        TG = [A.view(alloc(CAP * 4), [CAP], F32) for _ in range(2)]
        TS = [A.view(alloc(CAP * 2), [CAP], BF16) for _ in range(2)]
        TU = [A.view(alloc(CAP * 4), [CAP], F32) for _ in range(2)]
        GBS = [A.view(alloc(512 * 2), [512], BF16) for _ in range(2)]
        assert o <= A.nbytes, o
        self.arena_used = o
        Sg = [self.HTb.view(i * CAP * 2, [CAP], BF16) for i in range(NT)]
        base = NT * CAP * 2
        STv = [[self.HTb.view(base + (jt * S + r * 512) * 2, [512], BF16) for r in range(4)] for jt in range(NJT)]

        def STslice(jt, i):
            r, c = divmod(i, 4)
            v = STv[jt][r]
            return V(v.ap[:, c * 128:(c + 1) * 128], v.keys)

        wr_src = self.w_router[li].rearrange("(kc kp) e -> kp kc e", kp=128)
        self.op("pool", lambda h: h.dma_start(out=WR.ap, in_=wr_src), r=[], w=[WR], dma=True)
        self.op("sp", lambda h: h.dma_start(out=BRT.ap, in_=self.b_router[li:li + 1, :].partition_broadcast(128)), r=[], w=[BRT], dma=True)
        bgu_src = self.b_gu[li].rearrange("e (c p) -> (e c) p", p=128).rearrange("(t r) p -> r t p", r=128)
        self.op("sp", lambda h: h.dma_start(out=TG[0].ap.rearrange("p (t q) -> p t q", t=4), in_=bgu_src), r=[], w=[TG[0]], dma=True)
        pbg = self.psb(0)
        for t in range(4):
            self.op("pe", lambda h, t=t: h.transpose(out=pbg.ap[:, t * 128:(t + 1) * 128], in_=TG[0].ap[:, t * 128:(t + 1) * 128], identity=C["ident_f"].ap),
                    r=[TG[0], C["ident_f"]], w=[pbg])
        self.op("act", lambda h: h.activation(out=BGU.ap.rearrange("p e c -> p (e c)"), in_=pbg.ap, func=AF.Copy), r=[pbg], w=[BGU])

        pieces = []
        for e in range(NE):
            for p in range(4):
                pieces.append((e, "g", p))
                pieces.append((e, "u", p))
            for q in range(4):
                pieces.append((e, "d", q))
        self._ring_state = {"next": 0}
        piece_slot = {}

        def issue_piece():
            n = self._ring_state["next"]
            if n >= len(pieces):
                return
            e, kind, p = pieces[n]
            slot = RING[n % NRING]
            piece_slot[(e, kind, p)] = slot
            if kind == "d":
                src = self.w_dn[li, e].rearrange("(kc kp) d -> kp kc d", kp=128)[:, :, p * 256:(p + 1) * 256]
            else:
                c0 = p * 256 + (D if kind == "u" else 0)
                src = self.w_gu[li, e].rearrange("(kc kp) f -> kp kc f", kp=128)[:, :, c0:c0 + 256]
            self.op("pool", lambda h, slot=slot, src=src: h.dma_start(out=slot.ap, in_=src), r=[], w=[slot], dma=True)
            self._ring_state["next"] = n + 1

        for _ in range(NRING):
            issue_piece()

        scr = SM.ap
        for i in range(NT):
            pl = self.psb(4 + (i % 2), cols=NE)
            for k in range(8):
                self.op("pe", lambda h, k=k, i=i, pl=pl: h.matmul(pl.ap, lhsT=self.HT.ap[:, k, i * 128:(i + 1) * 128], rhs=WR.ap[:, k, :],
                                                               start=(k == 0), stop=(k == 7)), r=[self.HT, WR], w=[pl])
            lg = V(scr[:, 0:32], SM.keys)
            m8 = V(scr[:, 32:40], SM.keys)
            nm = V(scr[:, 40:41], SM.keys)
            mk = V(scr[:, 48:80], SM.keys)
            ex = V(scr[:, 80:112], SM.keys)
            sm = V(scr[:, 112:113], SM.keys)
            rk = V(scr[:, 128:160], SM.keys)
            self.op("dve", lambda h, pl=pl: h.tensor_tensor(out=lg.ap, in0=pl.ap, in1=BRT.ap, op=ALU.add), r=[pl, BRT], w=[SM])
            self.op("dve", lambda h: h.max(out=m8.ap, in_=lg.ap), r=[SM], w=[SM])
            self.op("dve", lambda h: h.tensor_scalar(out=mk.ap, in0=lg.ap, scalar1=m8.ap[:, 3:4], scalar2=None, op0=ALU.is_ge), r=[SM], w=[SM])
            self.op("dve", lambda h: h.tensor_scalar(out=nm.ap, in0=m8.ap[:, 0:1], scalar1=-1.0, scalar2=None, op0=ALU.mult), r=[SM], w=[SM])
            self.op("act", lambda h: h.activation(out=ex.ap, in_=lg.ap, func=AF.Exp, bias=nm.ap, scale=1.0), r=[SM], w=[SM])
            self.op("dve", lambda h: h.tensor_tensor(out=ex.ap, in0=ex.ap, in1=mk.ap, op=ALU.mult), r=[SM], w=[SM])
            self.op("dve", lambda h: h.tensor_reduce(out=sm.ap, in_=ex.ap, axis=AX.X, op=ALU.add), r=[SM], w=[SM])
            self.op("dve", lambda h: h.reciprocal(out=sm.ap, in_=sm.ap), r=[SM], w=[SM])
            self.op("dve", lambda h, i=i: h.tensor_scalar(out=RG.ap[:, i, 32:64], in0=ex.ap, scalar1=sm.ap, scalar2=None, op0=ALU.mult), r=[SM], w=[RG])
            self.op("dve", lambda h, i=i: h.tensor_copy(out=MSK.ap[:, i, :], in_=mk.ap), r=[SM], w=[MSK])
            if i == 0:
                self.op("dve", lambda h: h.memset(CM.ap[:, 0, :], 0.0), r=[], w=[CM])
            else:
                self.op("dve", lambda h, i=i: h.tensor_tensor(out=CM.ap[:, i, :], in0=CM.ap[:, i - 1, :], in1=MSK.ap[:, i - 1, :], op=ALU.add),
                        r=[CM, MSK], w=[CM])
            pr = self.psb(4 + (i % 2), cols=NE, c0=64)
            self.op("pe", lambda h, i=i, pr=pr: h.matmul(pr.ap, lhsT=C["ones"].ap, rhs=CM.ap[:, i, :], start=True, stop=False), r=[C["ones"], CM], w=[pr])
            self.op("pe", lambda h, i=i, pr=pr: h.matmul(pr.ap, lhsT=C["triu"].ap, rhs=MSK.ap[:, i, :], start=False, stop=True), r=[C["triu"], MSK], w=[pr])
            self.op("dve", lambda h, pr=pr: h.scalar_tensor_tensor(out=rk.ap, in0=pr.ap, scalar=1.0, in1=mk.ap, op0=ALU.add, op1=ALU.mult), r=[pr, SM], w=[SM])
            self.op("dve", lambda h, i=i: h.tensor_scalar(out=RG.ap[:, i, 0:32], in0=rk.ap, scalar1=-1.0, scalar2=None, op0=ALU.add), r=[SM], w=[RG])
            ptr = self.psb(4 + (i % 2), cols=128, parts=64, c0=128)
            self.op("pe", lambda h, i=i, ptr=ptr: h.transpose(out=ptr.ap, in_=RG.ap[:, i, :], identity=C["ident_f"].ap), r=[RG, C["ident_f"]], w=[ptr])
            self.op("act", lambda h, i=i, ptr=ptr: h.activation(out=RGT.ap[:, i * 128:(i + 1) * 128], in_=ptr.ap, func=AF.Copy), r=[ptr], w=[RGT])

        for i in range(NT):
            Ri = self.R[i]
            self.op("act", lambda h, Ri=Ri: h.activation(out=Ri.ap, in_=Ri.ap, func=AF.Copy, scale=ALPHA), r=[Ri], w=[Ri])

        identf = C["ident_f"].ap
        for e in range(NE):
            bdn = BDN[e % 2]
            self.op("pool", lambda h, e=e, bdn=bdn: h.dma_start(out=bdn.ap, in_=self.b_dn[li, e:e + 1, :]), r=[], w=[bdn], dma=True)
            for i in range(NT):
                eng = "dve"
                self.op(eng, lambda h, i=i, e=e: h.tensor_scalar(out=Sg[i].ap, in0=C["iota_row"].ap, scalar1=RG.ap[:, i, e:e + 1], scalar2=None,
                                                                op0=ALU.is_equal), r=[C["iota_row"], RG], w=[Sg[i]])
            for m in range(8):
                px = self.psb(m % 2)
                for i in range(NT):
                    self.op("pe", lambda h, m=m, i=i, px=px: h.matmul(px.ap, lhsT=self.HB[i].ap[:, m * 128:(m + 1) * 128], rhs=Sg[i].ap,
                                                                   start=(i == 0), stop=(i == NT - 1)), r=[self.HB[i], Sg[i]], w=[px])
                self.op("act", lambda h, m=m, px=px: h.activation(out=XT.ap[:, m, :], in_=px.ap, func=AF.Copy), r=[px], w=[XT])
            for r in range(4):
                prb = self.psb(2)
                pgb = self.psb(3)
                self.op("pe", lambda h, r=r, e=e, prb=prb: h.matmul(prb.ap, lhsT=identf[0:64, e:e + 1].broadcast_to([64, 128]),
                                                                    rhs=RGT.ap[:, r * 512:(r + 1) * 512], start=True, stop=True),
                        r=[C["ident_f"], RGT], w=[prb])
                self.op("pe", lambda h, r=r, e=e, pgb=pgb: h.matmul(pgb.ap, lhsT=identf[0:64, 32 + e:33 + e].broadcast_to([64, 128]),
                                                                    rhs=RGT.ap[:, r * 512:(r + 1) * 512], start=True, stop=True),
                        r=[C["ident_f"], RGT], w=[pgb])
                gbs = GBS[r % 2]
                self.op("act", lambda h, pgb=pgb, gbs=gbs: h.activation(out=gbs.ap, in_=pgb.ap, func=AF.Copy), r=[pgb], w=[gbs])
                for jt in range(NJT):
                    eng = "dve"
                    self.op(eng, lambda h, jt=jt, r=r, prb=prb, gbs=gbs: h.scalar_tensor_tensor(
                        out=STv[jt][r].ap, in0=prb.ap, scalar=C["piota"].ap[:, jt:jt + 1], in1=gbs.ap, op0=ALU.is_equal, op1=ALU.mult),
                        r=[prb, gbs, C["piota"]], w=[STv[jt][r]])
            for p in range(4):
                wg = piece_slot[(e, "g", p)]
                wu = piece_slot[(e, "u", p)]
                for sub in range(2):
                    c = 2 * p + sub
                    pg = self.psb(4 + (c % 2))
                    pu = self.psb(6 + (c % 2))
                    for k in range(8):
                        self.op("pe", lambda h, k=k, sub=sub, wg=wg, pg=pg: h.matmul(pg.ap, lhsT=wg.ap[:, k, sub * 128:(sub + 1) * 128], rhs=XT.ap[:, k, :],
                                                                                 start=(k == 0), stop=(k == 7)), r=[wg, XT], w=[pg])
                    for k in range(8):
                        self.op("pe", lambda h, k=k, sub=sub, wu=wu, pu=pu: h.matmul(pu.ap, lhsT=wu.ap[:, k, sub * 128:(sub + 1) * 128], rhs=XT.ap[:, k, :],
                                                                                 start=(k == 0), stop=(k == 7)), r=[wu, XT], w=[pu])
                    tg, ts, tu = TG[c % 2], TS[c % 2], TU[c % 2]
                    self.op("dve", lambda h, pg=pg, tg=tg, e=e, c=c: h.tensor_scalar(out=tg.ap, in0=pg.ap, scalar1=BGU.ap[:, e, c:c + 1], scalar2=7.0,
                                                                                 op0=ALU.add, op1=ALU.min), r=[pg, BGU], w=[tg])
                    self.op("act", lambda h, tg=tg, ts=ts: h.activation(out=ts.ap, in_=tg.ap, func=AF.Sigmoid, scale=1.702), r=[tg], w=[ts])
                    self.op("act", lambda h, pu=pu, tu=tu, e=e, c=c: h.activation(out=tu.ap, in_=pu.ap, func=AF.Identity, bias=BGU.ap[:, e, 8 + c:9 + c], scale=1.0),
                            r=[pu, BGU], w=[tu])
                    self.op("dve", lambda h, tu=tu: h.tensor_scalar(out=tu.ap, in0=tu.ap, scalar1=-7.0, scalar2=7.0, op0=ALU.max, op1=ALU.min), r=[tu], w=[tu])
                    self.op("dve", lambda h, tg=tg, ts=ts: h.tensor_tensor(out=tg.ap, in0=tg.ap, in1=ts.ap, op=ALU.mult), r=[tg, ts], w=[tg])
                    self.op("dve", lambda h, tg=tg, tu=tu, c=c: h.scalar_tensor_tensor(out=ACTT.ap[:, c, :], in0=tu.ap, scalar=1.0, in1=tg.ap, op0=ALU.add, op1=ALU.mult),
                            r=[tg, tu], w=[ACTT])
                issue_piece()
                issue_piece()
            for q in range(4):
                wd = piece_slot[(e, "d", q)]
                for jt in range(NJT):
                    py = self.psb(jt % 2, cols=256, c0=0)
                    for c in range(8):
                        self.op("pe", lambda h, c=c, jt=jt, wd=wd, py=py: h.matmul(py.ap, lhsT=ACTT.ap[:, c, jt * 128:(jt + 1) * 128], rhs=wd.ap[:, c, :],
                                                                               start=(c == 0), stop=False), r=[ACTT, wd], w=[py])
                    self.op("pe", lambda h, q=q, py=py, bdn=bdn: h.matmul(py.ap, lhsT=C["ones"].ap[0:1, :], rhs=bdn.ap[0:1, q * 256:(q + 1) * 256],
                                                                          start=False, stop=True), r=[C["ones"], bdn], w=[py])
                    self.op("act", lambda h, jt=jt, q=q, py=py: h.activation(out=YB.ap[:, jt, q * 256:(q + 1) * 256], in_=py.ap, func=AF.Copy), r=[py], w=[YB])
                issue_piece()
            for i in range(NT):
                for dr in range(2):
                    po = self.psb(2 + ((2 * i + dr) % 2))
                    for jt in range(NJT):
                        stv = STslice(jt, i)
                        self.op("pe", lambda h, jt=jt, dr=dr, stv=stv, po=po: h.matmul(po.ap, lhsT=stv.ap, rhs=YB.ap[:, jt, dr * 512:(dr + 1) * 512],
                                                                                   start=(jt == 0), stop=(jt == NJT - 1)), r=[stv, YB], w=[po])
                    Ri = self.R[i]
                    self.op("dve", lambda h, dr=dr, Ri=Ri, po=po: h.tensor_tensor(out=Ri.ap[:, dr * 512:(dr + 1) * 512], in0=Ri.ap[:, dr * 512:(dr + 1) * 512],
                                                                              in1=po.ap, op=ALU.add), r=[Ri, po], w=[Ri])


_CACHE = {}
_NPH = [8]

WEIGHT_KEYS = ["kv_w_k", "kv_w_v", "attn_w_q", "attn_w_o", "ssm_w_in", "ssm_conv_w", "ssm_conv_b", "ssm_dt_bias", "ssm_a_log", "ssm_d", "ssm_norm_g", "ssm_w_out",
               "moe_w_router", "moe_b_router", "moe_w_gate_up", "moe_b_gate_up", "moe_w_down", "moe_b_down",
               "ln_mix_g", "ln_mix_b", "ln_ffn_g", "ln_ffn_b"]


def kernel(**inputs):
    x = np.ascontiguousarray(np.asarray(inputs["x"], dtype=np.float32))
    nb = x.shape[0]
    if "nc" not in _CACHE:
        phases = [("prep", 0), ("mamba", 0), ("moe", 0), ("mamba", 1), ("moe", 1), ("attn", 2), ("moe", 2), ("attn", 3), ("moe", 3)][:_NPH[0] + 1]
        mk = MK(layers=[0, 1, 2, 3], phases=phases)
        _CACHE["nc"] = mk.build()
        _CACHE["mk"] = mk
    nc = _CACHE["nc"]
    shared = {k: np.ascontiguousarray(np.asarray(inputs[k], dtype=np.float32)) for k in WEIGHT_KEYS}
    for k, v in make_consts().items():
        shared["c_" + k] = v
    in_maps = []
    for b in range(nb):
        m = dict(shared)
        m["x"] = x[b]
        in_maps.append(m)
    res = run_bass_kernel_spmd(nc, in_maps, core_ids=list(range(nb)))
    return np.stack([np.asarray(r["y"], dtype=np.float32) for r in res.results], axis=0)
```

```python
import math
from contextlib import ExitStack

import numpy as np
import ml_dtypes

import concourse.bass as bass
import concourse.mybir as mybir
from concourse.bass_utils import run_bass_kernel_spmd

F32 = mybir.dt.float32
BF16 = mybir.dt.bfloat16
AF = mybir.ActivationFunctionType
ALU = mybir.AluOpType
AX = mybir.AxisListType

D = 1024
S = 2048
NT = 16
DEPTH = 4
NE = 32
CAP = 512
NJT = CAP // 128
ALPHA = (2 * DEPTH) ** 0.25
LN_EPS = 1e-5
G = 1024


class Op:
    __slots__ = ("eng", "fn", "deps", "dma", "signal", "sig", "idx")

    def __init__(self, eng, fn, deps, dma, idx):
        self.eng = eng
        self.fn = fn
        self.deps = deps
        self.dma = dma
        self.signal = False
        self.sig = None
        self.idx = idx


class Prog:
    NDMA = 24

    def __init__(self, nc):
        self.nc = nc
        self.ops = []
        self.last_writer = {}
        self.readers = {}

    def add(self, eng, fn, reads=(), writes=(), dma=False):
        idx = len(self.ops)
        ops = self.ops
        deps = set()
        for k in reads:
            w = self.last_writer.get(k)
            if w is not None:
                deps.add(w)
        for k in writes:
            w = self.last_writer.get(k)
            if w is not None:
                o = ops[w]
                if o.dma or dma or o.eng != eng:
                    deps.add(w)
            for r in self.readers.get(k, ()):
                o = ops[r]
                if o.dma or dma or o.eng != eng:
                    deps.add(r)
        if eng == "pe" and not dma:
            deps = {d for d in deps if ops[d].dma or ops[d].eng != "pe"}
        latest = {}
        pruned = set()
        for d in deps:
            o = ops[d]
            if o.dma:
                pruned.add(d)
            elif latest.get(o.eng, -1) < d:
                latest[o.eng] = d
        pruned.update(latest.values())
        deps = pruned
        op = Op(eng, fn, deps, dma, idx)
        ops.append(op)
        for k in reads:
            self.readers.setdefault(k, []).append(idx)
        for k in writes:
            self.last_writer[k] = idx
            self.readers[k] = []
        return op

    def emit(self, stack):
        nc = self.nc
        ops = self.ops
        for op in ops:
            for d in op.deps:
                ops[d].signal = True
        engs = ["pe", "act", "dve", "pool", "sp"]
        esem = {e: stack.enter_context(nc.semaphore("s_" + e)) for e in engs}
        dsem = {e: [stack.enter_context(nc.semaphore("d_%s_%d" % (e, i))) for i in range(self.NDMA)]
                for e in ("sp", "act", "pool")}
        cnt = {e: 0 for e in engs}
        dcnt = {e: 0 for e in dsem}
        prewait = {}
        for op in ops:
            if op.dma:
                op.signal = True
                i = dcnt[op.eng]
                dcnt[op.eng] += 1
                sem = dsem[op.eng][i % self.NDMA]
                op.sig = (sem, 16 * (i // self.NDMA + 1))
                if i >= self.NDMA:
                    prewait[op.idx] = (sem, 16 * (i // self.NDMA))
            elif op.signal:
                cnt[op.eng] += 1
                op.sig = (esem[op.eng], cnt[op.eng])
        per = {e: [op for op in ops if op.eng == e] for e in engs}
        block = stack.enter_context(nc.Block())
        self.nwaits = 0

        def body(e):
            def run(h):
                waited = {}
                for op in per[e]:
                    need = {}
                    if op.idx in prewait:
                        s, v = prewait[op.idx]
                        need[id(s)] = (s, v)
                    for d in op.deps:
                        s, v = ops[d].sig
                        cur = need.get(id(s))
                        if cur is None or cur[1] < v:
                            need[id(s)] = (s, v)
                    for sid, (s, v) in need.items():
                        if waited.get(sid, 0) < v:
                            h.wait_ge(s, v)
                            waited[sid] = v
                            self.nwaits += 1
                    ins = op.fn(h)
                    if op.signal and ins is not None:
                        s, v = op.sig
                        ins.then_inc(s, 16 if op.dma else 1)
            return run

        block.tensor(body("pe"))
        block.scalar(body("act"))
        block.vector(body("dve"))
        block.gpsimd(body("pool"))
        block.sync(body("sp"))
        self.counts = dict(cnt)


class V:
    __slots__ = ("ap", "keys")

    def __init__(self, ap, keys):
        self.ap = ap
        self.keys = keys


def _prod(s):
    n = 1
    for x in s:
        n *= x
    return n


_RE = {2: "p (a b) -> p a b", 3: "p (a b c) -> p a b c"}


class Buf:
    def __init__(self, M, name, nbytes):
        self.name = name
        self.nbytes = nbytes
        self.t = M.st.enter_context(M.nc.sbuf_tensor(name, [128, nbytes // 2], BF16))

    def view(self, off, shape, dt=BF16, p0=0, parts=128):
        esz = 4 if dt == F32 else 2
        n = _prod(shape)
        assert off % 4 == 0 and off + n * esz <= self.nbytes, (self.name, off, shape)
        a = self.t[p0:p0 + parts, off // 2:(off + n * esz) // 2]
        if dt == F32:
            a = a.bitcast(F32)
        if len(shape) == 2:
            a = a.rearrange("p (a b) -> p a b", a=shape[0])
        elif len(shape) == 3:
            a = a.rearrange("p (a b c) -> p a b c", a=shape[0], b=shape[1])
        keys = [(self.name, g) for g in range(off // G, (off + n * esz - 1) // G + 1)]
        return V(a, keys)


def keys_of(vs):
    out = []
    for v in vs:
        if isinstance(v, V):
            out.extend(v.keys)
        elif isinstance(v, (list, tuple)) and v and isinstance(v[0], (V,)):
            out.extend(keys_of(v))
        else:
            out.append(v)
    return out


def make_consts():
    c = {}
    c["ident_f"] = np.eye(128, dtype=np.float32)
    c["iota_row"] = np.broadcast_to(np.arange(CAP, dtype=np.float32), (128, CAP)).copy()
    c["piota"] = (np.arange(128, dtype=np.float32)[:, None] + 128.0 * np.arange(NJT, dtype=np.float32)[None, :]).copy()
    c["triu"] = np.triu(np.ones((128, 128), np.float32), k=1)
    c["ones"] = np.ones((128, 128), np.float32)
    m = np.where(np.arange(128)[None, :] >= np.arange(128)[:, None], 0.0, -30000.0).astype(np.float32)
    c["maskneg"] = np.tile(m, (1, 4))
    inv_freq = (500000.0 ** (-np.arange(0, 16, 2, dtype=np.float32) / 16.0)).astype(np.float32)
    ang = np.arange(S, dtype=np.float32)[:, None] * inv_freq[None, :]
    cos, sin = np.cos(ang).astype(np.float32), np.sin(ang).astype(np.float32)
    cosf = np.ones((128, S), np.float32)
    sinf = np.zeros((128, S), np.float32)
    rotm = np.zeros((128, 128), np.float32)
    for hl in range(2):
        for dd in range(16):
            cosf[64 * hl + dd] = cos[:, dd % 8]
            sinf[64 * hl + dd] = sin[:, dd % 8]
        for dd in range(8):
            rotm[64 * hl + dd + 8, 64 * hl + dd] = -1.0
            rotm[64 * hl + dd, 64 * hl + dd + 8] = 1.0
    c["cosf"], c["sinf"], c["rotm"] = cosf, sinf, rotm
    c["tri"] = np.triu(np.ones((128, 128), np.float32), k=0)
    padm = np.zeros((8, 8), np.float32)
    for cb in range(8):
        padm[cb, cb:] = -1e30
    c["padm"] = np.broadcast_to(padm.reshape(1, 64), (128, 64)).copy()
    return c


CONST_SHAPES = {"ident_f": [128, 128], "iota_row": [128, CAP], "piota": [128, NJT],
                "triu": [128, 128], "ones": [128, 128], "maskneg": [128, 512], "cosf": [128, S], "sinf": [128, S],
                "rotm": [128, 128], "tri": [128, 128], "padm": [128, 64]}


class MK:
    def __init__(self, layers, phases, debug_in=None):
        self.layers = layers
        self.phases = phases
        self.debug = debug_in
        self.nc = bass.Bass("TRN2", target_bir_lowering=False)
        self.st = ExitStack()

    def op(self, eng, fn, r=(), w=(), dma=False):
        return self.P.add(eng, fn, keys_of(r), keys_of(w), dma)

    def dram_in(self, name, shape):
        return self.nc.dram_tensor(name, list(shape), F32, kind="ExternalInput").ap()

    def psb(self, b, cols=512, parts=128, c0=0):
        return V(self.ps[0:parts, b, c0:c0 + cols], [("ps", b)])

    def build(self):
        nc, st = self.nc, self.st
        nl = len(self.layers)
        self.x = self.dram_in("x", [S, D])
        self.y = nc.dram_tensor("y", [S, D], F32, kind="ExternalOutput").ap()
        self.w_router = self.dram_in("moe_w_router", [nl, D, NE])
        self.b_router = self.dram_in("moe_b_router", [nl, NE])
        self.w_gu = self.dram_in("moe_w_gate_up", [nl, NE, D, 2 * D])
        self.b_gu = self.dram_in("moe_b_gate_up", [nl, NE, 2 * D])
        self.w_dn = self.dram_in("moe_w_down", [nl, NE, D, D])
        self.b_dn = self.dram_in("moe_b_down", [nl, NE, D])
        self.ln_g = {"mix": self.dram_in("ln_mix_g", [nl, D]), "moe": self.dram_in("ln_ffn_g", [nl, D])}
        self.ln_b = {"mix": self.dram_in("ln_mix_b", [nl, D]), "moe": self.dram_in("ln_ffn_b", [nl, D])}
        self.cst = {k: self.dram_in("c_" + k, s) for k, s in CONST_SHAPES.items()}
        na = max(1, sum(1 for l in self.layers if l < 2))
        self.ssm_w_in = self.dram_in("ssm_w_in", [na, D, 5152])
        self.ssm_conv_w = self.dram_in("ssm_conv_w", [na, 4, 3072])
        self.ssm_conv_b = self.dram_in("ssm_conv_b", [na, 3072])
        self.ssm_dt_bias = self.dram_in("ssm_dt_bias", [na, 32])
        self.ssm_a_log = self.dram_in("ssm_a_log", [na, 32])
        self.ssm_d = self.dram_in("ssm_d", [na, 32])
        self.ssm_norm_g = self.dram_in("ssm_norm_g", [na, 2048])
        self.ssm_w_out = self.dram_in("ssm_w_out", [na, 2048, D])
        self.attn_layers = [l for l in self.layers if l >= 2]
        nbl = max(1, len(self.attn_layers))
        self.kv_w_k = self.dram_in("kv_w_k", [D, D])
        self.kv_w_v = self.dram_in("kv_w_v", [D, D])
        self.attn_w_q = self.dram_in("attn_w_q", [nbl, D, D])
        self.attn_w_o = self.dram_in("attn_w_o", [nbl, D, D])
        self.kt_s = self.y[0:1024, :].rearrange("(p a) c -> p (a c)", p=128)
        self.va_s = self.y[1024:2048, :].rearrange("(p a) c -> p (a c)", p=128)

        self.dbg = nc.dram_tensor("dbg", [128, 2048], F32, kind="ExternalOutput").ap() if self.debug else None
        self.P = Prog(nc)
        self.ps = st.enter_context(nc.psum_tensor("ps", [128, 8, 512], F32))
        self.Rb = Buf(self, "R", NT * D * 4)
        self.HBb = Buf(self, "HB", NT * D * 2)
        self.HTb = Buf(self, "HT", 8 * S * 2)
        self.Cb = Buf(self, "CST", 6 * 1024)
        self.Ab = Buf(self, "ARENA", 72 * 1024)
        self.R = [self.Rb.view(i * D * 4, [D], F32) for i in range(NT)]
        self.HB = [self.HBb.view(i * D * 2, [D], BF16) for i in range(NT)]
        self.HT = self.HTb.view(0, [8, S], BF16)

        self.load_consts()
        for i in range(NT):
            self.op("sp", lambda h, i=i: h.dma_start(out=self.R[i].ap, in_=self.x[i * 128:(i + 1) * 128, :]),
                    r=["x"], w=[self.R[i]], dma=True)
        first = True
        for kind, l in self.phases:
            li = self.layers.index(l)
            if kind == "prep":
                for i in range(NT):
                    self.make_copies(i)
            elif kind == "nomix":
                for i in range(NT):
                    Ri = self.R[i]
                    self.op("act", lambda h, Ri=Ri: h.activation(out=Ri.ap, in_=Ri.ap, func=AF.Copy, scale=ALPHA), r=[Ri], w=[Ri])
                self.layer_norm("mix", li)
            elif kind == "attn":
                self.attention(l, li)
                self.layer_norm("mix", li)
            elif kind == "mamba":
                self.mamba(li)
                self.layer_norm("mix", li)
            elif kind == "moe":
                self.moe(li)
                self.layer_norm("moe", li)
        for i in range(NT):
            self.op("sp", lambda h, i=i: h.dma_start(out=self.y[i * 128:(i + 1) * 128, :], in_=self.R[i].ap),
                    r=[self.R[i]], w=["y%d" % i, "kt_s", "va_s"], dma=True)
        self.P.add("sp", lambda h: None, reads=["y%d" % i for i in range(NT)])
        self.P.emit(st)
        return nc

    def load_consts(self):
        cb = self.Cb
        off = 0
        self.C = {}

        def alloc(n_bytes):
            nonlocal off
            o = off
            off += (n_bytes + 3) // 4 * 4
            return o
        for k in ("maskneg",):
            v = cb.view(alloc(512 * 4), [512], F32)
            self.C[k] = v
            self.op("sp", lambda h, v=v, k=k: h.dma_start(out=v.ap, in_=self.cst[k]), r=[], w=[v], dma=True)
        v = cb.view(alloc(128 * 4), [128], F32)
        self.C["ones_f"] = v
        self.op("sp", lambda h, v=v: h.dma_start(out=v.ap, in_=self.cst["ones"]), r=[], w=[v], dma=True)
        for k in ("ident_f", "piota"):
            shp = CONST_SHAPES[k]
            v = cb.view(alloc(shp[1] * 4), [shp[1]], F32)
            self.C[k] = v
            self.op("sp", lambda h, v=v, k=k: h.dma_start(out=v.ap, in_=self.cst[k]), r=[], w=[v], dma=True)
        v = cb.view(alloc(CAP * 4), [CAP], F32)
        self.C["iota_row"] = v
        self.op("sp", lambda h, v=v: h.dma_start(out=v.ap, in_=self.cst["iota_row"]), r=[], w=[v], dma=True)
        for k in ("triu", "ones"):
            v = cb.view(alloc(128 * 2), [128], BF16)
            self.C[k] = v
            self.op("pool", lambda h, v=v, k=k: h.dma_start(out=v.ap, in_=self.cst[k]), r=[], w=[v], dma=True)
        v = cb.view(alloc(128 * 2), [128], BF16)
        self.C["ident_b"] = v
        self.op("pool", lambda h, v=v: h.dma_start(out=v.ap, in_=self.cst["ident_f"]), r=[], w=[v], dma=True)
        self.cst_off = off

    def layer_norm(self, which, li, scale_in_place=True):
        A = self.Ab
        gt = A.view(0, [D], F32)
        bt = A.view(4096, [D], F32)
        st6 = A.view(8192, [2, 6], F32)
        mv = A.view(8192 + 64, [2], F32)
        rstd = A.view(8192 + 128, [1], F32)
        nmr = A.view(8192 + 192, [1], F32)
        tmp = A.view(9216, [D], F32)
        self.op("sp", lambda h: h.dma_start(out=gt.ap, in_=self.ln_g[which][li:li + 1, :].partition_broadcast(128)),
                r=[], w=[gt], dma=True)
        self.op("sp", lambda h: h.dma_start(out=bt.ap, in_=self.ln_b[which][li:li + 1, :].partition_broadcast(128)),
                r=[], w=[bt], dma=True)
        for i in range(NT):
            Ri = self.R[i]
            for c in range(2):
                self.op("dve", lambda h, c=c, Ri=Ri: h.bn_stats(out=st6.ap[:, c, :], in_=Ri.ap[:, c * 512:(c + 1) * 512]),
                        r=[Ri], w=[st6])
            self.op("dve", lambda h: h.bn_aggr(out=mv.ap, in_=st6.ap.rearrange("p a b -> p (a b)")), r=[st6], w=[mv])
            self.op("act", lambda h: h.activation(out=rstd.ap, in_=mv.ap[:, 1:2], func=AF.Sqrt, bias=LN_EPS, scale=1.0),
                    r=[mv], w=[rstd])
            self.op("dve", lambda h: h.reciprocal(out=rstd.ap, in_=rstd.ap), r=[rstd], w=[rstd])
            self.op("dve", lambda h: h.scalar_tensor_tensor(out=nmr.ap, in0=mv.ap[:, 0:1], scalar=-1.0, in1=rstd.ap, op0=ALU.mult, op1=ALU.mult),
                    r=[mv, rstd], w=[nmr])
            self.op("act", lambda h, Ri=Ri: h.activation(out=tmp.ap, in_=Ri.ap, func=AF.Identity, bias=nmr.ap, scale=rstd.ap), r=[Ri, nmr, rstd], w=[tmp])
            self.op("dve", lambda h: h.tensor_tensor(out=tmp.ap, in0=tmp.ap, in1=gt.ap, op=ALU.mult), r=[tmp, gt], w=[tmp])
            self.op("dve", lambda h, Ri=Ri: h.tensor_tensor(out=Ri.ap, in0=tmp.ap, in1=bt.ap, op=ALU.add), r=[tmp, bt], w=[Ri])
            self.make_copies(i)

    def make_copies(self, i):
            Ri = self.R[i]
            HBi = self.HB[i]
            self.op("act", lambda h, Ri=Ri, HBi=HBi: h.activation(out=HBi.ap, in_=Ri.ap, func=AF.Copy), r=[Ri], w=[HBi])
            pb = 6 + (i % 2)
            pt = V(self.ps[:, pb, :].bitcast(BF16)[:, 0:1024].rearrange("p (a b) -> p a b", a=8), [("ps", pb)])
            for m in range(8):
                self.op("pe", lambda h, m=m, HBi=HBi, pt=pt: h.transpose(out=pt.ap[:, m, :], in_=HBi.ap[:, m * 128:(m + 1) * 128],
                                                                          identity=self.C["ident_b"].ap),
                        r=[HBi, self.C["ident_b"]], w=[pt])
            htv = V(self.HT.ap[:, :, i * 128:(i + 1) * 128], [("HT", g) for g in range(8 * S * 2 // G)])
            self.op("act", lambda h, pt=pt, htv=htv: h.activation(out=htv.ap, in_=pt.ap, func=AF.Copy), r=[pt], w=[htv])


    def mamba(self, li):
        A, HBb, C = self.Ab, self.HBb, self.C
        identf = C["ident_f"].ap
        op = self.op
        ACSF = HBb.view(0, [S], F32, parts=32)
        NBF = HBb.view(8192, [S], F32, parts=32)
        NBH = [HBb.view(16384 + 2048 * i, [512], F32, parts=32) for i in range(2)]
        ONESF = HBb.view(20480, [128], F32)
        PAR = HBb.view(21504, [8], F32, parts=32)
        ACST = HBb.view(24576, [16, 32], F32)
        W2T = HBb.view(26624, [16, 32], F32)
        EACST = HBb.view(28672, [16, 32], F32)
        DECC = HBb.view(30720, [16, 32], F32)
        o = 0

        def alloc(n):
            nonlocal o
            a = o
            o += (n + G - 1) // G * G
            return a
        CWT = A.view(alloc(6 * 4 * 4), [6, 4], F32)
        CBT = A.view(alloc(6 * 4), [6], F32)
        DBC = A.view(alloc(32 * 4), [32], F32)
        NGg = A.view(alloc(512 * 4), [512], F32)
        xf_off = alloc(4 * S * 2)
        XF = A.view(xf_off, [4, S], BF16)
        T1 = A.view(xf_off, [S], F32, parts=32)
        T2 = A.view(xf_off + S * 4, [S], F32, parts=32)
        bf_off = alloc(S * 2)
        BF = A.view(bf_off, [S], BF16)
        WDT = A.view(bf_off, [8, 32], BF16)
        CF = A.view(alloc(S * 2), [S], BF16)
        STATE = A.view(alloc(512 * 4), [512], F32)
        reg = o
        o = reg
        WX = A.view(alloc(8 * 512 * 2), [8, 512], BF16)
        WB = A.view(alloc(8 * 128 * 2), [8, 128], BF16)
        WC = A.view(alloc(8 * 128 * 2), [8, 128], BF16)
        U = A.view(alloc((S + 4) * 4), [S + 4], F32)
        ACC = [A.view(alloc(512 * 4), [512], F32) for _ in range(2)]
        o_conv = o
        o = reg
        WZ = A.view(alloc(8 * 512 * 2), [8, 512], BF16)
        WO = A.view(alloc(4 * D * 2), [4, D], BF16)
        GT4 = A.view(alloc(512 * 4), [4, 128], F32)
        ARG = [A.view(alloc(512 * 4), [512], F32) for _ in range(2)]
        MT = [A.view(alloc(512 * 2), [4, 128], BF16) for _ in range(8)]
        XTOK = A.view(alloc(512 * 2), [512], BF16)
        BTOK = A.view(alloc(128 * 2), [128], BF16)
        PREV = A.view(alloc(512 * 2), [512], BF16)
        xdt_off = alloc(512 * 2)
        XDT2 = A.view(xdt_off, [512], BF16)
        Y1 = A.view(alloc(512 * 4), [512], F32)
        xd_off = alloc(512 * 4)
        XD = A.view(xd_off, [512], F32)
        ZS = A.view(alloc(512 * 2), [512], BF16)
        JUNK = A.view(xd_off, [512], F32)
        SS = A.view(alloc(16), [4], F32)
        YN = A.view(alloc(512 * 2), [512], BF16)
        YNT = A.view(xdt_off, [4, 128], BF16)
        assert max(o, o_conv) <= A.nbytes, (o, o_conv)
        w_in = self.ssm_w_in[li].rearrange("(kc kp) c -> kp kc c", kp=128)

        for i in range(NT):
            Ri = self.R[i]
            op("act", lambda h, Ri=Ri: h.activation(out=Ri.ap, in_=Ri.ap, func=AF.Copy, scale=ALPHA), r=[Ri], w=[Ri])

        op("pool", lambda h: h.dma_start(out=WDT.ap, in_=w_in[:, :, 5120:5152]), r=[], w=[WDT], dma=True)
        op("sp", lambda h: h.dma_start(out=PAR.ap[:, 0:1], in_=self.ssm_dt_bias[li:li + 1, :].rearrange("o h -> h o")), r=[], w=[PAR], dma=True)
        op("sp", lambda h: h.dma_start(out=PAR.ap[:, 1:2], in_=self.ssm_a_log[li:li + 1, :].rearrange("o h -> h o")), r=[], w=[PAR], dma=True)
        op("sp", lambda h: h.dma_start(out=DBC.ap, in_=self.ssm_d[li:li + 1, :].partition_broadcast(128)), r=[], w=[DBC], dma=True)
        op("dve", lambda h: h.memset(ONESF.ap, 1.0), r=[], w=[ONESF])
        for r in range(4):
            pd = self.psb(r % 2, parts=32)
            for k in range(8):
                op("pe", lambda h, k=k, r=r, pd=pd: h.matmul(pd.ap, lhsT=WDT.ap[:, k, :], rhs=self.HT.ap[:, k, r * 512:(r + 1) * 512],
                                                           start=(k == 0), stop=(k == 7)), r=[WDT, self.HT], w=[pd])
            op("act", lambda h, r=r, pd=pd: h.activation(out=T1.ap[:, r * 512:(r + 1) * 512], in_=pd.ap, func=AF.Exp, bias=PAR.ap[:, 0:1], scale=1.0),
               r=[pd, PAR], w=[T1])
        op("act", lambda h: h.activation(out=T1.ap, in_=T1.ap, func=AF.Ln, bias=1.0, scale=1.0), r=[T1], w=[T1])
        op("act", lambda h: h.activation(out=T2.ap, in_=T1.ap, func=AF.Ln), r=[T1], w=[T2])
        op("act", lambda h: h.activation(out=PAR.ap[:, 2:3], in_=PAR.ap[:, 1:2], func=AF.Exp), r=[PAR], w=[PAR])
        op("dve", lambda h: h.tensor_scalar(out=PAR.ap[:, 2:3], in0=PAR.ap[:, 2:3], scalar1=-1.0, scalar2=None, op0=ALU.mult), r=[PAR], w=[PAR])
        op("dve", lambda h: h.tensor_scalar(out=T1.ap, in0=T1.ap, scalar1=PAR.ap[:, 2:3], scalar2=None, op0=ALU.mult), r=[T1, PAR], w=[T1])
        for c in range(16):
            op("dve", lambda h, c=c: h.tensor_tensor_scan(out=ACSF.ap[:, c * 128:(c + 1) * 128], data0=ONESF.ap[0:32, :], data1=T1.ap[:, c * 128:(c + 1) * 128],
                                                         initial=0.0, op0=ALU.mult, op1=ALU.add), r=[ONESF, T1], w=[ACSF])
        op("dve", lambda h: h.tensor_tensor(out=NBF.ap, in0=T2.ap, in1=ACSF.ap, op=ALU.subtract), r=[T2, ACSF], w=[NBF])
        for c in range(16):
            op("act", lambda h, c=c: h.activation(out=T2.ap[:, c * 128:(c + 1) * 128], in_=NBF.ap[:, c * 128:(c + 1) * 128], func=AF.Exp,
                                                  bias=ACSF.ap[:, c * 128 + 127:c * 128 + 128], scale=1.0), r=[NBF, ACSF], w=[T2])
        pa = self.psb(2)
        pw = self.psb(3)
        for c in range(16):
            op("pe", lambda h, c=c: h.transpose(out=pa.ap[:, c * 32:(c + 1) * 32], in_=ACSF.ap[:, c * 128:(c + 1) * 128], identity=identf[0:32, 0:32]),
               r=[ACSF, C["ident_f"]], w=[pa])
            op("pe", lambda h, c=c: h.transpose(out=pw.ap[:, c * 32:(c + 1) * 32], in_=T2.ap[:, c * 128:(c + 1) * 128], identity=identf[0:32, 0:32]),
               r=[T2, C["ident_f"]], w=[pw])
        op("act", lambda h: h.activation(out=ACST.ap.rearrange("p a b -> p (a b)"), in_=pa.ap, func=AF.Copy), r=[pa], w=[ACST])
        op("act", lambda h: h.activation(out=W2T.ap.rearrange("p a b -> p (a b)"), in_=pw.ap, func=AF.Copy), r=[pw], w=[W2T])
        op("act", lambda h: h.activation(out=EACST.ap, in_=ACST.ap, func=AF.Exp), r=[ACST], w=[EACST])
        op("dve", lambda h: h.tensor_scalar(out=Y1.ap, in0=ACST.ap.rearrange("p a b -> p (a b)"), scalar1=identf[:, 127:128], scalar2=None, op0=ALU.mult),
           r=[ACST, C["ident_f"]], w=[Y1])
        pdc = self.psb(0)
        op("pe", lambda h: h.matmul(pdc.ap, lhsT=C["ones_f"].ap, rhs=Y1.ap, start=True, stop=True), r=[C["ones_f"], Y1], w=[pdc])
        op("act", lambda h: h.activation(out=DECC.ap.rearrange("p a b -> p (a b)"), in_=pdc.ap, func=AF.Exp), r=[pdc], w=[DECC])

        if getattr(self, "dbg", None) is not None:
            for q, tv in enumerate((ACST, W2T, EACST, DECC)[3:], 3):
                op("sp", lambda h, q=q, tv=tv: h.dma_start(out=self.dbg[:, q * 512:(q + 1) * 512], in_=tv.ap.rearrange("p a b -> p (a b)")),
                   r=[tv], w=["dbg%d" % q], dma=True)
        for g in range(4):
            op("pool", lambda h, g=g: h.dma_start(out=WX.ap, in_=w_in[:, :, 2048 + g * 512:2048 + (g + 1) * 512]), r=[], w=[WX], dma=True)
            op("pool", lambda h, g=g: h.dma_start(out=WB.ap, in_=w_in[:, :, 4096 + g * 128:4096 + (g + 1) * 128]), r=[], w=[WB], dma=True)
            op("pool", lambda h, g=g: h.dma_start(out=WC.ap, in_=w_in[:, :, 4608 + g * 128:4608 + (g + 1) * 128]), r=[], w=[WC], dma=True)
            op("dve", lambda h: h.memset(U.ap[:, 0:4], 0.0), r=[], w=[U])
            bases = [g * 512 + j * 128 for j in range(4)] + [2048 + g * 128, 2560 + g * 128]
            for j, b0 in enumerate(bases):
                op("sp", lambda h, j=j, b0=b0: h.dma_start(out=CWT.ap[:, j, :], in_=self.ssm_conv_w[li, :, b0:b0 + 128].rearrange("k p -> p k"),
                                                          allow_slow_non_contiguous=True), r=[], w=[CWT], dma=True)
                op("sp", lambda h, j=j, b0=b0: h.dma_start(out=CBT.ap[:, j:j + 1], in_=self.ssm_conv_b[li:li + 1, b0:b0 + 128].rearrange("o p -> p o")),
                   r=[], w=[CBT], dma=True)
            n = 0
            for j in range(6):
                for r in range(4):
                    pc = self.psb(6 + (n % 2))
                    for k in range(8):
                        if j < 4:
                            lw = WX.ap[:, k, j * 128:(j + 1) * 128]
                            wv = WX
                        else:
                            wv = WB if j == 4 else WC
                            lw = wv.ap[:, k, :]
                        op("pe", lambda h, k=k, r=r, lw=lw, pc=pc: h.matmul(pc.ap, lhsT=lw, rhs=self.HT.ap[:, k, r * 512:(r + 1) * 512],
                                                                       start=(k == 0), stop=(k == 7)), r=[wv, self.HT], w=[pc])
                    op("act", lambda h, r=r, pc=pc: h.activation(out=U.ap[:, 4 + r * 512:4 + (r + 1) * 512], in_=pc.ap, func=AF.Copy), r=[pc], w=[U])
                    acc = ACC[n % 2]
                    eng = "dve"
                    op(eng, lambda h, r=r, j=j, acc=acc: h.tensor_scalar(out=acc.ap, in0=U.ap[:, 1 + r * 512:1 + (r + 1) * 512], scalar1=CWT.ap[:, j, 0:1],
                                                                        scalar2=None, op0=ALU.mult), r=[U, CWT], w=[acc])
                    for t in range(1, 4):
                        op(eng, lambda h, r=r, j=j, t=t, acc=acc: h.scalar_tensor_tensor(out=acc.ap, in0=U.ap[:, 1 + t + r * 512:1 + t + (r + 1) * 512],
                                                                                         scalar=CWT.ap[:, j, t:t + 1], in1=acc.ap, op0=ALU.mult, op1=ALU.add),
                           r=[U, CWT, acc], w=[acc])
                    if j < 4:
                        dst, dv = XF.ap[:, j, r * 512:(r + 1) * 512], XF
                    elif j == 4:
                        dst, dv = BF.ap[:, r * 512:(r + 1) * 512], BF
                    else:
                        dst, dv = CF.ap[:, r * 512:(r + 1) * 512], CF
                    op("act", lambda h, j=j, acc=acc, dst=dst: h.activation(out=dst, in_=acc.ap, func=AF.Silu, bias=CBT.ap[:, j:j + 1], scale=1.0),
                       r=[acc, CBT], w=[dv])
                    n += 1
            op("pool", lambda h, g=g: h.dma_start(out=WZ.ap, in_=w_in[:, :, g * 512:(g + 1) * 512]), r=[], w=[WZ], dma=True)
            op("pool", lambda h, g=g: h.dma_start(out=WO.ap, in_=self.ssm_w_out[li, g * 512:(g + 1) * 512, :].rearrange("(c p) d -> p c d", p=128)),
               r=[], w=[WO], dma=True)
            op("sp", lambda h, g=g: h.dma_start(out=NGg.ap, in_=self.ssm_norm_g[li:li + 1, g * 512:(g + 1) * 512].partition_broadcast(128)),
               r=[], w=[NGg], dma=True)
            op("dve", lambda h: h.memset(STATE.ap, 0.0), r=[], w=[STATE])
            hs = slice(8 * g, 8 * g + 8)
            for cb in range(4):
                pgt = self.psb(5)
                for c in range(4):
                    cc = 4 * cb + c
                    op("pe", lambda h, c=c, cc=cc: h.matmul(pgt.ap[:, c * 128:(c + 1) * 128], lhsT=BF.ap[:, cc * 128:(cc + 1) * 128],
                                                           rhs=CF.ap[:, cc * 128:(cc + 1) * 128], start=True, stop=True), r=[BF, CF], w=[pgt])
                op("act", lambda h: h.activation(out=GT4.ap.rearrange("p a b -> p (a b)"), in_=pgt.ap, func=AF.Copy), r=[pgt], w=[GT4])
                for hh in range(8):
                    hd = 8 * g + hh
                    nbh = NBH[hh % 2]
                    op("dve", lambda h, hd=hd, cb=cb, nbh=nbh: h.tensor_scalar(out=nbh.ap, in0=NBF.ap[:, cb * 512:(cb + 1) * 512], scalar1=identf[0:32, hd:hd + 1],
                                                                               scalar2=None, op0=ALU.mult), r=[NBF, C["ident_f"]], w=[nbh])
                    parg = self.psb(4)
                    op("pe", lambda h, hd=hd, cb=cb: h.matmul(parg.ap, lhsT=identf[0:32, hd:hd + 1].broadcast_to([32, 128]), rhs=ACSF.ap[:, cb * 512:(cb + 1) * 512],
                                                              start=True, stop=False), r=[C["ident_f"], ACSF], w=[parg])
                    for c in range(4):
                        op("pe", lambda h, c=c, nbh=nbh: h.matmul(parg.ap[:, c * 128:(c + 1) * 128], lhsT=nbh.ap[:, c * 128:(c + 1) * 128], rhs=ONESF.ap[0:32, :],
                                                                  start=False, stop=(c == 3)), r=[nbh, ONESF], w=[parg])
                    arg = ARG[hh % 2]
                    op("dve", lambda h, arg=arg: h.tensor_tensor(out=arg.ap, in0=parg.ap, in1=C["maskneg"].ap, op=ALU.add), r=[parg, C["maskneg"]], w=[arg])
                    op("act", lambda h, arg=arg: h.activation(out=arg.ap, in_=arg.ap, func=AF.Exp), r=[arg], w=[arg])
                    op("dve", lambda h, arg=arg, hh=hh: h.tensor_tensor(out=MT[hh].ap.rearrange("p a b -> p (a b)"), in0=arg.ap,
                                                                        in1=GT4.ap.rearrange("p a b -> p (a b)"), op=ALU.mult), r=[arg, GT4], w=[MT[hh]])
                for c in range(4):
                    cc = 4 * cb + c
                    tk = slice(cc * 128, (cc + 1) * 128)
                    ptx = V(self.ps[:, 5, :].bitcast(BF16)[:, 0:512], [("ps", 5)])
                    ptb = V(self.ps[:, 5, :].bitcast(BF16)[:, 512:640], [("ps", 5)])
                    for j in range(4):
                        op("pe", lambda h, j=j, tk=tk: h.transpose(out=ptx.ap[:, j * 128:(j + 1) * 128], in_=XF.ap[:, j, tk], identity=C["ident_b"].ap),
                           r=[XF, C["ident_b"]], w=[ptx])
                    op("pe", lambda h, tk=tk: h.transpose(out=ptb.ap, in_=BF.ap[:, tk], identity=C["ident_b"].ap), r=[BF, C["ident_b"]], w=[ptb])
                    op("act", lambda h: h.activation(out=XTOK.ap, in_=ptx.ap, func=AF.Copy), r=[ptx], w=[XTOK])
                    op("act", lambda h: h.activation(out=BTOK.ap, in_=ptb.ap, func=AF.Copy), r=[ptb], w=[BTOK])
                    op("act", lambda h: h.activation(out=PREV.ap, in_=STATE.ap, func=AF.Copy), r=[STATE], w=[PREV])
                    pyd, pyo, pst, pz = self.psb(0), self.psb(1), self.psb(2), self.psb(3)
                    for hh in range(8):
                        op("pe", lambda h, hh=hh, c=c: h.matmul(pyd.ap[:, hh * 64:(hh + 1) * 64], lhsT=MT[hh].ap[:, c, :], rhs=XTOK.ap[:, hh * 64:(hh + 1) * 64],
                                                               start=True, stop=True), r=[MT[hh], XTOK], w=[pyd])
                    op("pe", lambda h, tk=tk: h.matmul(pyo.ap, lhsT=CF.ap[:, tk], rhs=PREV.ap, start=True, stop=True), r=[CF, PREV], w=[pyo])
                    op("dve", lambda h, cc=cc, hs=hs: h.tensor_tensor(out=XDT2.ap.rearrange("p (a b) -> p a b", a=8), in0=XTOK.ap.rearrange("p (a b) -> p a b", a=8),
                                                               in1=W2T.ap[:, cc, hs].unsqueeze(2).broadcast_to([128, 8, 64]), op=ALU.mult), r=[XTOK, W2T], w=[XDT2])
                    op("pe", lambda h: h.matmul(pst.ap, lhsT=BTOK.ap, rhs=XDT2.ap, start=True, stop=True), r=[BTOK, XDT2], w=[pst])
                    op("dve", lambda h, cc=cc, hs=hs: h.tensor_tensor(out=Y1.ap.rearrange("p (a b) -> p a b", a=8), in0=pyo.ap.rearrange("p (a b) -> p a b", a=8),
                                                              in1=EACST.ap[:, cc, hs].unsqueeze(2).broadcast_to([128, 8, 64]), op=ALU.mult), r=[pyo, EACST], w=[Y1])
                    op("dve", lambda h: h.tensor_tensor(out=Y1.ap, in0=Y1.ap, in1=pyd.ap, op=ALU.add), r=[Y1, pyd], w=[Y1])
                    op("dve", lambda h, hs=hs: h.tensor_tensor(out=XD.ap.rearrange("p (a b) -> p a b", a=8), in0=XTOK.ap.rearrange("p (a b) -> p a b", a=8),
                                                        in1=DBC.ap[:, hs].unsqueeze(2).broadcast_to([128, 8, 64]), op=ALU.mult), r=[XTOK, DBC], w=[XD])
                    op("dve", lambda h: h.tensor_tensor(out=Y1.ap, in0=Y1.ap, in1=XD.ap, op=ALU.add), r=[Y1, XD], w=[Y1])
                    op("dve", lambda h, cc=cc, hs=hs: h.tensor_tensor(out=STATE.ap.rearrange("p (a b) -> p a b", a=8), in0=STATE.ap.rearrange("p (a b) -> p a b", a=8),
                                                              in1=DECC.ap[:, cc, hs].unsqueeze(2).broadcast_to([128, 8, 64]), op=ALU.mult), r=[STATE, DECC], w=[STATE])
                    op("dve", lambda h: h.tensor_tensor(out=STATE.ap, in0=STATE.ap, in1=pst.ap, op=ALU.add), r=[STATE, pst], w=[STATE])
                    if getattr(self, "dbg", None) is not None and g == 0 and cc == 0:
                        op("sp", lambda h: h.dma_start(out=self.dbg[:, 0:512], in_=STATE.ap), r=[STATE], w=["dbg0"], dma=True)
                        op("act", lambda h: h.activation(out=ARG[0].ap, in_=XDT2.ap, func=AF.Copy), r=[XDT2], w=[ARG[0]])
                        op("act", lambda h: h.activation(out=ARG[1].ap[:, 0:128], in_=BTOK.ap, func=AF.Copy), r=[BTOK], w=[ARG[1]])
                        op("sp", lambda h: h.dma_start(out=self.dbg[:, 1024:1536], in_=ARG[0].ap), r=[ARG[0]], w=["dbg2"], dma=True)
                        op("sp", lambda h: h.dma_start(out=self.dbg[:, 512:640], in_=ARG[1].ap[:, 0:128]), r=[ARG[1]], w=["dbg1"], dma=True)
                    for k in range(8):
                        op("pe", lambda h, k=k, tk=tk: h.matmul(pz.ap, lhsT=self.HT.ap[:, k, tk], rhs=WZ.ap[:, k, :], start=(k == 0), stop=(k == 7)),
                           r=[self.HT, WZ], w=[pz])
                    op("act", lambda h: h.activation(out=ZS.ap, in_=pz.ap, func=AF.Silu), r=[pz], w=[ZS])
                    op("dve", lambda h: h.tensor_tensor(out=Y1.ap, in0=Y1.ap, in1=ZS.ap, op=ALU.mult), r=[Y1, ZS], w=[Y1])
                    op("act", lambda h: h.activation(out=JUNK.ap, in_=Y1.ap, func=AF.Square, accum_out=SS.ap[:, 0:1]), r=[Y1], w=[JUNK, SS])
                    op("act", lambda h: h.activation(out=SS.ap[:, 1:2], in_=SS.ap[:, 0:1], func=AF.Sqrt, bias=1e-5, scale=1.0 / 512.0), r=[SS], w=[SS])
                    op("dve", lambda h: h.reciprocal(out=SS.ap[:, 2:3], in_=SS.ap[:, 1:2]), r=[SS], w=[SS])
                    op("dve", lambda h: h.scalar_tensor_tensor(out=YN.ap, in0=Y1.ap, scalar=SS.ap[:, 2:3], in1=NGg.ap, op0=ALU.mult, op1=ALU.mult),
                       r=[Y1, SS, NGg], w=[YN])
                    pty = V(self.ps[:, 5, :].bitcast(BF16)[:, 0:512].rearrange("p (a b) -> p a b", a=4), [("ps", 5)])
                    for j in range(4):
                        op("pe", lambda h, j=j: h.transpose(out=pty.ap[:, j, :], in_=YN.ap[:, j * 128:(j + 1) * 128], identity=C["ident_b"].ap),
                           r=[YN, C["ident_b"]], w=[pty])
                    op("act", lambda h: h.activation(out=YNT.ap, in_=pty.ap, func=AF.Copy), r=[pty], w=[YNT])
                    Rc = self.R[cc]
                    for dr in range(2):
                        pm = self.psb(6 + dr)
                        for j in range(4):
                            op("pe", lambda h, j=j, dr=dr, pm=pm: h.matmul(pm.ap, lhsT=YNT.ap[:, j, :], rhs=WO.ap[:, j, dr * 512:(dr + 1) * 512],
                                                                        start=(j == 0), stop=(j == 3)), r=[YNT, WO], w=[pm])
                        op("dve", lambda h, dr=dr, pm=pm, Rc=Rc: h.tensor_tensor(out=Rc.ap[:, dr * 512:(dr + 1) * 512], in0=Rc.ap[:, dr * 512:(dr + 1) * 512],
                                                                                in1=pm.ap, op=ALU.add), r=[Rc, pm], w=[Rc])


    def attention(self, l, li):
        A, HBb, C = self.Ab, self.HBb, self.C
        op = self.op
        ja = self.attn_layers.index(l)
        build_kv = (ja == 0)
        KT = HBb.view(0, [8, S], BF16)
        o = 0

        def alloc(n):
            nonlocal o
            a = o
            o += (n + G - 1) // G * G
            return a
        VA = A.view(alloc(16 * 16 * 66 * 2), [16, 16, 66], BF16)
        VAF = A.view(0, [16, 16, 33], F32)
        KTF = HBb.view(0, [8 * S // 2], F32)
        KMB = A.view(alloc(8 * 8 * 2), [8, 8], BF16)
        W1R = A.view(alloc(8 * 128 * 2), [8, 128], BF16)
        TRI = A.view(alloc(256), [128], BF16)
        PADM = A.view(alloc(256), [8, 8], F32)
        WSEL = A.view(alloc(16 * 2 * 8 * 4), [16, 2, 8], F32)
        GSC = A.view(alloc(1024), [256], F32)
        W1 = A.view(alloc(8 * 512 * 2), [8, 512], BF16)
        WOp = A.view(alloc(D * 2), [D], BF16)
        QTp = A.view(alloc(S * 2), [S], BF16)
        OTp = A.view(alloc(S * 2), [S], BF16)
        CS = A.view(alloc(512 * 4), [512], F32)
        SN = A.view(alloc(512 * 4), [512], F32)
        T1 = A.view(alloc(512 * 4), [512], F32)
        T2 = A.view(alloc(512 * 4), [512], F32)
        PT = [A.view(alloc(512 * 2), [512], BF16) for _ in range(2)]
        ACC = A.view(alloc(4 * 65 * 4), [4, 65], F32)
        OTOK = A.view(alloc(4 * 128 * 2), [4, 128], BF16)
        assert o <= A.nbytes, o

        for i in range(NT):
            Ri = self.R[i]
            op("act", lambda h, Ri=Ri: h.activation(out=Ri.ap, in_=Ri.ap, func=AF.Copy, scale=ALPHA), r=[Ri], w=[Ri])
        op("pool", lambda h: h.dma_start(out=TRI.ap, in_=self.cst["tri"]), r=[], w=[TRI], dma=True)
        op("sp", lambda h: h.dma_start(out=PADM.ap.rearrange("p a b -> p (a b)"), in_=self.cst["padm"]), r=[], w=[PADM], dma=True)

        def make_rot_weights(pp):
            op("dve", lambda h: h.memset(W1R.ap, 0.0), r=[], w=[W1R])
            for hl in range(2):
                b0 = pp * 128 + 64 * hl
                op("act", lambda h, hl=hl, b0=b0: h.activation(out=W1R.ap[:, :, 64 * hl:64 * hl + 8], in_=W1.ap[:, :, b0 + 8:b0 + 16], func=AF.Copy, scale=-1.0),
                   r=[W1], w=[W1R])
                op("act", lambda h, hl=hl, b0=b0: h.activation(out=W1R.ap[:, :, 64 * hl + 8:64 * hl + 16], in_=W1.ap[:, :, b0:b0 + 8], func=AF.Copy),
                   r=[W1], w=[W1R])

        def rope(pk, dst, dv, r):
            prot = self.psb(6)
            for k in range(8):
                op("pe", lambda h, k=k, r=r, prot=prot: h.matmul(prot.ap, lhsT=W1R.ap[:, k, :], rhs=self.HT.ap[:, k, r * 512:(r + 1) * 512],
                                                              start=(k == 0), stop=(k == 7)), r=[W1R, self.HT], w=[prot])
            op("sp", lambda h, r=r: h.dma_start(out=CS.ap, in_=self.cst["cosf"][:, r * 512:(r + 1) * 512]), r=[], w=[CS], dma=True)
            op("sp", lambda h, r=r: h.dma_start(out=SN.ap, in_=self.cst["sinf"][:, r * 512:(r + 1) * 512]), r=[], w=[SN], dma=True)
            op("dve", lambda h, pk=pk: h.tensor_tensor(out=T1.ap, in0=pk.ap, in1=CS.ap, op=ALU.mult), r=[pk, CS], w=[T1])
            op("dve", lambda h, prot=prot: h.tensor_tensor(out=T2.ap, in0=prot.ap, in1=SN.ap, op=ALU.mult), r=[prot, SN], w=[T2])
            op("dve", lambda h, dst=dst: h.tensor_tensor(out=dst, in0=T1.ap, in1=T2.ap, op=ALU.add), r=[T1, T2], w=[dv])

        def wsrc(w2d, half):
            return w2d.rearrange("(kc kp) c -> kp kc c", kp=128)[:, :, half * 512:(half + 1) * 512]

        def kmeans():
            for p in range(8):
                op("dve", lambda h, p=p: h.tensor_reduce(out=GSC.ap[:, 0:8], in_=KT.ap[:, p, :].rearrange("p (a b) -> p a b", a=8), axis=AX.X, op=ALU.add),
                   r=[KT], w=[GSC])
                op("dve", lambda h, p=p: h.tensor_scalar(out=KMB.ap[:, p, :], in0=GSC.ap[:, 0:8], scalar1=1.0 / 256.0, scalar2=None, op0=ALU.mult),
                   r=[GSC], w=[KMB])

        stop = getattr(self, "attn_stop", 0)
        if stop == -1:
            return
        if build_kv:
            for half in range(2):
                if stop == -2 and half == 1:
                    return
                op("pool", lambda h, half=half: h.dma_start(out=W1.ap, in_=wsrc(self.kv_w_k, half)), r=[], w=[W1], dma=True)
                for pp in range(4):
                    p = half * 4 + pp
                    make_rot_weights(pp)
                    for r in range(4):
                        pk = self.psb(4 + r % 2)
                        for k in range(8):
                            op("pe", lambda h, k=k, r=r, pp=pp, pk=pk: h.matmul(pk.ap, lhsT=W1.ap[:, k, pp * 128:(pp + 1) * 128],
                                                                             rhs=self.HT.ap[:, k, r * 512:(r + 1) * 512], start=(k == 0), stop=(k == 7)),
                               r=[W1, self.HT], w=[pk])
                        rope(pk, KT.ap[:, p, r * 512:(r + 1) * 512], KT, r)
            for half in range(2):
                op("pool", lambda h, half=half: h.dma_start(out=W1.ap, in_=wsrc(self.kv_w_v, half)), r=[], w=[W1], dma=True)
                for i in range(NT):
                    pv = self.psb(4 + i % 2)
                    for k in range(8):
                        op("pe", lambda h, k=k, i=i, pv=pv: h.matmul(pv.ap, lhsT=self.HT.ap[:, k, i * 128:(i + 1) * 128], rhs=W1.ap[:, k, :],
                                                                 start=(k == 0), stop=(k == 7)), r=[self.HT, W1], w=[pv])
                    op("act", lambda h, i=i, half=half, pv=pv: h.activation(out=VA.ap[:, i, half * 8:(half + 1) * 8, 0:64],
                                                                          in_=pv.ap.rearrange("p (a b) -> p a b", a=8), func=AF.Copy), r=[pv], w=[VA])
            if stop == -3:
                return
            op("dve", lambda h: h.memset(VA.ap[:, :, :, 64:65], 1.0), r=[], w=[VA])
            if stop == -4:
                return
            kmeans()
            if not getattr(self, "no_spill", False):
                op("sp", lambda h: h.dma_start(out=self.kt_s, in_=KTF.ap), r=[KT], w=["kt_s"], dma=True)
                for kt in range(16):
                    op("sp", lambda h, kt=kt: h.dma_start(out=self.va_s[:, kt * 512:(kt + 1) * 512].rearrange("p (b c) -> p b c", c=32), in_=VAF.ap[:, kt, :, 0:32]),
                       r=[VA], w=["va_s"], dma=True)
        else:
            op("sp", lambda h: h.dma_start(out=KTF.ap, in_=self.kt_s), r=["kt_s"], w=[KT], dma=True)
            for kt in range(16):
                op("sp", lambda h, kt=kt: h.dma_start(out=VAF.ap[:, kt, :, 0:32], in_=self.va_s[:, kt * 512:(kt + 1) * 512].rearrange("p (b c) -> p b c", c=32)),
                   r=["va_s"], w=[VA], dma=True)
            op("dve", lambda h: h.memset(VA.ap[:, :, :, 64:65], 1.0), r=[], w=[VA])
            kmeans()
        wq = self.attn_w_q[ja]
        stop = getattr(self, "attn_stop", 0)
        if stop == 1:
            return
        for p in range(8 if stop == 0 else 1):
            half, pp = divmod(p, 4)
            if pp == 0:
                op("pool", lambda h, half=half: h.dma_start(out=W1.ap, in_=wsrc(wq, half)), r=[], w=[W1], dma=True)
            op("pool", lambda h, p=p: h.dma_start(out=WOp.ap, in_=self.attn_w_o[ja, p * 128:(p + 1) * 128, :]), r=[], w=[WOp], dma=True)
            make_rot_weights(pp)
            for r in range(4):
                pq = self.psb(4 + r % 2)
                for k in range(8):
                    op("pe", lambda h, k=k, r=r, pp=pp, pq=pq: h.matmul(pq.ap, lhsT=W1.ap[:, k, pp * 128:(pp + 1) * 128],
                                                                     rhs=self.HT.ap[:, k, r * 512:(r + 1) * 512], start=(k == 0), stop=(k == 7)),
                       r=[W1, self.HT], w=[pq])
                rope(pq, QTp.ap[:, r * 512:(r + 1) * 512], QTp, r)
            for hl in range(2):
                rows = slice(64 * hl, 64 * hl + 64)
                for i in range(8, NT):
                    cblk = i // 2
                    pg = self.psb(6, cols=8)
                    op("pe", lambda h, i=i, rows=rows, p=p, pg=pg: h.matmul(pg.ap, lhsT=QTp.ap[rows, i * 128:(i + 1) * 128], rhs=KMB.ap[rows, p, :],
                                                                       start=True, stop=True), r=[QTp, KMB], w=[pg])
                    op("dve", lambda h, cblk=cblk, pg=pg: h.tensor_tensor(out=GSC.ap[:, 0:8], in0=pg.ap, in1=PADM.ap[:, cblk, :], op=ALU.add),
                       r=[pg, PADM], w=[GSC])
                    op("dve", lambda h: h.max(out=GSC.ap[:, 8:16], in_=GSC.ap[:, 0:8]), r=[GSC], w=[GSC])
                    op("dve", lambda h, i=i, hl=hl: h.tensor_scalar(out=WSEL.ap[:, i, hl, :], in0=GSC.ap[:, 0:8], scalar1=GSC.ap[:, 10:11], scalar2=None,
                                                                   op0=ALU.is_ge), r=[GSC], w=[WSEL])
            nst = 0
            if stop == 2:
                return
            for r in range(4):
                for hl in range(2):
                    rows = slice(64 * hl, 64 * hl + 64)
                    hd = 2 * p + hl
                    for n in range(2 * r + 2):
                        jqs = [jq for jq in range(4) if (4 * r + jq) // 2 >= n]
                        c0 = jqs[0] * 128
                        ncol = 512 - c0
                        pob = self.psb(2 + n % 2)
                        pts = {}
                        for kt in (2 * n, 2 * n + 1):
                            use = [jq for jq in jqs if kt <= 4 * r + jq]
                            if not use:
                                continue
                            pss = self.psb(nst % 2)
                            pt = PT[nst % 2]
                            nst += 1
                            pts[kt] = pt
                            op("pe", lambda h, kt=kt, rows=rows, r=r, c0=c0, ncol=ncol, pss=pss, p=p: h.matmul(
                                pss.ap[:, c0:c0 + ncol], lhsT=KT.ap[rows, p, kt * 128:(kt + 1) * 128], rhs=QTp.ap[rows, r * 512 + c0:(r + 1) * 512],
                                start=True, stop=True), r=[KT, QTp], w=[pss])
                            op("act", lambda h, c0=c0, ncol=ncol, pss=pss, pt=pt: h.activation(out=pt.ap[:, c0:c0 + ncol], in_=pss.ap[:, c0:c0 + ncol],
                                                                                           func=AF.Exp, scale=0.125), r=[pss], w=[pt])
                            for jq in use:
                                if kt == 4 * r + jq:
                                    op("dve", lambda h, jq=jq, pt=pt: h.tensor_tensor(out=pt.ap[:, jq * 128:(jq + 1) * 128], in0=pt.ap[:, jq * 128:(jq + 1) * 128],
                                                                                 in1=TRI.ap, op=ALU.mult), r=[pt, TRI], w=[pt])
                        for jq in jqs:
                            kts = [kt for kt in (2 * n, 2 * n + 1) if kt <= 4 * r + jq]
                            for kt in kts:
                                pt = pts[kt]
                                op("pe", lambda h, jq=jq, kt=kt, hd=hd, pt=pt, pob=pob, st=(kt == kts[0]), last=(kt == kts[-1]): h.matmul(
                                    pob.ap[:, jq * 65:(jq + 1) * 65], lhsT=pt.ap[:, jq * 128:(jq + 1) * 128], rhs=VA.ap[:, kt, hd, 0:65], start=st, stop=last),
                                    r=[pt, VA], w=[pob])
                        for jq in jqs:
                            i = 4 * r + jq
                            cblk = i // 2
                            if n == cblk or i < 8:
                                wgt = 1.0
                                rd = [pob]
                            else:
                                wgt = WSEL.ap[:, i, hl, n:n + 1]
                                rd = [pob, WSEL]
                            if n == 0:
                                op("dve", lambda h, jq=jq, wgt=wgt, pob=pob: h.tensor_scalar(out=ACC.ap[:, jq, :], in0=pob.ap[:, jq * 65:(jq + 1) * 65], scalar1=wgt,
                                                                                        scalar2=None, op0=ALU.mult), r=rd, w=[ACC])
                            else:
                                op("dve", lambda h, jq=jq, wgt=wgt, pob=pob: h.scalar_tensor_tensor(out=ACC.ap[:, jq, :], in0=pob.ap[:, jq * 65:(jq + 1) * 65], scalar=wgt,
                                                                                               in1=ACC.ap[:, jq, :], op0=ALU.mult, op1=ALU.add), r=rd + [ACC], w=[ACC])
                    for jq in range(4):
                        op("dve", lambda h, jq=jq: h.reciprocal(out=GSC.ap[:, 32 + jq:33 + jq], in_=ACC.ap[:, jq, 64:65]), r=[ACC], w=[GSC])
                        op("dve", lambda h, jq=jq, hl=hl: h.tensor_scalar(out=OTOK.ap[:, jq, 64 * hl:64 * hl + 64], in0=ACC.ap[:, jq, 0:64],
                                                                         scalar1=GSC.ap[:, 32 + jq:33 + jq], scalar2=None, op0=ALU.mult), r=[ACC, GSC], w=[OTOK])
                ptt = V(self.ps[:, 7, :].bitcast(BF16)[:, 0:512], [("ps", 7)])
                for jq in range(4):
                    op("pe", lambda h, jq=jq: h.transpose(out=ptt.ap[:, jq * 128:(jq + 1) * 128], in_=OTOK.ap[:, jq, :], identity=C["ident_b"].ap),
                       r=[OTOK, C["ident_b"]], w=[ptt])
                op("act", lambda h, r=r: h.activation(out=OTp.ap[:, r * 512:(r + 1) * 512], in_=ptt.ap, func=AF.Copy), r=[ptt], w=[OTp])
            for i in range(NT):
                Ri = self.R[i]
                for dr in range(2):
                    pm = self.psb(4 + dr)
                    op("pe", lambda h, i=i, dr=dr, pm=pm: h.matmul(pm.ap, lhsT=OTp.ap[:, i * 128:(i + 1) * 128], rhs=WOp.ap[:, dr * 512:(dr + 1) * 512],
                                                              start=True, stop=True), r=[OTp, WOp], w=[pm])
                    op("dve", lambda h, dr=dr, pm=pm, Ri=Ri: h.tensor_tensor(out=Ri.ap[:, dr * 512:(dr + 1) * 512], in0=Ri.ap[:, dr * 512:(dr + 1) * 512],
                                                                            in1=pm.ap, op=ALU.add), r=[Ri, pm], w=[Ri])

    def moe(self, li):
        A = self.Ab
        C = self.C
        o = 0

        def alloc(n):
            nonlocal o
            a = o
            o += (n + G - 1) // G * G
            return a
        WR = A.view(alloc(8 * NE * 2), [8, NE], BF16)
        BRT = A.view(alloc(NE * 4), [NE], F32)
        RG = A.view(alloc(NT * 64 * 4), [NT, 64], F32)
        RGB = A.view(alloc(NT * 64 * 2), [NT, 64], BF16)
        RGT = A.view(alloc(S * 2), [S], BF16, parts=64)
        MSK = A.view(alloc(NT * NE * 2), [NT, NE], BF16)
        CM = A.view(alloc(NT * NE * 2), [NT, NE], BF16)
        SM = A.view(alloc(1024), [256], F32)
        BGU = A.view(alloc(NE * 16 * 4), [NE, 16], F32)
        BDN = [A.view(alloc(D * 2), [D], BF16, parts=1) for _ in range(2)]
        NRING = 4
        RING = [A.view(alloc(8 * 256 * 2), [8, 256], BF16) for _ in range(NRING)]
        xt_off = alloc(8 * CAP * 2)
        XT = A.view(xt_off, [8, CAP], BF16)
        ACTT = A.view(alloc(8 * CAP * 2), [8, CAP], BF16)
        YB = A.view(xt_off, [NJT, D], BF16)
        TG = [A.view(alloc(CAP * 4), [CAP], F32) for _ in range(2)]
        TS = [A.view(alloc(CAP * 2), [CAP], BF16) for _ in range(2)]
        TU = [A.view(alloc(CAP * 4), [CAP], F32) for _ in range(2)]
        GBS = [A.view(alloc(512 * 2), [512], BF16) for _ in range(2)]
        assert o <= A.nbytes, o
        self.arena_used = o
        Sg = [self.HTb.view(i * 1024, [128], BF16) for i in range(NT)]
        base = NT * 1024
        STw = [self.HTb.view(base + w * 1024, [512], BF16) for w in range(4)]

        wr_src = self.w_router[li].rearrange("(kc kp) e -> kp kc e", kp=128)
        self.op("pool", lambda h: h.dma_start(out=WR.ap, in_=wr_src), r=[], w=[WR], dma=True)
        self.op("sp", lambda h: h.dma_start(out=BRT.ap, in_=self.b_router[li:li + 1, :].partition_broadcast(128)), r=[], w=[BRT], dma=True)
        bgu_src = self.b_gu[li].rearrange("e (c p) -> (e c) p", p=128).rearrange("(t r) p -> r t p", r=128)
        self.op("sp", lambda h: h.dma_start(out=TG[0].ap.rearrange("p (t q) -> p t q", t=4), in_=bgu_src), r=[], w=[TG[0]], dma=True)
        pbg = self.psb(0)
        for t in range(4):
            self.op("pe", lambda h, t=t: h.transpose(out=pbg.ap[:, t * 128:(t + 1) * 128], in_=TG[0].ap[:, t * 128:(t + 1) * 128], identity=C["ident_f"].ap),
                    r=[TG[0], C["ident_f"]], w=[pbg])
        self.op("act", lambda h: h.activation(out=BGU.ap.rearrange("p e c -> p (e c)"), in_=pbg.ap, func=AF.Copy), r=[pbg], w=[BGU])

        pieces = []
        for e in range(NE):
            for p in range(4):
                pieces.append((e, "g", p))
                pieces.append((e, "u", p))
            for q in range(4):
                pieces.append((e, "d", q))
        self._ring_state = {"next": 0}
        piece_slot = {}

        def issue_piece():
            n = self._ring_state["next"]
            if n >= len(pieces):
                return
            e, kind, p = pieces[n]
            slot = RING[n % NRING]
            piece_slot[(e, kind, p)] = slot
            if kind == "d":
                src = self.w_dn[li, e].rearrange("(kc kp) d -> kp kc d", kp=128)[:, :, p * 256:(p + 1) * 256]
            else:
                c0 = p * 256 + (D if kind == "u" else 0)
                src = self.w_gu[li, e].rearrange("(kc kp) f -> kp kc f", kp=128)[:, :, c0:c0 + 256]
            self.op("pool", lambda h, slot=slot, src=src: h.dma_start(out=slot.ap, in_=src), r=[], w=[slot], dma=True)
            self._ring_state["next"] = n + 1

        for _ in range(NRING):
            issue_piece()

        scr = SM.ap
        for i in range(NT):
            pl = self.psb(4 + (i % 2), cols=NE)
            for k in range(8):
                self.op("pe", lambda h, k=k, i=i, pl=pl: h.matmul(pl.ap, lhsT=self.HT.ap[:, k, i * 128:(i + 1) * 128], rhs=WR.ap[:, k, :],
                                                               start=(k == 0), stop=(k == 7)), r=[self.HT, WR], w=[pl])
            lg = V(scr[:, 0:32], SM.keys)
            m8 = V(scr[:, 32:40], SM.keys)
            nm = V(scr[:, 40:41], SM.keys)
            mk = V(scr[:, 48:80], SM.keys)
            ex = V(scr[:, 80:112], SM.keys)
            sm = V(scr[:, 112:113], SM.keys)
            rk = V(scr[:, 128:160], SM.keys)
            self.op("dve", lambda h, pl=pl: h.tensor_tensor(out=lg.ap, in0=pl.ap, in1=BRT.ap, op=ALU.add), r=[pl, BRT], w=[SM])
            self.op("dve", lambda h: h.max(out=m8.ap, in_=lg.ap), r=[SM], w=[SM])
            self.op("dve", lambda h: h.tensor_scalar(out=mk.ap, in0=lg.ap, scalar1=m8.ap[:, 3:4], scalar2=None, op0=ALU.is_ge), r=[SM], w=[SM])
            self.op("dve", lambda h: h.tensor_scalar(out=nm.ap, in0=m8.ap[:, 0:1], scalar1=-1.0, scalar2=None, op0=ALU.mult), r=[SM], w=[SM])
            self.op("act", lambda h: h.activation(out=ex.ap, in_=lg.ap, func=AF.Exp, bias=nm.ap, scale=1.0), r=[SM], w=[SM])
            self.op("dve", lambda h: h.tensor_tensor(out=ex.ap, in0=ex.ap, in1=mk.ap, op=ALU.mult), r=[SM], w=[SM])
            self.op("dve", lambda h: h.tensor_reduce(out=sm.ap, in_=ex.ap, axis=AX.X, op=ALU.add), r=[SM], w=[SM])
            self.op("dve", lambda h: h.reciprocal(out=sm.ap, in_=sm.ap), r=[SM], w=[SM])
            self.op("dve", lambda h, i=i: h.tensor_scalar(out=RG.ap[:, i, 32:64], in0=ex.ap, scalar1=sm.ap, scalar2=None, op0=ALU.mult), r=[SM], w=[RG])
            self.op("dve", lambda h, i=i: h.tensor_copy(out=MSK.ap[:, i, :], in_=mk.ap), r=[SM], w=[MSK])
            if i % 4 == 0:
                self.op("dve", lambda h, i=i: h.memset(CM.ap[:, i, :], 0.0), r=[], w=[CM])
            else:
                self.op("dve", lambda h, i=i: h.tensor_tensor(out=CM.ap[:, i, :], in0=CM.ap[:, i - 1, :], in1=MSK.ap[:, i - 1, :], op=ALU.add),
                        r=[CM, MSK], w=[CM])
            pr = self.psb(4 + (i % 2), cols=NE, c0=64)
            self.op("pe", lambda h, i=i, pr=pr: h.matmul(pr.ap, lhsT=C["ones"].ap, rhs=CM.ap[:, i, :], start=True, stop=False), r=[C["ones"], CM], w=[pr])
            self.op("pe", lambda h, i=i, pr=pr: h.matmul(pr.ap, lhsT=C["triu"].ap, rhs=MSK.ap[:, i, :], start=False, stop=True), r=[C["triu"], MSK], w=[pr])
            self.op("dve", lambda h, pr=pr: h.scalar_tensor_tensor(out=rk.ap, in0=pr.ap, scalar=1.0, in1=mk.ap, op0=ALU.add, op1=ALU.mult), r=[pr, SM], w=[SM])
            self.op("dve", lambda h, i=i: h.tensor_scalar(out=RG.ap[:, i, 0:32], in0=rk.ap, scalar1=-1.0, scalar2=None, op0=ALU.add), r=[SM], w=[RG])
            self.op("dve", lambda h, i=i: h.tensor_copy(out=RGB.ap[:, i, :], in_=RG.ap[:, i, :]), r=[RG], w=[RGB])
            ptr = V(self.ps[0:64, 4 + (i % 2), :].bitcast(BF16)[:, 256:384], [("ps", 4 + (i % 2))])
            self.op("pe", lambda h, i=i, ptr=ptr: h.transpose(out=ptr.ap, in_=RGB.ap[:, i, :], identity=C["ident_b"].ap), r=[RGB, C["ident_b"]], w=[ptr])
            self.op("act", lambda h, i=i, ptr=ptr: h.activation(out=RGT.ap[:, i * 128:(i + 1) * 128], in_=ptr.ap, func=AF.Copy), r=[ptr], w=[RGT])

        for i in range(NT):
            Ri = self.R[i]
            self.op("act", lambda h, Ri=Ri: h.activation(out=Ri.ap, in_=Ri.ap, func=AF.Copy, scale=ALPHA), r=[Ri], w=[Ri])

        identf = C["ident_f"].ap
        identb = C["ident_b"].ap
        for e in range(NE):
            bdn = BDN[e % 2]
            self.op("pool", lambda h, e=e, bdn=bdn: h.dma_start(out=bdn.ap, in_=self.b_dn[li, e:e + 1, :]), r=[], w=[bdn], dma=True)
            for i in range(NT):
                eng = "dve"
                self.op(eng, lambda h, i=i, e=e: h.tensor_scalar(out=Sg[i].ap, in0=C["iota_row"].ap[:, 0:128], scalar1=RG.ap[:, i, e:e + 1], scalar2=None,
                                                                op0=ALU.is_equal), r=[C["iota_row"], RG], w=[Sg[i]])
            for m in range(8):
                px = self.psb(m % 2)
                for i in range(NT):
                    w4, ii = divmod(i, 4)
                    self.op("pe", lambda h, m=m, i=i, px=px, w4=w4, ii=ii: h.matmul(px.ap[:, w4 * 128:(w4 + 1) * 128], lhsT=self.HB[i].ap[:, m * 128:(m + 1) * 128],
                                                                               rhs=Sg[i].ap, start=(ii == 0), stop=(ii == 3)), r=[self.HB[i], Sg[i]], w=[px])
                self.op("act", lambda h, m=m, px=px: h.activation(out=XT.ap[:, m, :], in_=px.ap, func=AF.Copy), r=[px], w=[XT])
            for r in range(4):
                prb = self.psb(2)
                pgb = self.psb(3)
                self.op("pe", lambda h, r=r, e=e, prb=prb: h.matmul(prb.ap, lhsT=identb[0:64, e:e + 1].broadcast_to([64, 128]),
                                                                    rhs=RGT.ap[:, r * 512:(r + 1) * 512], start=True, stop=True),
                        r=[C["ident_b"], RGT], w=[prb])
                self.op("pe", lambda h, r=r, e=e, pgb=pgb: h.matmul(pgb.ap, lhsT=identb[0:64, 32 + e:33 + e].broadcast_to([64, 128]),
                                                                    rhs=RGT.ap[:, r * 512:(r + 1) * 512], start=True, stop=True),
                        r=[C["ident_b"], RGT], w=[pgb])
                gbs = GBS[r % 2]
                self.op("act", lambda h, pgb=pgb, gbs=gbs: h.activation(out=gbs.ap, in_=pgb.ap, func=AF.Copy), r=[pgb], w=[gbs])
                self.op("dve", lambda h, r=r, prb=prb, gbs=gbs: h.scalar_tensor_tensor(
                    out=STw[r].ap, in0=prb.ap, scalar=C["piota"].ap[:, 0:1], in1=gbs.ap, op0=ALU.is_equal, op1=ALU.mult),
                    r=[prb, gbs, C["piota"]], w=[STw[r]])
            for p in range(4):
                wg = piece_slot[(e, "g", p)]
                wu = piece_slot[(e, "u", p)]
                for sub in range(2):
                    c = 2 * p + sub
                    pg = self.psb(4 + (c % 2))
                    pu = self.psb(6 + (c % 2))
                    for k in range(8):
                        self.op("pe", lambda h, k=k, sub=sub, wg=wg, pg=pg: h.matmul(pg.ap, lhsT=wg.ap[:, k, sub * 128:(sub + 1) * 128], rhs=XT.ap[:, k, :],
                                                                                 start=(k == 0), stop=(k == 7)), r=[wg, XT], w=[pg])
                    for k in range(8):
                        self.op("pe", lambda h, k=k, sub=sub, wu=wu, pu=pu: h.matmul(pu.ap, lhsT=wu.ap[:, k, sub * 128:(sub + 1) * 128], rhs=XT.ap[:, k, :],
                                                                                 start=(k == 0), stop=(k == 7)), r=[wu, XT], w=[pu])
                    tg, ts, tu = TG[c % 2], TS[c % 2], TU[c % 2]
                    self.op("dve", lambda h, pg=pg, tg=tg, e=e, c=c: h.tensor_scalar(out=tg.ap, in0=pg.ap, scalar1=BGU.ap[:, e, c:c + 1], scalar2=7.0,
                                                                                 op0=ALU.add, op1=ALU.min), r=[pg, BGU], w=[tg])
                    self.op("act", lambda h, tg=tg, ts=ts: h.activation(out=ts.ap, in_=tg.ap, func=AF.Sigmoid, scale=1.702), r=[tg], w=[ts])
                    self.op("act", lambda h, pu=pu, tu=tu, e=e, c=c: h.activation(out=tu.ap, in_=pu.ap, func=AF.Identity, bias=BGU.ap[:, e, 8 + c:9 + c], scale=1.0),
                            r=[pu, BGU], w=[tu])
                    self.op("dve", lambda h, tu=tu: h.tensor_scalar(out=tu.ap, in0=tu.ap, scalar1=-7.0, scalar2=7.0, op0=ALU.max, op1=ALU.min), r=[tu], w=[tu])
                    self.op("dve", lambda h, tg=tg, ts=ts: h.tensor_tensor(out=tg.ap, in0=tg.ap, in1=ts.ap, op=ALU.mult), r=[tg, ts], w=[tg])
                    self.op("dve", lambda h, tg=tg, tu=tu, c=c: h.scalar_tensor_tensor(out=ACTT.ap[:, c, :], in0=tu.ap, scalar=1.0, in1=tg.ap, op0=ALU.add, op1=ALU.mult),
                            r=[tg, tu], w=[ACTT])
                issue_piece()
                issue_piece()
            for q in range(4):
                wd = piece_slot[(e, "d", q)]
                for jt in range(NJT):
                    py = self.psb(jt % 2, cols=256, c0=0)
                    for c in range(8):
                        self.op("pe", lambda h, c=c, jt=jt, wd=wd, py=py: h.matmul(py.ap, lhsT=ACTT.ap[:, c, jt * 128:(jt + 1) * 128], rhs=wd.ap[:, c, :],
                                                                               start=(c == 0), stop=False), r=[ACTT, wd], w=[py])
                    self.op("pe", lambda h, q=q, py=py, bdn=bdn: h.matmul(py.ap, lhsT=C["ones"].ap[0:1, :], rhs=bdn.ap[0:1, q * 256:(q + 1) * 256],
                                                                          start=False, stop=True), r=[C["ones"], bdn], w=[py])
                    self.op("act", lambda h, jt=jt, q=q, py=py: h.activation(out=YB.ap[:, jt, q * 256:(q + 1) * 256], in_=py.ap, func=AF.Copy), r=[py], w=[YB])
                issue_piece()
            for i in range(NT):
                for dr in range(2):
                    po = self.psb(2 + ((2 * i + dr) % 2))
                    w4, ii = divmod(i, 4)
                    self.op("pe", lambda h, w4=w4, ii=ii, dr=dr, po=po: h.matmul(po.ap, lhsT=STw[w4].ap[:, ii * 128:(ii + 1) * 128],
                                                                             rhs=YB.ap[:, w4, dr * 512:(dr + 1) * 512], start=True, stop=True),
                            r=[STw[w4], YB], w=[po])
                    Ri = self.R[i]
                    self.op("dve", lambda h, dr=dr, Ri=Ri, po=po: h.tensor_tensor(out=Ri.ap[:, dr * 512:(dr + 1) * 512], in0=Ri.ap[:, dr * 512:(dr + 1) * 512],
                                                                              in1=po.ap, op=ALU.add), r=[Ri, po], w=[Ri])


_CACHE = {}
_NPH = [8]

WEIGHT_KEYS = ["kv_w_k", "kv_w_v", "attn_w_q", "attn_w_o", "ssm_w_in", "ssm_conv_w", "ssm_conv_b", "ssm_dt_bias", "ssm_a_log", "ssm_d", "ssm_norm_g", "ssm_w_out",
               "moe_w_router", "moe_b_router", "moe_w_gate_up", "moe_b_gate_up", "moe_w_down", "moe_b_down",
               "ln_mix_g", "ln_mix_b", "ln_ffn_g", "ln_ffn_b"]


def kernel(**inputs):
    x = np.ascontiguousarray(np.asarray(inputs["x"], dtype=np.float32))
    nb = x.shape[0]
    if "nc" not in _CACHE:
        phases = [("prep", 0), ("mamba", 0), ("moe", 0), ("mamba", 1), ("moe", 1), ("attn", 2), ("moe", 2), ("attn", 3), ("moe", 3)][:_NPH[0] + 1]
        mk = MK(layers=[0, 1, 2, 3], phases=phases)
        _CACHE["nc"] = mk.build()
        _CACHE["mk"] = mk
    nc = _CACHE["nc"]
    shared = {k: np.ascontiguousarray(np.asarray(inputs[k], dtype=np.float32)) for k in WEIGHT_KEYS}
    for k, v in make_consts().items():
        shared["c_" + k] = v
    in_maps = []
    for b in range(nb):
        m = dict(shared)
        m["x"] = x[b]
        in_maps.append(m)
    res = run_bass_kernel_spmd(nc, in_maps, core_ids=list(range(nb)))
    return np.stack([np.asarray(r["y"], dtype=np.float32) for r in res.results], axis=0)
```

```python
import math
from contextlib import ExitStack

import numpy as np
import ml_dtypes

import concourse.bass as bass
import concourse.mybir as mybir
from concourse.bass_utils import run_bass_kernel_spmd

F32 = mybir.dt.float32
BF16 = mybir.dt.bfloat16
AF = mybir.ActivationFunctionType
ALU = mybir.AluOpType
AX = mybir.AxisListType

D = 1024
S = 2048
NT = 16
DEPTH = 4
NE = 32
CAP = 512
NJT = CAP // 128
ALPHA = (2 * DEPTH) ** 0.25
LN_EPS = 1e-5
G = 1024


class Op:
    __slots__ = ("eng", "fn", "deps", "dma", "signal", "sig", "idx")

    def __init__(self, eng, fn, deps, dma, idx):
        self.eng = eng
        self.fn = fn
        self.deps = deps
        self.dma = dma
        self.signal = False
        self.sig = None
        self.idx = idx


class Prog:
    NDMA = 24

    def __init__(self, nc):
        self.nc = nc
        self.ops = []
        self.last_writer = {}
        self.readers = {}

    def add(self, eng, fn, reads=(), writes=(), dma=False):
        idx = len(self.ops)
        ops = self.ops
        deps = set()
        for k in reads:
            w = self.last_writer.get(k)
            if w is not None:
                deps.add(w)
        for k in writes:
            w = self.last_writer.get(k)
            if w is not None:
                o = ops[w]
                if o.dma or dma or o.eng != eng:
                    deps.add(w)
            for r in self.readers.get(k, ()):
                o = ops[r]
                if o.dma or dma or o.eng != eng:
                    deps.add(r)
        if eng == "pe" and not dma:
            deps = {d for d in deps if ops[d].dma or ops[d].eng != "pe"}
        latest = {}
        pruned = set()
        for d in deps:
            o = ops[d]
            if o.dma:
                pruned.add(d)
            elif latest.get(o.eng, -1) < d:
                latest[o.eng] = d
        pruned.update(latest.values())
        deps = pruned
        op = Op(eng, fn, deps, dma, idx)
        ops.append(op)
        for k in reads:
            self.readers.setdefault(k, []).append(idx)
        for k in writes:
            self.last_writer[k] = idx
            self.readers[k] = []
        return op

    def emit(self, stack):
        nc = self.nc
        ops = self.ops
        for op in ops:
            for d in op.deps:
                ops[d].signal = True
        engs = ["pe", "act", "dve", "pool", "sp"]
        esem = {e: stack.enter_context(nc.semaphore("s_" + e)) for e in engs}
        dsem = {e: [stack.enter_context(nc.semaphore("d_%s_%d" % (e, i))) for i in range(self.NDMA)]
                for e in ("sp", "act", "pool")}
        cnt = {e: 0 for e in engs}
        dcnt = {e: 0 for e in dsem}
        prewait = {}
        for op in ops:
            if op.dma:
                op.signal = True
                i = dcnt[op.eng]
                dcnt[op.eng] += 1
                sem = dsem[op.eng][i % self.NDMA]
                op.sig = (sem, 16 * (i // self.NDMA + 1))
                if i >= self.NDMA:
                    prewait[op.idx] = (sem, 16 * (i // self.NDMA))
            elif op.signal:
                cnt[op.eng] += 1
                op.sig = (esem[op.eng], cnt[op.eng])
        per = {e: [op for op in ops if op.eng == e] for e in engs}
        block = stack.enter_context(nc.Block())
        self.nwaits = 0

        def body(e):
            def run(h):
                waited = {}
                for op in per[e]:
                    need = {}
                    if op.idx in prewait:
                        s, v = prewait[op.idx]
                        need[id(s)] = (s, v)
                    for d in op.deps:
                        s, v = ops[d].sig
                        cur = need.get(id(s))
                        if cur is None or cur[1] < v:
                            need[id(s)] = (s, v)
                    for sid, (s, v) in need.items():
                        if waited.get(sid, 0) < v:
                            h.wait_ge(s, v)
                            waited[sid] = v
                            self.nwaits += 1
                    ins = op.fn(h)
                    if op.signal and ins is not None:
                        s, v = op.sig
                        ins.then_inc(s, 16 if op.dma else 1)
            return run

        block.tensor(body("pe"))
        block.scalar(body("act"))
        block.vector(body("dve"))
        block.gpsimd(body("pool"))
        block.sync(body("sp"))
        self.counts = dict(cnt)


class V:
    __slots__ = ("ap", "keys")

    def __init__(self, ap, keys):
        self.ap = ap
        self.keys = keys


def _prod(s):
    n = 1
    for x in s:
        n *= x
    return n


_RE = {2: "p (a b) -> p a b", 3: "p (a b c) -> p a b c"}


class Buf:
    def __init__(self, M, name, nbytes):
        self.name = name
        self.nbytes = nbytes
        self.t = M.st.enter_context(M.nc.sbuf_tensor(name, [128, nbytes // 2], BF16))

    def view(self, off, shape, dt=BF16, p0=0, parts=128):
        esz = 4 if dt == F32 else 2
        n = _prod(shape)
        assert off % 4 == 0 and off + n * esz <= self.nbytes, (self.name, off, shape)
        a = self.t[p0:p0 + parts, off // 2:(off + n * esz) // 2]
        if dt == F32:
            a = a.bitcast(F32)
        if len(shape) == 2:
            a = a.rearrange("p (a b) -> p a b", a=shape[0])
        elif len(shape) == 3:
            a = a.rearrange("p (a b c) -> p a b c", a=shape[0], b=shape[1])
        keys = [(self.name, g) for g in range(off // G, (off + n * esz - 1) // G + 1)]
        return V(a, keys)


def keys_of(vs):
    out = []
    for v in vs:
        if isinstance(v, V):
            out.extend(v.keys)
        elif isinstance(v, (list, tuple)) and v and isinstance(v[0], (V,)):
            out.extend(keys_of(v))
        else:
            out.append(v)
    return out


def make_consts():
    c = {}
    c["ident_f"] = np.eye(128, dtype=np.float32)
    c["iota_row"] = np.broadcast_to(np.arange(CAP, dtype=np.float32), (128, CAP)).copy()
    c["piota"] = (np.arange(128, dtype=np.float32)[:, None] + 128.0 * np.arange(NJT, dtype=np.float32)[None, :]).copy()
    c["triu"] = np.triu(np.ones((128, 128), np.float32), k=1)
    c["ones"] = np.ones((128, 128), np.float32)
    m = np.where(np.arange(128)[None, :] >= np.arange(128)[:, None], 0.0, -30000.0).astype(np.float32)
    c["maskneg"] = np.tile(m, (1, 4))
    inv_freq = (500000.0 ** (-np.arange(0, 16, 2, dtype=np.float32) / 16.0)).astype(np.float32)
    ang = np.arange(S, dtype=np.float32)[:, None] * inv_freq[None, :]
    cos, sin = np.cos(ang).astype(np.float32), np.sin(ang).astype(np.float32)
    cosf = np.ones((128, S), np.float32)
    sinf = np.zeros((128, S), np.float32)
    rotm = np.zeros((128, 128), np.float32)
    for hl in range(2):
        for dd in range(16):
            cosf[64 * hl + dd] = cos[:, dd % 8]
            sinf[64 * hl + dd] = sin[:, dd % 8]
        for dd in range(8):
            rotm[64 * hl + dd + 8, 64 * hl + dd] = -1.0
            rotm[64 * hl + dd, 64 * hl + dd + 8] = 1.0
    c["cosf"], c["sinf"], c["rotm"] = cosf, sinf, rotm
    c["tri"] = np.triu(np.ones((128, 128), np.float32), k=0)
    padm = np.zeros((8, 8), np.float32)
    for cb in range(8):
        padm[cb, cb:] = -1e30
    c["padm"] = np.broadcast_to(padm.reshape(1, 64), (128, 64)).copy()
    return c


CONST_SHAPES = {"ident_f": [128, 128], "iota_row": [128, CAP], "piota": [128, NJT],
                "triu": [128, 128], "ones": [128, 128], "maskneg": [128, 512], "cosf": [128, S], "sinf": [128, S],
                "rotm": [128, 128], "tri": [128, 128], "padm": [128, 64]}


class MK:
    def __init__(self, layers, phases, debug_in=None):
        self.layers = layers
        self.phases = phases
        self.debug = debug_in
        self.nc = bass.Bass("TRN2", target_bir_lowering=False)
        self.st = ExitStack()

    def op(self, eng, fn, r=(), w=(), dma=False):
        return self.P.add(eng, fn, keys_of(r), keys_of(w), dma)

    def dram_in(self, name, shape):
        return self.nc.dram_tensor(name, list(shape), F32, kind="ExternalInput").ap()

    def psb(self, b, cols=512, parts=128, c0=0):
        return V(self.ps[0:parts, b, c0:c0 + cols], [("ps", b)])

    def build(self):
        nc, st = self.nc, self.st
        nl = len(self.layers)
        self.x = self.dram_in("x", [S, D])
        self.y = nc.dram_tensor("y", [S, D], F32, kind="ExternalOutput").ap()
        self.w_router = self.dram_in("moe_w_router", [nl, D, NE])
        self.b_router = self.dram_in("moe_b_router", [nl, NE])
        self.w_gu = self.dram_in("moe_w_gate_up", [nl, NE, D, 2 * D])
        self.b_gu = self.dram_in("moe_b_gate_up", [nl, NE, 2 * D])
        self.w_dn = self.dram_in("moe_w_down", [nl, NE, D, D])
        self.b_dn = self.dram_in("moe_b_down", [nl, NE, D])
        self.ln_g = {"mix": self.dram_in("ln_mix_g", [nl, D]), "moe": self.dram_in("ln_ffn_g", [nl, D])}
        self.ln_b = {"mix": self.dram_in("ln_mix_b", [nl, D]), "moe": self.dram_in("ln_ffn_b", [nl, D])}
        self.cst = {k: self.dram_in("c_" + k, s) for k, s in CONST_SHAPES.items()}
        na = max(1, sum(1 for l in self.layers if l < 2))
        self.ssm_w_in = self.dram_in("ssm_w_in", [na, D, 5152])
        self.ssm_conv_w = self.dram_in("ssm_conv_w", [na, 4, 3072])
        self.ssm_conv_b = self.dram_in("ssm_conv_b", [na, 3072])
        self.ssm_dt_bias = self.dram_in("ssm_dt_bias", [na, 32])
        self.ssm_a_log = self.dram_in("ssm_a_log", [na, 32])
        self.ssm_d = self.dram_in("ssm_d", [na, 32])
        self.ssm_norm_g = self.dram_in("ssm_norm_g", [na, 2048])
        self.ssm_w_out = self.dram_in("ssm_w_out", [na, 2048, D])
        self.attn_layers = [l for l in self.layers if l >= 2]
        nbl = max(1, len(self.attn_layers))
        self.kv_w_k = self.dram_in("kv_w_k", [D, D])
        self.kv_w_v = self.dram_in("kv_w_v", [D, D])
        self.attn_w_q = self.dram_in("attn_w_q", [nbl, D, D])
        self.attn_w_o = self.dram_in("attn_w_o", [nbl, D, D])
        self.kt_s = self.y[0:1024, :].rearrange("(p a) c -> p (a c)", p=128)
        self.va_s = self.y[1024:2048, :].rearrange("(p a) c -> p (a c)", p=128)

        self.dbg = nc.dram_tensor("dbg", [128, 2048], F32, kind="ExternalOutput").ap() if self.debug else None
        self.P = Prog(nc)
        self.ps = st.enter_context(nc.psum_tensor("ps", [128, 8, 512], F32))
        self.Rb = Buf(self, "R", NT * D * 4)
        self.HBb = Buf(self, "HB", NT * D * 2)
        self.HTb = Buf(self, "HT", 8 * S * 2)
        self.Cb = Buf(self, "CST", 6 * 1024)
        self.Ab = Buf(self, "ARENA", 72 * 1024)
        self.R = [self.Rb.view(i * D * 4, [D], F32) for i in range(NT)]
        self.HB = [self.HBb.view(i * D * 2, [D], BF16) for i in range(NT)]
        self.HT = self.HTb.view(0, [8, S], BF16)

        self.load_consts()
        for i in range(NT):
            self.op("sp", lambda h, i=i: h.dma_start(out=self.R[i].ap, in_=self.x[i * 128:(i + 1) * 128, :]),
                    r=["x"], w=[self.R[i]], dma=True)
        first = True
        for kind, l in self.phases:
            li = self.layers.index(l)
            if kind == "prep":
                for i in range(NT):
                    self.make_copies(i)
            elif kind == "nomix":
                for i in range(NT):
                    Ri = self.R[i]
                    self.op("act", lambda h, Ri=Ri: h.activation(out=Ri.ap, in_=Ri.ap, func=AF.Copy, scale=ALPHA), r=[Ri], w=[Ri])
                self.layer_norm("mix", li)
            elif kind == "attn":
                self.attention(l, li)
                self.layer_norm("mix", li)
            elif kind == "mamba":
                self.mamba(li)
                self.layer_norm("mix", li)
            elif kind == "moe":
                self.moe(li)
                self.layer_norm("moe", li)
        for i in range(NT):
            self.op("sp", lambda h, i=i: h.dma_start(out=self.y[i * 128:(i + 1) * 128, :], in_=self.R[i].ap),
                    r=[self.R[i]], w=["y%d" % i, "kt_s", "va_s"], dma=True)
        self.P.add("sp", lambda h: None, reads=["y%d" % i for i in range(NT)])
        self.P.emit(st)
        return nc

    def load_consts(self):
        cb = self.Cb
        off = 0
        self.C = {}

        def alloc(n_bytes):
            nonlocal off
            o = off
            off += (n_bytes + 3) // 4 * 4
            return o
        for k in ("maskneg",):
            v = cb.view(alloc(512 * 4), [512], F32)
            self.C[k] = v
            self.op("sp", lambda h, v=v, k=k: h.dma_start(out=v.ap, in_=self.cst[k]), r=[], w=[v], dma=True)
        v = cb.view(alloc(128 * 4), [128], F32)
        self.C["ones_f"] = v
        self.op("sp", lambda h, v=v: h.dma_start(out=v.ap, in_=self.cst["ones"]), r=[], w=[v], dma=True)
        for k in ("ident_f", "piota"):
            shp = CONST_SHAPES[k]
            v = cb.view(alloc(shp[1] * 4), [shp[1]], F32)
            self.C[k] = v
            self.op("sp", lambda h, v=v, k=k: h.dma_start(out=v.ap, in_=self.cst[k]), r=[], w=[v], dma=True)
        v = cb.view(alloc(CAP * 4), [CAP], F32)
        self.C["iota_row"] = v
        self.op("sp", lambda h, v=v: h.dma_start(out=v.ap, in_=self.cst["iota_row"]), r=[], w=[v], dma=True)
        for k in ("triu", "ones"):
            v = cb.view(alloc(128 * 2), [128], BF16)
            self.C[k] = v
            self.op("pool", lambda h, v=v, k=k: h.dma_start(out=v.ap, in_=self.cst[k]), r=[], w=[v], dma=True)
        v = cb.view(alloc(128 * 2), [128], BF16)
        self.C["ident_b"] = v
        self.op("pool", lambda h, v=v: h.dma_start(out=v.ap, in_=self.cst["ident_f"]), r=[], w=[v], dma=True)
        self.cst_off = off

    def layer_norm(self, which, li, scale_in_place=True):
        A = self.Ab
        gt = A.view(0, [D], F32)
        bt = A.view(4096, [D], F32)
        st6 = A.view(8192, [2, 6], F32)
        mv = A.view(8192 + 64, [2], F32)
        rstd = A.view(8192 + 128, [1], F32)
        nmr = A.view(8192 + 192, [1], F32)
        tmp = A.view(9216, [D], F32)
        self.op("sp", lambda h: h.dma_start(out=gt.ap, in_=self.ln_g[which][li:li + 1, :].partition_broadcast(128)),
                r=[], w=[gt], dma=True)
        self.op("sp", lambda h: h.dma_start(out=bt.ap, in_=self.ln_b[which][li:li + 1, :].partition_broadcast(128)),
                r=[], w=[bt], dma=True)
        for i in range(NT):
            Ri = self.R[i]
            for c in range(2):
                self.op("dve", lambda h, c=c, Ri=Ri: h.bn_stats(out=st6.ap[:, c, :], in_=Ri.ap[:, c * 512:(c + 1) * 512]),
                        r=[Ri], w=[st6])
            self.op("dve", lambda h: h.bn_aggr(out=mv.ap, in_=st6.ap.rearrange("p a b -> p (a b)")), r=[st6], w=[mv])
            self.op("act", lambda h: h.activation(out=rstd.ap, in_=mv.ap[:, 1:2], func=AF.Sqrt, bias=LN_EPS, scale=1.0),
                    r=[mv], w=[rstd])
            self.op("dve", lambda h: h.reciprocal(out=rstd.ap, in_=rstd.ap), r=[rstd], w=[rstd])
            self.op("dve", lambda h: h.scalar_tensor_tensor(out=nmr.ap, in0=mv.ap[:, 0:1], scalar=-1.0, in1=rstd.ap, op0=ALU.mult, op1=ALU.mult),
                    r=[mv, rstd], w=[nmr])
            self.op("act", lambda h, Ri=Ri: h.activation(out=tmp.ap, in_=Ri.ap, func=AF.Identity, bias=nmr.ap, scale=rstd.ap), r=[Ri, nmr, rstd], w=[tmp])
            self.op("dve", lambda h: h.tensor_tensor(out=tmp.ap, in0=tmp.ap, in1=gt.ap, op=ALU.mult), r=[tmp, gt], w=[tmp])
            self.op("dve", lambda h, Ri=Ri: h.tensor_tensor(out=Ri.ap, in0=tmp.ap, in1=bt.ap, op=ALU.add), r=[tmp, bt], w=[Ri])
            self.make_copies(i)

    def make_copies(self, i):
            Ri = self.R[i]
            HBi = self.HB[i]
            self.op("act", lambda h, Ri=Ri, HBi=HBi: h.activation(out=HBi.ap, in_=Ri.ap, func=AF.Copy), r=[Ri], w=[HBi])
            pb = 6 + (i % 2)
            pt = V(self.ps[:, pb, :].bitcast(BF16)[:, 0:1024].rearrange("p (a b) -> p a b", a=8), [("ps", pb)])
            for m in range(8):
                self.op("pe", lambda h, m=m, HBi=HBi, pt=pt: h.transpose(out=pt.ap[:, m, :], in_=HBi.ap[:, m * 128:(m + 1) * 128],
                                                                          identity=self.C["ident_b"].ap),
                        r=[HBi, self.C["ident_b"]], w=[pt])
            htv = V(self.HT.ap[:, :, i * 128:(i + 1) * 128], [("HT", g) for g in range(8 * S * 2 // G)])
            self.op("act", lambda h, pt=pt, htv=htv: h.activation(out=htv.ap, in_=pt.ap, func=AF.Copy), r=[pt], w=[htv])


    def mamba(self, li):
        A, HBb, C = self.Ab, self.HBb, self.C
        identf = C["ident_f"].ap
        op = self.op
        ACSF = HBb.view(0, [S], F32, parts=32)
        NBF = HBb.view(8192, [S], F32, parts=32)
        NBH = [HBb.view(16384 + 2048 * i, [512], F32, parts=32) for i in range(2)]
        ONESF = HBb.view(20480, [128], F32)
        PAR = HBb.view(21504, [8], F32, parts=32)
        ACST = HBb.view(24576, [16, 32], F32)
        W2T = HBb.view(26624, [16, 32], F32)
        EACST = HBb.view(28672, [16, 32], F32)
        DECC = HBb.view(30720, [16, 32], F32)
        o = 0

        def alloc(n):
            nonlocal o
            a = o
            o += (n + G - 1) // G * G
            return a
        CWT = A.view(alloc(6 * 4 * 4), [6, 4], F32)
        CBT = A.view(alloc(6 * 4), [6], F32)
        DBC = A.view(alloc(32 * 4), [32], F32)
        NGg = A.view(alloc(512 * 4), [512], F32)
        xf_off = alloc(4 * S * 2)
        XF = A.view(xf_off, [4, S], BF16)
        T1 = A.view(xf_off, [S], F32, parts=32)
        T2 = A.view(xf_off + S * 4, [S], F32, parts=32)
        bf_off = alloc(S * 2)
        BF = A.view(bf_off, [S], BF16)
        WDT = A.view(bf_off, [8, 32], BF16)
        CF = A.view(alloc(S * 2), [S], BF16)
        STATE = A.view(alloc(512 * 4), [512], F32)
        reg = o
        o = reg
        WX = A.view(alloc(8 * 512 * 2), [8, 512], BF16)
        WB = A.view(alloc(8 * 128 * 2), [8, 128], BF16)
        WC = A.view(alloc(8 * 128 * 2), [8, 128], BF16)
        U = A.view(alloc((S + 4) * 4), [S + 4], F32)
        ACC = [A.view(alloc(512 * 4), [512], F32) for _ in range(2)]
        o_conv = o
        o = reg
        WZ = A.view(alloc(8 * 512 * 2), [8, 512], BF16)
        WO = A.view(alloc(4 * D * 2), [4, D], BF16)
        GT4 = A.view(alloc(512 * 4), [4, 128], F32)
        ARG = [A.view(alloc(512 * 4), [512], F32) for _ in range(2)]
        MT = [A.view(alloc(512 * 2), [4, 128], BF16) for _ in range(8)]
        XTOK = A.view(alloc(512 * 2), [512], BF16)
        BTOK = A.view(alloc(128 * 2), [128], BF16)
        PREV = A.view(alloc(512 * 2), [512], BF16)
        xdt_off = alloc(512 * 2)
        XDT2 = A.view(xdt_off, [512], BF16)
        Y1 = A.view(alloc(512 * 4), [512], F32)
        xd_off = alloc(512 * 4)
        XD = A.view(xd_off, [512], F32)
        ZS = A.view(alloc(512 * 2), [512], BF16)
        JUNK = A.view(xd_off, [512], F32)
        SS = A.view(alloc(16), [4], F32)
        YN = A.view(alloc(512 * 2), [512], BF16)
        YNT = A.view(xdt_off, [4, 128], BF16)
        assert max(o, o_conv) <= A.nbytes, (o, o_conv)
        w_in = self.ssm_w_in[li].rearrange("(kc kp) c -> kp kc c", kp=128)

        for i in range(NT):
            Ri = self.R[i]
            op("act", lambda h, Ri=Ri: h.activation(out=Ri.ap, in_=Ri.ap, func=AF.Copy, scale=ALPHA), r=[Ri], w=[Ri])

        op("pool", lambda h: h.dma_start(out=WDT.ap, in_=w_in[:, :, 5120:5152]), r=[], w=[WDT], dma=True)
        op("sp", lambda h: h.dma_start(out=PAR.ap[:, 0:1], in_=self.ssm_dt_bias[li:li + 1, :].rearrange("o h -> h o")), r=[], w=[PAR], dma=True)
        op("sp", lambda h: h.dma_start(out=PAR.ap[:, 1:2], in_=self.ssm_a_log[li:li + 1, :].rearrange("o h -> h o")), r=[], w=[PAR], dma=True)
        op("sp", lambda h: h.dma_start(out=DBC.ap, in_=self.ssm_d[li:li + 1, :].partition_broadcast(128)), r=[], w=[DBC], dma=True)
        op("dve", lambda h: h.memset(ONESF.ap, 1.0), r=[], w=[ONESF])
        for r in range(4):
            pd = self.psb(r % 2, parts=32)
            for k in range(8):
                op("pe", lambda h, k=k, r=r, pd=pd: h.matmul(pd.ap, lhsT=WDT.ap[:, k, :], rhs=self.HT.ap[:, k, r * 512:(r + 1) * 512],
                                                           start=(k == 0), stop=(k == 7)), r=[WDT, self.HT], w=[pd])
            op("act", lambda h, r=r, pd=pd: h.activation(out=T1.ap[:, r * 512:(r + 1) * 512], in_=pd.ap, func=AF.Exp, bias=PAR.ap[:, 0:1], scale=1.0),
               r=[pd, PAR], w=[T1])
        op("act", lambda h: h.activation(out=T1.ap, in_=T1.ap, func=AF.Ln, bias=1.0, scale=1.0), r=[T1], w=[T1])
        op("act", lambda h: h.activation(out=T2.ap, in_=T1.ap, func=AF.Ln), r=[T1], w=[T2])
        op("act", lambda h: h.activation(out=PAR.ap[:, 2:3], in_=PAR.ap[:, 1:2], func=AF.Exp), r=[PAR], w=[PAR])
        op("dve", lambda h: h.tensor_scalar(out=PAR.ap[:, 2:3], in0=PAR.ap[:, 2:3], scalar1=-1.0, scalar2=None, op0=ALU.mult), r=[PAR], w=[PAR])
        op("dve", lambda h: h.tensor_scalar(out=T1.ap, in0=T1.ap, scalar1=PAR.ap[:, 2:3], scalar2=None, op0=ALU.mult), r=[T1, PAR], w=[T1])
        for c in range(16):
            op("dve", lambda h, c=c: h.tensor_tensor_scan(out=ACSF.ap[:, c * 128:(c + 1) * 128], data0=ONESF.ap[0:32, :], data1=T1.ap[:, c * 128:(c + 1) * 128],
                                                         initial=0.0, op0=ALU.mult, op1=ALU.add), r=[ONESF, T1], w=[ACSF])
        op("dve", lambda h: h.tensor_tensor(out=NBF.ap, in0=T2.ap, in1=ACSF.ap, op=ALU.subtract), r=[T2, ACSF], w=[NBF])
        for c in range(16):
            op("act", lambda h, c=c: h.activation(out=T2.ap[:, c * 128:(c + 1) * 128], in_=NBF.ap[:, c * 128:(c + 1) * 128], func=AF.Exp,
                                                  bias=ACSF.ap[:, c * 128 + 127:c * 128 + 128], scale=1.0), r=[NBF, ACSF], w=[T2])
        pa = self.psb(2)
        pw = self.psb(3)
        for c in range(16):
            op("pe", lambda h, c=c: h.transpose(out=pa.ap[:, c * 32:(c + 1) * 32], in_=ACSF.ap[:, c * 128:(c + 1) * 128], identity=identf[0:32, 0:32]),
               r=[ACSF, C["ident_f"]], w=[pa])
            op("pe", lambda h, c=c: h.transpose(out=pw.ap[:, c * 32:(c + 1) * 32], in_=T2.ap[:, c * 128:(c + 1) * 128], identity=identf[0:32, 0:32]),
               r=[T2, C["ident_f"]], w=[pw])
        op("act", lambda h: h.activation(out=ACST.ap.rearrange("p a b -> p (a b)"), in_=pa.ap, func=AF.Copy), r=[pa], w=[ACST])
        op("act", lambda h: h.activation(out=W2T.ap.rearrange("p a b -> p (a b)"), in_=pw.ap, func=AF.Copy), r=[pw], w=[W2T])
        op("act", lambda h: h.activation(out=EACST.ap, in_=ACST.ap, func=AF.Exp), r=[ACST], w=[EACST])
        op("dve", lambda h: h.tensor_scalar(out=Y1.ap, in0=ACST.ap.rearrange("p a b -> p (a b)"), scalar1=identf[:, 127:128], scalar2=None, op0=ALU.mult),
           r=[ACST, C["ident_f"]], w=[Y1])
        pdc = self.psb(0)
        op("pe", lambda h: h.matmul(pdc.ap, lhsT=C["ones_f"].ap, rhs=Y1.ap, start=True, stop=True), r=[C["ones_f"], Y1], w=[pdc])
        op("act", lambda h: h.activation(out=DECC.ap.rearrange("p a b -> p (a b)"), in_=pdc.ap, func=AF.Exp), r=[pdc], w=[DECC])

        if getattr(self, "dbg", None) is not None:
            for q, tv in enumerate((ACST, W2T, EACST, DECC)[3:], 3):
                op("sp", lambda h, q=q, tv=tv: h.dma_start(out=self.dbg[:, q * 512:(q + 1) * 512], in_=tv.ap.rearrange("p a b -> p (a b)")),
                   r=[tv], w=["dbg%d" % q], dma=True)
        for g in range(4):
            op("pool", lambda h, g=g: h.dma_start(out=WX.ap, in_=w_in[:, :, 2048 + g * 512:2048 + (g + 1) * 512]), r=[], w=[WX], dma=True)
            op("pool", lambda h, g=g: h.dma_start(out=WB.ap, in_=w_in[:, :, 4096 + g * 128:4096 + (g + 1) * 128]), r=[], w=[WB], dma=True)
            op("pool", lambda h, g=g: h.dma_start(out=WC.ap, in_=w_in[:, :, 4608 + g * 128:4608 + (g + 1) * 128]), r=[], w=[WC], dma=True)
            op("dve", lambda h: h.memset(U.ap[:, 0:4], 0.0), r=[], w=[U])
            bases = [g * 512 + j * 128 for j in range(4)] + [2048 + g * 128, 2560 + g * 128]
            for j, b0 in enumerate(bases):
                op("sp", lambda h, j=j, b0=b0: h.dma_start(out=CWT.ap[:, j, :], in_=self.ssm_conv_w[li, :, b0:b0 + 128].rearrange("k p -> p k"),
                                                          allow_slow_non_contiguous=True), r=[], w=[CWT], dma=True)
                op("sp", lambda h, j=j, b0=b0: h.dma_start(out=CBT.ap[:, j:j + 1], in_=self.ssm_conv_b[li:li + 1, b0:b0 + 128].rearrange("o p -> p o")),
                   r=[], w=[CBT], dma=True)
            n = 0
            for j in range(6):
                for r in range(4):
                    pc = self.psb(6 + (n % 2))
                    for k in range(8):
                        if j < 4:
                            lw = WX.ap[:, k, j * 128:(j + 1) * 128]
                            wv = WX
                        else:
                            wv = WB if j == 4 else WC
                            lw = wv.ap[:, k, :]
                        op("pe", lambda h, k=k, r=r, lw=lw, pc=pc: h.matmul(pc.ap, lhsT=lw, rhs=self.HT.ap[:, k, r * 512:(r + 1) * 512],
                                                                       start=(k == 0), stop=(k == 7)), r=[wv, self.HT], w=[pc])
                    op("act", lambda h, r=r, pc=pc: h.activation(out=U.ap[:, 4 + r * 512:4 + (r + 1) * 512], in_=pc.ap, func=AF.Copy), r=[pc], w=[U])
                    acc = ACC[n % 2]
                    eng = "dve"
                    op(eng, lambda h, r=r, j=j, acc=acc: h.tensor_scalar(out=acc.ap, in0=U.ap[:, 1 + r * 512:1 + (r + 1) * 512], scalar1=CWT.ap[:, j, 0:1],
                                                                        scalar2=None, op0=ALU.mult), r=[U, CWT], w=[acc])
                    for t in range(1, 4):
                        op(eng, lambda h, r=r, j=j, t=t, acc=acc: h.scalar_tensor_tensor(out=acc.ap, in0=U.ap[:, 1 + t + r * 512:1 + t + (r + 1) * 512],
                                                                                         scalar=CWT.ap[:, j, t:t + 1], in1=acc.ap, op0=ALU.mult, op1=ALU.add),
                           r=[U, CWT, acc], w=[acc])
                    if j < 4:
                        dst, dv = XF.ap[:, j, r * 512:(r + 1) * 512], XF
                    elif j == 4:
                        dst, dv = BF.ap[:, r * 512:(r + 1) * 512], BF
                    else:
                        dst, dv = CF.ap[:, r * 512:(r + 1) * 512], CF
                    op("act", lambda h, j=j, acc=acc, dst=dst: h.activation(out=dst, in_=acc.ap, func=AF.Silu, bias=CBT.ap[:, j:j + 1], scale=1.0),
                       r=[acc, CBT], w=[dv])
                    n += 1
            op("pool", lambda h, g=g: h.dma_start(out=WZ.ap, in_=w_in[:, :, g * 512:(g + 1) * 512]), r=[], w=[WZ], dma=True)
            op("pool", lambda h, g=g: h.dma_start(out=WO.ap, in_=self.ssm_w_out[li, g * 512:(g + 1) * 512, :].rearrange("(c p) d -> p c d", p=128)),
               r=[], w=[WO], dma=True)
            op("sp", lambda h, g=g: h.dma_start(out=NGg.ap, in_=self.ssm_norm_g[li:li + 1, g * 512:(g + 1) * 512].partition_broadcast(128)),
               r=[], w=[NGg], dma=True)
            op("dve", lambda h: h.memset(STATE.ap, 0.0), r=[], w=[STATE])
            hs = slice(8 * g, 8 * g + 8)
            for cb in range(4):
                pgt = self.psb(5)
                for c in range(4):
                    cc = 4 * cb + c
                    op("pe", lambda h, c=c, cc=cc: h.matmul(pgt.ap[:, c * 128:(c + 1) * 128], lhsT=BF.ap[:, cc * 128:(cc + 1) * 128],
                                                           rhs=CF.ap[:, cc * 128:(cc + 1) * 128], start=True, stop=True), r=[BF, CF], w=[pgt])
                op("act", lambda h: h.activation(out=GT4.ap.rearrange("p a b -> p (a b)"), in_=pgt.ap, func=AF.Copy), r=[pgt], w=[GT4])
                for hh in range(8):
                    hd = 8 * g + hh
                    nbh = NBH[hh % 2]
                    op("dve", lambda h, hd=hd, cb=cb, nbh=nbh: h.tensor_scalar(out=nbh.ap, in0=NBF.ap[:, cb * 512:(cb + 1) * 512], scalar1=identf[0:32, hd:hd + 1],
                                                                               scalar2=None, op0=ALU.mult), r=[NBF, C["ident_f"]], w=[nbh])
                    parg = self.psb(4)
                    op("pe", lambda h, hd=hd, cb=cb: h.matmul(parg.ap, lhsT=identf[0:32, hd:hd + 1].broadcast_to([32, 128]), rhs=ACSF.ap[:, cb * 512:(cb + 1) * 512],
                                                              start=True, stop=False), r=[C["ident_f"], ACSF], w=[parg])
                    for c in range(4):
                        op("pe", lambda h, c=c, nbh=nbh: h.matmul(parg.ap[:, c * 128:(c + 1) * 128], lhsT=nbh.ap[:, c * 128:(c + 1) * 128], rhs=ONESF.ap[0:32, :],
                                                                  start=False, stop=(c == 3)), r=[nbh, ONESF], w=[parg])
                    arg = ARG[hh % 2]
                    op("dve", lambda h, arg=arg: h.tensor_tensor(out=arg.ap, in0=parg.ap, in1=C["maskneg"].ap, op=ALU.add), r=[parg, C["maskneg"]], w=[arg])
                    op("act", lambda h, arg=arg: h.activation(out=arg.ap, in_=arg.ap, func=AF.Exp), r=[arg], w=[arg])
                    op("dve", lambda h, arg=arg, hh=hh: h.tensor_tensor(out=MT[hh].ap.rearrange("p a b -> p (a b)"), in0=arg.ap,
                                                                        in1=GT4.ap.rearrange("p a b -> p (a b)"), op=ALU.mult), r=[arg, GT4], w=[MT[hh]])
                for c in range(4):
                    cc = 4 * cb + c
                    tk = slice(cc * 128, (cc + 1) * 128)
                    ptx = V(self.ps[:, 5, :].bitcast(BF16)[:, 0:512], [("ps", 5)])
                    ptb = V(self.ps[:, 5, :].bitcast(BF16)[:, 512:640], [("ps", 5)])
                    for j in range(4):
                        op("pe", lambda h, j=j, tk=tk: h.transpose(out=ptx.ap[:, j * 128:(j + 1) * 128], in_=XF.ap[:, j, tk], identity=C["ident_b"].ap),
                           r=[XF, C["ident_b"]], w=[ptx])
                    op("pe", lambda h, tk=tk: h.transpose(out=ptb.ap, in_=BF.ap[:, tk], identity=C["ident_b"].ap), r=[BF, C["ident_b"]], w=[ptb])
                    op("act", lambda h: h.activation(out=XTOK.ap, in_=ptx.ap, func=AF.Copy), r=[ptx], w=[XTOK])
                    op("act", lambda h: h.activation(out=BTOK.ap, in_=ptb.ap, func=AF.Copy), r=[ptb], w=[BTOK])
                    op("act", lambda h: h.activation(out=PREV.ap, in_=STATE.ap, func=AF.Copy), r=[STATE], w=[PREV])
                    pyd, pyo, pst, pz = self.psb(0), self.psb(1), self.psb(2), self.psb(3)
                    for hh in range(8):
                        op("pe", lambda h, hh=hh, c=c: h.matmul(pyd.ap[:, hh * 64:(hh + 1) * 64], lhsT=MT[hh].ap[:, c, :], rhs=XTOK.ap[:, hh * 64:(hh + 1) * 64],
                                                               start=True, stop=True), r=[MT[hh], XTOK], w=[pyd])
                    op("pe", lambda h, tk=tk: h.matmul(pyo.ap, lhsT=CF.ap[:, tk], rhs=PREV.ap, start=True, stop=True), r=[CF, PREV], w=[pyo])
                    op("dve", lambda h, cc=cc, hs=hs: h.tensor_tensor(out=XDT2.ap.rearrange("p (a b) -> p a b", a=8), in0=XTOK.ap.rearrange("p (a b) -> p a b", a=8),
                                                               in1=W2T.ap[:, cc, hs].unsqueeze(2).broadcast_to([128, 8, 64]), op=ALU.mult), r=[XTOK, W2T], w=[XDT2])
                    op("pe", lambda h: h.matmul(pst.ap, lhsT=BTOK.ap, rhs=XDT2.ap, start=True, stop=True), r=[BTOK, XDT2], w=[pst])
                    op("dve", lambda h, cc=cc, hs=hs: h.tensor_tensor(out=Y1.ap.rearrange("p (a b) -> p a b", a=8), in0=pyo.ap.rearrange("p (a b) -> p a b", a=8),
                                                              in1=EACST.ap[:, cc, hs].unsqueeze(2).broadcast_to([128, 8, 64]), op=ALU.mult), r=[pyo, EACST], w=[Y1])
                    op("dve", lambda h: h.tensor_tensor(out=Y1.ap, in0=Y1.ap, in1=pyd.ap, op=ALU.add), r=[Y1, pyd], w=[Y1])
                    op("dve", lambda h, hs=hs: h.tensor_tensor(out=XD.ap.rearrange("p (a b) -> p a b", a=8), in0=XTOK.ap.rearrange("p (a b) -> p a b", a=8),
                                                        in1=DBC.ap[:, hs].unsqueeze(2).broadcast_to([128, 8, 64]), op=ALU.mult), r=[XTOK, DBC], w=[XD])
                    op("dve", lambda h: h.tensor_tensor(out=Y1.ap, in0=Y1.ap, in1=XD.ap, op=ALU.add), r=[Y1, XD], w=[Y1])
                    op("dve", lambda h, cc=cc, hs=hs: h.tensor_tensor(out=STATE.ap.rearrange("p (a b) -> p a b", a=8), in0=STATE.ap.rearrange("p (a b) -> p a b", a=8),
                                                              in1=DECC.ap[:, cc, hs].unsqueeze(2).broadcast_to([128, 8, 64]), op=ALU.mult), r=[STATE, DECC], w=[STATE])
                    op("dve", lambda h: h.tensor_tensor(out=STATE.ap, in0=STATE.ap, in1=pst.ap, op=ALU.add), r=[STATE, pst], w=[STATE])
                    if getattr(self, "dbg", None) is not None and g == 0 and cc == 0:
                        op("sp", lambda h: h.dma_start(out=self.dbg[:, 0:512], in_=STATE.ap), r=[STATE], w=["dbg0"], dma=True)
                        op("act", lambda h: h.activation(out=ARG[0].ap, in_=XDT2.ap, func=AF.Copy), r=[XDT2], w=[ARG[0]])
                        op("act", lambda h: h.activation(out=ARG[1].ap[:, 0:128], in_=BTOK.ap, func=AF.Copy), r=[BTOK], w=[ARG[1]])
                        op("sp", lambda h: h.dma_start(out=self.dbg[:, 1024:1536], in_=ARG[0].ap), r=[ARG[0]], w=["dbg2"], dma=True)
                        op("sp", lambda h: h.dma_start(out=self.dbg[:, 512:640], in_=ARG[1].ap[:, 0:128]), r=[ARG[1]], w=["dbg1"], dma=True)
                    for k in range(8):
                        op("pe", lambda h, k=k, tk=tk: h.matmul(pz.ap, lhsT=self.HT.ap[:, k, tk], rhs=WZ.ap[:, k, :], start=(k == 0), stop=(k == 7)),
                           r=[self.HT, WZ], w=[pz])
                    op("act", lambda h: h.activation(out=ZS.ap, in_=pz.ap, func=AF.Silu), r=[pz], w=[ZS])
                    op("dve", lambda h: h.tensor_tensor(out=Y1.ap, in0=Y1.ap, in1=ZS.ap, op=ALU.mult), r=[Y1, ZS], w=[Y1])
                    op("act", lambda h: h.activation(out=JUNK.ap, in_=Y1.ap, func=AF.Square, accum_out=SS.ap[:, 0:1]), r=[Y1], w=[JUNK, SS])
                    op("act", lambda h: h.activation(out=SS.ap[:, 1:2], in_=SS.ap[:, 0:1], func=AF.Sqrt, bias=1e-5, scale=1.0 / 512.0), r=[SS], w=[SS])
                    op("dve", lambda h: h.reciprocal(out=SS.ap[:, 2:3], in_=SS.ap[:, 1:2]), r=[SS], w=[SS])
                    op("dve", lambda h: h.scalar_tensor_tensor(out=YN.ap, in0=Y1.ap, scalar=SS.ap[:, 2:3], in1=NGg.ap, op0=ALU.mult, op1=ALU.mult),
                       r=[Y1, SS, NGg], w=[YN])
                    pty = V(self.ps[:, 5, :].bitcast(BF16)[:, 0:512].rearrange("p (a b) -> p a b", a=4), [("ps", 5)])
                    for j in range(4):
                        op("pe", lambda h, j=j: h.transpose(out=pty.ap[:, j, :], in_=YN.ap[:, j * 128:(j + 1) * 128], identity=C["ident_b"].ap),
                           r=[YN, C["ident_b"]], w=[pty])
                    op("act", lambda h: h.activation(out=YNT.ap, in_=pty.ap, func=AF.Copy), r=[pty], w=[YNT])
                    Rc = self.R[cc]
                    for dr in range(2):
                        pm = self.psb(6 + dr)
                        for j in range(4):
                            op("pe", lambda h, j=j, dr=dr, pm=pm: h.matmul(pm.ap, lhsT=YNT.ap[:, j, :], rhs=WO.ap[:, j, dr * 512:(dr + 1) * 512],
                                                                        start=(j == 0), stop=(j == 3)), r=[YNT, WO], w=[pm])
                        op("dve", lambda h, dr=dr, pm=pm, Rc=Rc: h.tensor_tensor(out=Rc.ap[:, dr * 512:(dr + 1) * 512], in0=Rc.ap[:, dr * 512:(dr + 1) * 512],
                                                                                in1=pm.ap, op=ALU.add), r=[Rc, pm], w=[Rc])


    def attention(self, l, li):
        A, HBb, C = self.Ab, self.HBb, self.C
        op = self.op
        ja = self.attn_layers.index(l)
        build_kv = (ja == 0)
        KT = HBb.view(0, [8, S], BF16)
        o = 0

        def alloc(n):
            nonlocal o
            a = o
            o += (n + G - 1) // G * G
            return a
        VA = A.view(alloc(16 * 16 * 66 * 2), [16, 16, 66], BF16)
        VAF = A.view(0, [16, 16, 33], F32)
        KTF = HBb.view(0, [8 * S // 2], F32)
        KMB = A.view(alloc(8 * 8 * 2), [8, 8], BF16)
        W1R = A.view(alloc(8 * 128 * 2), [8, 128], BF16)
        TRI = A.view(alloc(256), [128], BF16)
        PADM = A.view(alloc(256), [8, 8], F32)
        WSEL = A.view(alloc(16 * 2 * 8 * 4), [16, 2, 8], F32)
        GSC = A.view(alloc(1024), [256], F32)
        W1 = A.view(alloc(8 * 512 * 2), [8, 512], BF16)
        WOp = A.view(alloc(D * 2), [D], BF16)
        QTp = A.view(alloc(S * 2), [S], BF16)
        OTp = A.view(alloc(S * 2), [S], BF16)
        CS = A.view(alloc(512 * 4), [512], F32)
        SN = A.view(alloc(512 * 4), [512], F32)
        T1 = A.view(alloc(512 * 4), [512], F32)
        T2 = A.view(alloc(512 * 4), [512], F32)
        PT = [A.view(alloc(512 * 2), [512], BF16) for _ in range(2)]
        ACC = A.view(alloc(4 * 65 * 4), [4, 65], F32)
        OTOK = A.view(alloc(4 * 128 * 2), [4, 128], BF16)
        assert o <= A.nbytes, o

        for i in range(NT):
            Ri = self.R[i]
            op("act", lambda h, Ri=Ri: h.activation(out=Ri.ap, in_=Ri.ap, func=AF.Copy, scale=ALPHA), r=[Ri], w=[Ri])
        op("pool", lambda h: h.dma_start(out=TRI.ap, in_=self.cst["tri"]), r=[], w=[TRI], dma=True)
        op("sp", lambda h: h.dma_start(out=PADM.ap.rearrange("p a b -> p (a b)"), in_=self.cst["padm"]), r=[], w=[PADM], dma=True)

        def make_rot_weights(pp):
            op("dve", lambda h: h.memset(W1R.ap, 0.0), r=[], w=[W1R])
            for hl in range(2):
                b0 = pp * 128 + 64 * hl
                op("act", lambda h, hl=hl, b0=b0: h.activation(out=W1R.ap[:, :, 64 * hl:64 * hl + 8], in_=W1.ap[:, :, b0 + 8:b0 + 16], func=AF.Copy, scale=-1.0),
                   r=[W1], w=[W1R])
                op("act", lambda h, hl=hl, b0=b0: h.activation(out=W1R.ap[:, :, 64 * hl + 8:64 * hl + 16], in_=W1.ap[:, :, b0:b0 + 8], func=AF.Copy),
                   r=[W1], w=[W1R])

        def rope(pk, dst, dv, r):
            prot = self.psb(6)
            for k in range(8):
                op("pe", lambda h, k=k, r=r, prot=prot: h.matmul(prot.ap, lhsT=W1R.ap[:, k, :], rhs=self.HT.ap[:, k, r * 512:(r + 1) * 512],
                                                              start=(k == 0), stop=(k == 7)), r=[W1R, self.HT], w=[prot])
            op("sp", lambda h, r=r: h.dma_start(out=CS.ap, in_=self.cst["cosf"][:, r * 512:(r + 1) * 512]), r=[], w=[CS], dma=True)
            op("sp", lambda h, r=r: h.dma_start(out=SN.ap, in_=self.cst["sinf"][:, r * 512:(r + 1) * 512]), r=[], w=[SN], dma=True)
            op("dve", lambda h, pk=pk: h.tensor_tensor(out=T1.ap, in0=pk.ap, in1=CS.ap, op=ALU.mult), r=[pk, CS], w=[T1])
            op("dve", lambda h, prot=prot: h.tensor_tensor(out=T2.ap, in0=prot.ap, in1=SN.ap, op=ALU.mult), r=[prot, SN], w=[T2])
            op("dve", lambda h, dst=dst: h.tensor_tensor(out=dst, in0=T1.ap, in1=T2.ap, op=ALU.add), r=[T1, T2], w=[dv])

        def wsrc(w2d, half):
            return w2d.rearrange("(kc kp) c -> kp kc c", kp=128)[:, :, half * 512:(half + 1) * 512]

        def kmeans():
            for p in range(8):
                op("dve", lambda h, p=p: h.tensor_reduce(out=GSC.ap[:, 0:8], in_=KT.ap[:, p, :].rearrange("p (a b) -> p a b", a=8), axis=AX.X, op=ALU.add),
                   r=[KT], w=[GSC])
                op("dve", lambda h, p=p: h.tensor_scalar(out=KMB.ap[:, p, :], in0=GSC.ap[:, 0:8], scalar1=1.0 / 256.0, scalar2=None, op0=ALU.mult),
                   r=[GSC], w=[KMB])

        stop = getattr(self, "attn_stop", 0)
        if stop == -1:
            return
        if build_kv:
            for half in range(2):
                if stop == -2 and half == 1:
                    return
                op("pool", lambda h, half=half: h.dma_start(out=W1.ap, in_=wsrc(self.kv_w_k, half)), r=[], w=[W1], dma=True)
                for pp in range(4):
                    p = half * 4 + pp
                    make_rot_weights(pp)
                    for r in range(4):
                        pk = self.psb(4 + r % 2)
                        for k in range(8):
                            op("pe", lambda h, k=k, r=r, pp=pp, pk=pk: h.matmul(pk.ap, lhsT=W1.ap[:, k, pp * 128:(pp + 1) * 128],
                                                                             rhs=self.HT.ap[:, k, r * 512:(r + 1) * 512], start=(k == 0), stop=(k == 7)),
                               r=[W1, self.HT], w=[pk])
                        rope(pk, KT.ap[:, p, r * 512:(r + 1) * 512], KT, r)
            for half in range(2):
                op("pool", lambda h, half=half: h.dma_start(out=W1.ap, in_=wsrc(self.kv_w_v, half)), r=[], w=[W1], dma=True)
                for i in range(NT):
                    pv = self.psb(4 + i % 2)
                    for k in range(8):
                        op("pe", lambda h, k=k, i=i, pv=pv: h.matmul(pv.ap, lhsT=self.HT.ap[:, k, i * 128:(i + 1) * 128], rhs=W1.ap[:, k, :],
                                                                 start=(k == 0), stop=(k == 7)), r=[self.HT, W1], w=[pv])
                    op("act", lambda h, i=i, half=half, pv=pv: h.activation(out=VA.ap[:, i, half * 8:(half + 1) * 8, 0:64],
                                                                          in_=pv.ap.rearrange("p (a b) -> p a b", a=8), func=AF.Copy), r=[pv], w=[VA])
            if stop == -3:
                return
            op("dve", lambda h: h.memset(VA.ap[:, :, :, 64:65], 1.0), r=[], w=[VA])
            if stop == -4:
                return
            kmeans()
            if not getattr(self, "no_spill", False):
                op("sp", lambda h: h.dma_start(out=self.kt_s, in_=KTF.ap), r=[KT], w=["kt_s"], dma=True)
                for kt in range(16):
                    op("sp", lambda h, kt=kt: h.dma_start(out=self.va_s[:, kt * 512:(kt + 1) * 512].rearrange("p (b c) -> p b c", c=32), in_=VAF.ap[:, kt, :, 0:32]),
                       r=[VA], w=["va_s"], dma=True)
        else:
            op("sp", lambda h: h.dma_start(out=KTF.ap, in_=self.kt_s), r=["kt_s"], w=[KT], dma=True)
            for kt in range(16):
                op("sp", lambda h, kt=kt: h.dma_start(out=VAF.ap[:, kt, :, 0:32], in_=self.va_s[:, kt * 512:(kt + 1) * 512].rearrange("p (b c) -> p b c", c=32)),
                   r=["va_s"], w=[VA], dma=True)
            op("dve", lambda h: h.memset(VA.ap[:, :, :, 64:65], 1.0), r=[], w=[VA])
            kmeans()
        wq = self.attn_w_q[ja]
        stop = getattr(self, "attn_stop", 0)
        if stop == 1:
            return
        for p in range(8 if stop == 0 else 1):
            half, pp = divmod(p, 4)
            if pp == 0:
                op("pool", lambda h, half=half: h.dma_start(out=W1.ap, in_=wsrc(wq, half)), r=[], w=[W1], dma=True)
            op("pool", lambda h, p=p: h.dma_start(out=WOp.ap, in_=self.attn_w_o[ja, p * 128:(p + 1) * 128, :]), r=[], w=[WOp], dma=True)
            make_rot_weights(pp)
            for r in range(4):
                pq = self.psb(4 + r % 2)
                for k in range(8):
                    op("pe", lambda h, k=k, r=r, pp=pp, pq=pq: h.matmul(pq.ap, lhsT=W1.ap[:, k, pp * 128:(pp + 1) * 128],
                                                                     rhs=self.HT.ap[:, k, r * 512:(r + 1) * 512], start=(k == 0), stop=(k == 7)),
                       r=[W1, self.HT], w=[pq])
                rope(pq, QTp.ap[:, r * 512:(r + 1) * 512], QTp, r)
            for hl in range(2):
                rows = slice(64 * hl, 64 * hl + 64)
                for i in range(8, NT):
                    cblk = i // 2
                    pg = self.psb(6, cols=8)
                    op("pe", lambda h, i=i, rows=rows, p=p, pg=pg: h.matmul(pg.ap, lhsT=QTp.ap[rows, i * 128:(i + 1) * 128], rhs=KMB.ap[rows, p, :],
                                                                       start=True, stop=True), r=[QTp, KMB], w=[pg])
                    op("dve", lambda h, cblk=cblk, pg=pg: h.tensor_tensor(out=GSC.ap[:, 0:8], in0=pg.ap, in1=PADM.ap[:, cblk, :], op=ALU.add),
                       r=[pg, PADM], w=[GSC])
                    op("dve", lambda h: h.max(out=GSC.ap[:, 8:16], in_=GSC.ap[:, 0:8]), r=[GSC], w=[GSC])
                    op("dve", lambda h, i=i, hl=hl: h.tensor_scalar(out=WSEL.ap[:, i, hl, :], in0=GSC.ap[:, 0:8], scalar1=GSC.ap[:, 10:11], scalar2=None,
                                                                   op0=ALU.is_ge), r=[GSC], w=[WSEL])
            nst = 0
            if stop == 2:
                return
            for r in range(4):
                for hl in range(2):
                    rows = slice(64 * hl, 64 * hl + 64)
                    hd = 2 * p + hl
                    for n in range(2 * r + 2):
                        jqs = [jq for jq in range(4) if (4 * r + jq) // 2 >= n]
                        c0 = jqs[0] * 128
                        ncol = 512 - c0
                        pob = self.psb(2 + n % 2)
                        pts = {}
                        for kt in (2 * n, 2 * n + 1):
                            use = [jq for jq in jqs if kt <= 4 * r + jq]
                            if not use:
                                continue
                            pss = self.psb(nst % 2)
                            pt = PT[nst % 2]
                            nst += 1
                            pts[kt] = pt
                            op("pe", lambda h, kt=kt, rows=rows, r=r, c0=c0, ncol=ncol, pss=pss, p=p: h.matmul(
                                pss.ap[:, c0:c0 + ncol], lhsT=KT.ap[rows, p, kt * 128:(kt + 1) * 128], rhs=QTp.ap[rows, r * 512 + c0:(r + 1) * 512],
                                start=True, stop=True), r=[KT, QTp], w=[pss])
                            op("act", lambda h, c0=c0, ncol=ncol, pss=pss, pt=pt: h.activation(out=pt.ap[:, c0:c0 + ncol], in_=pss.ap[:, c0:c0 + ncol],
                                                                                           func=AF.Exp, scale=0.125), r=[pss], w=[pt])
                            for jq in use:
                                if kt == 4 * r + jq:
                                    op("dve", lambda h, jq=jq, pt=pt: h.tensor_tensor(out=pt.ap[:, jq * 128:(jq + 1) * 128], in0=pt.ap[:, jq * 128:(jq + 1) * 128],
                                                                                 in1=TRI.ap, op=ALU.mult), r=[pt, TRI], w=[pt])
                        for jq in jqs:
                            kts = [kt for kt in (2 * n, 2 * n + 1) if kt <= 4 * r + jq]
                            for kt in kts:
                                pt = pts[kt]
                                op("pe", lambda h, jq=jq, kt=kt, hd=hd, pt=pt, pob=pob, st=(kt == kts[0]), last=(kt == kts[-1]): h.matmul(
                                    pob.ap[:, jq * 65:(jq + 1) * 65], lhsT=pt.ap[:, jq * 128:(jq + 1) * 128], rhs=VA.ap[:, kt, hd, 0:65], start=st, stop=last),
                                    r=[pt, VA], w=[pob])
                        for jq in jqs:
                            i = 4 * r + jq
                            cblk = i // 2
                            if n == cblk or i < 8:
                                wgt = 1.0
                                rd = [pob]
                            else:
                                wgt = WSEL.ap[:, i, hl, n:n + 1]
                                rd = [pob, WSEL]
                            if n == 0:
                                op("dve", lambda h, jq=jq, wgt=wgt, pob=pob: h.tensor_scalar(out=ACC.ap[:, jq, :], in0=pob.ap[:, jq * 65:(jq + 1) * 65], scalar1=wgt,
                                                                                        scalar2=None, op0=ALU.mult), r=rd, w=[ACC])
                            else:
                                op("dve", lambda h, jq=jq, wgt=wgt, pob=pob: h.scalar_tensor_tensor(out=ACC.ap[:, jq, :], in0=pob.ap[:, jq * 65:(jq + 1) * 65], scalar=wgt,
                                                                                               in1=ACC.ap[:, jq, :], op0=ALU.mult, op1=ALU.add), r=rd + [ACC], w=[ACC])
                    for jq in range(4):
                        op("dve", lambda h, jq=jq: h.reciprocal(out=GSC.ap[:, 32 + jq:33 + jq], in_=ACC.ap[:, jq, 64:65]), r=[ACC], w=[GSC])
                        op("dve", lambda h, jq=jq, hl=hl: h.tensor_scalar(out=OTOK.ap[:, jq, 64 * hl:64 * hl + 64], in0=ACC.ap[:, jq, 0:64],
                                                                         scalar1=GSC.ap[:, 32 + jq:33 + jq], scalar2=None, op0=ALU.mult), r=[ACC, GSC], w=[OTOK])
                ptt = V(self.ps[:, 7, :].bitcast(BF16)[:, 0:512], [("ps", 7)])
                for jq in range(4):
                    op("pe", lambda h, jq=jq: h.transpose(out=ptt.ap[:, jq * 128:(jq + 1) * 128], in_=OTOK.ap[:, jq, :], identity=C["ident_b"].ap),
                       r=[OTOK, C["ident_b"]], w=[ptt])
                op("act", lambda h, r=r: h.activation(out=OTp.ap[:, r * 512:(r + 1) * 512], in_=ptt.ap, func=AF.Copy), r=[ptt], w=[OTp])
            for i in range(NT):
                Ri = self.R[i]
                for dr in range(2):
                    pm = self.psb(4 + dr)
                    op("pe", lambda h, i=i, dr=dr, pm=pm: h.matmul(pm.ap, lhsT=OTp.ap[:, i * 128:(i + 1) * 128], rhs=WOp.ap[:, dr * 512:(dr + 1) * 512],
                                                              start=True, stop=True), r=[OTp, WOp], w=[pm])
                    op("dve", lambda h, dr=dr, pm=pm, Ri=Ri: h.tensor_tensor(out=Ri.ap[:, dr * 512:(dr + 1) * 512], in0=Ri.ap[:, dr * 512:(dr + 1) * 512],
                                                                            in1=pm.ap, op=ALU.add), r=[Ri, pm], w=[Ri])

    def moe(self, li):
        A = self.Ab
        C = self.C
        o = 0

        def alloc(n):
            nonlocal o
            a = o
            o += (n + G - 1) // G * G
            return a
        WR = A.view(alloc(8 * NE * 2), [8, NE], BF16)
        BRT = A.view(alloc(NE * 4), [NE], F32)
        RG = A.view(alloc(NT * 64 * 4), [NT, 64], F32)
        RGB = A.view(alloc(NT * 64 * 2), [NT, 64], BF16)
        RGT = A.view(alloc(S * 2), [S], BF16, parts=64)
        MSK = A.view(alloc(NT * NE * 2), [NT, NE], BF16)
        CM = A.view(alloc(NT * NE * 2), [NT, NE], BF16)
        SM = A.view(alloc(1024), [256], F32)
        BGU = A.view(alloc(NE * 16 * 4), [NE, 16], F32)
        BDN = [A.view(alloc(D * 2), [D], BF16, parts=1)] * 2
        NRING = 6
        ring_offs = [alloc(8 * 256 * 2) for _ in range(NRING)]
        assert all(ring_offs[k + 1] - ring_offs[k] == 4096 for k in range(NRING - 1))
        RING = [A.view(o_, [8, 256], BF16) for o_ in ring_offs]
        d_slot0 = 8 % NRING
        assert 12 % NRING == 0 and d_slot0 % 2 == 0 and d_slot0 + 3 < NRING + 0 * 1 or NRING == 4
        RING2 = [A.view(ring_offs[(d_slot0 + 2 * j) % NRING], [2, 8, 256], BF16) for j in range(2)]
        xt_off = alloc(8 * CAP * 2)
        XT = A.view(xt_off, [8, CAP], BF16)
        ACTT = A.view(alloc(8 * CAP * 2), [8, CAP], BF16)
        YB = A.view(xt_off, [NJT, D], BF16)
        TG = [A.view(alloc(CAP * 4), [CAP], F32) for _ in range(2)]
        TS = [A.view(alloc(CAP * 2), [CAP], BF16) for _ in range(2)]
        TU = [A.view(alloc(CAP * 4), [CAP], F32) for _ in range(2)]
        GBS = [A.view(alloc(512 * 2), [512], BF16)] * 2
        assert o <= A.nbytes, o
        self.arena_used = o
        Sg = [self.HTb.view(i * 1024, [128], BF16) for i in range(NT)]
        base = NT * 1024
        STw = [self.HTb.view(base + w * 1024, [512], BF16) for w in range(4)]

        wr_src = self.w_router[li].rearrange("(kc kp) e -> kp kc e", kp=128)
        self.op("pool", lambda h: h.dma_start(out=WR.ap, in_=wr_src), r=[], w=[WR], dma=True)
        self.op("sp", lambda h: h.dma_start(out=BRT.ap, in_=self.b_router[li:li + 1, :].partition_broadcast(128)), r=[], w=[BRT], dma=True)
        bgu_src = self.b_gu[li].rearrange("e (c p) -> (e c) p", p=128).rearrange("(t r) p -> r t p", r=128)
        self.op("sp", lambda h: h.dma_start(out=TG[0].ap.rearrange("p (t q) -> p t q", t=4), in_=bgu_src), r=[], w=[TG[0]], dma=True)
        pbg = self.psb(0)
        for t in range(4):
            self.op("pe", lambda h, t=t: h.transpose(out=pbg.ap[:, t * 128:(t + 1) * 128], in_=TG[0].ap[:, t * 128:(t + 1) * 128], identity=C["ident_f"].ap),
                    r=[TG[0], C["ident_f"]], w=[pbg])
        self.op("act", lambda h: h.activation(out=BGU.ap.rearrange("p e c -> p (e c)"), in_=pbg.ap, func=AF.Copy), r=[pbg], w=[BGU])

        pieces = []
        for e in range(NE):
            for p in range(4):
                pieces.append((e, "g", p))
                pieces.append((e, "u", p))
            for q in range(4):
                pieces.append((e, "d", q))
        self._ring_state = {"next": 0}
        piece_slot = {}

        def issue_piece():
            n = self._ring_state["next"]
            if n >= len(pieces):
                return
            e, kind, p = pieces[n]
            slot = RING[n % NRING]
            piece_slot[(e, kind, p)] = slot
            if kind == "d":
                src = self.w_dn[li, e].rearrange("(kc kp) d -> kp kc d", kp=128)[:, :, p * 256:(p + 1) * 256]
            else:
                c0 = p * 256 + (D if kind == "u" else 0)
                src = self.w_gu[li, e].rearrange("(kc kp) f -> kp kc f", kp=128)[:, :, c0:c0 + 256]
            self.op("pool", lambda h, slot=slot, src=src: h.dma_start(out=slot.ap, in_=src), r=[], w=[slot], dma=True)
            self._ring_state["next"] = n + 1

        for _ in range(NRING):
            issue_piece()

        scr = SM.ap
        for i in range(NT):
            pl = self.psb(4 + (i % 2), cols=NE)
            for k in range(8):
                self.op("pe", lambda h, k=k, i=i, pl=pl: h.matmul(pl.ap, lhsT=self.HT.ap[:, k, i * 128:(i + 1) * 128], rhs=WR.ap[:, k, :],
                                                               start=(k == 0), stop=(k == 7)), r=[self.HT, WR], w=[pl])
            lg = V(scr[:, 0:32], SM.keys)
            m8 = V(scr[:, 32:40], SM.keys)
            nm = V(scr[:, 40:41], SM.keys)
            mk = V(scr[:, 48:80], SM.keys)
            ex = V(scr[:, 80:112], SM.keys)
            sm = V(scr[:, 112:113], SM.keys)
            rk = V(scr[:, 128:160], SM.keys)
            self.op("dve", lambda h, pl=pl: h.tensor_tensor(out=lg.ap, in0=pl.ap, in1=BRT.ap, op=ALU.add), r=[pl, BRT], w=[SM])
            self.op("dve", lambda h: h.max(out=m8.ap, in_=lg.ap), r=[SM], w=[SM])
            self.op("dve", lambda h: h.tensor_scalar(out=mk.ap, in0=lg.ap, scalar1=m8.ap[:, 3:4], scalar2=None, op0=ALU.is_ge), r=[SM], w=[SM])
            self.op("dve", lambda h: h.tensor_scalar(out=nm.ap, in0=m8.ap[:, 0:1], scalar1=-1.0, scalar2=None, op0=ALU.mult), r=[SM], w=[SM])
            self.op("act", lambda h: h.activation(out=ex.ap, in_=lg.ap, func=AF.Exp, bias=nm.ap, scale=1.0), r=[SM], w=[SM])
            self.op("dve", lambda h: h.tensor_tensor(out=ex.ap, in0=ex.ap, in1=mk.ap, op=ALU.mult), r=[SM], w=[SM])
            self.op("dve", lambda h: h.tensor_reduce(out=sm.ap, in_=ex.ap, axis=AX.X, op=ALU.add), r=[SM], w=[SM])
            self.op("dve", lambda h: h.reciprocal(out=sm.ap, in_=sm.ap), r=[SM], w=[SM])
            self.op("dve", lambda h, i=i: h.tensor_scalar(out=RG.ap[:, i, 32:64], in0=ex.ap, scalar1=sm.ap, scalar2=None, op0=ALU.mult), r=[SM], w=[RG])
            self.op("dve", lambda h, i=i: h.tensor_copy(out=MSK.ap[:, i, :], in_=mk.ap), r=[SM], w=[MSK])
            if i % 4 == 0:
                self.op("dve", lambda h, i=i: h.memset(CM.ap[:, i, :], 0.0), r=[], w=[CM])
            else:
                self.op("dve", lambda h, i=i: h.tensor_tensor(out=CM.ap[:, i, :], in0=CM.ap[:, i - 1, :], in1=MSK.ap[:, i - 1, :], op=ALU.add),
                        r=[CM, MSK], w=[CM])
            pr = self.psb(4 + (i % 2), cols=NE, c0=64)
            self.op("pe", lambda h, i=i, pr=pr: h.matmul(pr.ap, lhsT=C["ones"].ap, rhs=CM.ap[:, i, :], start=True, stop=False), r=[C["ones"], CM], w=[pr])
            self.op("pe", lambda h, i=i, pr=pr: h.matmul(pr.ap, lhsT=C["triu"].ap, rhs=MSK.ap[:, i, :], start=False, stop=True), r=[C["triu"], MSK], w=[pr])
            self.op("dve", lambda h, pr=pr: h.scalar_tensor_tensor(out=rk.ap, in0=pr.ap, scalar=1.0, in1=mk.ap, op0=ALU.add, op1=ALU.mult), r=[pr, SM], w=[SM])
            self.op("dve", lambda h, i=i: h.tensor_scalar(out=RG.ap[:, i, 0:32], in0=rk.ap, scalar1=-1.0, scalar2=None, op0=ALU.add), r=[SM], w=[RG])
            self.op("dve", lambda h, i=i: h.tensor_copy(out=RGB.ap[:, i, :], in_=RG.ap[:, i, :]), r=[RG], w=[RGB])
            ptr = V(self.ps[0:64, 4 + (i % 2), :].bitcast(BF16)[:, 256:384], [("ps", 4 + (i % 2))])
            self.op("pe", lambda h, i=i, ptr=ptr: h.transpose(out=ptr.ap, in_=RGB.ap[:, i, :], identity=C["ident_b"].ap), r=[RGB, C["ident_b"]], w=[ptr])
            self.op("act", lambda h, i=i, ptr=ptr: h.activation(out=RGT.ap[:, i * 128:(i + 1) * 128], in_=ptr.ap, func=AF.Copy), r=[ptr], w=[RGT])

        for i in range(NT):
            Ri = self.R[i]
            self.op("act", lambda h, Ri=Ri: h.activation(out=Ri.ap, in_=Ri.ap, func=AF.Copy, scale=ALPHA), r=[Ri], w=[Ri])

        identf = C["ident_f"].ap
        identb = C["ident_b"].ap
        for e in range(NE):
            bdn = BDN[e % 2]
            self.op("pool", lambda h, e=e, bdn=bdn: h.dma_start(out=bdn.ap, in_=self.b_dn[li, e:e + 1, :]), r=[], w=[bdn], dma=True)
            for i in range(NT):
                eng = "dve"
                self.op(eng, lambda h, i=i, e=e: h.tensor_scalar(out=Sg[i].ap, in0=C["iota_row"].ap[:, 0:128], scalar1=RG.ap[:, i, e:e + 1], scalar2=None,
                                                                op0=ALU.is_equal), r=[C["iota_row"], RG], w=[Sg[i]])
            for m in range(8):
                px = self.psb(m % 2)
                for i in range(NT):
                    w4, ii = divmod(i, 4)
                    self.op("pe", lambda h, m=m, i=i, px=px, w4=w4, ii=ii: h.matmul(px.ap[:, w4 * 128:(w4 + 1) * 128], lhsT=self.HB[i].ap[:, m * 128:(m + 1) * 128],
                                                                               rhs=Sg[i].ap, start=(ii == 0), stop=(ii == 3)), r=[self.HB[i], Sg[i]], w=[px])
                self.op("act", lambda h, m=m, px=px: h.activation(out=XT.ap[:, m, :], in_=px.ap, func=AF.Copy), r=[px], w=[XT])
            for r in range(4):
                prb = self.psb(2)
                pgb = self.psb(3)
                self.op("pe", lambda h, r=r, e=e, prb=prb: h.matmul(prb.ap, lhsT=identb[0:64, e:e + 1].broadcast_to([64, 128]),
                                                                    rhs=RGT.ap[:, r * 512:(r + 1) * 512], start=True, stop=True),
                        r=[C["ident_b"], RGT], w=[prb])
                self.op("pe", lambda h, r=r, e=e, pgb=pgb: h.matmul(pgb.ap, lhsT=identb[0:64, 32 + e:33 + e].broadcast_to([64, 128]),
                                                                    rhs=RGT.ap[:, r * 512:(r + 1) * 512], start=True, stop=True),
                        r=[C["ident_b"], RGT], w=[pgb])
                gbs = GBS[r % 2]
                self.op("act", lambda h, pgb=pgb, gbs=gbs: h.activation(out=gbs.ap, in_=pgb.ap, func=AF.Copy), r=[pgb], w=[gbs])
                self.op("dve", lambda h, r=r, prb=prb, gbs=gbs: h.scalar_tensor_tensor(
                    out=STw[r].ap, in0=prb.ap, scalar=C["piota"].ap[:, 0:1], in1=gbs.ap, op0=ALU.is_equal, op1=ALU.mult),
                    r=[prb, gbs, C["piota"]], w=[STw[r]])
            for p in range(4):
                wg = piece_slot[(e, "g", p)]
                wu = piece_slot[(e, "u", p)]
                for sub in range(2):
                    c = 2 * p + sub
                    pg = self.psb(4 + (c % 2))
                    pu = self.psb(6 + (c % 2))
                    for k in range(8):
                        self.op("pe", lambda h, k=k, sub=sub, wg=wg, pg=pg: h.matmul(pg.ap, lhsT=wg.ap[:, k, sub * 128:(sub + 1) * 128], rhs=XT.ap[:, k, :],
                                                                                 start=(k == 0), stop=(k == 7)), r=[wg, XT], w=[pg])
                    for k in range(8):
                        self.op("pe", lambda h, k=k, sub=sub, wu=wu, pu=pu: h.matmul(pu.ap, lhsT=wu.ap[:, k, sub * 128:(sub + 1) * 128], rhs=XT.ap[:, k, :],
                                                                                 start=(k == 0), stop=(k == 7)), r=[wu, XT], w=[pu])
                    tg, ts, tu = TG[c % 2], TS[c % 2], TU[c % 2]
                    self.op("dve", lambda h, pg=pg, tg=tg, e=e, c=c: h.tensor_scalar(out=tg.ap, in0=pg.ap, scalar1=BGU.ap[:, e, c:c + 1], scalar2=7.0,
                                                                                 op0=ALU.add, op1=ALU.min), r=[pg, BGU], w=[tg])
                    self.op("act", lambda h, tg=tg, ts=ts: h.activation(out=ts.ap, in_=tg.ap, func=AF.Sigmoid, scale=1.702), r=[tg], w=[ts])
                    self.op("act", lambda h, pu=pu, tu=tu, e=e, c=c: h.activation(out=tu.ap, in_=pu.ap, func=AF.Identity, bias=BGU.ap[:, e, 8 + c:9 + c], scale=1.0),
                            r=[pu, BGU], w=[tu])
                    self.op("dve", lambda h, tu=tu: h.tensor_scalar(out=tu.ap, in0=tu.ap, scalar1=-7.0, scalar2=7.0, op0=ALU.max, op1=ALU.min), r=[tu], w=[tu])
                    self.op("dve", lambda h, tg=tg, ts=ts: h.tensor_tensor(out=tg.ap, in0=tg.ap, in1=ts.ap, op=ALU.mult), r=[tg, ts], w=[tg])
                    self.op("dve", lambda h, tg=tg, tu=tu, c=c: h.scalar_tensor_tensor(out=ACTT.ap[:, c, :], in0=tu.ap, scalar=1.0, in1=tg.ap, op0=ALU.add, op1=ALU.mult),
                            r=[tg, tu], w=[ACTT])
                issue_piece()
                issue_piece()
            for j in range(2):
                assert piece_slot[(e, "d", 2 * j)] is RING[(d_slot0 + 2 * j) % NRING] and piece_slot[(e, "d", 2 * j + 1)] is RING[(d_slot0 + 2 * j + 1) % NRING]
                wd2 = RING2[j]
                for jt in range(NJT):
                    py = self.psb(jt % 2)
                    for c in range(8):
                        self.op("pe", lambda h, c=c, jt=jt, wd2=wd2, py=py: h.matmul(py.ap.rearrange("p (a b) -> p a b", a=2), lhsT=ACTT.ap[:, c, jt * 128:(jt + 1) * 128],
                                                                                 rhs=wd2.ap[:, :, c, :], start=(c == 0), stop=False), r=[ACTT, wd2], w=[py])
                    self.op("pe", lambda h, j=j, py=py, bdn=bdn: h.matmul(py.ap, lhsT=C["ones"].ap[0:1, :], rhs=bdn.ap[0:1, j * 512:(j + 1) * 512],
                                                                          start=False, stop=True), r=[C["ones"], bdn], w=[py])
                    self.op("act", lambda h, jt=jt, j=j, py=py: h.activation(out=YB.ap[:, jt, j * 512:(j + 1) * 512], in_=py.ap, func=AF.Copy), r=[py], w=[YB])
                issue_piece()
                issue_piece()
            for i in range(NT):
                for dr in range(2):
                    po = self.psb(2 + ((2 * i + dr) % 2))
                    w4, ii = divmod(i, 4)
                    self.op("pe", lambda h, w4=w4, ii=ii, dr=dr, po=po: h.matmul(po.ap, lhsT=STw[w4].ap[:, ii * 128:(ii + 1) * 128],
                                                                             rhs=YB.ap[:, w4, dr * 512:(dr + 1) * 512], start=True, stop=True),
                            r=[STw[w4], YB], w=[po])
                    Ri = self.R[i]
                    self.op("dve", lambda h, dr=dr, Ri=Ri, po=po: h.tensor_tensor(out=Ri.ap[:, dr * 512:(dr + 1) * 512], in0=Ri.ap[:, dr * 512:(dr + 1) * 512],
                                                                              in1=po.ap, op=ALU.add), r=[Ri, po], w=[Ri])


_CACHE = {}
_NPH = [8]

WEIGHT_KEYS = ["kv_w_k", "kv_w_v", "attn_w_q", "attn_w_o", "ssm_w_in", "ssm_conv_w", "ssm_conv_b", "ssm_dt_bias", "ssm_a_log", "ssm_d", "ssm_norm_g", "ssm_w_out",
               "moe_w_router", "moe_b_router", "moe_w_gate_up", "moe_b_gate_up", "moe_w_down", "moe_b_down",
               "ln_mix_g", "ln_mix_b", "ln_ffn_g", "ln_ffn_b"]


def kernel(**inputs):
    x = np.ascontiguousarray(np.asarray(inputs["x"], dtype=np.float32))
    nb = x.shape[0]
    if "nc" not in _CACHE:
        phases = [("prep", 0), ("mamba", 0), ("moe", 0), ("mamba", 1), ("moe", 1), ("attn", 2), ("moe", 2), ("attn", 3), ("moe", 3)][:_NPH[0] + 1]
        mk = MK(layers=[0, 1, 2, 3], phases=phases)
        _CACHE["nc"] = mk.build()
        _CACHE["mk"] = mk
    nc = _CACHE["nc"]
    shared = {k: np.ascontiguousarray(np.asarray(inputs[k], dtype=np.float32)) for k in WEIGHT_KEYS}
    for k, v in make_consts().items():
        shared["c_" + k] = v
    in_maps = []
    for b in range(nb):
        m = dict(shared)
        m["x"] = x[b]
        in_maps.append(m)
    res = run_bass_kernel_spmd(nc, in_maps, core_ids=list(range(nb)))
    return np.stack([np.asarray(r["y"], dtype=np.float32) for r in res.results], axis=0)
```

```python
import math
from contextlib import ExitStack

import numpy as np
import ml_dtypes

import concourse.bass as bass
import concourse.mybir as mybir
from concourse.bass_utils import run_bass_kernel_spmd

F32 = mybir.dt.float32
BF16 = mybir.dt.bfloat16
AF = mybir.ActivationFunctionType
ALU = mybir.AluOpType
AX = mybir.AxisListType

D = 1024
S = 2048
NT = 16
DEPTH = 4
NE = 32
CAP = 512
NJT = CAP // 128
ALPHA = (2 * DEPTH) ** 0.25
LN_EPS = 1e-5
G = 1024


class Op:
    __slots__ = ("eng", "fn", "deps", "dma", "signal", "sig", "idx")

    def __init__(self, eng, fn, deps, dma, idx):
        self.eng = eng
        self.fn = fn
        self.deps = deps
        self.dma = dma
        self.signal = False
        self.sig = None
        self.idx = idx


class Prog:
    NDMA = 24

    def __init__(self, nc):
        self.nc = nc
        self.ops = []
        self.last_writer = {}
        self.readers = {}

    def add(self, eng, fn, reads=(), writes=(), dma=False):
        idx = len(self.ops)
        ops = self.ops
        deps = set()
        for k in reads:
            w = self.last_writer.get(k)
            if w is not None:
                deps.add(w)
        for k in writes:
            w = self.last_writer.get(k)
            if w is not None:
                o = ops[w]
                if o.dma or dma or o.eng != eng:
                    deps.add(w)
            for r in self.readers.get(k, ()):
                o = ops[r]
                if o.dma or dma or o.eng != eng:
                    deps.add(r)
        if eng == "pe" and not dma:
            deps = {d for d in deps if ops[d].dma or ops[d].eng != "pe"}
        latest = {}
        pruned = set()
        for d in deps:
            o = ops[d]
            if o.dma:
                pruned.add(d)
            elif latest.get(o.eng, -1) < d:
                latest[o.eng] = d
        pruned.update(latest.values())
        deps = pruned
        op = Op(eng, fn, deps, dma, idx)
        ops.append(op)
        for k in reads:
            self.readers.setdefault(k, []).append(idx)
        for k in writes:
            self.last_writer[k] = idx
            self.readers[k] = []
        return op

    def emit(self, stack):
        nc = self.nc
        ops = self.ops
        for op in ops:
            for d in op.deps:
                ops[d].signal = True
        engs = ["pe", "act", "dve", "pool", "sp"]
        esem = {e: stack.enter_context(nc.semaphore("s_" + e)) for e in engs}
        dsem = {e: [stack.enter_context(nc.semaphore("d_%s_%d" % (e, i))) for i in range(self.NDMA)]
                for e in ("sp", "act", "pool")}
        cnt = {e: 0 for e in engs}
        dcnt = {e: 0 for e in dsem}
        prewait = {}
        for op in ops:
            if op.dma:
                op.signal = True
                i = dcnt[op.eng]
                dcnt[op.eng] += 1
                sem = dsem[op.eng][i % self.NDMA]
                op.sig = (sem, 16 * (i // self.NDMA + 1))
                if i >= self.NDMA:
                    prewait[op.idx] = (sem, 16 * (i // self.NDMA))
            elif op.signal:
                cnt[op.eng] += 1
                op.sig = (esem[op.eng], cnt[op.eng])
        per = {e: [op for op in ops if op.eng == e] for e in engs}
        block = stack.enter_context(nc.Block())
        self.nwaits = 0

        def body(e):
            def run(h):
                waited = {}
                for op in per[e]:
                    need = {}
                    if op.idx in prewait:
                        s, v = prewait[op.idx]
                        need[id(s)] = (s, v)
                    for d in op.deps:
                        s, v = ops[d].sig
                        cur = need.get(id(s))
                        if cur is None or cur[1] < v:
                            need[id(s)] = (s, v)
                    for sid, (s, v) in need.items():
                        if waited.get(sid, 0) < v:
                            h.wait_ge(s, v)
                            waited[sid] = v
                            self.nwaits += 1
                    ins = op.fn(h)
                    if op.signal and ins is not None:
                        s, v = op.sig
                        ins.then_inc(s, 16 if op.dma else 1)
            return run

        block.tensor(body("pe"))
        block.scalar(body("act"))
        block.vector(body("dve"))
        block.gpsimd(body("pool"))
        block.sync(body("sp"))
        self.counts = dict(cnt)


class V:
    __slots__ = ("ap", "keys")

    def __init__(self, ap, keys):
        self.ap = ap
        self.keys = keys


def _prod(s):
    n = 1
    for x in s:
        n *= x
    return n


_RE = {2: "p (a b) -> p a b", 3: "p (a b c) -> p a b c"}


class Buf:
    def __init__(self, M, name, nbytes):
        self.name = name
        self.nbytes = nbytes
        self.t = M.st.enter_context(M.nc.sbuf_tensor(name, [128, nbytes // 2], BF16))

    def view(self, off, shape, dt=BF16, p0=0, parts=128):
        esz = 4 if dt == F32 else 2
        n = _prod(shape)
        assert off % 4 == 0 and off + n * esz <= self.nbytes, (self.name, off, shape)
        a = self.t[p0:p0 + parts, off // 2:(off + n * esz) // 2]
        if dt == F32:
            a = a.bitcast(F32)
        if len(shape) == 2:
            a = a.rearrange("p (a b) -> p a b", a=shape[0])
        elif len(shape) == 3:
            a = a.rearrange("p (a b c) -> p a b c", a=shape[0], b=shape[1])
        keys = [(self.name, g) for g in range(off // G, (off + n * esz - 1) // G + 1)]
        return V(a, keys)


def keys_of(vs):
    out = []
    for v in vs:
        if isinstance(v, V):
            out.extend(v.keys)
        elif isinstance(v, (list, tuple)) and v and isinstance(v[0], (V,)):
            out.extend(keys_of(v))
        else:
            out.append(v)
    return out


def make_consts():
    c = {}
    c["ident_f"] = np.eye(128, dtype=np.float32)
    c["iota_row"] = np.broadcast_to(np.arange(CAP, dtype=np.float32), (128, CAP)).copy()
    c["piota"] = (np.arange(128, dtype=np.float32)[:, None] + 128.0 * np.arange(NJT, dtype=np.float32)[None, :]).copy()
    c["triu"] = np.triu(np.ones((128, 128), np.float32), k=1)
    c["ones"] = np.ones((128, 128), np.float32)
    m = np.where(np.arange(128)[None, :] >= np.arange(128)[:, None], 0.0, -30000.0).astype(np.float32)
    c["maskneg"] = np.tile(m, (1, 4))
    inv_freq = (500000.0 ** (-np.arange(0, 16, 2, dtype=np.float32) / 16.0)).astype(np.float32)
    ang = np.arange(S, dtype=np.float32)[:, None] * inv_freq[None, :]
    cos, sin = np.cos(ang).astype(np.float32), np.sin(ang).astype(np.float32)
    cosf = np.ones((128, S), np.float32)
    sinf = np.zeros((128, S), np.float32)
    rotm = np.zeros((128, 128), np.float32)
    for hl in range(2):
        for dd in range(16):
            cosf[64 * hl + dd] = cos[:, dd % 8]
            sinf[64 * hl + dd] = sin[:, dd % 8]
        for dd in range(8):
            rotm[64 * hl + dd + 8, 64 * hl + dd] = -1.0
            rotm[64 * hl + dd, 64 * hl + dd + 8] = 1.0
    c["cosf"], c["sinf"], c["rotm"] = cosf, sinf, rotm
    c["tri"] = np.triu(np.ones((128, 128), np.float32), k=0)
    padm = np.zeros((8, 8), np.float32)
    for cb in range(8):
        padm[cb, cb:] = -1e30
    c["padm"] = np.broadcast_to(padm.reshape(1, 64), (128, 64)).copy()
    return c


CONST_SHAPES = {"ident_f": [128, 128], "iota_row": [128, CAP], "piota": [128, NJT],
                "triu": [128, 128], "ones": [128, 128], "maskneg": [128, 512], "cosf": [128, S], "sinf": [128, S],
                "rotm": [128, 128], "tri": [128, 128], "padm": [128, 64]}


class MK:
    def __init__(self, layers, phases, debug_in=None):
        self.layers = layers
        self.phases = phases
        self.debug = debug_in
        self.nc = bass.Bass("TRN2", target_bir_lowering=False)
        self.st = ExitStack()

    def op(self, eng, fn, r=(), w=(), dma=False):
        return self.P.add(eng, fn, keys_of(r), keys_of(w), dma)

    def dram_in(self, name, shape):
        return self.nc.dram_tensor(name, list(shape), F32, kind="ExternalInput").ap()

    def psb(self, b, cols=512, parts=128, c0=0):
        return V(self.ps[0:parts, b, c0:c0 + cols], [("ps", b)])

    def build(self):
        nc, st = self.nc, self.st
        nl = len(self.layers)
        self.x = self.dram_in("x", [S, D])
        self.y = nc.dram_tensor("y", [S, D], F32, kind="ExternalOutput").ap()
        self.w_router = self.dram_in("moe_w_router", [nl, D, NE])
        self.b_router = self.dram_in("moe_b_router", [nl, NE])
        self.w_gu = self.dram_in("moe_w_gate_up", [nl, NE, D, 2 * D])
        self.b_gu = self.dram_in("moe_b_gate_up", [nl, NE, 2 * D])
        self.w_dn = self.dram_in("moe_w_down", [nl, NE, D, D])
        self.b_dn = self.dram_in("moe_b_down", [nl, NE, D])
        self.ln_g = {"mix": self.dram_in("ln_mix_g", [nl, D]), "moe": self.dram_in("ln_ffn_g", [nl, D])}
        self.ln_b = {"mix": self.dram_in("ln_mix_b", [nl, D]), "moe": self.dram_in("ln_ffn_b", [nl, D])}
        self.cst = {k: self.dram_in("c_" + k, s) for k, s in CONST_SHAPES.items()}
        na = max(1, sum(1 for l in self.layers if l < 2))
        self.ssm_w_in = self.dram_in("ssm_w_in", [na, D, 5152])
        self.ssm_conv_w = self.dram_in("ssm_conv_w", [na, 4, 3072])
        self.ssm_conv_b = self.dram_in("ssm_conv_b", [na, 3072])
        self.ssm_dt_bias = self.dram_in("ssm_dt_bias", [na, 32])
        self.ssm_a_log = self.dram_in("ssm_a_log", [na, 32])
        self.ssm_d = self.dram_in("ssm_d", [na, 32])
        self.ssm_norm_g = self.dram_in("ssm_norm_g", [na, 2048])
        self.ssm_w_out = self.dram_in("ssm_w_out", [na, 2048, D])
        self.attn_layers = [l for l in self.layers if l >= 2]
        nbl = max(1, len(self.attn_layers))
        self.kv_w_k = self.dram_in("kv_w_k", [D, D])
        self.kv_w_v = self.dram_in("kv_w_v", [D, D])
        self.attn_w_q = self.dram_in("attn_w_q", [nbl, D, D])
        self.attn_w_o = self.dram_in("attn_w_o", [nbl, D, D])
        self.kt_s = self.y[0:1024, :].rearrange("(p a) c -> p (a c)", p=128)
        self.va_s = self.y[1024:2048, :].rearrange("(p a) c -> p (a c)", p=128)

        self.dbg = nc.dram_tensor("dbg", [128, 2048], F32, kind="ExternalOutput").ap() if self.debug else None
        self.P = Prog(nc)
        self.ps = st.enter_context(nc.psum_tensor("ps", [128, 8, 512], F32))
        self.Rb = Buf(self, "R", NT * D * 4)
        self.HBb = Buf(self, "HB", NT * D * 2)
        self.HTb = Buf(self, "HT", 8 * S * 2)
        self.Cb = Buf(self, "CST", 6 * 1024)
        self.Ab = Buf(self, "ARENA", 72 * 1024)
        self.R = [self.Rb.view(i * D * 4, [D], F32) for i in range(NT)]
        self.HB = [self.HBb.view(i * D * 2, [D], BF16) for i in range(NT)]
        self.HT = self.HTb.view(0, [8, S], BF16)

        self.load_consts()
        for i in range(NT):
            self.op("sp", lambda h, i=i: h.dma_start(out=self.R[i].ap, in_=self.x[i * 128:(i + 1) * 128, :]),
                    r=["x"], w=[self.R[i]], dma=True)
        first = True
        for kind, l in self.phases:
            li = self.layers.index(l)
            if kind == "prep":
                for i in range(NT):
                    self.make_copies(i)
            elif kind == "nomix":
                for i in range(NT):
                    Ri = self.R[i]
                    self.op("act", lambda h, Ri=Ri: h.activation(out=Ri.ap, in_=Ri.ap, func=AF.Copy, scale=ALPHA), r=[Ri], w=[Ri])
                self.layer_norm("mix", li)
            elif kind == "attn":
                self.attention(l, li)
                self.layer_norm("mix", li)
            elif kind == "mamba":
                self.mamba(li)
                self.layer_norm("mix", li)
            elif kind == "moe":
                self.moe(li)
                self.layer_norm("moe", li)
        for i in range(NT):
            self.op("sp", lambda h, i=i: h.dma_start(out=self.y[i * 128:(i + 1) * 128, :], in_=self.R[i].ap),
                    r=[self.R[i]], w=["y%d" % i, "kt_s", "va_s"], dma=True)
        self.P.add("sp", lambda h: None, reads=["y%d" % i for i in range(NT)])
        self.P.emit(st)
        return nc

    def load_consts(self):
        cb = self.Cb
        off = 0
        self.C = {}

        def alloc(n_bytes):
            nonlocal off
            o = off
            off += (n_bytes + 3) // 4 * 4
            return o
        for k in ("maskneg",):
            v = cb.view(alloc(512 * 4), [512], F32)
            self.C[k] = v
            self.op("sp", lambda h, v=v, k=k: h.dma_start(out=v.ap, in_=self.cst[k]), r=[], w=[v], dma=True)
        v = cb.view(alloc(128 * 4), [128], F32)
        self.C["ones_f"] = v
        self.op("sp", lambda h, v=v: h.dma_start(out=v.ap, in_=self.cst["ones"]), r=[], w=[v], dma=True)
        for k in ("ident_f", "piota"):
            shp = CONST_SHAPES[k]
            v = cb.view(alloc(shp[1] * 4), [shp[1]], F32)
            self.C[k] = v
            self.op("sp", lambda h, v=v, k=k: h.dma_start(out=v.ap, in_=self.cst[k]), r=[], w=[v], dma=True)
        v = cb.view(alloc(CAP * 4), [CAP], F32)
        self.C["iota_row"] = v
        self.op("sp", lambda h, v=v: h.dma_start(out=v.ap, in_=self.cst["iota_row"]), r=[], w=[v], dma=True)
        for k in ("triu", "ones"):
            v = cb.view(alloc(128 * 2), [128], BF16)
            self.C[k] = v
            self.op("pool", lambda h, v=v, k=k: h.dma_start(out=v.ap, in_=self.cst[k]), r=[], w=[v], dma=True)
        v = cb.view(alloc(128 * 2), [128], BF16)
        self.C["ident_b"] = v
        self.op("pool", lambda h, v=v: h.dma_start(out=v.ap, in_=self.cst["ident_f"]), r=[], w=[v], dma=True)
        self.cst_off = off

    def layer_norm(self, which, li, scale_in_place=True):
        A = self.Ab
        gt = A.view(0, [D], F32)
        bt = A.view(4096, [D], F32)
        st6 = A.view(8192, [2, 6], F32)
        mv = A.view(8192 + 64, [2], F32)
        rstd = A.view(8192 + 128, [1], F32)
        nmr = A.view(8192 + 192, [1], F32)
        tmp = A.view(9216, [D], F32)
        self.op("sp", lambda h: h.dma_start(out=gt.ap, in_=self.ln_g[which][li:li + 1, :].partition_broadcast(128)),
                r=[], w=[gt], dma=True)
        self.op("sp", lambda h: h.dma_start(out=bt.ap, in_=self.ln_b[which][li:li + 1, :].partition_broadcast(128)),
                r=[], w=[bt], dma=True)
        for i in range(NT):
            Ri = self.R[i]
            for c in range(2):
                self.op("dve", lambda h, c=c, Ri=Ri: h.bn_stats(out=st6.ap[:, c, :], in_=Ri.ap[:, c * 512:(c + 1) * 512]),
                        r=[Ri], w=[st6])
            self.op("dve", lambda h: h.bn_aggr(out=mv.ap, in_=st6.ap.rearrange("p a b -> p (a b)")), r=[st6], w=[mv])
            self.op("act", lambda h: h.activation(out=rstd.ap, in_=mv.ap[:, 1:2], func=AF.Sqrt, bias=LN_EPS, scale=1.0),
                    r=[mv], w=[rstd])
            self.op("dve", lambda h: h.reciprocal(out=rstd.ap, in_=rstd.ap), r=[rstd], w=[rstd])
            self.op("dve", lambda h: h.scalar_tensor_tensor(out=nmr.ap, in0=mv.ap[:, 0:1], scalar=-1.0, in1=rstd.ap, op0=ALU.mult, op1=ALU.mult),
                    r=[mv, rstd], w=[nmr])
            self.op("act", lambda h, Ri=Ri: h.activation(out=tmp.ap, in_=Ri.ap, func=AF.Identity, bias=nmr.ap, scale=rstd.ap), r=[Ri, nmr, rstd], w=[tmp])
            self.op("dve", lambda h: h.tensor_tensor(out=tmp.ap, in0=tmp.ap, in1=gt.ap, op=ALU.mult), r=[tmp, gt], w=[tmp])
            self.op("dve", lambda h, Ri=Ri: h.tensor_tensor(out=Ri.ap, in0=tmp.ap, in1=bt.ap, op=ALU.add), r=[tmp, bt], w=[Ri])
            self.make_copies(i)

    def make_copies(self, i):
            Ri = self.R[i]
            HBi = self.HB[i]
            self.op("act", lambda h, Ri=Ri, HBi=HBi: h.activation(out=HBi.ap, in_=Ri.ap, func=AF.Copy), r=[Ri], w=[HBi])
            pb = 6 + (i % 2)
            pt = V(self.ps[:, pb, :].bitcast(BF16)[:, 0:1024].rearrange("p (a b) -> p a b", a=8), [("ps", pb)])
            for m in range(8):
                self.op("pe", lambda h, m=m, HBi=HBi, pt=pt: h.transpose(out=pt.ap[:, m, :], in_=HBi.ap[:, m * 128:(m + 1) * 128],
                                                                          identity=self.C["ident_b"].ap),
                        r=[HBi, self.C["ident_b"]], w=[pt])
            htv = V(self.HT.ap[:, :, i * 128:(i + 1) * 128], [("HT", g) for g in range(8 * S * 2 // G)])
            self.op("act", lambda h, pt=pt, htv=htv: h.activation(out=htv.ap, in_=pt.ap, func=AF.Copy), r=[pt], w=[htv])


    def mamba(self, li):
        A, HBb, C = self.Ab, self.HBb, self.C
        identf = C["ident_f"].ap
        op = self.op
        ACSF = HBb.view(0, [S], F32, parts=32)
        NBF = HBb.view(8192, [S], F32, parts=32)
        NBH = [HBb.view(16384 + 2048 * i, [512], F32, parts=32) for i in range(2)]
        ONESF = HBb.view(20480, [128], F32)
        PAR = HBb.view(21504, [8], F32, parts=32)
        ACST = HBb.view(24576, [16, 32], F32)
        W2T = HBb.view(26624, [16, 32], F32)
        EACST = HBb.view(28672, [16, 32], F32)
        DECC = HBb.view(30720, [16, 32], F32)
        o = 0

        def alloc(n):
            nonlocal o
            a = o
            o += (n + G - 1) // G * G
            return a
        CWT = A.view(alloc(6 * 4 * 4), [6, 4], F32)
        CBT = A.view(alloc(6 * 4), [6], F32)
        DBC = A.view(alloc(32 * 4), [32], F32)
        NGg = A.view(alloc(512 * 4), [512], F32)
        xf_off = alloc(4 * S * 2)
        XF = A.view(xf_off, [4, S], BF16)
        T1 = A.view(xf_off, [S], F32, parts=32)
        T2 = A.view(xf_off + S * 4, [S], F32, parts=32)
        bf_off = alloc(S * 2)
        BF = A.view(bf_off, [S], BF16)
        WDT = A.view(bf_off, [8, 32], BF16)
        CF = A.view(alloc(S * 2), [S], BF16)
        STATE = A.view(alloc(512 * 4), [512], F32)
        reg = o
        o = reg
        WX = A.view(alloc(8 * 512 * 2), [8, 512], BF16)
        WB = A.view(alloc(8 * 128 * 2), [8, 128], BF16)
        WC = A.view(alloc(8 * 128 * 2), [8, 128], BF16)
        U = A.view(alloc((S + 4) * 4), [S + 4], F32)
        ACC = [A.view(alloc(512 * 4), [512], F32) for _ in range(2)]
        o_conv = o
        o = reg
        WZ = A.view(alloc(8 * 512 * 2), [8, 512], BF16)
        WO = A.view(alloc(4 * D * 2), [4, D], BF16)
        GT4 = A.view(alloc(512 * 4), [4, 128], F32)
        ARG = [A.view(alloc(512 * 4), [512], F32) for _ in range(2)]
        MT = [A.view(alloc(512 * 2), [4, 128], BF16) for _ in range(8)]
        XTOK = A.view(alloc(512 * 2), [512], BF16)
        BTOK = A.view(alloc(128 * 2), [128], BF16)
        PREV = A.view(alloc(512 * 2), [512], BF16)
        xdt_off = alloc(512 * 2)
        XDT2 = A.view(xdt_off, [512], BF16)
        Y1 = A.view(alloc(512 * 4), [512], F32)
        xd_off = alloc(512 * 4)
        XD = A.view(xd_off, [512], F32)
        ZS = A.view(alloc(512 * 2), [512], BF16)
        JUNK = A.view(xd_off, [512], F32)
        SS = A.view(alloc(16), [4], F32)
        YN = A.view(alloc(512 * 2), [512], BF16)
        YNT = A.view(xdt_off, [4, 128], BF16)
        assert max(o, o_conv) <= A.nbytes, (o, o_conv)
        w_in = self.ssm_w_in[li].rearrange("(kc kp) c -> kp kc c", kp=128)

        for i in range(NT):
            Ri = self.R[i]
            op("act", lambda h, Ri=Ri: h.activation(out=Ri.ap, in_=Ri.ap, func=AF.Copy, scale=ALPHA), r=[Ri], w=[Ri])

        op("pool", lambda h: h.dma_start(out=WDT.ap, in_=w_in[:, :, 5120:5152]), r=[], w=[WDT], dma=True)
        op("sp", lambda h: h.dma_start(out=PAR.ap[:, 0:1], in_=self.ssm_dt_bias[li:li + 1, :].rearrange("o h -> h o")), r=[], w=[PAR], dma=True)
        op("sp", lambda h: h.dma_start(out=PAR.ap[:, 1:2], in_=self.ssm_a_log[li:li + 1, :].rearrange("o h -> h o")), r=[], w=[PAR], dma=True)
        op("sp", lambda h: h.dma_start(out=DBC.ap, in_=self.ssm_d[li:li + 1, :].partition_broadcast(128)), r=[], w=[DBC], dma=True)
        op("dve", lambda h: h.memset(ONESF.ap, 1.0), r=[], w=[ONESF])
        for r in range(4):
            pd = self.psb(r % 2, parts=32)
            for k in range(8):
                op("pe", lambda h, k=k, r=r, pd=pd: h.matmul(pd.ap, lhsT=WDT.ap[:, k, :], rhs=self.HT.ap[:, k, r * 512:(r + 1) * 512],
                                                           start=(k == 0), stop=(k == 7)), r=[WDT, self.HT], w=[pd])
            op("act", lambda h, r=r, pd=pd: h.activation(out=T1.ap[:, r * 512:(r + 1) * 512], in_=pd.ap, func=AF.Exp, bias=PAR.ap[:, 0:1], scale=1.0),
               r=[pd, PAR], w=[T1])
        op("act", lambda h: h.activation(out=T1.ap, in_=T1.ap, func=AF.Ln, bias=1.0, scale=1.0), r=[T1], w=[T1])
        op("act", lambda h: h.activation(out=T2.ap, in_=T1.ap, func=AF.Ln), r=[T1], w=[T2])
        op("act", lambda h: h.activation(out=PAR.ap[:, 2:3], in_=PAR.ap[:, 1:2], func=AF.Exp), r=[PAR], w=[PAR])
        op("dve", lambda h: h.tensor_scalar(out=PAR.ap[:, 2:3], in0=PAR.ap[:, 2:3], scalar1=-1.0, scalar2=None, op0=ALU.mult), r=[PAR], w=[PAR])
        op("dve", lambda h: h.tensor_scalar(out=T1.ap, in0=T1.ap, scalar1=PAR.ap[:, 2:3], scalar2=None, op0=ALU.mult), r=[T1, PAR], w=[T1])
        for c in range(16):
            op("dve", lambda h, c=c: h.tensor_tensor_scan(out=ACSF.ap[:, c * 128:(c + 1) * 128], data0=ONESF.ap[0:32, :], data1=T1.ap[:, c * 128:(c + 1) * 128],
                                                         initial=0.0, op0=ALU.mult, op1=ALU.add), r=[ONESF, T1], w=[ACSF])
        op("dve", lambda h: h.tensor_tensor(out=NBF.ap, in0=T2.ap, in1=ACSF.ap, op=ALU.subtract), r=[T2, ACSF], w=[NBF])
        for c in range(16):
            op("act", lambda h, c=c: h.activation(out=T2.ap[:, c * 128:(c + 1) * 128], in_=NBF.ap[:, c * 128:(c + 1) * 128], func=AF.Exp,
                                                  bias=ACSF.ap[:, c * 128 + 127:c * 128 + 128], scale=1.0), r=[NBF, ACSF], w=[T2])
        pa = self.psb(2)
        pw = self.psb(3)
        for c in range(16):
            op("pe", lambda h, c=c: h.transpose(out=pa.ap[:, c * 32:(c + 1) * 32], in_=ACSF.ap[:, c * 128:(c + 1) * 128], identity=identf[0:32, 0:32]),
               r=[ACSF, C["ident_f"]], w=[pa])
            op("pe", lambda h, c=c: h.transpose(out=pw.ap[:, c * 32:(c + 1) * 32], in_=T2.ap[:, c * 128:(c + 1) * 128], identity=identf[0:32, 0:32]),
               r=[T2, C["ident_f"]], w=[pw])
        op("act", lambda h: h.activation(out=ACST.ap.rearrange("p a b -> p (a b)"), in_=pa.ap, func=AF.Copy), r=[pa], w=[ACST])
        op("act", lambda h: h.activation(out=W2T.ap.rearrange("p a b -> p (a b)"), in_=pw.ap, func=AF.Copy), r=[pw], w=[W2T])
        op("act", lambda h: h.activation(out=EACST.ap, in_=ACST.ap, func=AF.Exp), r=[ACST], w=[EACST])
        op("dve", lambda h: h.tensor_scalar(out=Y1.ap, in0=ACST.ap.rearrange("p a b -> p (a b)"), scalar1=identf[:, 127:128], scalar2=None, op0=ALU.mult),
           r=[ACST, C["ident_f"]], w=[Y1])
        pdc = self.psb(0)
        op("pe", lambda h: h.matmul(pdc.ap, lhsT=C["ones_f"].ap, rhs=Y1.ap, start=True, stop=True), r=[C["ones_f"], Y1], w=[pdc])
        op("act", lambda h: h.activation(out=DECC.ap.rearrange("p a b -> p (a b)"), in_=pdc.ap, func=AF.Exp), r=[pdc], w=[DECC])

        if getattr(self, "dbg", None) is not None:
            for q, tv in enumerate((ACST, W2T, EACST, DECC)[3:], 3):
                op("sp", lambda h, q=q, tv=tv: h.dma_start(out=self.dbg[:, q * 512:(q + 1) * 512], in_=tv.ap.rearrange("p a b -> p (a b)")),
                   r=[tv], w=["dbg%d" % q], dma=True)
        for g in range(4):
            op("pool", lambda h, g=g: h.dma_start(out=WX.ap, in_=w_in[:, :, 2048 + g * 512:2048 + (g + 1) * 512]), r=[], w=[WX], dma=True)
            op("pool", lambda h, g=g: h.dma_start(out=WB.ap, in_=w_in[:, :, 4096 + g * 128:4096 + (g + 1) * 128]), r=[], w=[WB], dma=True)
            op("pool", lambda h, g=g: h.dma_start(out=WC.ap, in_=w_in[:, :, 4608 + g * 128:4608 + (g + 1) * 128]), r=[], w=[WC], dma=True)
            op("dve", lambda h: h.memset(U.ap[:, 0:4], 0.0), r=[], w=[U])
            bases = [g * 512 + j * 128 for j in range(4)] + [2048 + g * 128, 2560 + g * 128]
            for j, b0 in enumerate(bases):
                op("sp", lambda h, j=j, b0=b0: h.dma_start(out=CWT.ap[:, j, :], in_=self.ssm_conv_w[li, :, b0:b0 + 128].rearrange("k p -> p k"),
                                                          allow_slow_non_contiguous=True), r=[], w=[CWT], dma=True)
                op("sp", lambda h, j=j, b0=b0: h.dma_start(out=CBT.ap[:, j:j + 1], in_=self.ssm_conv_b[li:li + 1, b0:b0 + 128].rearrange("o p -> p o")),
                   r=[], w=[CBT], dma=True)
            n = 0
            for j in range(6):
                for r in range(4):
                    pc = self.psb(6 + (n % 2))
                    for k in range(8):
                        if j < 4:
                            lw = WX.ap[:, k, j * 128:(j + 1) * 128]
                            wv = WX
                        else:
                            wv = WB if j == 4 else WC
                            lw = wv.ap[:, k, :]
                        op("pe", lambda h, k=k, r=r, lw=lw, pc=pc: h.matmul(pc.ap, lhsT=lw, rhs=self.HT.ap[:, k, r * 512:(r + 1) * 512],
                                                                       start=(k == 0), stop=(k == 7)), r=[wv, self.HT], w=[pc])
                    op("act", lambda h, r=r, pc=pc: h.activation(out=U.ap[:, 4 + r * 512:4 + (r + 1) * 512], in_=pc.ap, func=AF.Copy), r=[pc], w=[U])
                    acc = ACC[n % 2]
                    eng = "dve"
                    op(eng, lambda h, r=r, j=j, acc=acc: h.tensor_scalar(out=acc.ap, in0=U.ap[:, 1 + r * 512:1 + (r + 1) * 512], scalar1=CWT.ap[:, j, 0:1],
                                                                        scalar2=None, op0=ALU.mult), r=[U, CWT], w=[acc])
                    for t in range(1, 4):
                        op(eng, lambda h, r=r, j=j, t=t, acc=acc: h.scalar_tensor_tensor(out=acc.ap, in0=U.ap[:, 1 + t + r * 512:1 + t + (r + 1) * 512],
                                                                                         scalar=CWT.ap[:, j, t:t + 1], in1=acc.ap, op0=ALU.mult, op1=ALU.add),
                           r=[U, CWT, acc], w=[acc])
                    if j < 4:
                        dst, dv = XF.ap[:, j, r * 512:(r + 1) * 512], XF
                    elif j == 4:
                        dst, dv = BF.ap[:, r * 512:(r + 1) * 512], BF
                    else:
                        dst, dv = CF.ap[:, r * 512:(r + 1) * 512], CF
                    op("act", lambda h, j=j, acc=acc, dst=dst: h.activation(out=dst, in_=acc.ap, func=AF.Silu, bias=CBT.ap[:, j:j + 1], scale=1.0),
                       r=[acc, CBT], w=[dv])
                    n += 1
            op("pool", lambda h, g=g: h.dma_start(out=WZ.ap, in_=w_in[:, :, g * 512:(g + 1) * 512]), r=[], w=[WZ], dma=True)
            op("pool", lambda h, g=g: h.dma_start(out=WO.ap, in_=self.ssm_w_out[li, g * 512:(g + 1) * 512, :].rearrange("(c p) d -> p c d", p=128)),
               r=[], w=[WO], dma=True)
            op("sp", lambda h, g=g: h.dma_start(out=NGg.ap, in_=self.ssm_norm_g[li:li + 1, g * 512:(g + 1) * 512].partition_broadcast(128)),
               r=[], w=[NGg], dma=True)
            op("dve", lambda h: h.memset(STATE.ap, 0.0), r=[], w=[STATE])
            hs = slice(8 * g, 8 * g + 8)
            for cb in range(4):
                pgt = self.psb(5)
                for c in range(4):
                    cc = 4 * cb + c
                    op("pe", lambda h, c=c, cc=cc: h.matmul(pgt.ap[:, c * 128:(c + 1) * 128], lhsT=BF.ap[:, cc * 128:(cc + 1) * 128],
                                                           rhs=CF.ap[:, cc * 128:(cc + 1) * 128], start=True, stop=True), r=[BF, CF], w=[pgt])
                op("act", lambda h: h.activation(out=GT4.ap.rearrange("p a b -> p (a b)"), in_=pgt.ap, func=AF.Copy), r=[pgt], w=[GT4])
                for hh in range(8):
                    hd = 8 * g + hh
                    nbh = NBH[hh % 2]
                    op("dve", lambda h, hd=hd, cb=cb, nbh=nbh: h.tensor_scalar(out=nbh.ap, in0=NBF.ap[:, cb * 512:(cb + 1) * 512], scalar1=identf[0:32, hd:hd + 1],
                                                                               scalar2=None, op0=ALU.mult), r=[NBF, C["ident_f"]], w=[nbh])
                    parg = self.psb(4)
                    op("pe", lambda h, hd=hd, cb=cb: h.matmul(parg.ap, lhsT=identf[0:32, hd:hd + 1].broadcast_to([32, 128]), rhs=ACSF.ap[:, cb * 512:(cb + 1) * 512],
                                                              start=True, stop=False), r=[C["ident_f"], ACSF], w=[parg])
                    for c in range(4):
                        op("pe", lambda h, c=c, nbh=nbh: h.matmul(parg.ap[:, c * 128:(c + 1) * 128], lhsT=nbh.ap[:, c * 128:(c + 1) * 128], rhs=ONESF.ap[0:32, :],
                                                                  start=False, stop=(c == 3)), r=[nbh, ONESF], w=[parg])
                    arg = ARG[hh % 2]
                    op("dve", lambda h, arg=arg: h.tensor_tensor(out=arg.ap, in0=parg.ap, in1=C["maskneg"].ap, op=ALU.add), r=[parg, C["maskneg"]], w=[arg])
                    op("act", lambda h, arg=arg: h.activation(out=arg.ap, in_=arg.ap, func=AF.Exp), r=[arg], w=[arg])
                    op("dve", lambda h, arg=arg, hh=hh: h.tensor_tensor(out=MT[hh].ap.rearrange("p a b -> p (a b)"), in0=arg.ap,
                                                                        in1=GT4.ap.rearrange("p a b -> p (a b)"), op=ALU.mult), r=[arg, GT4], w=[MT[hh]])
                for c in range(4):
                    cc = 4 * cb + c
                    tk = slice(cc * 128, (cc + 1) * 128)
                    ptx = V(self.ps[:, 5, :].bitcast(BF16)[:, 0:512], [("ps", 5)])
                    ptb = V(self.ps[:, 5, :].bitcast(BF16)[:, 512:640], [("ps", 5)])
                    for j in range(4):
                        op("pe", lambda h, j=j, tk=tk: h.transpose(out=ptx.ap[:, j * 128:(j + 1) * 128], in_=XF.ap[:, j, tk], identity=C["ident_b"].ap),
                           r=[XF, C["ident_b"]], w=[ptx])
                    op("pe", lambda h, tk=tk: h.transpose(out=ptb.ap, in_=BF.ap[:, tk], identity=C["ident_b"].ap), r=[BF, C["ident_b"]], w=[ptb])
                    op("act", lambda h: h.activation(out=XTOK.ap, in_=ptx.ap, func=AF.Copy), r=[ptx], w=[XTOK])
                    op("act", lambda h: h.activation(out=BTOK.ap, in_=ptb.ap, func=AF.Copy), r=[ptb], w=[BTOK])
                    op("act", lambda h: h.activation(out=PREV.ap, in_=STATE.ap, func=AF.Copy), r=[STATE], w=[PREV])
                    pyd, pyo, pst, pz = self.psb(0), self.psb(1), self.psb(2), self.psb(3)
                    for hh in range(8):
                        op("pe", lambda h, hh=hh, c=c: h.matmul(pyd.ap[:, hh * 64:(hh + 1) * 64], lhsT=MT[hh].ap[:, c, :], rhs=XTOK.ap[:, hh * 64:(hh + 1) * 64],
                                                               start=True, stop=True), r=[MT[hh], XTOK], w=[pyd])
                    op("pe", lambda h, tk=tk: h.matmul(pyo.ap, lhsT=CF.ap[:, tk], rhs=PREV.ap, start=True, stop=True), r=[CF, PREV], w=[pyo])
                    op("dve", lambda h, cc=cc, hs=hs: h.tensor_tensor(out=XDT2.ap.rearrange("p (a b) -> p a b", a=8), in0=XTOK.ap.rearrange("p (a b) -> p a b", a=8),
                                                               in1=W2T.ap[:, cc, hs].unsqueeze(2).broadcast_to([128, 8, 64]), op=ALU.mult), r=[XTOK, W2T], w=[XDT2])
                    op("pe", lambda h: h.matmul(pst.ap, lhsT=BTOK.ap, rhs=XDT2.ap, start=True, stop=True), r=[BTOK, XDT2], w=[pst])
                    op("dve", lambda h, cc=cc, hs=hs: h.tensor_tensor(out=Y1.ap.rearrange("p (a b) -> p a b", a=8), in0=pyo.ap.rearrange("p (a b) -> p a b", a=8),
                                                              in1=EACST.ap[:, cc, hs].unsqueeze(2).broadcast_to([128, 8, 64]), op=ALU.mult), r=[pyo, EACST], w=[Y1])
                    op("dve", lambda h: h.tensor_tensor(out=Y1.ap, in0=Y1.ap, in1=pyd.ap, op=ALU.add), r=[Y1, pyd], w=[Y1])
                    op("dve", lambda h, hs=hs: h.tensor_tensor(out=XD.ap.rearrange("p (a b) -> p a b", a=8), in0=XTOK.ap.rearrange("p (a b) -> p a b", a=8),
                                                        in1=DBC.ap[:, hs].unsqueeze(2).broadcast_to([128, 8, 64]), op=ALU.mult), r=[XTOK, DBC], w=[XD])
                    op("dve", lambda h: h.tensor_tensor(out=Y1.ap, in0=Y1.ap, in1=XD.ap, op=ALU.add), r=[Y1, XD], w=[Y1])
                    op("dve", lambda h, cc=cc, hs=hs: h.tensor_tensor(out=STATE.ap.rearrange("p (a b) -> p a b", a=8), in0=STATE.ap.rearrange("p (a b) -> p a b", a=8),
                                                              in1=DECC.ap[:, cc, hs].unsqueeze(2).broadcast_to([128, 8, 64]), op=ALU.mult), r=[STATE, DECC], w=[STATE])
                    op("dve", lambda h: h.tensor_tensor(out=STATE.ap, in0=STATE.ap, in1=pst.ap, op=ALU.add), r=[STATE, pst], w=[STATE])
                    if getattr(self, "dbg", None) is not None and g == 0 and cc == 0:
                        op("sp", lambda h: h.dma_start(out=self.dbg[:, 0:512], in_=STATE.ap), r=[STATE], w=["dbg0"], dma=True)
                        op("act", lambda h: h.activation(out=ARG[0].ap, in_=XDT2.ap, func=AF.Copy), r=[XDT2], w=[ARG[0]])
                        op("act", lambda h: h.activation(out=ARG[1].ap[:, 0:128], in_=BTOK.ap, func=AF.Copy), r=[BTOK], w=[ARG[1]])
                        op("sp", lambda h: h.dma_start(out=self.dbg[:, 1024:1536], in_=ARG[0].ap), r=[ARG[0]], w=["dbg2"], dma=True)
                        op("sp", lambda h: h.dma_start(out=self.dbg[:, 512:640], in_=ARG[1].ap[:, 0:128]), r=[ARG[1]], w=["dbg1"], dma=True)
                    for k in range(8):
                        op("pe", lambda h, k=k, tk=tk: h.matmul(pz.ap, lhsT=self.HT.ap[:, k, tk], rhs=WZ.ap[:, k, :], start=(k == 0), stop=(k == 7)),
                           r=[self.HT, WZ], w=[pz])
                    op("act", lambda h: h.activation(out=ZS.ap, in_=pz.ap, func=AF.Silu), r=[pz], w=[ZS])
                    op("dve", lambda h: h.tensor_tensor(out=Y1.ap, in0=Y1.ap, in1=ZS.ap, op=ALU.mult), r=[Y1, ZS], w=[Y1])
                    op("act", lambda h: h.activation(out=JUNK.ap, in_=Y1.ap, func=AF.Square, accum_out=SS.ap[:, 0:1]), r=[Y1], w=[JUNK, SS])
                    op("act", lambda h: h.activation(out=SS.ap[:, 1:2], in_=SS.ap[:, 0:1], func=AF.Sqrt, bias=1e-5, scale=1.0 / 512.0), r=[SS], w=[SS])
                    op("dve", lambda h: h.reciprocal(out=SS.ap[:, 2:3], in_=SS.ap[:, 1:2]), r=[SS], w=[SS])
                    op("dve", lambda h: h.scalar_tensor_tensor(out=YN.ap, in0=Y1.ap, scalar=SS.ap[:, 2:3], in1=NGg.ap, op0=ALU.mult, op1=ALU.mult),
                       r=[Y1, SS, NGg], w=[YN])
                    pty = V(self.ps[:, 5, :].bitcast(BF16)[:, 0:512].rearrange("p (a b) -> p a b", a=4), [("ps", 5)])
                    for j in range(4):
                        op("pe", lambda h, j=j: h.transpose(out=pty.ap[:, j, :], in_=YN.ap[:, j * 128:(j + 1) * 128], identity=C["ident_b"].ap),
                           r=[YN, C["ident_b"]], w=[pty])
                    op("act", lambda h: h.activation(out=YNT.ap, in_=pty.ap, func=AF.Copy), r=[pty], w=[YNT])
                    Rc = self.R[cc]
                    for dr in range(2):
                        pm = self.psb(6 + dr)
                        for j in range(4):
                            op("pe", lambda h, j=j, dr=dr, pm=pm: h.matmul(pm.ap, lhsT=YNT.ap[:, j, :], rhs=WO.ap[:, j, dr * 512:(dr + 1) * 512],
                                                                        start=(j == 0), stop=(j == 3)), r=[YNT, WO], w=[pm])
                        op("dve", lambda h, dr=dr, pm=pm, Rc=Rc: h.tensor_tensor(out=Rc.ap[:, dr * 512:(dr + 1) * 512], in0=Rc.ap[:, dr * 512:(dr + 1) * 512],
                                                                                in1=pm.ap, op=ALU.add), r=[Rc, pm], w=[Rc])


    def attention(self, l, li):
        A, HBb, C = self.Ab, self.HBb, self.C
        op = self.op
        ja = self.attn_layers.index(l)
        build_kv = (ja == 0)
        KT = HBb.view(0, [8, S], BF16)
        o = 0

        def alloc(n):
            nonlocal o
            a = o
            o += (n + G - 1) // G * G
            return a
        VA = A.view(alloc(16 * 16 * 66 * 2), [16, 16, 66], BF16)
        VAF = A.view(0, [16, 16, 33], F32)
        KTF = HBb.view(0, [8 * S // 2], F32)
        KMB = A.view(alloc(8 * 8 * 2), [8, 8], BF16)
        W1R = A.view(alloc(8 * 128 * 2), [8, 128], BF16)
        TRI = A.view(alloc(256), [128], BF16)
        PADM = A.view(alloc(256), [8, 8], F32)
        WSEL = A.view(alloc(16 * 2 * 8 * 4), [16, 2, 8], F32)
        GSC = A.view(alloc(1024), [256], F32)
        W1 = A.view(alloc(8 * 512 * 2), [8, 512], BF16)
        WOp = A.view(alloc(D * 2), [D], BF16)
        QTp = A.view(alloc(S * 2), [S], BF16)
        OTp = A.view(alloc(S * 2), [S], BF16)
        CS = A.view(alloc(512 * 4), [512], F32)
        SN = A.view(alloc(512 * 4), [512], F32)
        T1 = A.view(alloc(512 * 4), [512], F32)
        T2 = A.view(alloc(512 * 4), [512], F32)
        PT = [A.view(alloc(512 * 2), [512], BF16) for _ in range(2)]
        ACC = A.view(alloc(4 * 65 * 4), [4, 65], F32)
        OTOK = A.view(alloc(4 * 128 * 2), [4, 128], BF16)
        assert o <= A.nbytes, o

        for i in range(NT):
            Ri = self.R[i]
            op("act", lambda h, Ri=Ri: h.activation(out=Ri.ap, in_=Ri.ap, func=AF.Copy, scale=ALPHA), r=[Ri], w=[Ri])
        op("pool", lambda h: h.dma_start(out=TRI.ap, in_=self.cst["tri"]), r=[], w=[TRI], dma=True)
        op("sp", lambda h: h.dma_start(out=PADM.ap.rearrange("p a b -> p (a b)"), in_=self.cst["padm"]), r=[], w=[PADM], dma=True)

        def make_rot_weights(pp):
            op("dve", lambda h: h.memset(W1R.ap, 0.0), r=[], w=[W1R])
            for hl in range(2):
                b0 = pp * 128 + 64 * hl
                op("act", lambda h, hl=hl, b0=b0: h.activation(out=W1R.ap[:, :, 64 * hl:64 * hl + 8], in_=W1.ap[:, :, b0 + 8:b0 + 16], func=AF.Copy, scale=-1.0),
                   r=[W1], w=[W1R])
                op("act", lambda h, hl=hl, b0=b0: h.activation(out=W1R.ap[:, :, 64 * hl + 8:64 * hl + 16], in_=W1.ap[:, :, b0:b0 + 8], func=AF.Copy),
                   r=[W1], w=[W1R])

        def rope(pk, dst, dv, r):
            prot = self.psb(6)
            for k in range(8):
                op("pe", lambda h, k=k, r=r, prot=prot: h.matmul(prot.ap, lhsT=W1R.ap[:, k, :], rhs=self.HT.ap[:, k, r * 512:(r + 1) * 512],
                                                              start=(k == 0), stop=(k == 7)), r=[W1R, self.HT], w=[prot])
            op("sp", lambda h, r=r: h.dma_start(out=CS.ap, in_=self.cst["cosf"][:, r * 512:(r + 1) * 512]), r=[], w=[CS], dma=True)
            op("sp", lambda h, r=r: h.dma_start(out=SN.ap, in_=self.cst["sinf"][:, r * 512:(r + 1) * 512]), r=[], w=[SN], dma=True)
            op("dve", lambda h, pk=pk: h.tensor_tensor(out=T1.ap, in0=pk.ap, in1=CS.ap, op=ALU.mult), r=[pk, CS], w=[T1])
            op("dve", lambda h, prot=prot: h.tensor_tensor(out=T2.ap, in0=prot.ap, in1=SN.ap, op=ALU.mult), r=[prot, SN], w=[T2])
            op("dve", lambda h, dst=dst: h.tensor_tensor(out=dst, in0=T1.ap, in1=T2.ap, op=ALU.add), r=[T1, T2], w=[dv])

        def wsrc(w2d, half):
            return w2d.rearrange("(kc kp) c -> kp kc c", kp=128)[:, :, half * 512:(half + 1) * 512]

        def kmeans():
            for p in range(8):
                op("dve", lambda h, p=p: h.tensor_reduce(out=GSC.ap[:, 0:8], in_=KT.ap[:, p, :].rearrange("p (a b) -> p a b", a=8), axis=AX.X, op=ALU.add),
                   r=[KT], w=[GSC])
                op("dve", lambda h, p=p: h.tensor_scalar(out=KMB.ap[:, p, :], in0=GSC.ap[:, 0:8], scalar1=1.0 / 256.0, scalar2=None, op0=ALU.mult),
                   r=[GSC], w=[KMB])

        stop = getattr(self, "attn_stop", 0)
        if stop == -1:
            return
        if build_kv:
            for half in range(2):
                if stop == -2 and half == 1:
                    return
                op("pool", lambda h, half=half: h.dma_start(out=W1.ap, in_=wsrc(self.kv_w_k, half)), r=[], w=[W1], dma=True)
                for pp in range(4):
                    p = half * 4 + pp
                    make_rot_weights(pp)
                    for r in range(4):
                        pk = self.psb(4 + r % 2)
                        for k in range(8):
                            op("pe", lambda h, k=k, r=r, pp=pp, pk=pk: h.matmul(pk.ap, lhsT=W1.ap[:, k, pp * 128:(pp + 1) * 128],
                                                                             rhs=self.HT.ap[:, k, r * 512:(r + 1) * 512], start=(k == 0), stop=(k == 7)),
                               r=[W1, self.HT], w=[pk])
                        rope(pk, KT.ap[:, p, r * 512:(r + 1) * 512], KT, r)
            for half in range(2):
                op("pool", lambda h, half=half: h.dma_start(out=W1.ap, in_=wsrc(self.kv_w_v, half)), r=[], w=[W1], dma=True)
                for i in range(NT):
                    pv = self.psb(4 + i % 2)
                    for k in range(8):
                        op("pe", lambda h, k=k, i=i, pv=pv: h.matmul(pv.ap, lhsT=self.HT.ap[:, k, i * 128:(i + 1) * 128], rhs=W1.ap[:, k, :],
                                                                 start=(k == 0), stop=(k == 7)), r=[self.HT, W1], w=[pv])
                    op("act", lambda h, i=i, half=half, pv=pv: h.activation(out=VA.ap[:, i, half * 8:(half + 1) * 8, 0:64],
                                                                          in_=pv.ap.rearrange("p (a b) -> p a b", a=8), func=AF.Copy), r=[pv], w=[VA])
            if stop == -3:
                return
            op("dve", lambda h: h.memset(VA.ap[:, :, :, 64:65], 1.0), r=[], w=[VA])
            if stop == -4:
                return
            kmeans()
            if not getattr(self, "no_spill", False):
                op("sp", lambda h: h.dma_start(out=self.kt_s, in_=KTF.ap), r=[KT], w=["kt_s"], dma=True)
                for kt in range(16):
                    op("sp", lambda h, kt=kt: h.dma_start(out=self.va_s[:, kt * 512:(kt + 1) * 512].rearrange("p (b c) -> p b c", c=32), in_=VAF.ap[:, kt, :, 0:32]),
                       r=[VA], w=["va_s"], dma=True)
        else:
            op("sp", lambda h: h.dma_start(out=KTF.ap, in_=self.kt_s), r=["kt_s"], w=[KT], dma=True)
            for kt in range(16):
                op("sp", lambda h, kt=kt: h.dma_start(out=VAF.ap[:, kt, :, 0:32], in_=self.va_s[:, kt * 512:(kt + 1) * 512].rearrange("p (b c) -> p b c", c=32)),
                   r=["va_s"], w=[VA], dma=True)
            op("dve", lambda h: h.memset(VA.ap[:, :, :, 64:65], 1.0), r=[], w=[VA])
            kmeans()
        wq = self.attn_w_q[ja]
        stop = getattr(self, "attn_stop", 0)
        if stop == 1:
            return
        for p in range(8 if stop == 0 else 1):
            half, pp = divmod(p, 4)
            if pp == 0:
                op("pool", lambda h, half=half: h.dma_start(out=W1.ap, in_=wsrc(wq, half)), r=[], w=[W1], dma=True)
            op("pool", lambda h, p=p: h.dma_start(out=WOp.ap, in_=self.attn_w_o[ja, p * 128:(p + 1) * 128, :]), r=[], w=[WOp], dma=True)
            make_rot_weights(pp)
            for r in range(4):
                pq = self.psb(4 + r % 2)
                for k in range(8):
                    op("pe", lambda h, k=k, r=r, pp=pp, pq=pq: h.matmul(pq.ap, lhsT=W1.ap[:, k, pp * 128:(pp + 1) * 128],
                                                                     rhs=self.HT.ap[:, k, r * 512:(r + 1) * 512], start=(k == 0), stop=(k == 7)),
                       r=[W1, self.HT], w=[pq])
                rope(pq, QTp.ap[:, r * 512:(r + 1) * 512], QTp, r)
            for hl in range(2):
                rows = slice(64 * hl, 64 * hl + 64)
                for i in range(8, NT):
                    cblk = i // 2
                    pg = self.psb(6, cols=8)
                    op("pe", lambda h, i=i, rows=rows, p=p, pg=pg: h.matmul(pg.ap, lhsT=QTp.ap[rows, i * 128:(i + 1) * 128], rhs=KMB.ap[rows, p, :],
                                                                       start=True, stop=True), r=[QTp, KMB], w=[pg])
                    op("dve", lambda h, cblk=cblk, pg=pg: h.tensor_tensor(out=GSC.ap[:, 0:8], in0=pg.ap, in1=PADM.ap[:, cblk, :], op=ALU.add),
                       r=[pg, PADM], w=[GSC])
                    op("dve", lambda h: h.max(out=GSC.ap[:, 8:16], in_=GSC.ap[:, 0:8]), r=[GSC], w=[GSC])
                    op("dve", lambda h, i=i, hl=hl: h.tensor_scalar(out=WSEL.ap[:, i, hl, :], in0=GSC.ap[:, 0:8], scalar1=GSC.ap[:, 10:11], scalar2=None,
                                                                   op0=ALU.is_ge), r=[GSC], w=[WSEL])
            nst = 0
            if stop == 2:
                return
            for r in range(4):
                for hl in range(2):
                    rows = slice(64 * hl, 64 * hl + 64)
                    hd = 2 * p + hl
                    for n in range(2 * r + 2):
                        jqs = [jq for jq in range(4) if (4 * r + jq) // 2 >= n]
                        c0 = jqs[0] * 128
                        ncol = 512 - c0
                        pob = self.psb(2 + n % 2)
                        pts = {}
                        for kt in (2 * n, 2 * n + 1):
                            use = [jq for jq in jqs if kt <= 4 * r + jq]
                            if not use:
                                continue
                            pss = self.psb(nst % 2)
                            pt = PT[nst % 2]
                            nst += 1
                            pts[kt] = pt
                            op("pe", lambda h, kt=kt, rows=rows, r=r, c0=c0, ncol=ncol, pss=pss, p=p: h.matmul(
                                pss.ap[:, c0:c0 + ncol], lhsT=KT.ap[rows, p, kt * 128:(kt + 1) * 128], rhs=QTp.ap[rows, r * 512 + c0:(r + 1) * 512],
                                start=True, stop=True), r=[KT, QTp], w=[pss])
                            op("act", lambda h, c0=c0, ncol=ncol, pss=pss, pt=pt: h.activation(out=pt.ap[:, c0:c0 + ncol], in_=pss.ap[:, c0:c0 + ncol],
                                                                                           func=AF.Exp, scale=0.125), r=[pss], w=[pt])
                            for jq in use:
                                if kt == 4 * r + jq:
                                    op("dve", lambda h, jq=jq, pt=pt: h.tensor_tensor(out=pt.ap[:, jq * 128:(jq + 1) * 128], in0=pt.ap[:, jq * 128:(jq + 1) * 128],
                                                                                 in1=TRI.ap, op=ALU.mult), r=[pt, TRI], w=[pt])
                        for jq in jqs:
                            kts = [kt for kt in (2 * n, 2 * n + 1) if kt <= 4 * r + jq]
                            for kt in kts:
                                pt = pts[kt]
                                op("pe", lambda h, jq=jq, kt=kt, hd=hd, pt=pt, pob=pob, st=(kt == kts[0]), last=(kt == kts[-1]): h.matmul(
                                    pob.ap[:, jq * 65:(jq + 1) * 65], lhsT=pt.ap[:, jq * 128:(jq + 1) * 128], rhs=VA.ap[:, kt, hd, 0:65], start=st, stop=last),
                                    r=[pt, VA], w=[pob])
                        for jq in jqs:
                            i = 4 * r + jq
                            cblk = i // 2
                            if n == cblk or i < 8:
                                wgt = 1.0
                                rd = [pob]
                            else:
                                wgt = WSEL.ap[:, i, hl, n:n + 1]
                                rd = [pob, WSEL]
                            if n == 0:
                                op("dve", lambda h, jq=jq, wgt=wgt, pob=pob: h.tensor_scalar(out=ACC.ap[:, jq, :], in0=pob.ap[:, jq * 65:(jq + 1) * 65], scalar1=wgt,
                                                                                        scalar2=None, op0=ALU.mult), r=rd, w=[ACC])
                            else:
                                op("dve", lambda h, jq=jq, wgt=wgt, pob=pob: h.scalar_tensor_tensor(out=ACC.ap[:, jq, :], in0=pob.ap[:, jq * 65:(jq + 1) * 65], scalar=wgt,
                                                                                               in1=ACC.ap[:, jq, :], op0=ALU.mult, op1=ALU.add), r=rd + [ACC], w=[ACC])
                    for jq in range(4):
                        op("dve", lambda h, jq=jq: h.reciprocal(out=GSC.ap[:, 32 + jq:33 + jq], in_=ACC.ap[:, jq, 64:65]), r=[ACC], w=[GSC])
                        op("dve", lambda h, jq=jq, hl=hl: h.tensor_scalar(out=OTOK.ap[:, jq, 64 * hl:64 * hl + 64], in0=ACC.ap[:, jq, 0:64],
                                                                         scalar1=GSC.ap[:, 32 + jq:33 + jq], scalar2=None, op0=ALU.mult), r=[ACC, GSC], w=[OTOK])
                ptt = V(self.ps[:, 7, :].bitcast(BF16)[:, 0:512], [("ps", 7)])
                for jq in range(4):
                    op("pe", lambda h, jq=jq: h.transpose(out=ptt.ap[:, jq * 128:(jq + 1) * 128], in_=OTOK.ap[:, jq, :], identity=C["ident_b"].ap),
                       r=[OTOK, C["ident_b"]], w=[ptt])
                op("act", lambda h, r=r: h.activation(out=OTp.ap[:, r * 512:(r + 1) * 512], in_=ptt.ap, func=AF.Copy), r=[ptt], w=[OTp])
            for i in range(NT):
                Ri = self.R[i]
                for dr in range(2):
                    pm = self.psb(4 + dr)
                    op("pe", lambda h, i=i, dr=dr, pm=pm: h.matmul(pm.ap, lhsT=OTp.ap[:, i * 128:(i + 1) * 128], rhs=WOp.ap[:, dr * 512:(dr + 1) * 512],
                                                              start=True, stop=True), r=[OTp, WOp], w=[pm])
                    op("dve", lambda h, dr=dr, pm=pm, Ri=Ri: h.tensor_tensor(out=Ri.ap[:, dr * 512:(dr + 1) * 512], in0=Ri.ap[:, dr * 512:(dr + 1) * 512],
                                                                            in1=pm.ap, op=ALU.add), r=[Ri, pm], w=[Ri])

    def moe(self, li):
        A = self.Ab
        C = self.C
        o = 0

        def alloc(n):
            nonlocal o
            a = o
            o += (n + G - 1) // G * G
            return a
        WR = A.view(alloc(8 * NE * 2), [8, NE], BF16)
        BRT = A.view(alloc(NE * 4), [NE], F32)
        RG = A.view(alloc(NT * 64 * 4), [NT, 64], F32)
        RGB = A.view(alloc(NT * 64 * 2), [NT, 64], BF16)
        RGT = A.view(alloc(S * 2), [S], BF16, parts=64)
        MSK = A.view(alloc(NT * NE * 2), [NT, NE], BF16)
        CM = A.view(alloc(NT * NE * 2), [NT, NE], BF16)
        SM = A.view(alloc(1024), [256], F32)
        BGU = A.view(alloc(NE * 16 * 4), [NE, 16], F32)
        BDN = [A.view(alloc(D * 2), [D], BF16, parts=1)] * 2
        NRING = 4
        ring_offs = [alloc(8 * 256 * 2) for _ in range(NRING)]
        assert all(ring_offs[k + 1] - ring_offs[k] == 4096 for k in range(NRING - 1))
        RING = [A.view(o_, [8, 256], BF16) for o_ in ring_offs]
        d_slot0 = 8 % NRING
        assert 12 % NRING == 0 and d_slot0 % 2 == 0 and d_slot0 + 3 < NRING + 0 * 1 or NRING == 4
        RING2 = [A.view(ring_offs[(d_slot0 + 2 * j) % NRING], [2, 8, 256], BF16) for j in range(2)]
        xt_off = alloc(8 * CAP * 2)
        XT = A.view(xt_off, [8, CAP], BF16)
        ACTT = A.view(alloc(8 * CAP * 2), [8, CAP], BF16)
        xtb_off = alloc(8 * CAP * 2)
        XTs = [XT, A.view(xtb_off, [8, CAP], BF16)]
        YBs = [A.view(xt_off, [NJT, D], BF16), A.view(xtb_off, [NJT, D], BF16)]
        TG = [A.view(alloc(CAP * 4), [CAP], F32) for _ in range(2)]
        TS = [A.view(alloc(CAP * 2), [CAP], BF16) for _ in range(2)]
        TU = [A.view(alloc(CAP * 4), [CAP], F32) for _ in range(2)]
        GBS = [A.view(alloc(512 * 2), [512], BF16)] * 2
        assert o <= A.nbytes, o
        self.arena_used = o
        Sg = [self.HTb.view(i * 1024, [128], BF16) for i in range(NT)]
        base = NT * 1024
        STw = [self.HTb.view(base + w * 1024, [512], BF16) for w in range(4)]

        wr_src = self.w_router[li].rearrange("(kc kp) e -> kp kc e", kp=128)
        self.op("pool", lambda h: h.dma_start(out=WR.ap, in_=wr_src), r=[], w=[WR], dma=True)
        self.op("sp", lambda h: h.dma_start(out=BRT.ap, in_=self.b_router[li:li + 1, :].partition_broadcast(128)), r=[], w=[BRT], dma=True)
        bgu_src = self.b_gu[li].rearrange("e (c p) -> (e c) p", p=128).rearrange("(t r) p -> r t p", r=128)
        self.op("sp", lambda h: h.dma_start(out=TG[0].ap.rearrange("p (t q) -> p t q", t=4), in_=bgu_src), r=[], w=[TG[0]], dma=True)
        pbg = self.psb(0)
        for t in range(4):
            self.op("pe", lambda h, t=t: h.transpose(out=pbg.ap[:, t * 128:(t + 1) * 128], in_=TG[0].ap[:, t * 128:(t + 1) * 128], identity=C["ident_f"].ap),
                    r=[TG[0], C["ident_f"]], w=[pbg])
        self.op("act", lambda h: h.activation(out=BGU.ap.rearrange("p e c -> p (e c)"), in_=pbg.ap, func=AF.Copy), r=[pbg], w=[BGU])

        pieces = []
        for e in range(NE):
            for p in range(4):
                pieces.append((e, "g", p))
                pieces.append((e, "u", p))
            for q in range(4):
                pieces.append((e, "d", q))
        self._ring_state = {"next": 0}
        piece_slot = {}

        def issue_piece():
            n = self._ring_state["next"]
            if n >= len(pieces):
                return
            e, kind, p = pieces[n]
            slot = RING[n % NRING]
            piece_slot[(e, kind, p)] = slot
            if kind == "d":
                src = self.w_dn[li, e].rearrange("(kc kp) d -> kp kc d", kp=128)[:, :, p * 256:(p + 1) * 256]
            else:
                c0 = p * 256 + (D if kind == "u" else 0)
                src = self.w_gu[li, e].rearrange("(kc kp) f -> kp kc f", kp=128)[:, :, c0:c0 + 256]
            self.op("pool", lambda h, slot=slot, src=src: h.dma_start(out=slot.ap, in_=src), r=[], w=[slot], dma=True)
            self._ring_state["next"] = n + 1

        for _ in range(NRING):
            issue_piece()

        scr = SM.ap
        for i in range(NT):
            pl = self.psb(4 + (i % 2), cols=NE)
            for k in range(8):
                self.op("pe", lambda h, k=k, i=i, pl=pl: h.matmul(pl.ap, lhsT=self.HT.ap[:, k, i * 128:(i + 1) * 128], rhs=WR.ap[:, k, :],
                                                               start=(k == 0), stop=(k == 7)), r=[self.HT, WR], w=[pl])
            lg = V(scr[:, 0:32], SM.keys)
            m8 = V(scr[:, 32:40], SM.keys)
            nm = V(scr[:, 40:41], SM.keys)
            mk = V(scr[:, 48:80], SM.keys)
            ex = V(scr[:, 80:112], SM.keys)
            sm = V(scr[:, 112:113], SM.keys)
            rk = V(scr[:, 128:160], SM.keys)
            self.op("dve", lambda h, pl=pl: h.tensor_tensor(out=lg.ap, in0=pl.ap, in1=BRT.ap, op=ALU.add), r=[pl, BRT], w=[SM])
            self.op("dve", lambda h: h.max(out=m8.ap, in_=lg.ap), r=[SM], w=[SM])
            self.op("dve", lambda h: h.tensor_scalar(out=mk.ap, in0=lg.ap, scalar1=m8.ap[:, 3:4], scalar2=None, op0=ALU.is_ge), r=[SM], w=[SM])
            self.op("dve", lambda h: h.tensor_scalar(out=nm.ap, in0=m8.ap[:, 0:1], scalar1=-1.0, scalar2=None, op0=ALU.mult), r=[SM], w=[SM])
            self.op("act", lambda h: h.activation(out=ex.ap, in_=lg.ap, func=AF.Exp, bias=nm.ap, scale=1.0), r=[SM], w=[SM])
            self.op("dve", lambda h: h.tensor_tensor(out=ex.ap, in0=ex.ap, in1=mk.ap, op=ALU.mult), r=[SM], w=[SM])
            self.op("dve", lambda h: h.tensor_reduce(out=sm.ap, in_=ex.ap, axis=AX.X, op=ALU.add), r=[SM], w=[SM])
            self.op("dve", lambda h: h.reciprocal(out=sm.ap, in_=sm.ap), r=[SM], w=[SM])
            self.op("dve", lambda h, i=i: h.tensor_scalar(out=RG.ap[:, i, 32:64], in0=ex.ap, scalar1=sm.ap, scalar2=None, op0=ALU.mult), r=[SM], w=[RG])
            self.op("dve", lambda h, i=i: h.tensor_copy(out=MSK.ap[:, i, :], in_=mk.ap), r=[SM], w=[MSK])
            if i % 4 == 0:
                self.op("dve", lambda h, i=i: h.memset(CM.ap[:, i, :], 0.0), r=[], w=[CM])
            else:
                self.op("dve", lambda h, i=i: h.tensor_tensor(out=CM.ap[:, i, :], in0=CM.ap[:, i - 1, :], in1=MSK.ap[:, i - 1, :], op=ALU.add),
                        r=[CM, MSK], w=[CM])
            pr = self.psb(4 + (i % 2), cols=NE, c0=64)
            self.op("pe", lambda h, i=i, pr=pr: h.matmul(pr.ap, lhsT=C["ones"].ap, rhs=CM.ap[:, i, :], start=True, stop=False), r=[C["ones"], CM], w=[pr])
            self.op("pe", lambda h, i=i, pr=pr: h.matmul(pr.ap, lhsT=C["triu"].ap, rhs=MSK.ap[:, i, :], start=False, stop=True), r=[C["triu"], MSK], w=[pr])
            self.op("dve", lambda h, pr=pr: h.scalar_tensor_tensor(out=rk.ap, in0=pr.ap, scalar=1.0, in1=mk.ap, op0=ALU.add, op1=ALU.mult), r=[pr, SM], w=[SM])
            self.op("dve", lambda h, i=i: h.tensor_scalar(out=RG.ap[:, i, 0:32], in0=rk.ap, scalar1=-1.0, scalar2=None, op0=ALU.add), r=[SM], w=[RG])
            self.op("dve", lambda h, i=i: h.tensor_copy(out=RGB.ap[:, i, :], in_=RG.ap[:, i, :]), r=[RG], w=[RGB])
            ptr = V(self.ps[0:64, 4 + (i % 2), :].bitcast(BF16)[:, 256:384], [("ps", 4 + (i % 2))])
            self.op("pe", lambda h, i=i, ptr=ptr: h.transpose(out=ptr.ap, in_=RGB.ap[:, i, :], identity=C["ident_b"].ap), r=[RGB, C["ident_b"]], w=[ptr])
            self.op("act", lambda h, i=i, ptr=ptr: h.activation(out=RGT.ap[:, i * 128:(i + 1) * 128], in_=ptr.ap, func=AF.Copy), r=[ptr], w=[RGT])

        for i in range(NT):
            Ri = self.R[i]
            self.op("act", lambda h, Ri=Ri: h.activation(out=Ri.ap, in_=Ri.ap, func=AF.Copy, scale=ALPHA), r=[Ri], w=[Ri])

        identf = C["ident_f"].ap
        identb = C["ident_b"].ap
        Sg2 = [self.HTb.view(i * 1024, [256], BF16) for i in range(NT)]

        def build_S(e0):
            for i in range(NT):
                for e2 in range(2):
                    self.op("dve", lambda h, i=i, e2=e2, e0=e0: h.tensor_scalar(out=Sg2[i].ap[:, e2 * 128:(e2 + 1) * 128], in0=C["iota_row"].ap[:, 0:128],
                                                                              scalar1=RG.ap[:, i, e0 + e2:e0 + e2 + 1], scalar2=None, op0=ALU.is_equal),
                            r=[C["iota_row"], RG], w=[Sg2[i]])

        def gather_pair():
            for m in range(8):
                banks = (0, 1) if m % 2 == 0 else (2, 3)
                for half in range(2):
                    px = self.psb(banks[half])
                    for wl in range(2):
                        for ii in range(4):
                            i = 4 * (2 * half + wl) + ii
                            self.op("pe", lambda h, m=m, i=i, wl=wl, ii=ii, px=px: h.matmul(px.ap[:, wl * 256:(wl + 1) * 256], lhsT=self.HB[i].ap[:, m * 128:(m + 1) * 128],
                                                                                       rhs=Sg2[i].ap, start=(ii == 0), stop=(ii == 3)), r=[self.HB[i], Sg2[i]], w=[px])
                    for e2 in range(2):
                        self.op("act", lambda h, m=m, half=half, e2=e2, px=px: h.activation(
                            out=XTs[e2].ap[:, m, half * 256:(half + 1) * 256].rearrange("p (a b) -> p a b", a=2),
                            in_=px.ap.rearrange("p (a e b) -> p a e b", a=2, e=2)[:, :, e2, :], func=AF.Copy), r=[px], w=[XTs[e2]])

        build_S(0)
        for e in range(NE):
            bdn = BDN[e % 2]
            self.op("pool", lambda h, e=e, bdn=bdn: h.dma_start(out=bdn.ap, in_=self.b_dn[li, e:e + 1, :]), r=[], w=[bdn], dma=True)
            if e % 2 == 0:
                gather_pair()
            XT = XTs[e % 2]
            YB = YBs[e % 2]
            for p in range(4):
                wg = piece_slot[(e, "g", p)]
                wu = piece_slot[(e, "u", p)]
                for sub in range(2):
                    c = 2 * p + sub
                    pg = self.psb(4 + (c % 2))
                    pu = self.psb(6 + (c % 2))
                    for k in range(8):
                        self.op("pe", lambda h, k=k, sub=sub, wg=wg, pg=pg, XT=XT: h.matmul(pg.ap, lhsT=wg.ap[:, k, sub * 128:(sub + 1) * 128], rhs=XT.ap[:, k, :],
                                                                                 start=(k == 0), stop=(k == 7)), r=[wg, XT], w=[pg])
                    for k in range(8):
                        self.op("pe", lambda h, k=k, sub=sub, wu=wu, pu=pu, XT=XT: h.matmul(pu.ap, lhsT=wu.ap[:, k, sub * 128:(sub + 1) * 128], rhs=XT.ap[:, k, :],
                                                                                 start=(k == 0), stop=(k == 7)), r=[wu, XT], w=[pu])
                    tg, ts, tu = TG[c % 2], TS[c % 2], TU[c % 2]
                    self.op("dve", lambda h, pg=pg, tg=tg, e=e, c=c: h.tensor_scalar(out=tg.ap, in0=pg.ap, scalar1=BGU.ap[:, e, c:c + 1], scalar2=7.0,
                                                                                 op0=ALU.add, op1=ALU.min), r=[pg, BGU], w=[tg])
                    self.op("act", lambda h, tg=tg, ts=ts: h.activation(out=ts.ap, in_=tg.ap, func=AF.Sigmoid, scale=1.702), r=[tg], w=[ts])
                    self.op("act", lambda h, pu=pu, tu=tu, e=e, c=c: h.activation(out=tu.ap, in_=pu.ap, func=AF.Identity, bias=BGU.ap[:, e, 8 + c:9 + c], scale=1.0),
                            r=[pu, BGU], w=[tu])
                    self.op("dve", lambda h, tu=tu: h.tensor_scalar(out=tu.ap, in0=tu.ap, scalar1=-7.0, scalar2=7.0, op0=ALU.max, op1=ALU.min), r=[tu], w=[tu])
                    self.op("dve", lambda h, tg=tg, ts=ts: h.tensor_tensor(out=tg.ap, in0=tg.ap, in1=ts.ap, op=ALU.mult), r=[tg, ts], w=[tg])
                    self.op("dve", lambda h, tg=tg, tu=tu, c=c: h.scalar_tensor_tensor(out=ACTT.ap[:, c, :], in0=tu.ap, scalar=1.0, in1=tg.ap, op0=ALU.add, op1=ALU.mult),
                            r=[tg, tu], w=[ACTT])
                issue_piece()
                issue_piece()
            for r in range(4):
                prb = self.psb(2)
                pgb = self.psb(3)
                self.op("pe", lambda h, r=r, e=e, prb=prb: h.matmul(prb.ap, lhsT=identb[0:64, e:e + 1].broadcast_to([64, 128]),
                                                                    rhs=RGT.ap[:, r * 512:(r + 1) * 512], start=True, stop=True),
                        r=[C["ident_b"], RGT], w=[prb])
                self.op("pe", lambda h, r=r, e=e, pgb=pgb: h.matmul(pgb.ap, lhsT=identb[0:64, 32 + e:33 + e].broadcast_to([64, 128]),
                                                                    rhs=RGT.ap[:, r * 512:(r + 1) * 512], start=True, stop=True),
                        r=[C["ident_b"], RGT], w=[pgb])
                gbs = GBS[r % 2]
                self.op("act", lambda h, pgb=pgb, gbs=gbs: h.activation(out=gbs.ap, in_=pgb.ap, func=AF.Copy), r=[pgb], w=[gbs])
                self.op("dve", lambda h, r=r, prb=prb, gbs=gbs: h.scalar_tensor_tensor(
                    out=STw[r].ap, in0=prb.ap, scalar=C["piota"].ap[:, 0:1], in1=gbs.ap, op0=ALU.is_equal, op1=ALU.mult),
                    r=[prb, gbs, C["piota"]], w=[STw[r]])
            if e % 2 == 0 and e + 2 < NE:
                build_S(e + 2)
            for j in range(2):
                assert piece_slot[(e, "d", 2 * j)] is RING[(d_slot0 + 2 * j) % NRING] and piece_slot[(e, "d", 2 * j + 1)] is RING[(d_slot0 + 2 * j + 1) % NRING]
                wd2 = RING2[j]
                for jt in range(NJT):
                    py = self.psb(jt % 2)
                    for c in range(8):
                        self.op("pe", lambda h, c=c, jt=jt, wd2=wd2, py=py: h.matmul(py.ap.rearrange("p (a b) -> p a b", a=2), lhsT=ACTT.ap[:, c, jt * 128:(jt + 1) * 128],
                                                                                 rhs=wd2.ap[:, :, c, :], start=(c == 0), stop=False), r=[ACTT, wd2], w=[py])
                    self.op("pe", lambda h, j=j, py=py, bdn=bdn: h.matmul(py.ap, lhsT=C["ones"].ap[0:1, :], rhs=bdn.ap[0:1, j * 512:(j + 1) * 512],
                                                                          start=False, stop=True), r=[C["ones"], bdn], w=[py])
                    self.op("act", lambda h, jt=jt, j=j, py=py, YB=YB: h.activation(out=YB.ap[:, jt, j * 512:(j + 1) * 512], in_=py.ap, func=AF.Copy), r=[py], w=[YB])
                issue_piece()
                issue_piece()
            for i in range(NT):
                for dr in range(2):
                    po = self.psb(2 + ((2 * i + dr) % 6))
                    w4, ii = divmod(i, 4)
                    self.op("pe", lambda h, w4=w4, ii=ii, dr=dr, po=po, YB=YB: h.matmul(po.ap, lhsT=STw[w4].ap[:, ii * 128:(ii + 1) * 128],
                                                                             rhs=YB.ap[:, w4, dr * 512:(dr + 1) * 512], start=True, stop=True),
                            r=[STw[w4], YB], w=[po])
                    Ri = self.R[i]
                    self.op("dve", lambda h, dr=dr, Ri=Ri, po=po: h.tensor_tensor(out=Ri.ap[:, dr * 512:(dr + 1) * 512], in0=Ri.ap[:, dr * 512:(dr + 1) * 512],
                                                                              in1=po.ap, op=ALU.add), r=[Ri, po], w=[Ri])


_CACHE = {}
_NPH = [8]

WEIGHT_KEYS = ["kv_w_k", "kv_w_v", "attn_w_q", "attn_w_o", "ssm_w_in", "ssm_conv_w", "ssm_conv_b", "ssm_dt_bias", "ssm_a_log", "ssm_d", "ssm_norm_g", "ssm_w_out",
               "moe_w_router", "moe_b_router", "moe_w_gate_up", "moe_b_gate_up", "moe_w_down", "moe_b_down",
               "ln_mix_g", "ln_mix_b", "ln_ffn_g", "ln_ffn_b"]


def kernel(**inputs):
    x = np.ascontiguousarray(np.asarray(inputs["x"], dtype=np.float32))
    nb = x.shape[0]
    if "nc" not in _CACHE:
        phases = [("prep", 0), ("mamba", 0), ("moe", 0), ("mamba", 1), ("moe", 1), ("attn", 2), ("moe", 2), ("attn", 3), ("moe", 3)][:_NPH[0] + 1]
        mk = MK(layers=[0, 1, 2, 3], phases=phases)
        _CACHE["nc"] = mk.build()
        _CACHE["mk"] = mk
    nc = _CACHE["nc"]
    shared = {k: np.ascontiguousarray(np.asarray(inputs[k], dtype=np.float32)) for k in WEIGHT_KEYS}
    for k, v in make_consts().items():
        shared["c_" + k] = v
    in_maps = []
    for b in range(nb):
        m = dict(shared)
        m["x"] = x[b]
        in_maps.append(m)
    res = run_bass_kernel_spmd(nc, in_maps, core_ids=list(range(nb)))
    return np.stack([np.asarray(r["y"], dtype=np.float32) for r in res.results], axis=0)
```

```python
import math
from contextlib import ExitStack

import numpy as np
import ml_dtypes

import concourse.bass as bass
import concourse.mybir as mybir
from concourse.bass_utils import run_bass_kernel_spmd

F32 = mybir.dt.float32
BF16 = mybir.dt.bfloat16
AF = mybir.ActivationFunctionType
ALU = mybir.AluOpType
AX = mybir.AxisListType

D = 1024
S = 2048
NT = 16
DEPTH = 4
NE = 32
CAP = 512
NJT = CAP // 128
ALPHA = (2 * DEPTH) ** 0.25
LN_EPS = 1e-5
G = 1024


class Op:
    __slots__ = ("eng", "fn", "deps", "dma", "signal", "sig", "idx")

    def __init__(self, eng, fn, deps, dma, idx):
        self.eng = eng
        self.fn = fn
        self.deps = deps
        self.dma = dma
        self.signal = False
        self.sig = None
        self.idx = idx


class Prog:
    NDMA = 24

    def __init__(self, nc):
        self.nc = nc
        self.ops = []
        self.last_writer = {}
        self.readers = {}

    def add(self, eng, fn, reads=(), writes=(), dma=False):
        idx = len(self.ops)
        ops = self.ops
        deps = set()
        for k in reads:
            w = self.last_writer.get(k)
            if w is not None:
                deps.add(w)
        for k in writes:
            w = self.last_writer.get(k)
            if w is not None:
                o = ops[w]
                if o.dma or dma or o.eng != eng:
                    deps.add(w)
            for r in self.readers.get(k, ()):
                o = ops[r]
                if o.dma or dma or o.eng != eng:
                    deps.add(r)
        if eng == "pe" and not dma:
            deps = {d for d in deps if ops[d].dma or ops[d].eng != "pe"}
        latest = {}
        pruned = set()
        for d in deps:
            o = ops[d]
            if o.dma:
                pruned.add(d)
            elif latest.get(o.eng, -1) < d:
                latest[o.eng] = d
        pruned.update(latest.values())
        deps = pruned
        op = Op(eng, fn, deps, dma, idx)
        ops.append(op)
        for k in reads:
            self.readers.setdefault(k, []).append(idx)
        for k in writes:
            self.last_writer[k] = idx
            self.readers[k] = []
        return op

    def emit(self, stack):
        nc = self.nc
        ops = self.ops
        for op in ops:
            for d in op.deps:
                ops[d].signal = True
        engs = ["pe", "act", "dve", "pool", "sp"]
        esem = {e: stack.enter_context(nc.semaphore("s_" + e)) for e in engs}
        dsem = {e: [stack.enter_context(nc.semaphore("d_%s_%d" % (e, i))) for i in range(self.NDMA)]
                for e in ("sp", "act", "pool")}
        cnt = {e: 0 for e in engs}
        dcnt = {e: 0 for e in dsem}
        prewait = {}
        for op in ops:
            if op.dma:
                op.signal = True
                i = dcnt[op.eng]
                dcnt[op.eng] += 1
                sem = dsem[op.eng][i % self.NDMA]
                op.sig = (sem, 16 * (i // self.NDMA + 1))
                if i >= self.NDMA:
                    prewait[op.idx] = (sem, 16 * (i // self.NDMA))
            elif op.signal:
                cnt[op.eng] += 1
                op.sig = (esem[op.eng], cnt[op.eng])
        per = {e: [op for op in ops if op.eng == e] for e in engs}
        block = stack.enter_context(nc.Block())
        self.nwaits = 0

        def body(e):
            def run(h):
                waited = {}
                for op in per[e]:
                    need = {}
                    if op.idx in prewait:
                        s, v = prewait[op.idx]
                        need[id(s)] = (s, v)
                    for d in op.deps:
                        s, v = ops[d].sig
                        cur = need.get(id(s))
                        if cur is None or cur[1] < v:
                            need[id(s)] = (s, v)
                    for sid, (s, v) in need.items():
                        if waited.get(sid, 0) < v:
                            h.wait_ge(s, v)
                            waited[sid] = v
                            self.nwaits += 1
                    ins = op.fn(h)
                    if op.signal and ins is not None:
                        s, v = op.sig
                        ins.then_inc(s, 16 if op.dma else 1)
            return run

        block.tensor(body("pe"))
        block.scalar(body("act"))
        block.vector(body("dve"))
        block.gpsimd(body("pool"))
        block.sync(body("sp"))
        self.counts = dict(cnt)


class V:
    __slots__ = ("ap", "keys")

    def __init__(self, ap, keys):
        self.ap = ap
        self.keys = keys


def _prod(s):
    n = 1
    for x in s:
        n *= x
    return n


_RE = {2: "p (a b) -> p a b", 3: "p (a b c) -> p a b c"}


class Buf:
    def __init__(self, M, name, nbytes):
        self.name = name
        self.nbytes = nbytes
        self.t = M.st.enter_context(M.nc.sbuf_tensor(name, [128, nbytes // 2], BF16))

    def view(self, off, shape, dt=BF16, p0=0, parts=128):
        esz = 4 if dt == F32 else 2
        n = _prod(shape)
        assert off % 4 == 0 and off + n * esz <= self.nbytes, (self.name, off, shape)
        a = self.t[p0:p0 + parts, off // 2:(off + n * esz) // 2]
        if dt == F32:
            a = a.bitcast(F32)
        if len(shape) == 2:
            a = a.rearrange("p (a b) -> p a b", a=shape[0])
        elif len(shape) == 3:
            a = a.rearrange("p (a b c) -> p a b c", a=shape[0], b=shape[1])
        keys = [(self.name, g) for g in range(off // G, (off + n * esz - 1) // G + 1)]
        return V(a, keys)


def keys_of(vs):
    out = []
    for v in vs:
        if isinstance(v, V):
            out.extend(v.keys)
        elif isinstance(v, (list, tuple)) and v and isinstance(v[0], (V,)):
            out.extend(keys_of(v))
        else:
            out.append(v)
    return out


def make_consts():
    c = {}
    c["ident_f"] = np.eye(128, dtype=np.float32)
    c["iota_row"] = np.broadcast_to(np.arange(CAP, dtype=np.float32), (128, CAP)).copy()
    c["piota"] = (np.arange(128, dtype=np.float32)[:, None] + 128.0 * np.arange(NJT, dtype=np.float32)[None, :]).copy()
    c["triu"] = np.triu(np.ones((128, 128), np.float32), k=1)
    c["ones"] = np.ones((128, 128), np.float32)
    m = np.where(np.arange(128)[None, :] >= np.arange(128)[:, None], 0.0, -30000.0).astype(np.float32)
    c["maskneg"] = np.tile(m, (1, 4))
    inv_freq = (500000.0 ** (-np.arange(0, 16, 2, dtype=np.float32) / 16.0)).astype(np.float32)
    ang = np.arange(S, dtype=np.float32)[:, None] * inv_freq[None, :]
    cos, sin = np.cos(ang).astype(np.float32), np.sin(ang).astype(np.float32)
    cosf = np.ones((128, S), np.float32)
    sinf = np.zeros((128, S), np.float32)
    rotm = np.zeros((128, 128), np.float32)
    for hl in range(2):
        for dd in range(16):
            cosf[64 * hl + dd] = cos[:, dd % 8]
            sinf[64 * hl + dd] = sin[:, dd % 8]
        for dd in range(8):
            rotm[64 * hl + dd + 8, 64 * hl + dd] = -1.0
            rotm[64 * hl + dd, 64 * hl + dd + 8] = 1.0
    c["cosf"], c["sinf"], c["rotm"] = cosf, sinf, rotm
    c["tri"] = np.triu(np.ones((128, 128), np.float32), k=0)
    padm = np.zeros((8, 8), np.float32)
    for cb in range(8):
        padm[cb, cb:] = -1e30
    c["padm"] = np.broadcast_to(padm.reshape(1, 64), (128, 64)).copy()
    return c


CONST_SHAPES = {"ident_f": [128, 128], "iota_row": [128, CAP], "piota": [128, NJT],
                "triu": [128, 128], "ones": [128, 128], "maskneg": [128, 512], "cosf": [128, S], "sinf": [128, S],
                "rotm": [128, 128], "tri": [128, 128], "padm": [128, 64]}


class MK:
    def __init__(self, layers, phases, debug_in=None):
        self.layers = layers
        self.phases = phases
        self.debug = debug_in
        self.nc = bass.Bass("TRN2", target_bir_lowering=False)
        self.st = ExitStack()

    def op(self, eng, fn, r=(), w=(), dma=False):
        return self.P.add(eng, fn, keys_of(r), keys_of(w), dma)

    def dram_in(self, name, shape):
        return self.nc.dram_tensor(name, list(shape), F32, kind="ExternalInput").ap()

    def psb(self, b, cols=512, parts=128, c0=0):
        return V(self.ps[0:parts, b, c0:c0 + cols], [("ps", b)])

    def build(self):
        nc, st = self.nc, self.st
        nl = len(self.layers)
        self.x = self.dram_in("x", [S, D])
        self.y = nc.dram_tensor("y", [S, D], F32, kind="ExternalOutput").ap()
        self.w_router = self.dram_in("moe_w_router", [nl, D, NE])
        self.b_router = self.dram_in("moe_b_router", [nl, NE])
        self.w_gu = self.dram_in("moe_w_gate_up", [nl, NE, D, 2 * D])
        self.b_gu = self.dram_in("moe_b_gate_up", [nl, NE, 2 * D])
        self.w_dn = self.dram_in("moe_w_down", [nl, NE, D, D])
        self.b_dn = self.dram_in("moe_b_down", [nl, NE, D])
        self.ln_g = {"mix": self.dram_in("ln_mix_g", [nl, D]), "moe": self.dram_in("ln_ffn_g", [nl, D])}
        self.ln_b = {"mix": self.dram_in("ln_mix_b", [nl, D]), "moe": self.dram_in("ln_ffn_b", [nl, D])}
        self.cst = {k: self.dram_in("c_" + k, s) for k, s in CONST_SHAPES.items()}
        na = max(1, sum(1 for l in self.layers if l < 2))
        self.ssm_w_in = self.dram_in("ssm_w_in", [na, D, 5152])
        self.ssm_conv_w = self.dram_in("ssm_conv_w", [na, 4, 3072])
        self.ssm_conv_b = self.dram_in("ssm_conv_b", [na, 3072])
        self.ssm_dt_bias = self.dram_in("ssm_dt_bias", [na, 32])
        self.ssm_a_log = self.dram_in("ssm_a_log", [na, 32])
        self.ssm_d = self.dram_in("ssm_d", [na, 32])
        self.ssm_norm_g = self.dram_in("ssm_norm_g", [na, 2048])
        self.ssm_w_out = self.dram_in("ssm_w_out", [na, 2048, D])
        self.attn_layers = [l for l in self.layers if l >= 2]
        nbl = max(1, len(self.attn_layers))
        self.kv_w_k = self.dram_in("kv_w_k", [D, D])
        self.kv_w_v = self.dram_in("kv_w_v", [D, D])
        self.attn_w_q = self.dram_in("attn_w_q", [nbl, D, D])
        self.attn_w_o = self.dram_in("attn_w_o", [nbl, D, D])
        self.kt_s = self.y[0:1024, :].rearrange("(p a) c -> p (a c)", p=128)
        self.va_s = self.y[1024:2048, :].rearrange("(p a) c -> p (a c)", p=128)

        self.dbg = nc.dram_tensor("dbg", [128, 2048], F32, kind="ExternalOutput").ap() if self.debug else None
        self.P = Prog(nc)
        self.ps = st.enter_context(nc.psum_tensor("ps", [128, 8, 512], F32))
        self.Rb = Buf(self, "R", NT * D * 4)
        self.HBb = Buf(self, "HB", NT * D * 2)
        self.HTb = Buf(self, "HT", 8 * S * 2)
        self.Cb = Buf(self, "CST", 6 * 1024)
        self.Ab = Buf(self, "ARENA", 72 * 1024)
        self.R = [self.Rb.view(i * D * 4, [D], F32) for i in range(NT)]
        self.HB = [self.HBb.view(i * D * 2, [D], BF16) for i in range(NT)]
        self.HT = self.HTb.view(0, [8, S], BF16)

        self.load_consts()
        for i in range(NT):
            self.op("sp", lambda h, i=i: h.dma_start(out=self.R[i].ap, in_=self.x[i * 128:(i + 1) * 128, :]),
                    r=["x"], w=[self.R[i]], dma=True)
        first = True
        for kind, l in self.phases:
            li = self.layers.index(l)
            if kind == "prep":
                for i in range(NT):
                    self.make_copies(i)
            elif kind == "nomix":
                for i in range(NT):
                    Ri = self.R[i]
                    self.op("act", lambda h, Ri=Ri: h.activation(out=Ri.ap, in_=Ri.ap, func=AF.Copy, scale=ALPHA), r=[Ri], w=[Ri])
                self.layer_norm("mix", li)
            elif kind == "attn":
                self.attention(l, li)
                self.layer_norm("mix", li)
            elif kind == "mamba":
                self.mamba(li)
                self.layer_norm("mix", li)
            elif kind == "moe":
                self.moe(li)
                self.layer_norm("moe", li)
        for i in range(NT):
            self.op("sp", lambda h, i=i: h.dma_start(out=self.y[i * 128:(i + 1) * 128, :], in_=self.R[i].ap),
                    r=[self.R[i]], w=["y%d" % i, "kt_s", "va_s"], dma=True)
        self.P.add("sp", lambda h: None, reads=["y%d" % i for i in range(NT)])
        self.P.emit(st)
        return nc

    def load_consts(self):
        cb = self.Cb
        off = 0
        self.C = {}

        def alloc(n_bytes):
            nonlocal off
            o = off
            off += (n_bytes + 3) // 4 * 4
            return o
        for k in ("maskneg",):
            v = cb.view(alloc(512 * 4), [512], F32)
            self.C[k] = v
            self.op("sp", lambda h, v=v, k=k: h.dma_start(out=v.ap, in_=self.cst[k]), r=[], w=[v], dma=True)
        v = cb.view(alloc(128 * 4), [128], F32)
        self.C["ones_f"] = v
        self.op("sp", lambda h, v=v: h.dma_start(out=v.ap, in_=self.cst["ones"]), r=[], w=[v], dma=True)
        for k in ("ident_f", "piota"):
            shp = CONST_SHAPES[k]
            v = cb.view(alloc(shp[1] * 4), [shp[1]], F32)
            self.C[k] = v
            self.op("sp", lambda h, v=v, k=k: h.dma_start(out=v.ap, in_=self.cst[k]), r=[], w=[v], dma=True)
        v = cb.view(alloc(CAP * 4), [CAP], F32)
        self.C["iota_row"] = v
        self.op("sp", lambda h, v=v: h.dma_start(out=v.ap, in_=self.cst["iota_row"]), r=[], w=[v], dma=True)
        for k in ("triu", "ones"):
            v = cb.view(alloc(128 * 2), [128], BF16)
            self.C[k] = v
            self.op("pool", lambda h, v=v, k=k: h.dma_start(out=v.ap, in_=self.cst[k]), r=[], w=[v], dma=True)
        v = cb.view(alloc(128 * 2), [128], BF16)
        self.C["ident_b"] = v
        self.op("pool", lambda h, v=v: h.dma_start(out=v.ap, in_=self.cst["ident_f"]), r=[], w=[v], dma=True)
        self.cst_off = off

    def layer_norm(self, which, li, scale_in_place=True):
        A = self.Ab
        gt = A.view(0, [D], F32)
        bt = A.view(4096, [D], F32)
        st6 = A.view(8192, [2, 6], F32)
        mv = A.view(8192 + 64, [2], F32)
        rstd = A.view(8192 + 128, [1], F32)
        nmr = A.view(8192 + 192, [1], F32)
        tmp = A.view(9216, [D], F32)
        self.op("sp", lambda h: h.dma_start(out=gt.ap, in_=self.ln_g[which][li:li + 1, :].partition_broadcast(128)),
                r=[], w=[gt], dma=True)
        self.op("sp", lambda h: h.dma_start(out=bt.ap, in_=self.ln_b[which][li:li + 1, :].partition_broadcast(128)),
                r=[], w=[bt], dma=True)
        for i in range(NT):
            Ri = self.R[i]
            for c in range(2):
                self.op("dve", lambda h, c=c, Ri=Ri: h.bn_stats(out=st6.ap[:, c, :], in_=Ri.ap[:, c * 512:(c + 1) * 512]),
                        r=[Ri], w=[st6])
            self.op("dve", lambda h: h.bn_aggr(out=mv.ap, in_=st6.ap.rearrange("p a b -> p (a b)")), r=[st6], w=[mv])
            self.op("act", lambda h: h.activation(out=rstd.ap, in_=mv.ap[:, 1:2], func=AF.Sqrt, bias=LN_EPS, scale=1.0),
                    r=[mv], w=[rstd])
            self.op("dve", lambda h: h.reciprocal(out=rstd.ap, in_=rstd.ap), r=[rstd], w=[rstd])
            self.op("dve", lambda h: h.scalar_tensor_tensor(out=nmr.ap, in0=mv.ap[:, 0:1], scalar=-1.0, in1=rstd.ap, op0=ALU.mult, op1=ALU.mult),
                    r=[mv, rstd], w=[nmr])
            self.op("act", lambda h, Ri=Ri: h.activation(out=tmp.ap, in_=Ri.ap, func=AF.Identity, bias=nmr.ap, scale=rstd.ap), r=[Ri, nmr, rstd], w=[tmp])
            self.op("dve", lambda h: h.tensor_tensor(out=tmp.ap, in0=tmp.ap, in1=gt.ap, op=ALU.mult), r=[tmp, gt], w=[tmp])
            self.op("dve", lambda h, Ri=Ri: h.tensor_tensor(out=Ri.ap, in0=tmp.ap, in1=bt.ap, op=ALU.add), r=[tmp, bt], w=[Ri])
            self.make_copies(i)

    def make_copies(self, i):
            Ri = self.R[i]
            HBi = self.HB[i]
            self.op("act", lambda h, Ri=Ri, HBi=HBi: h.activation(out=HBi.ap, in_=Ri.ap, func=AF.Copy), r=[Ri], w=[HBi])
            pb = 6 + (i % 2)
            pt = V(self.ps[:, pb, :].bitcast(BF16)[:, 0:1024].rearrange("p (a b) -> p a b", a=8), [("ps", pb)])
            for m in range(8):
                self.op("pe", lambda h, m=m, HBi=HBi, pt=pt: h.transpose(out=pt.ap[:, m, :], in_=HBi.ap[:, m * 128:(m + 1) * 128],
                                                                          identity=self.C["ident_b"].ap),
                        r=[HBi, self.C["ident_b"]], w=[pt])
            htv = V(self.HT.ap[:, :, i * 128:(i + 1) * 128], [("HT", g) for g in range(8 * S * 2 // G)])
            self.op("act", lambda h, pt=pt, htv=htv: h.activation(out=htv.ap, in_=pt.ap, func=AF.Copy), r=[pt], w=[htv])


    def mamba(self, li):
        A, HBb, C = self.Ab, self.HBb, self.C
        identf = C["ident_f"].ap
        op = self.op
        ACSF = HBb.view(0, [S], F32, parts=32)
        NBF = HBb.view(8192, [S], F32, parts=32)
        NBH = [HBb.view(16384 + 2048 * i, [512], F32, parts=32) for i in range(2)]
        ONESF = HBb.view(20480, [128], F32)
        PAR = HBb.view(21504, [8], F32, parts=32)
        ACST = HBb.view(24576, [16, 32], F32)
        W2T = HBb.view(26624, [16, 32], F32)
        EACST = HBb.view(28672, [16, 32], F32)
        DECC = HBb.view(30720, [16, 32], F32)
        o = 0

        def alloc(n):
            nonlocal o
            a = o
            o += (n + G - 1) // G * G
            return a
        CWT = A.view(alloc(6 * 4 * 4), [6, 4], F32)
        CBT = A.view(alloc(6 * 4), [6], F32)
        DBC = A.view(alloc(32 * 4), [32], F32)
        NGg = A.view(alloc(512 * 4), [512], F32)
        xf_off = alloc(4 * S * 2)
        XF = A.view(xf_off, [4, S], BF16)
        T1 = A.view(xf_off, [S], F32, parts=32)
        T2 = A.view(xf_off + S * 4, [S], F32, parts=32)
        bf_off = alloc(S * 2)
        BF = A.view(bf_off, [S], BF16)
        WDT = A.view(bf_off, [8, 32], BF16)
        CF = A.view(alloc(S * 2), [S], BF16)
        STATE = A.view(alloc(512 * 4), [512], F32)
        reg = o
        o = reg
        WX = A.view(alloc(8 * 512 * 2), [8, 512], BF16)
        WB = A.view(alloc(8 * 128 * 2), [8, 128], BF16)
        WC = A.view(alloc(8 * 128 * 2), [8, 128], BF16)
        U = A.view(alloc((S + 4) * 4), [S + 4], F32)
        ACC = [A.view(alloc(512 * 4), [512], F32) for _ in range(2)]
        o_conv = o
        o = reg
        WZ = A.view(alloc(8 * 512 * 2), [8, 512], BF16)
        WO = A.view(alloc(4 * D * 2), [4, D], BF16)
        GT4 = A.view(alloc(512 * 4), [4, 128], F32)
        ARG = [A.view(alloc(512 * 4), [512], F32) for _ in range(2)]
        MT = [A.view(alloc(512 * 2), [4, 128], BF16) for _ in range(8)]
        XTOK = A.view(alloc(512 * 2), [512], BF16)
        BTOK = A.view(alloc(128 * 2), [128], BF16)
        PREV = A.view(alloc(512 * 2), [512], BF16)
        xdt_off = alloc(512 * 2)
        XDT2 = A.view(xdt_off, [512], BF16)
        Y1 = A.view(alloc(512 * 4), [512], F32)
        xd_off = alloc(512 * 4)
        XD = A.view(xd_off, [512], F32)
        ZS = A.view(alloc(512 * 2), [512], BF16)
        JUNK = A.view(xd_off, [512], F32)
        SS = A.view(alloc(16), [4], F32)
        YN = A.view(alloc(512 * 2), [512], BF16)
        YNT = A.view(xdt_off, [4, 128], BF16)
        assert max(o, o_conv) <= A.nbytes, (o, o_conv)
        w_in = self.ssm_w_in[li].rearrange("(kc kp) c -> kp kc c", kp=128)

        for i in range(NT):
            Ri = self.R[i]
            op("act", lambda h, Ri=Ri: h.activation(out=Ri.ap, in_=Ri.ap, func=AF.Copy, scale=ALPHA), r=[Ri], w=[Ri])

        op("pool", lambda h: h.dma_start(out=WDT.ap, in_=w_in[:, :, 5120:5152]), r=[], w=[WDT], dma=True)
        op("sp", lambda h: h.dma_start(out=PAR.ap[:, 0:1], in_=self.ssm_dt_bias[li:li + 1, :].rearrange("o h -> h o")), r=[], w=[PAR], dma=True)
        op("sp", lambda h: h.dma_start(out=PAR.ap[:, 1:2], in_=self.ssm_a_log[li:li + 1, :].rearrange("o h -> h o")), r=[], w=[PAR], dma=True)
        op("sp", lambda h: h.dma_start(out=DBC.ap, in_=self.ssm_d[li:li + 1, :].partition_broadcast(128)), r=[], w=[DBC], dma=True)
        op("dve", lambda h: h.memset(ONESF.ap, 1.0), r=[], w=[ONESF])
        for r in range(4):
            pd = self.psb(r % 2, parts=32)
            for k in range(8):
                op("pe", lambda h, k=k, r=r, pd=pd: h.matmul(pd.ap, lhsT=WDT.ap[:, k, :], rhs=self.HT.ap[:, k, r * 512:(r + 1) * 512],
                                                           start=(k == 0), stop=(k == 7)), r=[WDT, self.HT], w=[pd])
            op("act", lambda h, r=r, pd=pd: h.activation(out=T1.ap[:, r * 512:(r + 1) * 512], in_=pd.ap, func=AF.Exp, bias=PAR.ap[:, 0:1], scale=1.0),
               r=[pd, PAR], w=[T1])
        op("act", lambda h: h.activation(out=T1.ap, in_=T1.ap, func=AF.Ln, bias=1.0, scale=1.0), r=[T1], w=[T1])
        op("act", lambda h: h.activation(out=T2.ap, in_=T1.ap, func=AF.Ln), r=[T1], w=[T2])
        op("act", lambda h: h.activation(out=PAR.ap[:, 2:3], in_=PAR.ap[:, 1:2], func=AF.Exp), r=[PAR], w=[PAR])
        op("dve", lambda h: h.tensor_scalar(out=PAR.ap[:, 2:3], in0=PAR.ap[:, 2:3], scalar1=-1.0, scalar2=None, op0=ALU.mult), r=[PAR], w=[PAR])
        op("dve", lambda h: h.tensor_scalar(out=T1.ap, in0=T1.ap, scalar1=PAR.ap[:, 2:3], scalar2=None, op0=ALU.mult), r=[T1, PAR], w=[T1])
        for c in range(16):
            op("dve", lambda h, c=c: h.tensor_tensor_scan(out=ACSF.ap[:, c * 128:(c + 1) * 128], data0=ONESF.ap[0:32, :], data1=T1.ap[:, c * 128:(c + 1) * 128],
                                                         initial=0.0, op0=ALU.mult, op1=ALU.add), r=[ONESF, T1], w=[ACSF])
        op("dve", lambda h: h.tensor_tensor(out=NBF.ap, in0=T2.ap, in1=ACSF.ap, op=ALU.subtract), r=[T2, ACSF], w=[NBF])
        for c in range(16):
            op("act", lambda h, c=c: h.activation(out=T2.ap[:, c * 128:(c + 1) * 128], in_=NBF.ap[:, c * 128:(c + 1) * 128], func=AF.Exp,
                                                  bias=ACSF.ap[:, c * 128 + 127:c * 128 + 128], scale=1.0), r=[NBF, ACSF], w=[T2])
        pa = self.psb(2)
        pw = self.psb(3)
        for c in range(16):
            op("pe", lambda h, c=c: h.transpose(out=pa.ap[:, c * 32:(c + 1) * 32], in_=ACSF.ap[:, c * 128:(c + 1) * 128], identity=identf[0:32, 0:32]),
               r=[ACSF, C["ident_f"]], w=[pa])
            op("pe", lambda h, c=c: h.transpose(out=pw.ap[:, c * 32:(c + 1) * 32], in_=T2.ap[:, c * 128:(c + 1) * 128], identity=identf[0:32, 0:32]),
               r=[T2, C["ident_f"]], w=[pw])
        op("act", lambda h: h.activation(out=ACST.ap.rearrange("p a b -> p (a b)"), in_=pa.ap, func=AF.Copy), r=[pa], w=[ACST])
        op("act", lambda h: h.activation(out=W2T.ap.rearrange("p a b -> p (a b)"), in_=pw.ap, func=AF.Copy), r=[pw], w=[W2T])
        op("act", lambda h: h.activation(out=EACST.ap, in_=ACST.ap, func=AF.Exp), r=[ACST], w=[EACST])
        op("dve", lambda h: h.tensor_scalar(out=Y1.ap, in0=ACST.ap.rearrange("p a b -> p (a b)"), scalar1=identf[:, 127:128], scalar2=None, op0=ALU.mult),
           r=[ACST, C["ident_f"]], w=[Y1])
        pdc = self.psb(0)
        op("pe", lambda h: h.matmul(pdc.ap, lhsT=C["ones_f"].ap, rhs=Y1.ap, start=True, stop=True), r=[C["ones_f"], Y1], w=[pdc])
        op("act", lambda h: h.activation(out=DECC.ap.rearrange("p a b -> p (a b)"), in_=pdc.ap, func=AF.Exp), r=[pdc], w=[DECC])

        if getattr(self, "dbg", None) is not None:
            for q, tv in enumerate((ACST, W2T, EACST, DECC)[3:], 3):
                op("sp", lambda h, q=q, tv=tv: h.dma_start(out=self.dbg[:, q * 512:(q + 1) * 512], in_=tv.ap.rearrange("p a b -> p (a b)")),
                   r=[tv], w=["dbg%d" % q], dma=True)
        for g in range(4):
            op("pool", lambda h, g=g: h.dma_start(out=WX.ap, in_=w_in[:, :, 2048 + g * 512:2048 + (g + 1) * 512]), r=[], w=[WX], dma=True)
            op("pool", lambda h, g=g: h.dma_start(out=WB.ap, in_=w_in[:, :, 4096 + g * 128:4096 + (g + 1) * 128]), r=[], w=[WB], dma=True)
            op("pool", lambda h, g=g: h.dma_start(out=WC.ap, in_=w_in[:, :, 4608 + g * 128:4608 + (g + 1) * 128]), r=[], w=[WC], dma=True)
            op("dve", lambda h: h.memset(U.ap[:, 0:4], 0.0), r=[], w=[U])
            bases = [g * 512 + j * 128 for j in range(4)] + [2048 + g * 128, 2560 + g * 128]
            for j, b0 in enumerate(bases):
                op("sp", lambda h, j=j, b0=b0: h.dma_start(out=CWT.ap[:, j, :], in_=self.ssm_conv_w[li, :, b0:b0 + 128].rearrange("k p -> p k"),
                                                          allow_slow_non_contiguous=True), r=[], w=[CWT], dma=True)
                op("sp", lambda h, j=j, b0=b0: h.dma_start(out=CBT.ap[:, j:j + 1], in_=self.ssm_conv_b[li:li + 1, b0:b0 + 128].rearrange("o p -> p o")),
                   r=[], w=[CBT], dma=True)
            n = 0
            for j in range(6):
                for r in range(4):
                    pc = self.psb(6 + (n % 2))
                    for k in range(8):
                        if j < 4:
                            lw = WX.ap[:, k, j * 128:(j + 1) * 128]
                            wv = WX
                        else:
                            wv = WB if j == 4 else WC
                            lw = wv.ap[:, k, :]
                        op("pe", lambda h, k=k, r=r, lw=lw, pc=pc: h.matmul(pc.ap, lhsT=lw, rhs=self.HT.ap[:, k, r * 512:(r + 1) * 512],
                                                                       start=(k == 0), stop=(k == 7)), r=[wv, self.HT], w=[pc])
                    op("act", lambda h, r=r, pc=pc: h.activation(out=U.ap[:, 4 + r * 512:4 + (r + 1) * 512], in_=pc.ap, func=AF.Copy), r=[pc], w=[U])
                    acc = ACC[n % 2]
                    eng = "dve"
                    op(eng, lambda h, r=r, j=j, acc=acc: h.tensor_scalar(out=acc.ap, in0=U.ap[:, 1 + r * 512:1 + (r + 1) * 512], scalar1=CWT.ap[:, j, 0:1],
                                                                        scalar2=None, op0=ALU.mult), r=[U, CWT], w=[acc])
                    for t in range(1, 4):
                        op(eng, lambda h, r=r, j=j, t=t, acc=acc: h.scalar_tensor_tensor(out=acc.ap, in0=U.ap[:, 1 + t + r * 512:1 + t + (r + 1) * 512],
                                                                                         scalar=CWT.ap[:, j, t:t + 1], in1=acc.ap, op0=ALU.mult, op1=ALU.add),
                           r=[U, CWT, acc], w=[acc])
                    if j < 4:
                        dst, dv = XF.ap[:, j, r * 512:(r + 1) * 512], XF
                    elif j == 4:
                        dst, dv = BF.ap[:, r * 512:(r + 1) * 512], BF
                    else:
                        dst, dv = CF.ap[:, r * 512:(r + 1) * 512], CF
                    op("act", lambda h, j=j, acc=acc, dst=dst: h.activation(out=dst, in_=acc.ap, func=AF.Silu, bias=CBT.ap[:, j:j + 1], scale=1.0),
                       r=[acc, CBT], w=[dv])
                    n += 1
            op("pool", lambda h, g=g: h.dma_start(out=WZ.ap, in_=w_in[:, :, g * 512:(g + 1) * 512]), r=[], w=[WZ], dma=True)
            op("pool", lambda h, g=g: h.dma_start(out=WO.ap, in_=self.ssm_w_out[li, g * 512:(g + 1) * 512, :].rearrange("(c p) d -> p c d", p=128)),
               r=[], w=[WO], dma=True)
            op("sp", lambda h, g=g: h.dma_start(out=NGg.ap, in_=self.ssm_norm_g[li:li + 1, g * 512:(g + 1) * 512].partition_broadcast(128)),
               r=[], w=[NGg], dma=True)
            op("dve", lambda h: h.memset(STATE.ap, 0.0), r=[], w=[STATE])
            hs = slice(8 * g, 8 * g + 8)
            for cb in range(4):
                pgt = self.psb(5)
                for c in range(4):
                    cc = 4 * cb + c
                    op("pe", lambda h, c=c, cc=cc: h.matmul(pgt.ap[:, c * 128:(c + 1) * 128], lhsT=BF.ap[:, cc * 128:(cc + 1) * 128],
                                                           rhs=CF.ap[:, cc * 128:(cc + 1) * 128], start=True, stop=True), r=[BF, CF], w=[pgt])
                op("act", lambda h: h.activation(out=GT4.ap.rearrange("p a b -> p (a b)"), in_=pgt.ap, func=AF.Copy), r=[pgt], w=[GT4])
                for hh in range(8):
                    hd = 8 * g + hh
                    nbh = NBH[hh % 2]
                    op("dve", lambda h, hd=hd, cb=cb, nbh=nbh: h.tensor_scalar(out=nbh.ap, in0=NBF.ap[:, cb * 512:(cb + 1) * 512], scalar1=identf[0:32, hd:hd + 1],
                                                                               scalar2=None, op0=ALU.mult), r=[NBF, C["ident_f"]], w=[nbh])
                    parg = self.psb(4)
                    op("pe", lambda h, hd=hd, cb=cb: h.matmul(parg.ap, lhsT=identf[0:32, hd:hd + 1].broadcast_to([32, 128]), rhs=ACSF.ap[:, cb * 512:(cb + 1) * 512],
                                                              start=True, stop=False), r=[C["ident_f"], ACSF], w=[parg])
                    for c in range(4):
                        op("pe", lambda h, c=c, nbh=nbh: h.matmul(parg.ap[:, c * 128:(c + 1) * 128], lhsT=nbh.ap[:, c * 128:(c + 1) * 128], rhs=ONESF.ap[0:32, :],
                                                                  start=False, stop=(c == 3)), r=[nbh, ONESF], w=[parg])
                    arg = ARG[hh % 2]
                    op("dve", lambda h, arg=arg: h.tensor_tensor(out=arg.ap, in0=parg.ap, in1=C["maskneg"].ap, op=ALU.add), r=[parg, C["maskneg"]], w=[arg])
                    op("act", lambda h, arg=arg: h.activation(out=arg.ap, in_=arg.ap, func=AF.Exp), r=[arg], w=[arg])
                    op("dve", lambda h, arg=arg, hh=hh: h.tensor_tensor(out=MT[hh].ap.rearrange("p a b -> p (a b)"), in0=arg.ap,
                                                                        in1=GT4.ap.rearrange("p a b -> p (a b)"), op=ALU.mult), r=[arg, GT4], w=[MT[hh]])
                for c in range(4):
                    cc = 4 * cb + c
                    tk = slice(cc * 128, (cc + 1) * 128)
                    ptx = V(self.ps[:, 5, :].bitcast(BF16)[:, 0:512], [("ps", 5)])
                    ptb = V(self.ps[:, 5, :].bitcast(BF16)[:, 512:640], [("ps", 5)])
                    for j in range(4):
                        op("pe", lambda h, j=j, tk=tk: h.transpose(out=ptx.ap[:, j * 128:(j + 1) * 128], in_=XF.ap[:, j, tk], identity=C["ident_b"].ap),
                           r=[XF, C["ident_b"]], w=[ptx])
                    op("pe", lambda h, tk=tk: h.transpose(out=ptb.ap, in_=BF.ap[:, tk], identity=C["ident_b"].ap), r=[BF, C["ident_b"]], w=[ptb])
                    op("act", lambda h: h.activation(out=XTOK.ap, in_=ptx.ap, func=AF.Copy), r=[ptx], w=[XTOK])
                    op("act", lambda h: h.activation(out=BTOK.ap, in_=ptb.ap, func=AF.Copy), r=[ptb], w=[BTOK])
                    op("act", lambda h: h.activation(out=PREV.ap, in_=STATE.ap, func=AF.Copy), r=[STATE], w=[PREV])
                    pyd, pyo, pst, pz = self.psb(0), self.psb(1), self.psb(2), self.psb(3)
                    for hh in range(8):
                        op("pe", lambda h, hh=hh, c=c: h.matmul(pyd.ap[:, hh * 64:(hh + 1) * 64], lhsT=MT[hh].ap[:, c, :], rhs=XTOK.ap[:, hh * 64:(hh + 1) * 64],
                                                               start=True, stop=True), r=[MT[hh], XTOK], w=[pyd])
                    op("pe", lambda h, tk=tk: h.matmul(pyo.ap, lhsT=CF.ap[:, tk], rhs=PREV.ap, start=True, stop=True), r=[CF, PREV], w=[pyo])
                    op("dve", lambda h, cc=cc, hs=hs: h.tensor_tensor(out=XDT2.ap.rearrange("p (a b) -> p a b", a=8), in0=XTOK.ap.rearrange("p (a b) -> p a b", a=8),
                                                               in1=W2T.ap[:, cc, hs].unsqueeze(2).broadcast_to([128, 8, 64]), op=ALU.mult), r=[XTOK, W2T], w=[XDT2])
                    op("pe", lambda h: h.matmul(pst.ap, lhsT=BTOK.ap, rhs=XDT2.ap, start=True, stop=True), r=[BTOK, XDT2], w=[pst])
                    op("dve", lambda h, cc=cc, hs=hs: h.tensor_tensor(out=Y1.ap.rearrange("p (a b) -> p a b", a=8), in0=pyo.ap.rearrange("p (a b) -> p a b", a=8),
                                                              in1=EACST.ap[:, cc, hs].unsqueeze(2).broadcast_to([128, 8, 64]), op=ALU.mult), r=[pyo, EACST], w=[Y1])
                    op("dve", lambda h: h.tensor_tensor(out=Y1.ap, in0=Y1.ap, in1=pyd.ap, op=ALU.add), r=[Y1, pyd], w=[Y1])
                    op("dve", lambda h, hs=hs: h.tensor_tensor(out=XD.ap.rearrange("p (a b) -> p a b", a=8), in0=XTOK.ap.rearrange("p (a b) -> p a b", a=8),
                                                        in1=DBC.ap[:, hs].unsqueeze(2).broadcast_to([128, 8, 64]), op=ALU.mult), r=[XTOK, DBC], w=[XD])
                    op("dve", lambda h: h.tensor_tensor(out=Y1.ap, in0=Y1.ap, in1=XD.ap, op=ALU.add), r=[Y1, XD], w=[Y1])
                    op("dve", lambda h, cc=cc, hs=hs: h.tensor_tensor(out=STATE.ap.rearrange("p (a b) -> p a b", a=8), in0=STATE.ap.rearrange("p (a b) -> p a b", a=8),
                                                              in1=DECC.ap[:, cc, hs].unsqueeze(2).broadcast_to([128, 8, 64]), op=ALU.mult), r=[STATE, DECC], w=[STATE])
                    op("dve", lambda h: h.tensor_tensor(out=STATE.ap, in0=STATE.ap, in1=pst.ap, op=ALU.add), r=[STATE, pst], w=[STATE])
                    if getattr(self, "dbg", None) is not None and g == 0 and cc == 0:
                        op("sp", lambda h: h.dma_start(out=self.dbg[:, 0:512], in_=STATE.ap), r=[STATE], w=["dbg0"], dma=True)
                        op("act", lambda h: h.activation(out=ARG[0].ap, in_=XDT2.ap, func=AF.Copy), r=[XDT2], w=[ARG[0]])
                        op("act", lambda h: h.activation(out=ARG[1].ap[:, 0:128], in_=BTOK.ap, func=AF.Copy), r=[BTOK], w=[ARG[1]])
                        op("sp", lambda h: h.dma_start(out=self.dbg[:, 1024:1536], in_=ARG[0].ap), r=[ARG[0]], w=["dbg2"], dma=True)
                        op("sp", lambda h: h.dma_start(out=self.dbg[:, 512:640], in_=ARG[1].ap[:, 0:128]), r=[ARG[1]], w=["dbg1"], dma=True)
                    for k in range(8):
                        op("pe", lambda h, k=k, tk=tk: h.matmul(pz.ap, lhsT=self.HT.ap[:, k, tk], rhs=WZ.ap[:, k, :], start=(k == 0), stop=(k == 7)),
                           r=[self.HT, WZ], w=[pz])
                    op("act", lambda h: h.activation(out=ZS.ap, in_=pz.ap, func=AF.Silu), r=[pz], w=[ZS])
                    op("dve", lambda h: h.tensor_tensor(out=Y1.ap, in0=Y1.ap, in1=ZS.ap, op=ALU.mult), r=[Y1, ZS], w=[Y1])
                    op("act", lambda h: h.activation(out=JUNK.ap, in_=Y1.ap, func=AF.Square, accum_out=SS.ap[:, 0:1]), r=[Y1], w=[JUNK, SS])
                    op("act", lambda h: h.activation(out=SS.ap[:, 1:2], in_=SS.ap[:, 0:1], func=AF.Sqrt, bias=1e-5, scale=1.0 / 512.0), r=[SS], w=[SS])
                    op("dve", lambda h: h.reciprocal(out=SS.ap[:, 2:3], in_=SS.ap[:, 1:2]), r=[SS], w=[SS])
                    op("dve", lambda h: h.scalar_tensor_tensor(out=YN.ap, in0=Y1.ap, scalar=SS.ap[:, 2:3], in1=NGg.ap, op0=ALU.mult, op1=ALU.mult),
                       r=[Y1, SS, NGg], w=[YN])
                    pty = V(self.ps[:, 5, :].bitcast(BF16)[:, 0:512].rearrange("p (a b) -> p a b", a=4), [("ps", 5)])
                    for j in range(4):
                        op("pe", lambda h, j=j: h.transpose(out=pty.ap[:, j, :], in_=YN.ap[:, j * 128:(j + 1) * 128], identity=C["ident_b"].ap),
                           r=[YN, C["ident_b"]], w=[pty])
                    op("act", lambda h: h.activation(out=YNT.ap, in_=pty.ap, func=AF.Copy), r=[pty], w=[YNT])
                    Rc = self.R[cc]
                    for dr in range(2):
                        pm = self.psb(6 + dr)
                        for j in range(4):
                            op("pe", lambda h, j=j, dr=dr, pm=pm: h.matmul(pm.ap, lhsT=YNT.ap[:, j, :], rhs=WO.ap[:, j, dr * 512:(dr + 1) * 512],
                                                                        start=(j == 0), stop=(j == 3)), r=[YNT, WO], w=[pm])
                        op("dve", lambda h, dr=dr, pm=pm, Rc=Rc: h.tensor_tensor(out=Rc.ap[:, dr * 512:(dr + 1) * 512], in0=Rc.ap[:, dr * 512:(dr + 1) * 512],
                                                                                in1=pm.ap, op=ALU.add), r=[Rc, pm], w=[Rc])


    def attention(self, l, li):
        A, HBb, C = self.Ab, self.HBb, self.C
        op = self.op
        ja = self.attn_layers.index(l)
        build_kv = (ja == 0)
        KT = HBb.view(0, [8, S], BF16)
        o = 0

        def alloc(n):
            nonlocal o
            a = o
            o += (n + G - 1) // G * G
            return a
        VA = A.view(alloc(16 * 16 * 66 * 2), [16, 16, 66], BF16)
        VAF = A.view(0, [16, 16, 33], F32)
        KTF = HBb.view(0, [8 * S // 2], F32)
        KMB = A.view(alloc(8 * 8 * 2), [8, 8], BF16)
        W1R = A.view(alloc(8 * 128 * 2), [8, 128], BF16)
        TRI = A.view(alloc(256), [128], BF16)
        PADM = A.view(alloc(256), [8, 8], F32)
        WSEL = A.view(alloc(16 * 2 * 8 * 4), [16, 2, 8], F32)
        GSC = A.view(alloc(1024), [256], F32)
        W1 = A.view(alloc(8 * 512 * 2), [8, 512], BF16)
        WOp = A.view(alloc(D * 2), [D], BF16)
        QTp = A.view(alloc(S * 2), [S], BF16)
        OTp = A.view(alloc(S * 2), [S], BF16)
        CS = A.view(alloc(512 * 4), [512], F32)
        SN = A.view(alloc(512 * 4), [512], F32)
        T1 = A.view(alloc(512 * 4), [512], F32)
        T2 = A.view(alloc(512 * 4), [512], F32)
        PT = [A.view(alloc(512 * 2), [512], BF16) for _ in range(2)]
        ACC = A.view(alloc(4 * 65 * 4), [4, 65], F32)
        OTOK = A.view(alloc(4 * 128 * 2), [4, 128], BF16)
        assert o <= A.nbytes, o

        for i in range(NT):
            Ri = self.R[i]
            op("act", lambda h, Ri=Ri: h.activation(out=Ri.ap, in_=Ri.ap, func=AF.Copy, scale=ALPHA), r=[Ri], w=[Ri])
        op("pool", lambda h: h.dma_start(out=TRI.ap, in_=self.cst["tri"]), r=[], w=[TRI], dma=True)
        op("sp", lambda h: h.dma_start(out=PADM.ap.rearrange("p a b -> p (a b)"), in_=self.cst["padm"]), r=[], w=[PADM], dma=True)

        def make_rot_weights(pp):
            op("dve", lambda h: h.memset(W1R.ap, 0.0), r=[], w=[W1R])
            for hl in range(2):
                b0 = pp * 128 + 64 * hl
                op("act", lambda h, hl=hl, b0=b0: h.activation(out=W1R.ap[:, :, 64 * hl:64 * hl + 8], in_=W1.ap[:, :, b0 + 8:b0 + 16], func=AF.Copy, scale=-1.0),
                   r=[W1], w=[W1R])
                op("act", lambda h, hl=hl, b0=b0: h.activation(out=W1R.ap[:, :, 64 * hl + 8:64 * hl + 16], in_=W1.ap[:, :, b0:b0 + 8], func=AF.Copy),
                   r=[W1], w=[W1R])

        def rope(pk, dst, dv, r):
            prot = self.psb(6)
            for k in range(8):
                op("pe", lambda h, k=k, r=r, prot=prot: h.matmul(prot.ap, lhsT=W1R.ap[:, k, :], rhs=self.HT.ap[:, k, r * 512:(r + 1) * 512],
                                                              start=(k == 0), stop=(k == 7)), r=[W1R, self.HT], w=[prot])
            op("sp", lambda h, r=r: h.dma_start(out=CS.ap, in_=self.cst["cosf"][:, r * 512:(r + 1) * 512]), r=[], w=[CS], dma=True)
            op("sp", lambda h, r=r: h.dma_start(out=SN.ap, in_=self.cst["sinf"][:, r * 512:(r + 1) * 512]), r=[], w=[SN], dma=True)
            op("dve", lambda h, pk=pk: h.tensor_tensor(out=T1.ap, in0=pk.ap, in1=CS.ap, op=ALU.mult), r=[pk, CS], w=[T1])
            op("dve", lambda h, prot=prot: h.tensor_tensor(out=T2.ap, in0=prot.ap, in1=SN.ap, op=ALU.mult), r=[prot, SN], w=[T2])
            op("dve", lambda h, dst=dst: h.tensor_tensor(out=dst, in0=T1.ap, in1=T2.ap, op=ALU.add), r=[T1, T2], w=[dv])

        def wsrc(w2d, half):
            return w2d.rearrange("(kc kp) c -> kp kc c", kp=128)[:, :, half * 512:(half + 1) * 512]

        def kmeans():
            for p in range(8):
                op("dve", lambda h, p=p: h.tensor_reduce(out=GSC.ap[:, 0:8], in_=KT.ap[:, p, :].rearrange("p (a b) -> p a b", a=8), axis=AX.X, op=ALU.add),
                   r=[KT], w=[GSC])
                op("dve", lambda h, p=p: h.tensor_scalar(out=KMB.ap[:, p, :], in0=GSC.ap[:, 0:8], scalar1=1.0 / 256.0, scalar2=None, op0=ALU.mult),
                   r=[GSC], w=[KMB])

        stop = getattr(self, "attn_stop", 0)
        if stop == -1:
            return
        if build_kv:
            for half in range(2):
                if stop == -2 and half == 1:
                    return
                op("pool", lambda h, half=half: h.dma_start(out=W1.ap, in_=wsrc(self.kv_w_k, half)), r=[], w=[W1], dma=True)
                for pp in range(4):
                    p = half * 4 + pp
                    make_rot_weights(pp)
                    for r in range(4):
                        pk = self.psb(4 + r % 2)
                        for k in range(8):
                            op("pe", lambda h, k=k, r=r, pp=pp, pk=pk: h.matmul(pk.ap, lhsT=W1.ap[:, k, pp * 128:(pp + 1) * 128],
                                                                             rhs=self.HT.ap[:, k, r * 512:(r + 1) * 512], start=(k == 0), stop=(k == 7)),
                               r=[W1, self.HT], w=[pk])
                        rope(pk, KT.ap[:, p, r * 512:(r + 1) * 512], KT, r)
            for half in range(2):
                op("pool", lambda h, half=half: h.dma_start(out=W1.ap, in_=wsrc(self.kv_w_v, half)), r=[], w=[W1], dma=True)
                for i in range(NT):
                    pv = self.psb(4 + i % 2)
                    for k in range(8):
                        op("pe", lambda h, k=k, i=i, pv=pv: h.matmul(pv.ap, lhsT=self.HT.ap[:, k, i * 128:(i + 1) * 128], rhs=W1.ap[:, k, :],
                                                                 start=(k == 0), stop=(k == 7)), r=[self.HT, W1], w=[pv])
                    op("act", lambda h, i=i, half=half, pv=pv: h.activation(out=VA.ap[:, i, half * 8:(half + 1) * 8, 0:64],
                                                                          in_=pv.ap.rearrange("p (a b) -> p a b", a=8), func=AF.Copy), r=[pv], w=[VA])
            if stop == -3:
                return
            op("dve", lambda h: h.memset(VA.ap[:, :, :, 64:65], 1.0), r=[], w=[VA])
            if stop == -4:
                return
            kmeans()
            if not getattr(self, "no_spill", False):
                op("sp", lambda h: h.dma_start(out=self.kt_s, in_=KTF.ap), r=[KT], w=["kt_s"], dma=True)
                for kt in range(16):
                    op("sp", lambda h, kt=kt: h.dma_start(out=self.va_s[:, kt * 512:(kt + 1) * 512].rearrange("p (b c) -> p b c", c=32), in_=VAF.ap[:, kt, :, 0:32]),
                       r=[VA], w=["va_s"], dma=True)
        else:
            op("sp", lambda h: h.dma_start(out=KTF.ap, in_=self.kt_s), r=["kt_s"], w=[KT], dma=True)
            for kt in range(16):
                op("sp", lambda h, kt=kt: h.dma_start(out=VAF.ap[:, kt, :, 0:32], in_=self.va_s[:, kt * 512:(kt + 1) * 512].rearrange("p (b c) -> p b c", c=32)),
                   r=["va_s"], w=[VA], dma=True)
            op("dve", lambda h: h.memset(VA.ap[:, :, :, 64:65], 1.0), r=[], w=[VA])
            kmeans()
        wq = self.attn_w_q[ja]
        stop = getattr(self, "attn_stop", 0)
        if stop == 1:
            return
        for p in range(8 if stop == 0 else 1):
            half, pp = divmod(p, 4)
            if pp == 0:
                op("pool", lambda h, half=half: h.dma_start(out=W1.ap, in_=wsrc(wq, half)), r=[], w=[W1], dma=True)
            op("pool", lambda h, p=p: h.dma_start(out=WOp.ap, in_=self.attn_w_o[ja, p * 128:(p + 1) * 128, :]), r=[], w=[WOp], dma=True)
            make_rot_weights(pp)
            for r in range(4):
                pq = self.psb(4 + r % 2)
                for k in range(8):
                    op("pe", lambda h, k=k, r=r, pp=pp, pq=pq: h.matmul(pq.ap, lhsT=W1.ap[:, k, pp * 128:(pp + 1) * 128],
                                                                     rhs=self.HT.ap[:, k, r * 512:(r + 1) * 512], start=(k == 0), stop=(k == 7)),
                       r=[W1, self.HT], w=[pq])
                rope(pq, QTp.ap[:, r * 512:(r + 1) * 512], QTp, r)
            for hl in range(2):
                rows = slice(64 * hl, 64 * hl + 64)
                for i in range(8, NT):
                    cblk = i // 2
                    pg = self.psb(6, cols=8)
                    op("pe", lambda h, i=i, rows=rows, p=p, pg=pg: h.matmul(pg.ap, lhsT=QTp.ap[rows, i * 128:(i + 1) * 128], rhs=KMB.ap[rows, p, :],
                                                                       start=True, stop=True), r=[QTp, KMB], w=[pg])
                    op("dve", lambda h, cblk=cblk, pg=pg: h.tensor_tensor(out=GSC.ap[:, 0:8], in0=pg.ap, in1=PADM.ap[:, cblk, :], op=ALU.add),
                       r=[pg, PADM], w=[GSC])
                    op("dve", lambda h: h.max(out=GSC.ap[:, 8:16], in_=GSC.ap[:, 0:8]), r=[GSC], w=[GSC])
                    op("dve", lambda h, i=i, hl=hl: h.tensor_scalar(out=WSEL.ap[:, i, hl, :], in0=GSC.ap[:, 0:8], scalar1=GSC.ap[:, 10:11], scalar2=None,
                                                                   op0=ALU.is_ge), r=[GSC], w=[WSEL])
            nst = 0
            if stop == 2:
                return
            for r in range(4):
                for hl in range(2):
                    rows = slice(64 * hl, 64 * hl + 64)
                    hd = 2 * p + hl
                    for n in range(2 * r + 2):
                        jqs = [jq for jq in range(4) if (4 * r + jq) // 2 >= n]
                        c0 = jqs[0] * 128
                        ncol = 512 - c0
                        pob = self.psb(2 + n % 2)
                        pts = {}
                        for kt in (2 * n, 2 * n + 1):
                            use = [jq for jq in jqs if kt <= 4 * r + jq]
                            if not use:
                                continue
                            pss = self.psb(nst % 2)
                            pt = PT[nst % 2]
                            nst += 1
                            pts[kt] = pt
                            op("pe", lambda h, kt=kt, rows=rows, r=r, c0=c0, ncol=ncol, pss=pss, p=p: h.matmul(
                                pss.ap[:, c0:c0 + ncol], lhsT=KT.ap[rows, p, kt * 128:(kt + 1) * 128], rhs=QTp.ap[rows, r * 512 + c0:(r + 1) * 512],
                                start=True, stop=True), r=[KT, QTp], w=[pss])
                            op("act", lambda h, c0=c0, ncol=ncol, pss=pss, pt=pt: h.activation(out=pt.ap[:, c0:c0 + ncol], in_=pss.ap[:, c0:c0 + ncol],
                                                                                           func=AF.Exp, scale=0.125), r=[pss], w=[pt])
                            for jq in use:
                                if kt == 4 * r + jq:
                                    op("dve", lambda h, jq=jq, pt=pt: h.tensor_tensor(out=pt.ap[:, jq * 128:(jq + 1) * 128], in0=pt.ap[:, jq * 128:(jq + 1) * 128],
                                                                                 in1=TRI.ap, op=ALU.mult), r=[pt, TRI], w=[pt])
                        for jq in jqs:
                            kts = [kt for kt in (2 * n, 2 * n + 1) if kt <= 4 * r + jq]
                            for kt in kts:
                                pt = pts[kt]
                                op("pe", lambda h, jq=jq, kt=kt, hd=hd, pt=pt, pob=pob, st=(kt == kts[0]), last=(kt == kts[-1]): h.matmul(
                                    pob.ap[:, jq * 65:(jq + 1) * 65], lhsT=pt.ap[:, jq * 128:(jq + 1) * 128], rhs=VA.ap[:, kt, hd, 0:65], start=st, stop=last),
                                    r=[pt, VA], w=[pob])
                        for jq in jqs:
                            i = 4 * r + jq
                            cblk = i // 2
                            if n == cblk or i < 8:
                                wgt = 1.0
                                rd = [pob]
                            else:
                                wgt = WSEL.ap[:, i, hl, n:n + 1]
                                rd = [pob, WSEL]
                            if n == 0:
                                op("dve", lambda h, jq=jq, wgt=wgt, pob=pob: h.tensor_scalar(out=ACC.ap[:, jq, :], in0=pob.ap[:, jq * 65:(jq + 1) * 65], scalar1=wgt,
                                                                                        scalar2=None, op0=ALU.mult), r=rd, w=[ACC])
                            else:
                                op("dve", lambda h, jq=jq, wgt=wgt, pob=pob: h.scalar_tensor_tensor(out=ACC.ap[:, jq, :], in0=pob.ap[:, jq * 65:(jq + 1) * 65], scalar=wgt,
                                                                                               in1=ACC.ap[:, jq, :], op0=ALU.mult, op1=ALU.add), r=rd + [ACC], w=[ACC])
                    for jq in range(4):
                        op("dve", lambda h, jq=jq: h.reciprocal(out=GSC.ap[:, 32 + jq:33 + jq], in_=ACC.ap[:, jq, 64:65]), r=[ACC], w=[GSC])
                        op("dve", lambda h, jq=jq, hl=hl: h.tensor_scalar(out=OTOK.ap[:, jq, 64 * hl:64 * hl + 64], in0=ACC.ap[:, jq, 0:64],
                                                                         scalar1=GSC.ap[:, 32 + jq:33 + jq], scalar2=None, op0=ALU.mult), r=[ACC, GSC], w=[OTOK])
                ptt = V(self.ps[:, 7, :].bitcast(BF16)[:, 0:512], [("ps", 7)])
                for jq in range(4):
                    op("pe", lambda h, jq=jq: h.transpose(out=ptt.ap[:, jq * 128:(jq + 1) * 128], in_=OTOK.ap[:, jq, :], identity=C["ident_b"].ap),
                       r=[OTOK, C["ident_b"]], w=[ptt])
                op("act", lambda h, r=r: h.activation(out=OTp.ap[:, r * 512:(r + 1) * 512], in_=ptt.ap, func=AF.Copy), r=[ptt], w=[OTp])
            for i in range(NT):
                Ri = self.R[i]
                for dr in range(2):
                    pm = self.psb(4 + dr)
                    op("pe", lambda h, i=i, dr=dr, pm=pm: h.matmul(pm.ap, lhsT=OTp.ap[:, i * 128:(i + 1) * 128], rhs=WOp.ap[:, dr * 512:(dr + 1) * 512],
                                                              start=True, stop=True), r=[OTp, WOp], w=[pm])
                    op("dve", lambda h, dr=dr, pm=pm, Ri=Ri: h.tensor_tensor(out=Ri.ap[:, dr * 512:(dr + 1) * 512], in0=Ri.ap[:, dr * 512:(dr + 1) * 512],
                                                                            in1=pm.ap, op=ALU.add), r=[Ri, pm], w=[Ri])

    def moe(self, li):
        A = self.Ab
        C = self.C
        o = 0

        def alloc(n):
            nonlocal o
            a = o
            o += (n + G - 1) // G * G
            return a
        WR = A.view(alloc(8 * NE * 2), [8, NE], BF16)
        BRT = A.view(alloc(NE * 4), [NE], F32)
        RG = A.view(alloc(NT * 64 * 4), [NT, 64], F32)
        RGB = A.view(alloc(NT * 64 * 2), [NT, 64], BF16)
        RGT = A.view(alloc(S * 2), [S], BF16, parts=64)
        MSK = A.view(alloc(NT * NE * 2), [NT, NE], BF16)
        CM = A.view(alloc(NT * NE * 2), [NT, NE], BF16)
        SM = A.view(alloc(1024), [256], F32)
        BGU = A.view(alloc(NE * 16 * 4), [NE, 16], F32)
        BDN = [A.view(alloc(D * 2), [D], BF16, parts=1)] * 2
        NRING = 4
        ring_offs = [alloc(8 * 256 * 2) for _ in range(NRING)]
        assert all(ring_offs[k + 1] - ring_offs[k] == 4096 for k in range(NRING - 1))
        RING = [A.view(o_, [8, 256], BF16) for o_ in ring_offs]
        d_slot0 = 8 % NRING
        assert 12 % NRING == 0 and d_slot0 % 2 == 0 and d_slot0 + 3 < NRING + 0 * 1 or NRING == 4
        RING2 = [A.view(ring_offs[(d_slot0 + 2 * j) % NRING], [2, 8, 256], BF16) for j in range(2)]
        xt_off = alloc(8 * CAP * 2)
        XT = A.view(xt_off, [8, CAP], BF16)
        ACTT = A.view(alloc(8 * CAP * 2), [8, CAP], BF16)
        xtb_off = alloc(8 * CAP * 2)
        XTs = [XT, A.view(xtb_off, [8, CAP], BF16)]
        YBs = [A.view(xt_off, [NJT, D], BF16), A.view(xtb_off, [NJT, D], BF16)]
        TG = [A.view(alloc(CAP * 4), [CAP], F32) for _ in range(2)]
        TS = [A.view(alloc(CAP * 2), [CAP], BF16) for _ in range(2)]
        TU = [A.view(alloc(CAP * 4), [CAP], F32) for _ in range(2)]
        GBS = [A.view(alloc(512 * 2), [512], BF16)] * 2
        assert o <= A.nbytes, o
        self.arena_used = o
        Sg = [self.HTb.view(i * 1024, [128], BF16) for i in range(NT)]
        base = NT * 1024
        STw = [self.HTb.view(base + w * 1024, [512], BF16) for w in range(4)]

        wr_src = self.w_router[li].rearrange("(kc kp) e -> kp kc e", kp=128)
        self.op("pool", lambda h: h.dma_start(out=WR.ap, in_=wr_src), r=[], w=[WR], dma=True)
        self.op("sp", lambda h: h.dma_start(out=BRT.ap, in_=self.b_router[li:li + 1, :].partition_broadcast(128)), r=[], w=[BRT], dma=True)
        bgu_src = self.b_gu[li].rearrange("e (c p) -> (e c) p", p=128).rearrange("(t r) p -> r t p", r=128)
        self.op("sp", lambda h: h.dma_start(out=TG[0].ap.rearrange("p (t q) -> p t q", t=4), in_=bgu_src), r=[], w=[TG[0]], dma=True)
        pbg = self.psb(0)
        for t in range(4):
            self.op("pe", lambda h, t=t: h.transpose(out=pbg.ap[:, t * 128:(t + 1) * 128], in_=TG[0].ap[:, t * 128:(t + 1) * 128], identity=C["ident_f"].ap),
                    r=[TG[0], C["ident_f"]], w=[pbg])
        self.op("act", lambda h: h.activation(out=BGU.ap.rearrange("p e c -> p (e c)"), in_=pbg.ap, func=AF.Copy), r=[pbg], w=[BGU])

        pieces = []
        for e in range(NE):
            for p in range(4):
                pieces.append((e, "g", p))
                pieces.append((e, "u", p))
            for q in range(4):
                pieces.append((e, "d", q))
        self._ring_state = {"next": 0}
        piece_slot = {}

        def issue_piece():
            n = self._ring_state["next"]
            if n >= len(pieces):
                return
            e, kind, p = pieces[n]
            slot = RING[n % NRING]
            piece_slot[(e, kind, p)] = slot
            if kind == "d":
                src = self.w_dn[li, e].rearrange("(kc kp) d -> kp kc d", kp=128)[:, :, p * 256:(p + 1) * 256]
            else:
                c0 = p * 256 + (D if kind == "u" else 0)
                src = self.w_gu[li, e].rearrange("(kc kp) f -> kp kc f", kp=128)[:, :, c0:c0 + 256]
            self.op("pool", lambda h, slot=slot, src=src: h.dma_start(out=slot.ap, in_=src), r=[], w=[slot], dma=True)
            self._ring_state["next"] = n + 1

        for _ in range(NRING):
            issue_piece()

        scr = SM.ap
        for i in range(NT):
            pl = self.psb(4 + (i % 2), cols=NE)
            for k in range(8):
                self.op("pe", lambda h, k=k, i=i, pl=pl: h.matmul(pl.ap, lhsT=self.HT.ap[:, k, i * 128:(i + 1) * 128], rhs=WR.ap[:, k, :],
                                                               start=(k == 0), stop=(k == 7)), r=[self.HT, WR], w=[pl])
            lg = V(scr[:, 0:32], SM.keys)
            m8 = V(scr[:, 32:40], SM.keys)
            nm = V(scr[:, 40:41], SM.keys)
            mk = V(scr[:, 48:80], SM.keys)
            ex = V(scr[:, 80:112], SM.keys)
            sm = V(scr[:, 112:113], SM.keys)
            rk = V(scr[:, 128:160], SM.keys)
            self.op("dve", lambda h, pl=pl: h.tensor_tensor(out=lg.ap, in0=pl.ap, in1=BRT.ap, op=ALU.add), r=[pl, BRT], w=[SM])
            self.op("dve", lambda h: h.max(out=m8.ap, in_=lg.ap), r=[SM], w=[SM])
            self.op("dve", lambda h: h.tensor_scalar(out=mk.ap, in0=lg.ap, scalar1=m8.ap[:, 3:4], scalar2=None, op0=ALU.is_ge), r=[SM], w=[SM])
            self.op("dve", lambda h: h.tensor_scalar(out=nm.ap, in0=m8.ap[:, 0:1], scalar1=-1.0, scalar2=None, op0=ALU.mult), r=[SM], w=[SM])
            self.op("act", lambda h: h.activation(out=ex.ap, in_=lg.ap, func=AF.Exp, bias=nm.ap, scale=1.0), r=[SM], w=[SM])
            self.op("dve", lambda h: h.tensor_tensor(out=ex.ap, in0=ex.ap, in1=mk.ap, op=ALU.mult), r=[SM], w=[SM])
            self.op("dve", lambda h: h.tensor_reduce(out=sm.ap, in_=ex.ap, axis=AX.X, op=ALU.add), r=[SM], w=[SM])
            self.op("dve", lambda h: h.reciprocal(out=sm.ap, in_=sm.ap), r=[SM], w=[SM])
            self.op("dve", lambda h, i=i: h.tensor_scalar(out=RG.ap[:, i, 32:64], in0=ex.ap, scalar1=sm.ap, scalar2=None, op0=ALU.mult), r=[SM], w=[RG])
            self.op("dve", lambda h, i=i: h.tensor_copy(out=MSK.ap[:, i, :], in_=mk.ap), r=[SM], w=[MSK])
            if i % 4 == 0:
                self.op("dve", lambda h, i=i: h.memset(CM.ap[:, i, :], 0.0), r=[], w=[CM])
            else:
                self.op("dve", lambda h, i=i: h.tensor_tensor(out=CM.ap[:, i, :], in0=CM.ap[:, i - 1, :], in1=MSK.ap[:, i - 1, :], op=ALU.add),
                        r=[CM, MSK], w=[CM])
            pr = self.psb(4 + (i % 2), cols=NE, c0=64)
            self.op("pe", lambda h, i=i, pr=pr: h.matmul(pr.ap, lhsT=C["ones"].ap, rhs=CM.ap[:, i, :], start=True, stop=False), r=[C["ones"], CM], w=[pr])
            self.op("pe", lambda h, i=i, pr=pr: h.matmul(pr.ap, lhsT=C["triu"].ap, rhs=MSK.ap[:, i, :], start=False, stop=True), r=[C["triu"], MSK], w=[pr])
            self.op("dve", lambda h, pr=pr: h.scalar_tensor_tensor(out=rk.ap, in0=pr.ap, scalar=1.0, in1=mk.ap, op0=ALU.add, op1=ALU.mult), r=[pr, SM], w=[SM])
            self.op("dve", lambda h, i=i: h.tensor_scalar(out=RG.ap[:, i, 0:32], in0=rk.ap, scalar1=-1.0, scalar2=None, op0=ALU.add), r=[SM], w=[RG])
            self.op("dve", lambda h, i=i: h.tensor_copy(out=RGB.ap[:, i, :], in_=RG.ap[:, i, :]), r=[RG], w=[RGB])
            ptr = V(self.ps[0:64, 4 + (i % 2), :].bitcast(BF16)[:, 256:384], [("ps", 4 + (i % 2))])
            self.op("pe", lambda h, i=i, ptr=ptr: h.transpose(out=ptr.ap, in_=RGB.ap[:, i, :], identity=C["ident_b"].ap), r=[RGB, C["ident_b"]], w=[ptr])
            self.op("act", lambda h, i=i, ptr=ptr: h.activation(out=RGT.ap[:, i * 128:(i + 1) * 128], in_=ptr.ap, func=AF.Copy), r=[ptr], w=[RGT])

        for i in range(NT):
            Ri = self.R[i]
            self.op("act", lambda h, Ri=Ri: h.activation(out=Ri.ap, in_=Ri.ap, func=AF.Copy, scale=ALPHA), r=[Ri], w=[Ri])

        identf = C["ident_f"].ap
        identb = C["ident_b"].ap
        Sg2 = [self.HTb.view(i * 1024, [256], BF16) for i in range(NT)]

        def build_S(e0):
            for i in range(NT):
                for e2 in range(2):
                    self.op("dve", lambda h, i=i, e2=e2, e0=e0: h.tensor_scalar(out=Sg2[i].ap[:, e2 * 128:(e2 + 1) * 128], in0=C["iota_row"].ap[:, 0:128],
                                                                              scalar1=RG.ap[:, i, e0 + e2:e0 + e2 + 1], scalar2=None, op0=ALU.is_equal),
                            r=[C["iota_row"], RG], w=[Sg2[i]])

        def gather_pair():
            for m in range(8):
                banks = (0, 1) if m % 2 == 0 else (2, 3)
                for half in range(2):
                    px = self.psb(banks[half])
                    for wl in range(2):
                        for ii in range(4):
                            i = 4 * (2 * half + wl) + ii
                            self.op("pe", lambda h, m=m, i=i, wl=wl, ii=ii, px=px: h.matmul(px.ap[:, wl * 256:(wl + 1) * 256], lhsT=self.HB[i].ap[:, m * 128:(m + 1) * 128],
                                                                                       rhs=Sg2[i].ap, start=(ii == 0), stop=(ii == 3)), r=[self.HB[i], Sg2[i]], w=[px])
                    for e2 in range(2):
                        self.op("act", lambda h, m=m, half=half, e2=e2, px=px: h.activation(
                            out=XTs[e2].ap[:, m, half * 256:(half + 1) * 256].rearrange("p (a b) -> p a b", a=2),
                            in_=px.ap.rearrange("p (a e b) -> p a e b", a=2, e=2)[:, :, e2, :], func=AF.Copy), r=[px], w=[XTs[e2].keys[m]])

        build_S(0)
        for e in range(NE):
            bdn = BDN[e % 2]
            self.op("pool", lambda h, e=e, bdn=bdn: h.dma_start(out=bdn.ap, in_=self.b_dn[li, e:e + 1, :]), r=[], w=[bdn], dma=True)
            if e % 2 == 0:
                gather_pair()
            XT = XTs[e % 2]
            YB = YBs[e % 2]
            for p in range(4):
                wg = piece_slot[(e, "g", p)]
                wu = piece_slot[(e, "u", p)]
                for sub in range(2):
                    c = 2 * p + sub
                    pg = self.psb(4 + (c % 2))
                    pu = self.psb(6 + (c % 2))
                    for k in range(8):
                        self.op("pe", lambda h, k=k, sub=sub, wg=wg, pg=pg, XT=XT: h.matmul(pg.ap, lhsT=wg.ap[:, k, sub * 128:(sub + 1) * 128], rhs=XT.ap[:, k, :],
                                                                                 start=(k == 0), stop=(k == 7)), r=[wg, XT.keys[k]], w=[pg])
                    for k in range(8):
                        self.op("pe", lambda h, k=k, sub=sub, wu=wu, pu=pu, XT=XT: h.matmul(pu.ap, lhsT=wu.ap[:, k, sub * 128:(sub + 1) * 128], rhs=XT.ap[:, k, :],
                                                                                 start=(k == 0), stop=(k == 7)), r=[wu, XT.keys[k]], w=[pu])
                    tg, ts, tu = TG[c % 2], TS[c % 2], TU[c % 2]
                    self.op("dve", lambda h, pg=pg, tg=tg, e=e, c=c: h.tensor_scalar(out=tg.ap, in0=pg.ap, scalar1=BGU.ap[:, e, c:c + 1], scalar2=7.0,
                                                                                 op0=ALU.add, op1=ALU.min), r=[pg, BGU], w=[tg])
                    self.op("act", lambda h, tg=tg, ts=ts: h.activation(out=ts.ap, in_=tg.ap, func=AF.Sigmoid, scale=1.702), r=[tg], w=[ts])
                    self.op("act", lambda h, pu=pu, tu=tu, e=e, c=c: h.activation(out=tu.ap, in_=pu.ap, func=AF.Identity, bias=BGU.ap[:, e, 8 + c:9 + c], scale=1.0),
                            r=[pu, BGU], w=[tu])
                    self.op("dve", lambda h, tu=tu: h.tensor_scalar(out=tu.ap, in0=tu.ap, scalar1=-7.0, scalar2=7.0, op0=ALU.max, op1=ALU.min), r=[tu], w=[tu])
                    self.op("dve", lambda h, tg=tg, ts=ts: h.tensor_tensor(out=tg.ap, in0=tg.ap, in1=ts.ap, op=ALU.mult), r=[tg, ts], w=[tg])
                    self.op("dve", lambda h, tg=tg, tu=tu, c=c: h.scalar_tensor_tensor(out=ACTT.ap[:, c, :], in0=tu.ap, scalar=1.0, in1=tg.ap, op0=ALU.add, op1=ALU.mult),
                            r=[tg, tu], w=[ACTT.keys[c]])
                issue_piece()
                issue_piece()
            for r in range(4):
                prb = self.psb(2)
                pgb = self.psb(3)
                self.op("pe", lambda h, r=r, e=e, prb=prb: h.matmul(prb.ap, lhsT=identb[0:64, e:e + 1].broadcast_to([64, 128]),
                                                                    rhs=RGT.ap[:, r * 512:(r + 1) * 512], start=True, stop=True),
                        r=[C["ident_b"], RGT], w=[prb])
                self.op("pe", lambda h, r=r, e=e, pgb=pgb: h.matmul(pgb.ap, lhsT=identb[0:64, 32 + e:33 + e].broadcast_to([64, 128]),
                                                                    rhs=RGT.ap[:, r * 512:(r + 1) * 512], start=True, stop=True),
                        r=[C["ident_b"], RGT], w=[pgb])
                gbs = GBS[r % 2]
                self.op("act", lambda h, pgb=pgb, gbs=gbs: h.activation(out=gbs.ap, in_=pgb.ap, func=AF.Copy), r=[pgb], w=[gbs])
                self.op("dve", lambda h, r=r, prb=prb, gbs=gbs: h.scalar_tensor_tensor(
                    out=STw[r].ap, in0=prb.ap, scalar=C["piota"].ap[:, 0:1], in1=gbs.ap, op0=ALU.is_equal, op1=ALU.mult),
                    r=[prb, gbs, C["piota"]], w=[STw[r]])
            if e % 2 == 0 and e + 2 < NE:
                build_S(e + 2)
            for j in range(2):
                assert piece_slot[(e, "d", 2 * j)] is RING[(d_slot0 + 2 * j) % NRING] and piece_slot[(e, "d", 2 * j + 1)] is RING[(d_slot0 + 2 * j + 1) % NRING]
                wd2 = RING2[j]
                for jt in range(NJT):
                    py = self.psb(jt % 2)
                    for c in range(8):
                        self.op("pe", lambda h, c=c, jt=jt, wd2=wd2, py=py: h.matmul(py.ap.rearrange("p (a b) -> p a b", a=2), lhsT=ACTT.ap[:, c, jt * 128:(jt + 1) * 128],
                                                                                 rhs=wd2.ap[:, :, c, :], start=(c == 0), stop=False), r=[ACTT.keys[c], wd2], w=[py])
                    self.op("pe", lambda h, j=j, py=py, bdn=bdn: h.matmul(py.ap, lhsT=C["ones"].ap[0:1, :], rhs=bdn.ap[0:1, j * 512:(j + 1) * 512],
                                                                          start=False, stop=True), r=[C["ones"], bdn], w=[py])
                    self.op("act", lambda h, jt=jt, j=j, py=py, YB=YB: h.activation(out=YB.ap[:, jt, j * 512:(j + 1) * 512], in_=py.ap, func=AF.Copy), r=[py], w=[YB.keys[2 * jt + j]])
                issue_piece()
                issue_piece()
            for i in range(NT):
                for dr in range(2):
                    po = self.psb(2 + ((2 * i + dr) % 6))
                    w4, ii = divmod(i, 4)
                    self.op("pe", lambda h, w4=w4, ii=ii, dr=dr, po=po, YB=YB: h.matmul(po.ap, lhsT=STw[w4].ap[:, ii * 128:(ii + 1) * 128],
                                                                             rhs=YB.ap[:, w4, dr * 512:(dr + 1) * 512], start=True, stop=True),
                            r=[STw[w4], YB.keys[2 * w4 + dr]], w=[po])
                    Ri = self.R[i]
                    self.op("dve", lambda h, dr=dr, Ri=Ri, po=po: h.tensor_tensor(out=Ri.ap[:, dr * 512:(dr + 1) * 512], in0=Ri.ap[:, dr * 512:(dr + 1) * 512],
                                                                              in1=po.ap, op=ALU.add), r=[Ri, po], w=[Ri])


_CACHE = {}
_NPH = [8]

WEIGHT_KEYS = ["kv_w_k", "kv_w_v", "attn_w_q", "attn_w_o", "ssm_w_in", "ssm_conv_w", "ssm_conv_b", "ssm_dt_bias", "ssm_a_log", "ssm_d", "ssm_norm_g", "ssm_w_out",
               "moe_w_router", "moe_b_router", "moe_w_gate_up", "moe_b_gate_up", "moe_w_down", "moe_b_down",
               "ln_mix_g", "ln_mix_b", "ln_ffn_g", "ln_ffn_b"]


def kernel(**inputs):
    x = np.ascontiguousarray(np.asarray(inputs["x"], dtype=np.float32))
    nb = x.shape[0]
    if "nc" not in _CACHE:
        phases = [("prep", 0), ("mamba", 0), ("moe", 0), ("mamba", 1), ("moe", 1), ("attn", 2), ("moe", 2), ("attn", 3), ("moe", 3)][:_NPH[0] + 1]
        mk = MK(layers=[0, 1, 2, 3], phases=phases)
        _CACHE["nc"] = mk.build()
        _CACHE["mk"] = mk
    nc = _CACHE["nc"]
    shared = {k: np.ascontiguousarray(np.asarray(inputs[k], dtype=np.float32)) for k in WEIGHT_KEYS}
    for k, v in make_consts().items():
        shared["c_" + k] = v
    in_maps = []
    for b in range(nb):
        m = dict(shared)
        m["x"] = x[b]
        in_maps.append(m)
    res = run_bass_kernel_spmd(nc, in_maps, core_ids=list(range(nb)))
    return np.stack([np.asarray(r["y"], dtype=np.float32) for r in res.results], axis=0)
```
